# Optimizing a Trainium2 kernel written in Bass

```python
import jax
import jax.numpy as jnp
from jax import lax
import numpy as np

D_MODEL = 1024
BATCH = 2
SEQ = 16384
DEPTH = 4

EPS = 1e-6
N_BRANCH = 3
MLA_HEADS = 8
MLA_Q_LORA = 256
MLA_KV_LORA = 128
MLA_NOPE = 64
MLA_ROPE = 32
MLA_V = 64
ROPE_THETA = 10000.0
Q_BLOCK = 128
MLSTM_HEADS = 4
MLSTM_QK = 64
MLSTM_V = 128
MLSTM_CHUNK = 64
CONV_K = 4
DIL_HEADS = 8
DIL_HEAD_DIM = 64
DIL_PATTERNS = ((128, 1), (512, 4), (2048, 16))
DIL_WIDTH = DIL_HEADS * DIL_HEAD_DIM
N_GROUPS = 4
EXPERTS_PER_GROUP = 4
N_EXPERTS = N_GROUPS * EXPERTS_PER_GROUP
TOP_K_FINE = 2
EXPERT_FF = 256
IN_SIZES = (MLA_Q_LORA, MLA_KV_LORA, MLA_ROPE,
            2 * MLSTM_HEADS * MLSTM_QK, MLSTM_HEADS * MLSTM_V, MLSTM_HEADS * MLSTM_V, MLSTM_HEADS, MLSTM_HEADS,
            DIL_WIDTH, DIL_WIDTH, DIL_WIDTH,
            N_BRANCH * D_MODEL)
IN_WIDTH = sum(IN_SIZES)

kernel_name = 'hybrid_mla_mlstm_dilated_hmoe_block'


def rmsnorm(x, gain):
    xf = x.astype(jnp.float32)
    y = xf * lax.rsqrt(jnp.mean(xf * xf, axis=-1, keepdims=True) + EPS)
    return (y * gain.astype(jnp.float32)).astype(x.dtype)


def rope(x, pos):
    half = x.shape[-1] // 2
    inv = ROPE_THETA ** (-jnp.arange(half, dtype=jnp.float32) / half)
    ang = pos.astype(jnp.float32)[:, None] * inv[None, :]
    cos = jnp.cos(ang)[None, :, None, :]
    sin = jnp.sin(ang)[None, :, None, :]
    xf = x.astype(jnp.float32)
    x1, x2 = xf[..., :half], xf[..., half:]
    return jnp.concatenate([x1 * cos - x2 * sin, x1 * sin + x2 * cos], axis=-1).astype(x.dtype)


def causal_conv(x, w, b):
    C = x.shape[-1]
    y = lax.conv_general_dilated(x, w[:, None, :], window_strides=(1,), padding=((CONV_K - 1, 0),),
                                 dimension_numbers=('NWC', 'WIO', 'NWC'), feature_group_count=C)
    return y + b


def blocked_causal_attention(q, k, v):
    B, S, H, Dq = q.shape
    Dv = v.shape[-1]
    nq = S // Q_BLOCK
    scale = Dq ** -0.5
    qb = q.reshape(B, nq, Q_BLOCK, H, Dq).transpose(1, 0, 2, 3, 4)
    kpos = jnp.arange(S)

    def one_block(args):
        qi, j = args
        s = jnp.einsum('bqhd,bkhd->bhqk', qi, k).astype(jnp.float32) * scale
        qpos = j * Q_BLOCK + jnp.arange(Q_BLOCK)
        s = jnp.where(kpos[None, :] <= qpos[:, None], s, -jnp.inf)
        p = jax.nn.softmax(s, axis=-1)
        return jnp.einsum('bhqk,bkhd->bqhd', p.astype(v.dtype), v)

    o = lax.map(one_block, (qb, jnp.arange(nq)))
    return o.transpose(1, 0, 2, 3, 4).reshape(B, S, H, Dv)


def mla_branch(cq_raw, ckv_raw, kr, q_norm, kv_norm, w_uq, w_ukv, q_gain, k_gain):
    B, S, _ = cq_raw.shape
    cq = rmsnorm(cq_raw, q_norm)
    ckv = rmsnorm(ckv_raw, kv_norm)
    q = jnp.einsum('bsr,rn->bsn', cq, w_uq).reshape(B, S, MLA_HEADS, MLA_NOPE + MLA_ROPE)
    kv = jnp.einsum('bsr,rn->bsn', ckv, w_ukv).reshape(B, S, MLA_HEADS, MLA_NOPE + MLA_V)
    k_nope, v = kv[..., :MLA_NOPE], kv[..., MLA_NOPE:]
    k = jnp.concatenate([k_nope, jnp.broadcast_to(kr[:, :, None, :], (B, S, MLA_HEADS, MLA_ROPE))], axis=-1)
    q = rmsnorm(q, q_gain)
    k = rmsnorm(k, k_gain)
    pos = jnp.arange(S)
    q = jnp.concatenate([q[..., :MLA_NOPE], rope(q[..., MLA_NOPE:], pos)], axis=-1)
    k = jnp.concatenate([k[..., :MLA_NOPE], rope(k[..., MLA_NOPE:], pos)], axis=-1)
    o = blocked_causal_attention(q, k, v)
    return o.reshape(B, S, MLA_HEADS * MLA_V)


def mlstm_chunkwise(q, k, v, ig, fg):
    out_dtype = v.dtype
    B, S, H, dk = q.shape
    dv = v.shape[-1]
    L = MLSTM_CHUNK
    nc = S // L
    q = q.astype(jnp.float32)
    k = k.astype(jnp.float32) * (dk ** -0.5)
    v = v.astype(jnp.float32)
    ig = ig.astype(jnp.float32)
    logf = jax.nn.log_sigmoid(fg.astype(jnp.float32))

    def to_chunks(t):
        t = t.reshape((B, nc, L) + t.shape[2:])
        return jnp.swapaxes(jnp.moveaxis(t, 1, 0), 2, 3)

    causal = jnp.tril(jnp.ones((L, L), dtype=bool))

    def step(carry, xs):
        C, n, m = carry
        qc, kc, vc, ic, lf = xs
        b = jnp.cumsum(lf, axis=-1)
        dlog = jnp.where(causal, b[..., :, None] - b[..., None, :] + ic[..., None, :], -jnp.inf)
        inter = b + m[..., None]
        m_t = jnp.maximum(inter, jnp.max(dlog, axis=-1))
        sc = jnp.einsum('bhtd,bhsd->bhts', qc, kc) * jnp.exp(dlog - m_t[..., None])
        decay = jnp.exp(inter - m_t)
        num = decay[..., None] * jnp.einsum('bhtd,bhdv->bhtv', qc, C) + jnp.einsum('bhts,bhsv->bhtv', sc, vc)
        den = decay * jnp.einsum('bhtd,bhd->bht', qc, n) + jnp.sum(sc, axis=-1)
        h = num / jnp.maximum(jnp.abs(den), jnp.exp(-m_t))[..., None]
        b_end = b[..., -1]
        wlog = b_end[..., None] - b + ic
        m_new = jnp.maximum(b_end + m, jnp.max(wlog, axis=-1))
        carry_decay = jnp.exp(b_end + m - m_new)
        w = jnp.exp(wlog - m_new[..., None])
        C_new = carry_decay[..., None, None] * C + jnp.einsum('bhs,bhsd,bhsv->bhdv', w, kc, vc)
        n_new = carry_decay[..., None] * n + jnp.einsum('bhs,bhsd->bhd', w, kc)
        return (C_new, n_new, m_new), h

    init = (jnp.zeros((B, H, dk, dv), jnp.float32), jnp.zeros((B, H, dk), jnp.float32), jnp.zeros((B, H), jnp.float32))
    _, hs = lax.scan(step, init, (to_chunks(q), to_chunks(k), to_chunks(v), to_chunks(ig), to_chunks(logf)))
    hs = jnp.moveaxis(jnp.swapaxes(hs, 2, 3), 0, 1).reshape(B, S, H, dv)
    return hs.astype(out_dtype)


def mlstm_branch(qk_raw, v_raw, o_raw, i_raw, f_raw, conv_w, conv_b, b_i, b_f, head_gain):
    B, S, _ = qk_raw.shape
    qk = jax.nn.silu(causal_conv(qk_raw, conv_w, conv_b))
    q = qk[..., :MLSTM_HEADS * MLSTM_QK].reshape(B, S, MLSTM_HEADS, MLSTM_QK)
    k = qk[..., MLSTM_HEADS * MLSTM_QK:].reshape(B, S, MLSTM_HEADS, MLSTM_QK)
    v = v_raw.reshape(B, S, MLSTM_HEADS, MLSTM_V)
    h = mlstm_chunkwise(q, k, v, i_raw + b_i, f_raw + b_f)
    h = rmsnorm(h, head_gain.reshape(MLSTM_HEADS, MLSTM_V))
    return h.reshape(B, S, MLSTM_HEADS * MLSTM_V) * jax.nn.sigmoid(o_raw)


def dilated_window_attention(q, k, v, window, dilation):
    B, S, H, Dh = q.shape
    blk = window // dilation
    span = blk * dilation
    s_pad = -(-S // span) * span
    nb = s_pad // span

    def fold(t):
        t = jnp.pad(t, ((0, 0), (0, s_pad - S), (0, 0), (0, 0)))
        return t.reshape(B, nb, blk, dilation, H, Dh)

    def with_prev(t):
        prev = jnp.concatenate([jnp.zeros_like(t[:, :1]), t[:, :-1]], axis=1)
        return jnp.concatenate([prev, t], axis=2)

    qb = fold(q)
    kk = with_prev(fold(k))
    vv = with_prev(fold(v))
    s = jnp.einsum('bnqrhd,bnkrhd->bnrhqk', qb, kk).astype(jnp.float32) * (Dh ** -0.5)
    iq = jnp.arange(blk)[:, None]
    ik = jnp.arange(2 * blk)[None, :]
    dist = iq + blk - ik
    in_band = (dist >= 0) & (dist <= blk)
    before_start = (jnp.arange(nb) == 0)[:, None, None] & (ik < blk)[None]
    mask = in_band[None] & ~before_start
    s = jnp.where(mask[None, :, None, None], s, -jnp.inf)
    m = jnp.max(s, axis=-1, keepdims=True)
    p = jnp.exp(s - m)
    den = jnp.sum(p, axis=-1)
    o = jnp.einsum('bnrhqk,bnkrhd->bnqrhd', p, vv.astype(jnp.float32))
    o = o / jnp.transpose(den, (0, 1, 4, 2, 3))[..., None]
    lse = jnp.transpose(m[..., 0] + jnp.log(den), (0, 1, 4, 2, 3))
    o = o.reshape(B, s_pad, H, Dh)[:, :S]
    lse = lse.reshape(B, s_pad, H)[:, :S]
    return o, lse


def dilated_branch(q_raw, k_raw, v_raw, q_gain, k_gain):
    B, S, _ = q_raw.shape
    q = rmsnorm(q_raw.reshape(B, S, DIL_HEADS, DIL_HEAD_DIM), q_gain)
    k = rmsnorm(k_raw.reshape(B, S, DIL_HEADS, DIL_HEAD_DIM), k_gain)
    v = v_raw.reshape(B, S, DIL_HEADS, DIL_HEAD_DIM)
    outs, lses = [], []
    for window, dilation in DIL_PATTERNS:
        o, lse = dilated_window_attention(q, k, v, window, dilation)
        outs.append(o)
        lses.append(lse)
    w = jax.nn.softmax(jnp.stack(lses, axis=-1), axis=-1)
    o = jnp.einsum('bshp,bshpd->bshd', w, jnp.stack(outs, axis=-2))
    return o.astype(v_raw.dtype).reshape(B, S, DIL_WIDTH)


def token_mixer(u, w_in, mla_q_norm, mla_kv_norm, mla_w_uq, mla_w_ukv, mla_q_gain, mla_k_gain,
                mlstm_conv_w, mlstm_conv_b, mlstm_b_i, mlstm_b_f, mlstm_head_gain, dil_q_gain, dil_k_gain,
                w_branch_a, w_branch_b, w_branch_c, w_out):
    B, S, D = u.shape
    proj = jnp.einsum('bsd,dn->bsn', u, w_in)
    points = np.cumsum(IN_SIZES)[:-1].tolist()
    (cq, ckv, kr, m_qk, m_v, m_o, m_i, m_f, d_q, d_k, d_v, gates) = jnp.split(proj, points, axis=-1)
    ya = mla_branch(cq, ckv, kr, mla_q_norm, mla_kv_norm, mla_w_uq, mla_w_ukv, mla_q_gain, mla_k_gain)
    yb = mlstm_branch(m_qk, m_v, m_o, m_i, m_f, mlstm_conv_w, mlstm_conv_b, mlstm_b_i, mlstm_b_f, mlstm_head_gain)
    yc = dilated_branch(d_q, d_k, d_v, dil_q_gain, dil_k_gain)
    g = jax.nn.sigmoid(gates).reshape(B, S, N_BRANCH, D)
    merged = (g[:, :, 0] * jnp.einsum('bsn,nd->bsd', ya, w_branch_a)
              + g[:, :, 1] * jnp.einsum('bsn,nd->bsd', yb, w_branch_b)
              + g[:, :, 2] * jnp.einsum('bsn,nd->bsd', yc, w_branch_c))
    return jnp.einsum('bsd,de->bse', merged, w_out)


def hier_moe(h, w_rg, b_rg, w_re, b_re, w_gate, w_up, w_down):
    B, S, D = h.shape
    lg = jnp.einsum('bsd,dg->bsg', h, w_rg).astype(jnp.float32) + b_rg.astype(jnp.float32)
    p_group = jax.nn.softmax(lg, axis=-1)
    g_idx = jnp.argmax(lg, axis=-1)
    p_sel = jnp.take_along_axis(p_group, g_idx[..., None], axis=-1)[..., 0]
    le = (jnp.einsum('bsd,de->bse', h, w_re).astype(jnp.float32) + b_re.astype(jnp.float32))
    le = le.reshape(B, S, N_GROUPS, EXPERTS_PER_GROUP)
    le = jnp.take_along_axis(le, g_idx[..., None, None], axis=2)[:, :, 0]
    top_v, top_i = lax.top_k(le, TOP_K_FINE)
    w2 = jax.nn.softmax(top_v, axis=-1) * p_sel[..., None]
    expert_id = g_idx[..., None] * EXPERTS_PER_GROUP + top_i
    combine = jnp.einsum('bsk,bske->bse', w2, jax.nn.one_hot(expert_id, N_EXPERTS, dtype=jnp.float32))
    hid = jax.nn.silu(jnp.einsum('bsd,edf->bsef', h, w_gate)) * jnp.einsum('bsd,edf->bsef', h, w_up)
    hid = hid * combine.astype(h.dtype)[..., None]
    return jnp.einsum('bsef,efd->bsd', hid, w_down)


def setup_inputs(seed: int = 0) -> dict:
    key = jax.random.key(seed)
    ks = iter(jax.random.split(key, 40))
    L, D = DEPTH, D_MODEL

    def nrm(shape, scale):
        return scale * jax.random.normal(next(ks), shape, jnp.float32)

    def gain(shape):
        return 1.0 + 0.02 * jax.random.normal(next(ks), shape, jnp.float32)

    return {
        'x': nrm((BATCH, SEQ, D), 1.0),
        'c': nrm((BATCH, D), 1.0),
        'w_ada': nrm((L, D, 6 * D), 0.5 * D ** -0.5),
        'b_ada': nrm((L, 6 * D), 0.02),
        'attn_norm': gain((L, D)),
        'w_in': nrm((L, D, IN_WIDTH), D ** -0.5),
        'mla_q_norm': gain((L, MLA_Q_LORA)),
        'mla_kv_norm': gain((L, MLA_KV_LORA)),
        'mla_w_uq': nrm((L, MLA_Q_LORA, MLA_HEADS * (MLA_NOPE + MLA_ROPE)), MLA_Q_LORA ** -0.5),
        'mla_w_ukv': nrm((L, MLA_KV_LORA, MLA_HEADS * (MLA_NOPE + MLA_V)), MLA_KV_LORA ** -0.5),
        'mla_q_gain': gain((L, MLA_NOPE + MLA_ROPE)),
        'mla_k_gain': gain((L, MLA_NOPE + MLA_ROPE)),
        'mlstm_conv_w': nrm((L, CONV_K, 2 * MLSTM_HEADS * MLSTM_QK), CONV_K ** -0.5),
        'mlstm_conv_b': nrm((L, 2 * MLSTM_HEADS * MLSTM_QK), 0.02),
        'mlstm_b_i': nrm((L, MLSTM_HEADS), 0.1),
        'mlstm_b_f': 3.0 + 3.0 * jax.random.uniform(next(ks), (L, MLSTM_HEADS), jnp.float32),
        'mlstm_head_gain': gain((L, MLSTM_HEADS * MLSTM_V)),
        'dil_q_gain': gain((L, DIL_HEAD_DIM)),
        'dil_k_gain': gain((L, DIL_HEAD_DIM)),
        'w_branch_a': nrm((L, MLA_HEADS * MLA_V, D), (MLA_HEADS * MLA_V) ** -0.5),
        'w_branch_b': nrm((L, MLSTM_HEADS * MLSTM_V, D), (MLSTM_HEADS * MLSTM_V) ** -0.5),
        'w_branch_c': nrm((L, DIL_WIDTH, D), DIL_WIDTH ** -0.5),
        'w_out': nrm((L, D, D), D ** -0.5),
        'ffn_norm': gain((L, D)),
        'w_router_group': nrm((L, D, N_GROUPS), D ** -0.5),
        'b_router_group': nrm((L, N_GROUPS), 0.01),
        'w_router_expert': nrm((L, D, N_EXPERTS), D ** -0.5),
        'b_router_expert': nrm((L, N_EXPERTS), 0.01),
        'w_exp_gate': nrm((L, N_EXPERTS, D, EXPERT_FF), D ** -0.5),
        'w_exp_up': nrm((L, N_EXPERTS, D, EXPERT_FF), D ** -0.5),
        'w_exp_down': nrm((L, N_EXPERTS, EXPERT_FF, D), EXPERT_FF ** -0.5),
    }


def reference(x, c, w_ada, b_ada, attn_norm, w_in, mla_q_norm, mla_kv_norm, mla_w_uq, mla_w_ukv,
              mla_q_gain, mla_k_gain, mlstm_conv_w, mlstm_conv_b, mlstm_b_i, mlstm_b_f, mlstm_head_gain,
              dil_q_gain, dil_k_gain, w_branch_a, w_branch_b, w_branch_c, w_out, ffn_norm,
              w_router_group, b_router_group, w_router_expert, b_router_expert,
              w_exp_gate, w_exp_up, w_exp_down):
    B, S, D = x.shape
    c_act = jax.nn.silu(c)
    for l in range(DEPTH):
        mod = (jnp.einsum('bd,dm->bm', c_act, w_ada[l]) + b_ada[l]).reshape(B, 6, D)[:, :, None, :]
        sh_a, sc_a, g_a, sh_f, sc_f, g_f = (mod[:, i] for i in range(6))
        u = rmsnorm(x, attn_norm[l]) * (1.0 + sc_a) + sh_a
        y = token_mixer(u, w_in[l], mla_q_norm[l], mla_kv_norm[l], mla_w_uq[l], mla_w_ukv[l],
                        mla_q_gain[l], mla_k_gain[l], mlstm_conv_w[l], mlstm_conv_b[l], mlstm_b_i[l],
                        mlstm_b_f[l], mlstm_head_gain[l], dil_q_gain[l], dil_k_gain[l],
                        w_branch_a[l], w_branch_b[l], w_branch_c[l], w_out[l])
        x = x + g_a * y
        hf = rmsnorm(x, ffn_norm[l]) * (1.0 + sc_f) + sh_f
        x = x + g_f * hier_moe(hf, w_router_group[l], b_router_group[l], w_router_expert[l],
                               b_router_expert[l], w_exp_gate[l], w_exp_up[l], w_exp_down[l])
    return x
```

```python
import numpy as np
import ml_dtypes
from contextlib import ExitStack
import concourse.bass as bass
import concourse.mybir as mybir
from concourse.bass_utils import run_bass_kernel_spmd

F32 = mybir.dt.float32
BF16 = mybir.dt.bfloat16
AF = mybir.ActivationFunctionType
ALU = mybir.AluOpType
AX = mybir.AxisListType
NPBF = ml_dtypes.bfloat16

D = 1024
S_LEN = 16384
NB = 2
DEPTH = 4
EPS = 1e-6
TOK = 4096
NT = 512


class T:
    __slots__ = ("ap", "name", "lw", "rs", "dsem", "dcnt")

    def __init__(self, ap, name):
        self.ap = ap
        self.name = name
        self.lw = None
        self.rs = {}
        self.dsem = None
        self.dcnt = 0

    def __getitem__(self, k):
        return self.ap[k]


class Sched:
    SEM_MAX = 30000

    def __init__(self, nc, stack):
        self.nc = nc
        self.root = stack
        self.eng = {'pe': nc.tensor, 'act': nc.scalar, 'dve': nc.vector, 'pool': nc.gpsimd, 'sp': nc.sync}
        self.sem = {}
        self.cnt = {}
        self.nsem = 0
        for e in self.eng:
            self._newsem(e)
        self.waited = {e: {} for e in self.eng}
        self.phase = None
        self.phase_tiles = []
        self.dma_pool = []
        self.all_dma = {}
        self.cc_sem = None
        self.ninst = 0

    def _newsem(self, e):
        self.nsem += 1
        self.sem[e] = self.root.enter_context(self.nc.semaphore("s_%s_%d" % (e, self.nsem)))
        self.cnt[e] = 0

    def begin_phase(self):
        self.phase = ExitStack()
        self.phase_tiles = []

    def end_phase(self):
        deps = []
        for t in self.phase_tiles:
            if t.lw is not None:
                deps.append(t.lw)
            deps.extend(t.rs.values())
        self._wait('sp', deps, True)
        self.sp_mark()
        self.barrier()
        for t in self.phase_tiles:
            if t.dsem is not None:
                self.dma_pool.append((t.dsem, t.dcnt))
        self.phase.close()
        self.phase = None
        self.phase_tiles = []

    def barrier(self):
        tags = [(self.sem[e], self.cnt[e], e) for e in self.eng if self.cnt[e] > 0]
        for e in self.eng:
            self._wait(e, tags, False)

    def sp_mark(self):
        ins = self.eng['sp'].sem_inc(self.sem['sp'], 1)
        self.cnt['sp'] += 1

    def collective(self, src_ap, dst_ap, deps, groups=((0, 1, 2, 3), (4, 5, 6, 7))):
        if self.cc_sem is None:
            self.cc_sem = self.root.enter_context(self.nc.semaphore("cc_sem"))
            self.cc_cnt = 0
        self._wait('pool', list(deps), True)
        ins = self.eng['pool'].collective_compute("AllGather", ALU.bypass, replica_groups=[list(g) for g in groups], ins=[src_ap], outs=[dst_ap])
        self.cc_cnt += 16
        ins.then_inc(self.cc_sem, 16)
        tag = (self.cc_sem, self.cc_cnt, 'dma')
        self.all_dma[id(self.cc_sem)] = tag
        self._wait('sp', [tag], True)
        return tag

    def sb(self, shape, dt, name):
        st = self.phase if self.phase is not None else self.root
        self.nsem += 1
        t = T(st.enter_context(self.nc.sbuf_tensor("sb_%s_%d" % (name, self.nsem), list(shape), dt)), name)
        if self.phase is not None:
            self.phase_tiles.append(t)
        return t

    def ps(self, shape, dt, name):
        st = self.phase if self.phase is not None else self.root
        self.nsem += 1
        t = T(st.enter_context(self.nc.psum_tensor("ps_%s_%d" % (name, self.nsem), list(shape), dt)), name)
        if self.phase is not None:
            self.phase_tiles.append(t)
        return t

    def sub(self, ap, name):
        t = T(ap, name)
        if self.phase is not None:
            self.phase_tiles.append(t)
        return t

    def _wait(self, e, deps, is_dma):
        w = self.waited[e]
        for (sem, val, de) in deps:
            if de == e and not is_dma and e == 'pe':
                continue
            key = id(sem)
            if w.get(key, 0) >= val:
                continue
            self.eng[e].wait_ge(sem, val)
            w[key] = val

    def _deps(self, R, W):
        deps = []
        for t in R:
            if t.lw is not None:
                deps.append(t.lw)
        for t in W:
            if t.lw is not None:
                deps.append(t.lw)
            deps.extend(t.rs.values())
        return deps

    def _mark(self, tag, R, W):
        sem = tag[0]
        for t in W:
            t.lw = tag
            t.rs = {}
        for t in R:
            t.rs[id(sem)] = tag

    def op(self, e, fn, R=(), W=()):
        self._wait(e, self._deps(R, W), False)
        if self.cnt[e] >= self.SEM_MAX:
            self._newsem(e)
        ins = fn(self.eng[e])
        self.cnt[e] += 1
        self.ninst += 1
        ins.then_inc(self.sem[e], 1)
        self._mark((self.sem[e], self.cnt[e], e), R, W)
        return ins

    def dma(self, q, out_ap, in_ap, R=(), W=(), owner=None):
        if q == 'pool':
            q = 'sp'
        self._wait(q, self._deps(R, W), True)
        if owner is None:
            owner = (list(W) + list(R))[0]
        if owner.dsem is None or owner.dcnt >= self.SEM_MAX:
            if owner.dsem is None and self.dma_pool:
                owner.dsem, owner.dcnt = self.dma_pool.pop()
            else:
                owner.dsem = self.root.enter_context(self.nc.semaphore("d_%d" % self.nsem))
                self.nsem += 1
                owner.dcnt = 0
        ins = self.eng[q].dma_start(out=out_ap, in_=in_ap)
        owner.dcnt += 16
        self.ninst += 1
        ins.then_inc(owner.dsem, 16)
        tag = (owner.dsem, owner.dcnt, 'dma')
        self.all_dma[id(owner.dsem)] = tag
        self._mark(tag, R, W)
        return tag

    def finish_all(self):
        self._wait('sp', list(self.all_dma.values()), True)
        self.barrier()

    def finish(self, tiles):
        deps = []
        for t in tiles:
            if t.lw is not None:
                deps.append(t.lw)
            deps.extend(t.rs.values())
        self._wait('sp', deps, True)


def bank(S, name, dt=F32):
    return S.ps([128, 512 if dt == F32 else 1024], dt, name)


def rsqrt_mean(S, out_ap, in_ap, n, tmp_ap, R, W):
    S.op('act', lambda a: a.activation(tmp_ap, in_ap, AF.Ln, scale=1.0 / n, bias=S.eps_col[0:tmp_ap.shape[0], 0:1]), R=R + [S.eps_t], W=W)
    S.op('act', lambda a: a.activation(out_ap, tmp_ap, AF.Exp, scale=-0.5), R=W, W=W)


def load_consts(S, io):
    c = {}
    c['ident_b'] = S.sb([128, 128], BF16, "ident_b")
    c['ident_f'] = S.sb([128, 128], F32, "ident_f")
    c['tri_b'] = S.sb([128, 128], BF16, "tri_b")
    c['E65'] = S.sb([65, 64], F32, "E65")
    c['ones_f'] = S.sb([128, 128], F32, "ones_f")
    S.eps_t = S.sb([128, 1], F32, "eps_t")
    S.eps_col = S.eps_t.ap
    S.dma('sp', c['ident_b'][:], io['c_ident_b'], W=[c['ident_b']])
    S.dma('sp', c['ident_f'][:], io['c_ident_f'], W=[c['ident_f']])
    S.dma('sp', c['tri_b'][:], io['c_tri_b'], W=[c['tri_b']])
    S.dma('sp', c['E65'][:], io['c_E65'], W=[c['E65']])
    S.op('pool', lambda g: g.memset(c['ones_f'][:], 1.0), W=[c['ones_f']])
    S.op('pool', lambda g: g.memset(S.eps_t[:], EPS), W=[S.eps_t])
    return c


def emit_mla(S, io, c, n_tiles=32):
    S.begin_phase()
    uT = io['uT']
    ident_b, tri_b, E65 = c['ident_b'], c['tri_b'], c['E65']
    wl_f = S.sb([128, 8, 416], F32, "wl_f")
    S.dma('sp', wl_f[:], io['WB'][:, 0:416].rearrange("(k p) n -> p k n", p=128), W=[wl_f])
    wl = S.sb([128, 8, 416], BF16, "wl")
    S.op('pool', lambda g: g.tensor_copy(wl[:], wl_f[:]), R=[wl_f], W=[wl])
    wuq_f = S.sb([128, 2, 192], F32, "wuq_f")
    S.dma('sp', wuq_f[:], io['wuq'].rearrange("(k p) n -> p k n", p=128), W=[wuq_f])
    wukv_f = S.sb([128, 256], F32, "wukv_f")
    S.dma('sp', wukv_f[:], io['wukv'], W=[wukv_f])
    ncol = S.sb([128, 3], F32, "ncol")
    S.dma('sp', ncol[:], io['mla_ncol'], W=[ncol])
    wuq = S.sb([128, 2, 192], BF16, "wuq")
    wukv = S.sb([128, 256], BF16, "wukv")
    for k in range(2):
        S.op('dve', lambda v: v.tensor_scalar(wuq[:, k, :], wuq_f[:, k, :], ncol[:, k:k + 1], None, ALU.mult), R=[wuq_f, ncol], W=[wuq])
    S.op('dve', lambda v: v.tensor_scalar(wukv[:], wukv_f[:], ncol[:, 2:3], None, ALU.mult), R=[wukv_f, ncol], W=[wukv])
    g4 = S.sb([128, 384], F32, "g4")
    S.dma('sp', g4[:], io['mla_grow'].partition_broadcast(128), W=[g4])
    S.op('dve', lambda v: v.tensor_scalar(g4[:, 0:192], g4[:, 0:192], 96 ** -0.5, None, ALU.mult), R=[g4], W=[g4])
    g4v = g4[:].rearrange("p (a b) -> p a b", a=4)
    cs = S.sb([128, 128 * 32], F32, "cs")
    S.dma('sp', cs[:], io['c_rope'], W=[cs])
    csv = cs[:].rearrange("p (t c) -> p t c", c=32)
    KT = S.sb([96, 128, 2, 128], BF16, "KT")
    VA = S.sb([128, 128, 2, 65], BF16, "VA")
    KT_t = [S.sub(KT[:, 4 * i:4 * i + 4, :, :], "KT%d" % i) for i in range(32)]
    VA_t = [S.sub(VA[:, 4 * i:4 * i + 4, :, :], "VA%d" % i) for i in range(32)]
    S.op('pool', lambda g: g.memset(VA[:, :, :, 64:65], 1.0), W=VA_t)
    QT = [S.sb([96, 2, 512], BF16, "QT%d" % i) for i in range(2)]
    uts = [S.sb([128, 8, 512], BF16, "ut%d" % i) for i in range(2)]
    p_lat = bank(S, "p_lat")
    p_trq = bank(S, "p_trq", BF16)
    p_tr = S.sub(p_trq[:, 0:512], "p_tr")
    p_qkT = S.sub(p_trq[:, 512:1024], "p_qkT")
    p_qkv = bank(S, "p_qkv")
    p_s = [bank(S, "p_s%d" % i) for i in range(3)]
    p_o0 = bank(S, "p_o0")
    p_o = [p_o0, p_o0]
    p_den = bank(S, "p_den")
    trv = p_tr[:].rearrange("p (a b) -> p a b", b=128)
    qkTv = p_qkT[:].rearrange("p (a b) -> p a b", b=128)
    R2 = 2
    junk = [S.sb([128, 256], F32, "junk%d" % i) for i in range(R2)]
    ss = [S.sb([128, 2], F32, "ss%d" % i) for i in range(R2)]
    sst = [S.sb([128, 2], F32, "sst%d" % i) for i in range(R2)]
    rstd = [S.sb([128, 2], F32, "rstd%d" % i) for i in range(R2)]
    cn = [S.sb([128, 384], BF16, "cn%d" % i) for i in range(R2)]
    cnT = [S.sb([128, 3, 128], BF16, "cnT%d" % i) for i in range(R2)]
    qk = [S.sb([128, 4, 96], F32, "qk%d" % i) for i in range(R2)]
    sq = [S.sb([128, 4, 96], F32, "sq%d" % i) for i in range(R2)]
    ss4 = [S.sb([128, 4], F32, "ss4%d" % i) for i in range(R2)]
    ss4t = [S.sb([128, 4], F32, "ss4t%d" % i) for i in range(R2)]
    rs4 = [S.sb([128, 4], F32, "rs4%d" % i) for i in range(R2)]
    qkn = [S.sb([128, 4, 96], F32, "qkn%d" % i) for i in range(R2)]
    rt = [[S.sb([128, 4, 16], F32, "rt%d_%d" % (j, i)) for j in range(4)] for i in range(R2)]
    qkr = [S.sb([128, 4, 96], BF16, "qkr%d" % i) for i in range(R2)]
    pts = [S.sb([128, 512], BF16, "pt%d" % i) for i in range(3)]
    o_sb = [S.sb([65, 512], F32, "o_sb%d" % i) for i in range(2)]
    rden = [S.sb([64, 512], F32, "rden%d" % i) for i in range(2)]
    yts = [S.sb([128, 512], BF16, "yt%d" % i) for i in range(2)]

    def load_u(i):
        S.dma('sp', uts[i % 2][:], uT[:, :, i * 512:(i + 1) * 512].rearrange("k p n -> p k n"), W=[uts[i % 2]])

    import os
    STOP = int(os.environ.get('DBG_STOP', '99'))

    def proj_sub(i, s):
        ut = uts[i % 2]
        r = (i * 4 + s) % R2
        blk = i * 4 + s
        for k in range(8):
            S.op('pe', lambda p: p.matmul(p_lat[:, 0:416], ut[:, k, s * 128:(s + 1) * 128], wl[:, k, :], start=(k == 0), stop=(k == 7)), R=[ut, wl], W=[p_lat])
        yield
        S.op('act', lambda a: a.activation(junk[r][:, 0:256], p_lat[:, 0:256], AF.Square, accum_out=ss[r][:, 0:1]), R=[p_lat], W=[junk[r], ss[r]])
        S.op('act', lambda a: a.activation(junk[r][:, 0:128], p_lat[:, 256:384], AF.Square, accum_out=ss[r][:, 1:2]), R=[p_lat], W=[junk[r], ss[r]])
        yield
        S.op('act', lambda a: a.activation(sst[r][:, 0:1], ss[r][:, 0:1], AF.Ln, scale=1.0 / 256, bias=S.eps_col[:, 0:1]), R=[ss[r], S.eps_t], W=[sst[r]])
        S.op('act', lambda a: a.activation(sst[r][:, 1:2], ss[r][:, 1:2], AF.Ln, scale=1.0 / 128, bias=S.eps_col[:, 0:1]), R=[ss[r], S.eps_t], W=[sst[r]])
        S.op('act', lambda a: a.activation(rstd[r][:], sst[r][:], AF.Exp, scale=-0.5), R=[sst[r]], W=[rstd[r]])
        yield
        S.op('act', lambda a: a.activation(cn[r][:, 0:256], p_lat[:, 0:256], AF.Copy, scale=rstd[r][:, 0:1]), R=[p_lat, rstd[r]], W=[cn[r]])
        S.op('dve', lambda v: v.tensor_scalar(cn[r][:, 256:384], p_lat[:, 256:384], rstd[r][:, 1:2], None, ALU.mult), R=[p_lat, rstd[r]], W=[cn[r]])
        yield
        for h in range(2):
            S.op('dve', lambda v: v.tensor_copy(qk[r][:, 2 + h, 64:96], p_lat[:, 384:416]), R=[p_lat], W=[qk[r]])
        yield
        for j in range(3):
            S.op('pe', lambda p: p.transpose(trv[:, j, :], cn[r][:, j * 128:(j + 1) * 128], ident_b[:]), R=[cn[r], ident_b], W=[p_tr])
        yield
        S.op('dve', lambda v: v.tensor_copy(cnT[r][:], trv[:, 0:3, :]), R=[p_tr], W=[cnT[r]])
        yield
        S.op('pe', lambda p: p.matmul(p_qkv[:, 0:192], cnT[r][:, 0, :], wuq[:, 0, :], start=True, stop=False), R=[cnT[r], wuq], W=[p_qkv])
        S.op('pe', lambda p: p.matmul(p_qkv[:, 0:192], cnT[r][:, 1, :], wuq[:, 1, :], start=False, stop=False), R=[cnT[r], wuq], W=[p_qkv])
        S.op('pe', lambda p: p.matmul(p_qkv[:, 192:448], cnT[r][:, 2, :], wukv[:], start=False, stop=True), R=[cnT[r], wukv], W=[p_qkv])
        yield
        kvv = p_qkv[:, 192:448].rearrange("p (a b) -> p a b", a=2)
        S.op('act', lambda a: a.activation(qk[r][:, 0:2, :], p_qkv[:, 0:192].rearrange("p (a b) -> p a b", a=2), AF.Copy), R=[p_qkv], W=[qk[r]])
        S.op('dve', lambda v: v.tensor_copy(qk[r][:, 2:4, 0:64], kvv[:, :, 0:64]), R=[p_qkv], W=[qk[r]])
        S.op('act', lambda a: a.activation(VA[:, blk, :, 0:64], kvv[:, :, 64:128], AF.Copy), R=[p_qkv], W=[VA_t[i]])
        yield
        S.op('dve', lambda v: v.tensor_tensor(sq[r][:], qk[r][:], qk[r][:], ALU.mult), R=[qk[r]], W=[sq[r]])
        S.op('dve', lambda v: v.tensor_reduce(ss4[r][:], sq[r][:], AX.X, ALU.add), R=[sq[r]], W=[ss4[r]])
        yield
        S.op('act', lambda a: a.activation(ss4t[r][:], ss4[r][:], AF.Ln, scale=1.0 / 96, bias=S.eps_col[:, 0:1]), R=[ss4[r], S.eps_t], W=[ss4t[r]])
        S.op('act', lambda a: a.activation(rs4[r][:], ss4t[r][:], AF.Exp, scale=-0.5), R=[ss4t[r]], W=[rs4[r]])
        yield
        S.op('pool', lambda g: g.tensor_tensor(sq[r][:], qk[r][:], g4v, ALU.mult), R=[qk[r], g4], W=[sq[r]])
        for sl in range(4):
            S.op('dve', lambda v: v.tensor_scalar(qkn[r][:, sl, :], sq[r][:, sl, :], rs4[r][:, sl:sl + 1], None, ALU.mult), R=[sq[r], rs4[r]], W=[qkn[r]])
        cosb = csv[:, blk:blk + 1, 0:16].to_broadcast([128, 4, 16])
        sinb = csv[:, blk:blk + 1, 16:32].to_broadcast([128, 4, 16])
        x1 = qkn[r][:, :, 64:80]
        x2 = qkn[r][:, :, 80:96]
        t1, t2, t3, t4 = rt[r]
        S.op('pool', lambda g: g.tensor_copy(qkr[r][:, :, 0:64], qkn[r][:, :, 0:64]), R=[qkn[r]], W=[qkr[r]])
        S.op('dve', lambda v: v.tensor_tensor(t1[:], x1, cosb, ALU.mult), R=[qkn[r], cs], W=[t1])
        S.op('pool', lambda g: g.tensor_tensor(t2[:], x2, sinb, ALU.mult), R=[qkn[r], cs], W=[t2])
        S.op('pool', lambda g: g.tensor_tensor(t3[:], x1, sinb, ALU.mult), R=[qkn[r], cs], W=[t3])
        S.op('dve', lambda v: v.tensor_tensor(t4[:], x2, cosb, ALU.mult), R=[qkn[r], cs], W=[t4])
        yield
        S.op('dve', lambda v: v.tensor_tensor(qkr[r][:, :, 64:80], t1[:], t2[:], ALU.subtract), R=[t1, t2], W=[qkr[r]])
        S.op('pool', lambda g: g.tensor_tensor(qkr[r][:, :, 80:96], t3[:], t4[:], ALU.add), R=[t3, t4], W=[qkr[r]])
        yield
        for sl in range(4):
            S.op('pe', lambda p: p.transpose(qkTv[0:96, sl, :], qkr[r][:, sl, :], ident_b[:]), R=[qkr[r], ident_b], W=[p_qkT])
        yield
        S.op('act', lambda a: a.activation(QT[i % 2][:, :, s * 128:(s + 1) * 128], qkTv[0:96, 0:2, :], AF.Copy), R=[p_qkT], W=[QT[i % 2]])
        S.op('act', lambda a: a.activation(KT[:, blk, :, :], qkTv[0:96, 2:4, :], AF.Copy), R=[p_qkT], W=[KT_t[i]])

    cnt = [0]

    def attention(i, fillers):
        nblk = 4 * i + 4
        qt = QT[i % 2]
        yt = yts[i % 2]
        items = [(h, kb) for h in range(2) for kb in range(nblk)]
        NST = 60
        done_st = [0]

        def advance(idx):
            if fillers is None:
                return
            want = min(NST, ((idx + 1) * NST + len(items) - 1) // len(items))
            while done_st[0] < want:
                try:
                    next(fillers)
                except StopIteration:
                    done_st[0] = NST
                    return
                done_st[0] += 1

        def qk(idx):
            h, kb = items[idx]
            d = kb - 4 * i
            q0 = max(d, 0) * 128
            n = cnt[0] + idx
            sT = p_s[n % 3]
            S.op('pe', lambda p: p.matmul(sT[:, q0:512], KT[:, kb, h, :], qt[:, h, q0:512], start=True, stop=True), R=[KT_t[kb // 4], qt], W=[sT])

        qk(0)
        if len(items) > 1:
            qk(1)
        for idx, (h, kb) in enumerate(items):
            d = kb - 4 * i
            q0 = max(d, 0) * 128
            n = cnt[0] + idx
            sT = p_s[n % 3]
            pt = pts[n % 3]
            po = p_o[h]
            if idx + 2 < len(items):
                qk(idx + 2)
            S.op('act', lambda a: a.activation(pt[:, q0:512], sT[:, q0:512], AF.Exp), R=[sT], W=[pt])
            if d >= 0:
                S.op('pool', lambda g: g.tensor_tensor(pt[:, q0:q0 + 128], pt[:, q0:q0 + 128], tri_b[:], ALU.mult), R=[pt, tri_b], W=[pt])
            S.op('pe', lambda p: p.matmul(po[0:65, q0:512], VA[:, kb, h, :], pt[:, q0:512], start=(kb == 0), stop=(kb == nblk - 1)), R=[VA_t[kb // 4], pt], W=[po])
            advance(idx)
            if kb == nblk - 1:
                osb = o_sb[h]
                S.op('act', lambda a: a.activation(osb[:], po[0:65, :], AF.Copy), R=[po], W=[osb])
                S.op('pe', lambda p: p.matmul(p_den[0:64, :], E65[:], osb[:], start=True, stop=True), R=[E65, osb], W=[p_den])
                S.op('dve', lambda v: v.reciprocal(rden[h][:], p_den[0:64, :]), R=[p_den], W=[rden[h]])
                S.op('dve', lambda v: v.tensor_tensor(yt[h * 64:(h + 1) * 64, :], osb[0:64, :], rden[h][:], ALU.mult), R=[osb, rden[h]], W=[yt])
        cnt[0] += len(items)
        if fillers is not None:
            for _ in fillers:
                pass
        S.dma('sp', io['yaT'][:, i * 512:(i + 1) * 512], yt[:], R=[yt])

    import os
    lvl = int(os.environ.get("DBG_LVL", "9"))
    load_u(0)
    if n_tiles > 1:
        load_u(1)
    def proj_gen(i):
        for s_ in range(4):
            yield from proj_sub(i, s_)

    if lvl >= 1:
        for _ in proj_gen(0):
            pass
    if lvl < 2:
        n_tiles = 0
        S.dma('pool', io['yaT'][:, 0:512], uts[0][:, 0, :], R=[uts[0]])
    for i in range(n_tiles):
        fillers = proj_gen(i + 1) if i + 1 < n_tiles else None
        attention(i, fillers)
        if i + 2 < n_tiles:
            load_u(i + 2)
    S.end_phase()


def host_consts():
    c = {}
    c['c_ident_b'] = np.eye(128, dtype=np.float32).astype(NPBF)
    c['c_ident_f'] = np.eye(128, dtype=np.float32)
    p = np.arange(128)[:, None]
    f = np.arange(128)[None, :]
    c['c_tri_b'] = (p <= f).astype(np.float32).astype(NPBF)
    e = np.zeros((65, 64), np.float32)
    e[64, :] = 1.0
    c['c_E65'] = e
    half = 16
    inv = (np.float32(10000.0) ** (-np.arange(half, dtype=np.float32) / np.float32(half))).astype(np.float32)
    pos = np.arange(S_LEN, dtype=np.float32)
    ang = (pos[:, None] * inv[None, :]).astype(np.float32)
    tab = np.concatenate([np.cos(ang), np.sin(ang)], axis=1).astype(np.float32)
    c['c_rope'] = np.ascontiguousarray(tab.reshape(128, 128, 32).transpose(1, 0, 2).reshape(128, 128 * 32))
    return c


B_CONST_KEYS = ['c_ident_b', 'c_ident_f', 'c_tri_b', 'c_E65', 'c_rope']


def prep_B_weights(inp, l, j):
    w_in = inp['w_in'][l]
    hq = slice(416 + j * 64, 416 + (j + 1) * 64)
    hk = slice(416 + 256 + j * 64, 416 + 256 + (j + 1) * 64)
    hv = slice(928 + j * 128, 928 + (j + 1) * 128)
    ho = slice(1440 + j * 128, 1440 + (j + 1) * 128)
    hi = slice(1952 + j, 1953 + j)
    hf = slice(1956 + j, 1957 + j)
    dq = slice(1960 + j * 128, 1960 + (j + 1) * 128)
    dk = slice(2472 + j * 128, 2472 + (j + 1) * 128)
    dv = slice(2984 + j * 128, 2984 + (j + 1) * 128)
    WB = np.concatenate([w_in[:, 0:416], w_in[:, hq], w_in[:, hk], w_in[:, hv], w_in[:, ho], w_in[:, hi], w_in[:, hf],
                         w_in[:, dq], w_in[:, dk], w_in[:, dv]], axis=1)
    d = {'WB': np.ascontiguousarray(WB)}
    d['wuq'] = np.ascontiguousarray(inp['mla_w_uq'][l][:, j * 192:(j + 1) * 192])
    d['wukv'] = np.ascontiguousarray(inp['mla_w_ukv'][l][:, j * 256:(j + 1) * 256])
    qn = inp['mla_q_norm'][l].reshape(2, 128).T
    kvn = inp['mla_kv_norm'][l].reshape(1, 128).T
    d['mla_ncol'] = np.ascontiguousarray(np.concatenate([qn, kvn], axis=1))
    qg = inp['mla_q_gain'][l]
    kg = inp['mla_k_gain'][l]
    d['mla_grow'] = np.ascontiguousarray(np.concatenate([qg, qg, kg, kg])[None, :])
    return d


DIL_R = (1, 4, 16)


def sst_(c0, r, n=128):
    return slice(c0, c0 + (n - 1) * r + 1, r)


def emit_dil(S, io, c, n_sb=8):
    S.begin_phase()
    uT = io['uT']
    ident_b, E65 = c['ident_b'], c['E65']
    C0 = 802
    wd_f = S.sb([128, 8, 384], F32, "wd_f")
    S.dma('sp', wd_f[:], io['WB'][:, C0:C0 + 384].rearrange("(k p) n -> p k n", p=128), W=[wd_f])
    wd = S.sb([128, 8, 384], BF16, "wd")
    S.op('pool', lambda g: g.tensor_copy(wd[:], wd_f[:]), R=[wd_f], W=[wd])
    gcol = S.sb([128, 2], F32, "gcol")
    S.dma('sp', gcol[:], io['dil_gcol'], W=[gcol])
    S.op('dve', lambda v: v.tensor_scalar(gcol[:, 0:1], gcol[:, 0:1], 64 ** -0.5, None, ALU.mult), R=[gcol], W=[gcol])
    Bd = S.sb([128, 128], F32, "Bd")
    S.dma('sp', Bd[:], io['c_Bd'], W=[Bd])
    M4 = S.sb([128, 512], BF16, "M4")
    S.dma('sp', M4[:], io['c_M4'], W=[M4])
    uts = [S.sb([128, 8, 512], BF16, "dut%d" % i) for i in range(2)]
    KTd = [S.sb([128, 2048], BF16, "KTd%d" % i) for i in range(2)]
    QTd = [S.sb([128, 2048], BF16, "QTd%d" % i) for i in range(2)]
    VTd = [S.sb([128, 2048], BF16, "VTd%d" % i) for i in range(2)]
    Vr = [[S.sb([128, 16, 2, 65], BF16, "Vr%d_%d" % (p, ri)) for ri in range(3)] for p in range(2)]
    for p in range(2):
        for ri in range(3):
            S.op('pool', lambda g: g.memset(Vr[p][ri][:, :, :, 64:65], 1.0), W=[Vr[p][ri]])
    P0 = bank(S, "dP0")
    P1 = bank(S, "dP1")
    p_tr = bank(S, "dp_tr", BF16)
    p_sc = bank(S, "dp_sc")
    p_acc = [bank(S, "dp_acc%d" % i) for i in range(4)]
    trv = p_tr[:].rearrange("p (a b) -> p a b", b=128)
    raw = [S.sb([128, 512], F32, "draw%d" % i) for i in range(2)]
    sqt = [S.sb([128, 512], F32, "dsq%d" % i) for i in range(2)]
    lnt = [S.sb([128, 512], F32, "dln%d" % i) for i in range(2)]
    rst = [S.sb([128, 512], F32, "drs%d" % i) for i in range(2)]
    pts = [S.sb([128, 512], BF16, "dpt%d" % i) for i in range(3)]
    o_sb = [S.sb([65, 512], F32, "do_sb%d" % i) for i in range(2)]
    rden = [S.sb([64, 512], F32, "drden%d" % i) for i in range(2)]
    yts = [S.sb([128, 2048], BF16, "dyt%d" % i) for i in range(2)]
    cnt = [0, 0]

    def load_u(t):
        S.dma('sp', uts[t % 2][:], uT[:, :, t * 512:(t + 1) * 512].rearrange("k p n -> p k n"), W=[uts[t % 2]])

    def proj_tile(sb, tt):
        t = sb * 4 + tt
        ut = uts[t % 2]
        par = sb % 2
        cols = slice(tt * 512, (tt + 1) * 512)
        for which in range(3):
            for k in range(8):
                S.op('pe', lambda p: p.matmul(P0[:, :], wd[:, k, which * 128:(which + 1) * 128], ut[:, k, :], start=(k == 0), stop=(k == 7)), R=[wd, ut], W=[P0])
            if which == 2:
                S.op('act', lambda a: a.activation(VTd[par][:, cols], P0[:, :], AF.Copy), R=[P0], W=[VTd[par]])
                continue
            x = cnt[1] % 2
            cnt[1] += 1
            S.op('act', lambda a: a.activation(raw[x][:], P0[:, :], AF.Copy), R=[P0], W=[raw[x]])
            S.op('act', lambda a: a.activation(sqt[x][:], P0[:, :], AF.Square), R=[P0], W=[sqt[x]])
            S.op('pe', lambda p: p.matmul(P1[:, :], Bd[:], sqt[x][:], start=True, stop=True), R=[Bd, sqt[x]], W=[P1])
            S.op('act', lambda a: a.activation(lnt[x][:], P1[:, :], AF.Ln, scale=1.0 / 64, bias=S.eps_col[:, 0:1]), R=[P1, S.eps_t], W=[lnt[x]])
            S.op('act', lambda a: a.activation(rst[x][:], lnt[x][:], AF.Exp, scale=-0.5), R=[lnt[x]], W=[rst[x]])
            dst = QTd[par] if which == 0 else KTd[par]
            S.op('dve', lambda v: v.scalar_tensor_tensor(dst[:, cols], raw[x][:], gcol[:, which:which + 1], rst[x][:], ALU.mult, ALU.mult), R=[raw[x], gcol, rst[x]], W=[dst])

    def vtrans(sb):
        par = sb % 2
        for ri, r in enumerate(DIL_R):
            for b in range(16):
                n, rho = divmod(b, r)
                c0 = n * 128 * r + rho
                S.op('pe', lambda p: p.transpose(trv[:, b % 4, :], VTd[par][:, sst_(c0, r)], ident_b[:]), R=[VTd[par], ident_b], W=[p_tr])
                if b % 4 == 3:
                    S.op('act', lambda a: a.activation(Vr[par][ri][:, b - 3:b + 1, :, 0:64], trv[:, 0:4, :].rearrange("p a (h d) -> p a h d", h=2), AF.Copy), R=[p_tr], W=[Vr[par][ri]])

    def attention(sb, h):
        par = sb % 2
        hp = slice(h * 64, (h + 1) * 64)
        blocks = []
        for ri, r in enumerate(DIL_R):
            for b in range(16):
                n, rho = divmod(b, r)
                c0 = n * 128 * r + rho
                cur = (par, b, c0)
                if n > 0:
                    prev = (par, b - r, c0 - 128 * r)
                elif sb > 0:
                    nb = 16 // r - 1
                    prev = (1 - par, nb * r + rho, nb * 128 * r + rho)
                else:
                    prev = None
                blocks.append((ri, r, b, c0, cur, prev))
        pairs = [blocks[i:i + 2] for i in range(0, len(blocks), 2)]
        pv_all = []
        for pi, pair in enumerate(pairs):
            for u, (ri, r, b, c0, cur, prev) in enumerate(pair):
                for part, kb in enumerate((cur, prev)):
                    if kb is None:
                        continue
                    base = u * 256 + part * 128
                    lhs = Vr[kb[0]][ri][:, kb[1], h, :]
                    if r == 1:
                        pv_all.append((pi, c0 // 512, slice(c0 % 512, c0 % 512 + 128), lhs, slice(base, base + 128), Vr[kb[0]][ri]))
                    elif r == 4:
                        pv_all.append((pi, c0 // 512, sst_(c0 % 512, 4), lhs, slice(base, base + 128), Vr[kb[0]][ri]))
                    else:
                        for jb in range(4):
                            pv_all.append((pi, jb, sst_(c0, 16, 32), lhs, slice(base + 32 * jb, base + 32 * jb + 32), Vr[kb[0]][ri]))
        first = {}
        last = {}
        for idx, op in enumerate(pv_all):
            first.setdefault(op[1], idx)
            last[op[1]] = idx
        idx = 0
        for pi, pair in enumerate(pairs):
            pt = pts[cnt[0] % 3]
            cnt[0] += 1
            nmm = 0
            for u, (ri, r, b, c0, cur, prev) in enumerate(pair):
                qap = QTd[par][hp, sst_(c0, r)]
                for part, kb in enumerate((cur, prev)):
                    if kb is None:
                        kb = cur
                    kap = KTd[kb[0]][hp, sst_(kb[2], r)]
                    base = u * 256 + part * 128
                    S.op('pe', lambda p: p.matmul(p_sc[:, base:base + 128], kap, qap, start=(nmm == 0), stop=(nmm == 3)), R=[KTd[kb[0]], QTd[par]], W=[p_sc])
                    nmm += 1
            S.op('act', lambda a: a.activation(pt[:], p_sc[:, :], AF.Exp), R=[p_sc], W=[pt])
            S.op('pool', lambda g: g.tensor_tensor(pt[:], pt[:], M4[:], ALU.mult), R=[pt, M4], W=[pt])
            while idx < len(pv_all) and pv_all[idx][0] == pi:
                _, bk, osl, lhs, psl, vt = pv_all[idx]
                S.op('pe', lambda p: p.matmul(p_acc[bk][0:65, osl], lhs, pt[:, psl], start=(first[bk] == idx), stop=(last[bk] == idx)), R=[vt, pt], W=[p_acc[bk]])
                idx += 1
        yt = yts[sb % 2]
        for jb in range(4):
            osb = o_sb[jb % 2]
            rd = rden[jb % 2]
            S.op('act', lambda a: a.activation(osb[:], p_acc[jb][0:65, :], AF.Copy), R=[p_acc[jb]], W=[osb])
            S.op('pe', lambda p: p.matmul(P1[0:64, :], E65[:], osb[:], start=True, stop=True), R=[E65, osb], W=[P1])
            S.op('dve', lambda v: v.reciprocal(rd[:], P1[0:64, :]), R=[P1], W=[rd])
            S.op('dve', lambda v: v.tensor_tensor(yt[h * 64:(h + 1) * 64, jb * 512:(jb + 1) * 512], osb[0:64, :], rd[:], ALU.mult), R=[osb, rd], W=[yt])

    load_u(0)
    load_u(1)
    for sb in range(n_sb):
        for tt in range(4):
            proj_tile(sb, tt)
            if sb * 4 + tt + 2 < n_sb * 4:
                load_u(sb * 4 + tt + 2)
        vtrans(sb)
        for h in range(2):
            attention(sb, h)
        S.dma('sp', io['ycT'][:, sb * 2048:(sb + 1) * 2048], yts[sb % 2][:], R=[yts[sb % 2]])
    S.end_phase()


def host_consts_dil():
    c = {}
    bd = np.zeros((128, 128), np.float32)
    bd[0:64, 0:64] = 1.0
    bd[64:128, 64:128] = 1.0
    c['c_Bd'] = bd
    p = np.arange(128)[:, None]
    f = np.arange(128)[None, :]
    mc = (p <= f).astype(np.float32)
    mp = (p >= f).astype(np.float32)
    c['c_M4'] = np.concatenate([mc, mp, mc, mp], axis=1).astype(NPBF)
    return c


def prep_dil_vecs(inp, l):
    return {'dil_gcol': np.ascontiguousarray(np.stack([np.tile(inp['dil_q_gain'][l], 2), np.tile(inp['dil_k_gain'][l], 2)], axis=1))}


def emit_mlstm(S, io, c, n_tiles=32):
    S.begin_phase()
    uT = io['uT']
    ident_b, tri_b = c['ident_b'], c['tri_b']
    wm_f = S.sb([128, 8, 386], F32, "wm_f")
    S.dma('sp', wm_f[:], io['WB'][:, 416:802].rearrange("(k p) n -> p k n", p=128), W=[wm_f])
    wm = S.sb([128, 8, 386], BF16, "wm")
    S.op('pool', lambda g: g.tensor_copy(wm[:], wm_f[:]), R=[wm_f], W=[wm])
    cw = S.sb([64, 10], F32, "cw")
    S.dma('sp', cw[:], io['ml_conv'], W=[cw])
    gb = S.sb([1, 2], F32, "gb")
    S.dma('sp', gb[:], io['ml_gate'], W=[gb])
    nbf = S.sb([1, 1], F32, "nbf")
    S.op('dve', lambda v: v.tensor_scalar(nbf[:], gb[0:1, 1:2], -1.0, None, ALU.mult), R=[gb], W=[nbf])
    hg = S.sb([128, 128], F32, "hg")
    S.dma('sp', hg[:], io['ml_hg'].partition_broadcast(128), W=[hg])
    one = S.sb([1, 1], F32, "one")
    S.op('pool', lambda g: g.memset(one[:], 1.0), W=[one])
    ones_r = S.sb([1, 128], F32, "ones_r")
    zeros_r = S.sb([1, 128], F32, "zeros_r")
    S.op('pool', lambda g: g.memset(ones_r[:], 1.0), W=[ones_r])
    S.op('pool', lambda g: g.memset(zeros_r[:], 0.0), W=[zeros_r])
    uts = [S.sb([128, 8, 512], BF16, "mut%d" % i) for i in range(2)]
    xq = [[S.sb([64, 515], F32, "xq%d_%d" % (w, i)) for i in range(2)] for w in range(2)]
    for w in range(2):
        S.op('pool', lambda g: g.memset(xq[w][0][:, 0:3], 0.0), W=[xq[w][0]])
    cv = [S.sb([64, 512], F32, "cv%d" % i) for i in range(2)]
    ex = [S.sb([64, 512], F32, "ex%d" % i) for i in range(2)]
    qkT = [[S.sb([64, 512], BF16, "qkT%d_%d" % (w, i)) for i in range(2)] for w in range(2)]
    Brow = [S.sb([1, 128], F32, "Brow%d" % i) for i in range(2)]
    Grow = [S.sb([1, 128], F32, "Grow%d" % i) for i in range(2)]
    zero1 = S.sb([1, 1], F32, "zero1")
    S.op('pool', lambda g: g.memset(zero1[:], 0.0), W=[zero1])
    t1 = [S.sb([1, 128], F32, "mt1_%d" % i) for i in range(2)]
    t2 = [S.sb([1, 128], F32, "mt2_%d" % i) for i in range(2)]
    arow = [S.sb([1, 128], F32, "arow%d" % i) for i in range(2)]
    bg = [S.sb([1, 128], F32, "bg%d" % i) for i in range(2)]
    ngp = [S.sb([1, 1], F32, "ngp%d" % i) for i in range(2)]
    rows3 = [S.sb([1, 3, 128], F32, "rows3_%d" % i) for i in range(2)]
    cols = [S.sb([128, 4], F32, "cols%d" % i) for i in range(3)]
    ones_c = S.sb([128, 4], F32, "ones_c")
    S.op('pool', lambda g: g.memset(ones_c[:], 1.0), W=[ones_c])
    Vp = [S.sb([128, 129], BF16, "Vp%d" % i) for i in range(2)]
    so = [S.sb([128, 128], F32, "so%d" % i) for i in range(2)]
    ktok = [S.sb([128, 64], BF16, "ktok%d" % i) for i in range(2)]
    scm = [S.sb([128, 128], BF16, "scm%d" % i) for i in range(2)]
    Dst = S.sb([64, 129], F32, "Dst")
    Cb = S.sb([64, 129], BF16, "Cb")
    S.op('pool', lambda g: g.memset(Dst[:], 0.0), W=[Dst])
    sm = [[S.sb([128, 1], F32, "sm%d_%d" % (j, i)) for j in range(6)] for i in range(2)]
    hh = [S.sb([128, 128], F32, "hh%d" % i) for i in range(2)]
    hj = [S.sb([128, 128], F32, "hj%d" % i) for i in range(2)]
    y1 = [S.sb([128, 128], F32, "y1_%d" % i) for i in range(2)]
    y2 = [S.sb([128, 128], BF16, "y2_%d" % i) for i in range(2)]
    ybt = [S.sb([128, 512], BF16, "ybt%d" % i) for i in range(2)]
    P_qk = bank(S, "mP_qk")
    P_vo = [bank(S, "mP_vo%d" % i) for i in range(2)]
    P_gc = bank(S, "mP_gc")
    P_su = bank(S, "mP_su")
    P_h = [bank(S, "mP_h%d" % i) for i in range(2)]
    p_tr = bank(S, "mp_tr", BF16)

    def load_u(t):
        S.dma('sp', uts[t % 2][:], uT[:, :, t * 512:(t + 1) * 512].rearrange("k p n -> p k n"), W=[uts[t % 2]])

    def qk_tile(t):
        ut = uts[t % 2]
        for w in range(2):
            xb = xq[w][t % 2]
            for k in range(8):
                S.op('pe', lambda p: p.matmul(P_qk[0:64, :], wm[:, k, w * 64:(w + 1) * 64], ut[:, k, :], start=(k == 0), stop=(k == 7)), R=[wm, ut], W=[P_qk])
            if t > 0:
                S.op('pool', lambda g: g.tensor_copy(xb[:, 0:3], xq[w][(t - 1) % 2][:, 512:515]), R=[xq[w][(t - 1) % 2]], W=[xb])
            S.op('act', lambda a: a.activation(xb[:, 3:515], P_qk[0:64, :], AF.Copy), R=[P_qk], W=[xb])
            o = w * 5
            cvt = cv[w]
            S.op('dve', lambda v: v.tensor_scalar(cvt[:], xb[:, 3:515], cw[:, o + 3:o + 4], cw[:, o + 4:o + 5], ALU.mult, ALU.add), R=[xb, cw], W=[cvt])
            for j in (2, 1, 0):
                S.op('dve', lambda v: v.scalar_tensor_tensor(cvt[:], xb[:, j:j + 512], cw[:, o + j:o + j + 1], cvt[:], ALU.mult, ALU.add), R=[xb, cw, cvt], W=[cvt])
            ext = ex[w]
            S.op('act', lambda a: a.activation(ext[:], cvt[:], AF.Exp, scale=-1.0), R=[cvt], W=[ext])
            S.op('dve', lambda v: v.tensor_scalar_add(ext[:], ext[:], 1.0), R=[ext], W=[ext])
            S.op('dve', lambda v: v.reciprocal(ext[:], ext[:]), R=[ext], W=[ext])
            S.op('dve', lambda v: v.scalar_tensor_tensor(qkT[w][t % 2][:], cvt[:], (0.125 if w == 0 else 1.0), ext[:], ALU.mult, ALU.mult), R=[cvt, ext], W=[qkT[w][t % 2]])

    def gates_a(g):
        t, cc = divmod(g, 4)
        ut = uts[t % 2]
        x = g % 2
        csl = slice(cc * 128, (cc + 1) * 128)
        for w in range(2):
            for k in range(8):
                S.op('pe', lambda p: p.matmul(P_gc[0:1, w * 128:(w + 1) * 128], wm[:, k, 384 + w:385 + w], ut[:, k, csl], start=(k == 0), stop=(k == 7)), R=[wm, ut], W=[P_gc])
        S.op('act', lambda a: a.activation(t1[x][:], P_gc[0:1, 128:256], AF.Exp, scale=-1.0, bias=nbf[0:1, 0:1]), R=[P_gc, nbf], W=[t1[x]])
        S.op('act', lambda a: a.activation(t2[x][:], t1[x][:], AF.Ln, bias=one[0:1, 0:1]), R=[t1[x], one], W=[t2[x]])
        bprev = Brow[1 - x][0:1, 127:128] if g > 0 else zero1[0:1, 0:1]
        gprev = Grow[1 - x][0:1, 127:128] if g > 0 else zero1[0:1, 0:1]
        prevB = [Brow[1 - x]] if g > 0 else [zero1]
        prevG = [Grow[1 - x]] if g > 0 else [zero1]
        S.op('dve', lambda v: v.tensor_tensor_scan(Brow[x][:], ones_r[:], t2[x][:], bprev, ALU.mult, ALU.subtract), R=[ones_r, t2[x]] + prevB, W=[Brow[x]])
        S.op('dve', lambda v: v.scalar_tensor_tensor(arow[x][:], P_gc[0:1, 0:128], gb[0:1, 0:1], Brow[x][:], ALU.add, ALU.subtract), R=[P_gc, gb, Brow[x]], W=[arow[x]])
        S.op('dve', lambda v: v.tensor_tensor_scan(Grow[x][:], zeros_r[:], arow[x][:], gprev, ALU.add, ALU.max), R=[zeros_r, arow[x]] + prevG, W=[Grow[x]])
        S.op('dve', lambda v: v.tensor_scalar(ngp[x][:], gprev, -1.0, None, ALU.mult), R=prevG, W=[ngp[x]])
        S.op('dve', lambda v: v.tensor_tensor(bg[x][:], Brow[x][:], Grow[x][:], ALU.add), R=[Brow[x], Grow[x]], W=[bg[x]])
        S.op('act', lambda a: a.activation(rows3[x][0:1, 0, :], arow[x][:], AF.Exp, bias=ngp[x][0:1, 0:1]), R=[arow[x], ngp[x]], W=[rows3[x]])
        S.op('act', lambda a: a.activation(rows3[x][0:1, 1, :], Grow[x][:], AF.Exp, scale=-1.0, bias=gprev), R=[Grow[x]] + prevG, W=[rows3[x]])
        S.op('act', lambda a: a.activation(rows3[x][0:1, 2, :], bg[x][:], AF.Exp, scale=-1.0), R=[bg[x]], W=[rows3[x]])

    def gates_b(g):
        x = g % 2
        cl = cols[g % 3]
        for j in range(3):
            S.op('pe', lambda p: p.matmul(P_gc[:, 256 + j:257 + j], rows3[x][0:1, j, :], one[0:1, 0:1], start=(j == 0), stop=False), R=[rows3[x], one], W=[P_gc])
        S.op('pe', lambda p: p.matmul(P_gc[:, 259:260], rows3[x][0:1, 1, 127:128].to_broadcast([1, 128]), one[0:1, 0:1], start=False, stop=True), R=[rows3[x], one], W=[P_gc])
        S.op('act', lambda a: a.activation(cl[:], P_gc[:, 256:260], AF.Copy), R=[P_gc], W=[cl])

    def main(g):
        t, cc = divmod(g, 4)
        ut = uts[t % 2]
        x = g % 2
        csl = slice(cc * 128, (cc + 1) * 128)
        cl = cols[g % 3]
        clp = cols[(g - 1) % 3] if g > 0 else ones_c
        qT = qkT[0][t % 2]
        kT = qkT[1][t % 2]
        pvo = P_vo[x]
        ph = P_h[x]
        for k in range(8):
            S.op('pe', lambda p: p.matmul(pvo[:, 0:256], ut[:, k, csl], wm[:, k, 128:384], start=(k == 0), stop=(k == 7)), R=[ut, wm], W=[pvo])
        S.op('dve', lambda v: v.tensor_scalar(Vp[x][:, 0:128], pvo[:, 0:128], cl[:, 0:1], None, ALU.mult), R=[pvo, cl], W=[Vp[x]])
        S.op('act', lambda a: a.activation(Vp[x][:, 128:129], cl[:, 0:1], AF.Copy), R=[cl], W=[Vp[x]])
        S.op('act', lambda a: a.activation(so[x][:], pvo[:, 128:256], AF.Exp, scale=-1.0), R=[pvo], W=[so[x]])
        S.op('dve', lambda v: v.tensor_scalar_add(so[x][:], so[x][:], 1.0), R=[so[x]], W=[so[x]])
        S.op('dve', lambda v: v.reciprocal(so[x][:], so[x][:]), R=[so[x]], W=[so[x]])
        S.op('pe', lambda p: p.transpose(p_tr[:, 0:64], kT[:, csl], ident_b[0:64, 0:64]), R=[kT, ident_b], W=[p_tr])
        S.op('act', lambda a: a.activation(ktok[x][:], p_tr[:, 0:64], AF.Copy), R=[p_tr], W=[ktok[x]])
        S.op('pe', lambda p: p.matmul(P_su[:, 0:128], kT[:, csl], qT[:, csl], start=True, stop=True), R=[kT, qT], W=[P_su])
        S.op('dve', lambda v: v.tensor_tensor(scm[x][:], P_su[:, 0:128], tri_b[:], ALU.mult), R=[P_su, tri_b], W=[scm[x]])
        S.op('dve', lambda v: v.tensor_scalar(Cb[:], Dst[:], clp[0:64, 3:4], None, ALU.mult), R=[Dst, clp], W=[Cb])
        S.op('pe', lambda p: p.matmul(ph[:, 0:129], qT[:, csl], Cb[:], start=True, stop=False), R=[qT, Cb], W=[ph])
        S.op('pe', lambda p: p.matmul(ph[:, 0:129], scm[x][:], Vp[x][:], start=False, stop=True), R=[scm[x], Vp[x]], W=[ph])
        S.op('pe', lambda p: p.matmul(P_su[0:64, 128:257], ktok[x][:], Vp[x][:], start=True, stop=True), R=[ktok[x], Vp[x]], W=[P_su])
        S.op('dve', lambda v: v.scalar_tensor_tensor(Dst[:], Dst[:], clp[0:64, 3:4], P_su[0:64, 128:257], ALU.mult, ALU.add), R=[Dst, clp, P_su], W=[Dst])
        ta, tb, rr, r2, ssq, rs = sm[x]
        S.op('act', lambda a: a.activation(ta[:], ph[:, 128:129], AF.Abs), R=[ph], W=[ta])
        S.op('dve', lambda v: v.scalar_tensor_tensor(tb[:], ta[:], cl[:, 1:2], cl[:, 2:3], ALU.mult, ALU.max), R=[ta, cl], W=[tb])
        S.op('dve', lambda v: v.reciprocal(rr[:], tb[:]), R=[tb], W=[rr])
        S.op('dve', lambda v: v.tensor_tensor(r2[:], rr[:], cl[:, 1:2], ALU.mult), R=[rr, cl], W=[r2])
        S.op('dve', lambda v: v.tensor_scalar(hh[x][:], ph[:, 0:128], r2[:, 0:1], None, ALU.mult), R=[ph, r2], W=[hh[x]])
        S.op('act', lambda a: a.activation(hj[x][:], hh[x][:], AF.Square, accum_out=ssq[:, 0:1]), R=[hh[x]], W=[hj[x], ssq])
        S.op('act', lambda a: a.activation(ssq[:], ssq[:], AF.Ln, scale=1.0 / 128, bias=S.eps_col[:, 0:1]), R=[ssq, S.eps_t], W=[ssq])
        S.op('act', lambda a: a.activation(rs[:], ssq[:], AF.Exp, scale=-0.5), R=[ssq], W=[rs])
        S.op('dve', lambda v: v.scalar_tensor_tensor(y1[x][:], hh[x][:], rs[:, 0:1], hg[:], ALU.mult, ALU.mult), R=[hh[x], rs, hg], W=[y1[x]])
        S.op('dve', lambda v: v.tensor_tensor(y2[x][:], y1[x][:], so[x][:], ALU.mult), R=[y1[x], so[x]], W=[y2[x]])
        S.op('pe', lambda p: p.transpose(p_tr[:, 128:256], y2[x][:], ident_b[:]), R=[y2[x], ident_b], W=[p_tr])
        S.op('act', lambda a: a.activation(ybt[t % 2][:, csl], p_tr[:, 128:256], AF.Copy), R=[p_tr], W=[ybt[t % 2]])

    n_ch = n_tiles * 4
    load_u(0)
    if n_tiles > 1:
        load_u(1)
    qk_tile(0)
    gates_a(0)
    gates_b(0)
    for g in range(n_ch):
        t, cc = divmod(g, 4)
        if g + 1 < n_ch:
            if cc == 3:
                qk_tile(t + 1)
            gates_a(g + 1)
        main(g)
        if g + 1 < n_ch:
            gates_b(g + 1)
        if cc == 3:
            S.dma('sp', io['ybT'][:, t * 512:(t + 1) * 512], ybt[t % 2][:], R=[ybt[t % 2]])
            if t + 2 < n_tiles:
                load_u(t + 2)
    S.end_phase()


def prep_mlstm_vecs(inp, l, j):
    cwq = inp['mlstm_conv_w'][l][:, j * 64:(j + 1) * 64].T
    cbq = inp['mlstm_conv_b'][l][j * 64:(j + 1) * 64][:, None]
    cwk = inp['mlstm_conv_w'][l][:, 256 + j * 64:256 + (j + 1) * 64].T
    cbk = inp['mlstm_conv_b'][l][256 + j * 64:256 + (j + 1) * 64][:, None]
    d = {'ml_conv': np.ascontiguousarray(np.concatenate([cwq, cbq, cwk, cbk], axis=1))}
    d['ml_gate'] = np.ascontiguousarray(np.array([[inp['mlstm_b_i'][l][j], inp['mlstm_b_f'][l][j]]], np.float32))
    d['ml_hg'] = np.ascontiguousarray(inp['mlstm_head_gain'][l][j * 128:(j + 1) * 128][None, :])
    return d


def emit_mod(S, io):
    S.begin_phase()
    cc = S.sb([128, 8], F32, "cc")
    S.dma('sp', cc[:], io['c_col'], W=[cc])
    ca = S.sb([128, 8], F32, "ca")
    S.op('act', lambda a: a.activation(ca[:], cc[:], AF.Silu), R=[cc], W=[ca])
    brow = S.sb([1, 6144], F32, "brow")
    S.dma('sp', brow[:], io['b_ada'], W=[brow])
    orow = S.sb([1, 6144], F32, "orow")
    wt = [S.sb([128, 8, 512], F32, "wada%d" % i) for i in range(2)]
    pm = [bank(S, "pm%d" % i) for i in range(2)]
    for gi in range(12):
        w = wt[gi % 2]
        S.dma('sp', w[:], io['w_ada'][:, gi * 512:(gi + 1) * 512].rearrange("(k p) n -> p k n", p=128), W=[w])
        p = pm[gi % 2]
        for k in range(8):
            S.op('pe', lambda pe: pe.matmul(p[0:1, :], ca[:, k:k + 1], w[:, k, :], start=(k == 0), stop=(k == 7)), R=[ca, w], W=[p])
        S.op('dve', lambda v: v.tensor_tensor(orow[0:1, gi * 512:(gi + 1) * 512], p[0:1, :], brow[0:1, gi * 512:(gi + 1) * 512], ALU.add), R=[p, brow], W=[orow])
    S.dma('sp', io['mod_out'], orow[:], R=[orow])
    S.end_phase()


def norm_mod(S, xt, Acol, Bcol, outs, sq, ones_f, pss, lnt, rst, tmp, R_extra):
    n = xt.ap.shape[2]
    S.op('pool', lambda g: g.tensor_tensor(sq[:], xt[:], xt[:], ALU.mult), R=[xt], W=[sq])
    for k in range(8):
        S.op('pe', lambda p: p.matmul(pss[:, 0:n], ones_f[:], sq[:, k, :], start=(k == 0), stop=(k == 7)), R=[ones_f, sq], W=[pss])
    S.op('act', lambda a: a.activation(lnt[:], pss[:, 0:n], AF.Ln, scale=1.0 / D, bias=S.eps_col[:, 0:1]), R=[pss, S.eps_t], W=[lnt])
    S.op('act', lambda a: a.activation(rst[:], lnt[:], AF.Exp, scale=-0.5), R=[lnt], W=[rst])
    for k in range(8):
        t = tmp[k % len(tmp)]
        S.op('dve', lambda v: v.scalar_tensor_tensor(t[:], xt[:, k, :], Acol[:, k:k + 1], rst[:], ALU.mult, ALU.mult), R=[xt, rst] + R_extra, W=[t])
        for o in outs:
            S.op('act', lambda a: a.activation(o[:, k, :], t[:], AF.Identity, bias=Bcol[:, k:k + 1]), R=[t] + R_extra, W=[o])


def scol(S, name, ap_dram, shape):
    t = S.sb(list(shape), F32, name)
    S.dma('sp', t[:], ap_dram, W=[t])
    return t


def emit_C1a(S, io, c, n_tiles=8):
    S.begin_phase()
    NTL = 512
    Wg = S.sb([128, 8, 3072], BF16, "Wg")
    Wbr = [S.sb([128, 4, 1024], BF16, "Wbr%d" % i) for i in range(3)]
    stg = [S.sb([128, 8, 512], F32, "stgA%d" % i) for i in range(2)]
    for gi in range(6):
        st = stg[gi % 2]
        S.dma('sp', st[:], io['w_gate'][:, gi * 512:(gi + 1) * 512].rearrange("(k p) n -> p k n", p=128), W=[st])
        S.op('pool', lambda g: g.tensor_copy(Wg[:, :, gi * 512:(gi + 1) * 512], st[:]), R=[st], W=[Wg])
    for br in range(3):
        for hh_ in range(2):
            st = stg[(br * 2 + hh_) % 2]
            stv = st[:].rearrange("p (a k) n -> p a k n", a=2)[:, 0, :, :]
            S.dma('sp', stv, io['w_br'][br, :, hh_ * 512:(hh_ + 1) * 512].rearrange("(k p) n -> p k n", p=128), W=[st])
            S.op('pool', lambda g: g.tensor_copy(Wbr[br][:, :, hh_ * 512:(hh_ + 1) * 512], stv), R=[st], W=[Wbr[br]])
    uts = [S.sb([128, 8, NTL], BF16, "cut%d" % i) for i in range(2)]
    yts = [[S.sb([128, 4, NTL], BF16, "cyt%d_%d" % (br, i)) for i in range(2)] for br in range(3)]
    mgs = [S.sb([128, 8, NTL], BF16, "mg%d" % i) for i in range(2)]
    eg = [S.sb([128, NTL], F32, "eg%d" % i) for i in range(3)]
    mm = [S.sb([128, NTL], F32, "mm%d" % i) for i in range(2)]
    tt = [S.sb([128, NTL], F32, "tt%d" % i) for i in range(2)]
    PG = [bank(S, "PG%d" % i) for i in range(3)]
    PB = [bank(S, "PB%d" % i) for i in range(3)]

    def load(t):
        sl = slice(t * NTL, (t + 1) * NTL)
        S.dma('sp', uts[t % 2][:], io['uT_loc'][:, :, sl].rearrange("k p n -> p k n"), W=[uts[t % 2]])
        for br in range(3):
            S.dma('sp', yts[br][t % 2][:], io['yT'][br, :, :, sl].rearrange("k p n -> p k n"), W=[yts[br][t % 2]])

    load(0)
    for t in range(n_tiles):
        if t + 1 < n_tiles:
            load(t + 1)
        ut = uts[t % 2]
        mg = mgs[t % 2]
        for dc in range(8):
            for br in range(3):
                for k in range(8):
                    S.op('pe', lambda p: p.matmul(PG[br][:, :], Wg[:, k, br * 1024 + dc * 128:br * 1024 + (dc + 1) * 128], ut[:, k, :], start=(k == 0), stop=(k == 7)), R=[Wg, ut], W=[PG[br]])
                S.op('act', lambda a: a.activation(eg[br][:], PG[br][:, :], AF.Sigmoid), R=[PG[br]], W=[eg[br]])
            for br in range(3):
                yt = yts[br][t % 2]
                for k in range(4):
                    S.op('pe', lambda p: p.matmul(PB[br][:, :], Wbr[br][:, k, dc * 128:(dc + 1) * 128], yt[:, k, :], start=(k == 0), stop=(k == 3)), R=[Wbr[br], yt], W=[PB[br]])
            m = mm[dc % 2]
            S.op('dve', lambda v: v.tensor_tensor(m[:], PB[0][:, :], eg[0][:], ALU.mult), R=[PB[0], eg[0]], W=[m])
            for br in (1, 2):
                t1 = tt[br % 2]
                S.op('dve', lambda v: v.tensor_tensor(t1[:], PB[br][:, :], eg[br][:], ALU.mult), R=[PB[br], eg[br]], W=[t1])
                if br == 1:
                    S.op('pool', lambda g: g.tensor_tensor(m[:], m[:], t1[:], ALU.add), R=[m, t1], W=[m])
                else:
                    S.op('pool', lambda g: g.tensor_tensor(mg[:, dc, :], m[:], t1[:], ALU.add), R=[m, t1], W=[mg])
        S.dma('sp', io['mgT'][:, :, t * NTL:(t + 1) * NTL].rearrange("k p n -> p k n"), mg[:], R=[mg])
    S.end_phase()


def emit_C1b(S, io, c, n_tiles=8):
    S.begin_phase()
    NTL = 512
    ones_f, ident_f = c['ones_f'], c['ident_f']
    Wo = S.sb([128, 8, 1024], BF16, "Wo")
    stg = [S.sb([128, 8, 512], F32, "stgB%d" % i) for i in range(2)]
    for gi in range(2):
        st = stg[gi]
        S.dma('sp', st[:], io['w_out'][:, gi * 512:(gi + 1) * 512].rearrange("(k p) n -> p k n", p=128), W=[st])
        S.op('pool', lambda g: g.tensor_copy(Wo[:, :, gi * 512:(gi + 1) * 512], st[:]), R=[st], W=[Wo])
    Wr = S.sb([128, 8, 20], F32, "Wr")
    S.dma('sp', Wr[:], io['w_router'].rearrange("(k p) n -> p k n", p=128), W=[Wr])
    rb = S.sb([128, 20], F32, "rb")
    S.dma('sp', rb[:], io['b_router'].partition_broadcast(128), W=[rb])
    modc = scol(S, "modc", io['modc'], [128, 48])
    fng = scol(S, "fng", io['ffn_norm_col'], [128, 8])
    Af = S.sb([128, 8], F32, "Af")
    S.op('dve', lambda v: v.scalar_tensor_tensor(Af[:], modc[:, 32:40], 1.0, fng[:], ALU.add, ALU.mult), R=[modc, fng], W=[Af])
    xts = [S.sb([128, 8, NTL], F32, "xt%d" % i) for i in range(2)]
    mgs = [S.sb([128, 8, NTL], BF16, "bmg%d" % i) for i in range(2)]
    sq = S.sb([128, 8, NTL], F32, "bsq")
    hf32 = S.sb([128, 8, NTL], F32, "hf32")
    hfb = [S.sb([128, 8, NTL], BF16, "hfb%d" % i) for i in range(2)]
    lnt = S.sb([128, NTL], F32, "blnt")
    rst = S.sb([128, NTL], F32, "brst")
    tmp = [S.sb([128, NTL], F32, "btmp%d" % i) for i in range(2)]
    cwT = [S.sb([16, NTL], F32, "cwT%d" % i) for i in range(2)]
    PO = [bank(S, "PO%d" % i) for i in range(2)]
    PSS = bank(S, "PSS")
    PR = bank(S, "PR")
    PT = bank(S, "PT")
    def rt(nm, shp):
        return [S.sb(shp, F32, "%s%d" % (nm, i)) for i in range(2)]
    lgb = rt("lgb", [128, 20]); gmax = rt("gmax", [128, 1]); ngm = rt("ngm", [128, 1]); oh = rt("oh", [128, 4])
    egj = rt("egj", [128, 4]); sume = rt("sume", [128, 1]); psel = rt("psel", [128, 1]); m1 = rt("m1", [128, 4])
    is1 = rt("is1", [128, 4, 4]); E2 = rt("E2", [128, 4, 4]); m2 = rt("m2", [128, 4]); sel = rt("sel", [128, 4, 4])
    exx = rt("exx", [128, 4, 4]); den = rt("den", [128, 4]); fac = rt("fac", [128, 4]); cw = rt("cw", [128, 4, 4])

    def load(t):
        sl = slice(t * NTL, (t + 1) * NTL)
        S.dma('sp', xts[t % 2][:], io['xT'][:, :, sl].rearrange("k p n -> p k n"), W=[xts[t % 2]])
        S.dma('sp', mgs[t % 2][:], io['mgT'][:, :, sl].rearrange("k p n -> p k n"), W=[mgs[t % 2]])

    def bc4(ap):
        return ap.unsqueeze(2).to_broadcast([128, 4, 4])

    load(0)
    for t in range(n_tiles):
        if t + 1 < n_tiles:
            load(t + 1)
        xt = xts[t % 2]
        mg = mgs[t % 2]
        sl = slice(t * NTL, (t + 1) * NTL)
        for dc in range(8):
            po = PO[dc % 2]
            for k in range(8):
                S.op('pe', lambda p: p.matmul(po[:, :], Wo[:, k, dc * 128:(dc + 1) * 128], mg[:, k, :], start=(k == 0), stop=(k == 7)), R=[Wo, mg], W=[po])
            S.op('dve', lambda v: v.scalar_tensor_tensor(xt[:, dc, :], po[:, :], modc[:, 16 + dc:17 + dc], xt[:, dc, :], ALU.mult, ALU.add), R=[po, modc, xt], W=[xt])
        S.dma('sp', io['x1T'][:, :, sl].rearrange("k p n -> p k n"), xt[:], R=[xt])
        hb = hfb[t % 2]
        norm_mod(S, xt, Af[:], modc[:, 24:32], [hb, hf32], sq, ones_f, PSS, lnt, rst, tmp, [Af, modc])
        S.dma('sp', io['hfT'][:, :, sl].rearrange("k p n -> p k n"), hb[:], R=[hb])
        ct = cwT[t % 2]
        for s_ in range(4):
            x = s_ % 2
            for k in range(8):
                S.op('pe', lambda p: p.matmul(PR[:, 0:20], hf32[:, k, s_ * 128:(s_ + 1) * 128], Wr[:, k, :], start=(k == 0), stop=(k == 7)), R=[hf32, Wr], W=[PR])
            S.op('dve', lambda v: v.tensor_tensor(lgb[x][:], PR[:, 0:20], rb[:], ALU.add), R=[PR, rb], W=[lgb[x]])
            G = lgb[x][:, 0:4]
            E = lgb[x][:, 4:20].rearrange("p (g e) -> p g e", g=4)
            S.op('dve', lambda v: v.tensor_reduce(gmax[x][:], G, AX.X, ALU.max), R=[lgb[x]], W=[gmax[x]])
            S.op('dve', lambda v: v.tensor_scalar(ngm[x][:], gmax[x][:], -1.0, None, ALU.mult), R=[gmax[x]], W=[ngm[x]])
            S.op('dve', lambda v: v.tensor_scalar(oh[x][:], G, gmax[x][:, 0:1], None, ALU.is_equal), R=[lgb[x], gmax[x]], W=[oh[x]])
            S.op('act', lambda a: a.activation(egj[x][:], G, AF.Exp, bias=ngm[x][:, 0:1], accum_out=sume[x][:, 0:1]), R=[lgb[x], ngm[x]], W=[egj[x], sume[x]])
            S.op('dve', lambda v: v.reciprocal(psel[x][:], sume[x][:]), R=[sume[x]], W=[psel[x]])
            S.op('dve', lambda v: v.tensor_reduce(m1[x][:], E, AX.X, ALU.max), R=[lgb[x]], W=[m1[x]])
            S.op('dve', lambda v: v.tensor_tensor(is1[x][:], E, bc4(m1[x][:]), ALU.is_equal), R=[lgb[x], m1[x]], W=[is1[x]])
            S.op('dve', lambda v: v.scalar_tensor_tensor(E2[x][:], is1[x][:], -1e30, E, ALU.mult, ALU.add), R=[is1[x], lgb[x]], W=[E2[x]])
            S.op('dve', lambda v: v.tensor_reduce(m2[x][:], E2[x][:], AX.X, ALU.max), R=[E2[x]], W=[m2[x]])
            S.op('dve', lambda v: v.tensor_tensor(sel[x][:], E, bc4(m2[x][:]), ALU.is_ge), R=[lgb[x], m2[x]], W=[sel[x]])
            S.op('dve', lambda v: v.tensor_tensor(exx[x][:], E, bc4(m1[x][:]), ALU.subtract), R=[lgb[x], m1[x]], W=[exx[x]])
            S.op('act', lambda a: a.activation(exx[x][:], exx[x][:], AF.Exp), R=[exx[x]], W=[exx[x]])
            S.op('dve', lambda v: v.tensor_tensor(exx[x][:], exx[x][:], sel[x][:], ALU.mult), R=[exx[x], sel[x]], W=[exx[x]])
            S.op('dve', lambda v: v.tensor_reduce(den[x][:], exx[x][:], AX.X, ALU.add), R=[exx[x]], W=[den[x]])
            S.op('dve', lambda v: v.reciprocal(den[x][:], den[x][:]), R=[den[x]], W=[den[x]])
            S.op('dve', lambda v: v.tensor_tensor(fac[x][:], den[x][:], oh[x][:], ALU.mult), R=[den[x], oh[x]], W=[fac[x]])
            S.op('dve', lambda v: v.tensor_scalar(fac[x][:], fac[x][:], psel[x][:, 0:1], None, ALU.mult), R=[fac[x], psel[x]], W=[fac[x]])
            S.op('dve', lambda v: v.tensor_tensor(cw[x][:], exx[x][:], bc4(fac[x][:]), ALU.mult), R=[exx[x], fac[x]], W=[cw[x]])
            S.op('pe', lambda p: p.transpose(PT[0:16, 0:128], cw[x][:].rearrange("p g e -> p (g e)"), ident_f[:]), R=[cw[x], ident_f], W=[PT])
            S.op('act', lambda a: a.activation(ct[:, s_ * 128:(s_ + 1) * 128], PT[0:16, 0:128], AF.Copy), R=[PT], W=[ct])
        S.dma('sp', io['cwT'][:, sl], ct[:], R=[ct])
    S.end_phase()


def emit_C2(S, io, c, last_layer, n_half=2):
    S.begin_phase()
    NTL = 512
    NE = 256
    HT = 2048
    ones_f = c['ones_f']
    modc = scol(S, "modc2", io['modc'], [128, 48])
    Sel = S.sb([16, 16 * 128], F32, "Sel")
    S.dma('sp', Sel[:], io['c_Sel'], W=[Sel])
    if not last_layer:
        modn = scol(S, "modn", io['modn'], [128, 16])
        ang = scol(S, "ang", io['attn_norm_col'], [128, 8])
        Aa = S.sb([128, 8], F32, "Aa")
        S.op('dve', lambda v: v.scalar_tensor_tensor(Aa[:], modn[:, 8:16], 1.0, ang[:], ALU.add, ALU.mult), R=[modn, ang], W=[Aa])
    hf = S.sb([128, 8, HT], BF16, "hfh")
    cwt = S.sb([16, HT], F32, "cwh")
    acc = S.sb([128, 8, HT], F32, "macc")
    acc_t = [[S.sub(acc[:, dc, tl * NTL:(tl + 1) * NTL], "acc%d_%d" % (dc, tl)) for tl in range(4)] for dc in range(8)]
    stg = [S.sb([128, 8, 256], F32, "stgC%d" % i) for i in range(2)]
    Wge = [S.sb([128, 8, 256], BF16, "Wge%d" % i) for i in range(2)]
    Wue = [S.sb([128, 8, 256], BF16, "Wue%d" % i) for i in range(2)]
    Wde = [S.sb([128, 2, 1024], BF16, "Wde%d" % i) for i in range(2)]
    cwb = [S.sb([128, NTL], F32, "cwb%d" % i) for i in range(2)]
    sg = [S.sb([128, NTL], F32, "sg%d" % i) for i in range(2)]
    h1 = [S.sb([128, NTL], F32, "h1_%d" % i) for i in range(2)]
    pt_ = [S.sb([128, NTL], F32, "ptl%d" % i) for i in range(2)]
    hid = [S.sb([128, 2, NTL], BF16, "hid%d" % i) for i in range(2)]
    xt = S.sb([128, 8, NE], F32, "c2xt")
    sq = S.sb([128, 8, NE], F32, "c2sq")
    ub = S.sb([128, 8, NE], BF16, "c2ub")
    lnt = S.sb([128, NE], F32, "c2ln")
    rst = S.sb([128, NE], F32, "c2rs")
    tmp = [S.sb([128, NE], F32, "c2tmp%d" % i) for i in range(2)]
    PGU = [bank(S, "PGU%d" % i) for i in range(4)]
    PD = [bank(S, "PD%d" % i) for i in range(2)]
    PCW = bank(S, "PCW")
    PSS = bank(S, "PSS2")
    nst = [0]

    def load_expert(e, slot):
        for (dst, src) in ((Wge[slot], io['w_eg'][e]), (Wue[slot], io['w_eu'][e])):
            st = stg[nst[0] % 2]
            nst[0] += 1
            S.dma('sp', st[:], src.rearrange("(k p) n -> p k n", p=128), W=[st])
            S.op('pool', lambda g: g.tensor_copy(dst[:], st[:]), R=[st], W=[dst])
        st = stg[nst[0] % 2]
        nst[0] += 1
        stv = st[:].rearrange("p (a k) n -> p a (k n)", a=2)
        S.dma('sp', stv, io['w_ed'][e].rearrange("(k p) n -> p k n", p=128), W=[st])
        S.op('pool', lambda g: g.tensor_copy(Wde[slot][:], stv), R=[st], W=[Wde[slot]])

    nx = [0]
    for half in range(n_half):
        h0 = half * HT
        S.dma('sp', hf[:], io['hfT'][:, :, h0:h0 + HT].rearrange("k p n -> p k n"), W=[hf])
        S.dma('sp', cwt[:], io['cwT'][:, h0:h0 + HT], W=[cwt])
        load_expert(0, 0)
        for e in range(16):
            slot = e % 2
            if e + 1 < 16:
                load_expert(e + 1, (e + 1) % 2)
            for tl in range(4):
                tsl = slice(tl * NTL, (tl + 1) * NTL)
                hd = hid[nx[0] % 2]
                cb = cwb[nx[0] % 2]
                nx[0] += 1
                S.op('pe', lambda p: p.matmul(PCW[:, :], Sel[:, e * 128:(e + 1) * 128], cwt[:, tsl], start=True, stop=True), R=[Sel, cwt], W=[PCW])
                S.op('act', lambda a: a.activation(cb[:], PCW[:, :], AF.Copy), R=[PCW], W=[cb])
                for fc in range(2):
                    pg = PGU[fc * 2]
                    pu = PGU[fc * 2 + 1]
                    for k in range(8):
                        S.op('pe', lambda p: p.matmul(pg[:, :], Wge[slot][:, k, fc * 128:(fc + 1) * 128], hf[:, k, tsl], start=(k == 0), stop=(k == 7)), R=[Wge[slot], hf], W=[pg])
                    for k in range(8):
                        S.op('pe', lambda p: p.matmul(pu[:, :], Wue[slot][:, k, fc * 128:(fc + 1) * 128], hf[:, k, tsl], start=(k == 0), stop=(k == 7)), R=[Wue[slot], hf], W=[pu])
                    s1 = sg[fc]
                    S.op('act', lambda a: a.activation(s1[:], pg[:, :], AF.Silu), R=[pg], W=[s1])
                    S.op('dve', lambda v: v.tensor_tensor(h1[fc][:], pu[:, :], s1[:], ALU.mult), R=[pu, s1], W=[h1[fc]])
                    S.op('pool', lambda g: g.tensor_tensor(hd[:, fc, :], h1[fc][:], cb[:], ALU.mult), R=[h1[fc], cb], W=[hd])
                for dc in range(8):
                    pd = PD[dc % 2]
                    for fc in range(2):
                        S.op('pe', lambda p: p.matmul(pd[:, :], Wde[slot][:, fc, dc * 128:(dc + 1) * 128], hd[:, fc, :], start=(fc == 0), stop=(fc == 1)), R=[Wde[slot], hd], W=[pd])
                    at = acc_t[dc][tl]
                    if e == 0:
                        S.op('act', lambda a: a.activation(acc[:, dc, tsl], pd[:, :], AF.Copy), R=[pd], W=[at])
                    else:
                        S.op('dve', lambda v: v.tensor_tensor(acc[:, dc, tsl], pd[:, :], acc[:, dc, tsl], ALU.add), R=[pd, at], W=[at])
        for tl in range(HT // NE):
            gsl = slice(h0 + tl * NE, h0 + (tl + 1) * NE)
            tsl = slice(tl * NE, (tl + 1) * NE)
            ats = [acc_t[dc][(tl * NE) // NTL] for dc in range(8)]
            S.dma('sp', xt[:], io['x1T'][:, :, gsl].rearrange("k p n -> p k n"), W=[xt])
            for dc in range(8):
                S.op('dve', lambda v: v.scalar_tensor_tensor(xt[:, dc, :], acc[:, dc, tsl], modc[:, 40 + dc:41 + dc], xt[:, dc, :], ALU.mult, ALU.add), R=[ats[dc], modc, xt], W=[xt])
            S.dma('sp', io['xT_out'][:, :, gsl].rearrange("k p n -> p k n"), xt[:], R=[xt])
            if not last_layer:
                norm_mod(S, xt, Aa[:], modn[:, 0:8], [ub], sq, ones_f, PSS, lnt, rst, tmp, [Aa, modn])
                S.dma('sp', io['uT_out'][:, :, gsl].rearrange("k p n -> p k n"), ub[:], R=[ub])
    S.end_phase()


def emit_A(S, io, c, n_tiles=8):
    S.begin_phase()
    NTL = 512
    ones_f = c['ones_f']
    modn = scol(S, "modnA", io['modn'], [128, 16])
    ang = scol(S, "angA", io['attn_norm_col'], [128, 8])
    Aa = S.sb([128, 8], F32, "AaA")
    S.op('dve', lambda v: v.scalar_tensor_tensor(Aa[:], modn[:, 8:16], 1.0, ang[:], ALU.add, ALU.mult), R=[modn, ang], W=[Aa])
    xt = [S.sb([128, 8, NTL], F32, "axt%d" % i) for i in range(2)]
    ub = [S.sb([128, 8, NTL], BF16, "aub%d" % i) for i in range(2)]
    sq = S.sb([128, 8, NTL], F32, "asq")
    lnt = S.sb([128, NTL], F32, "aln")
    rst = S.sb([128, NTL], F32, "ars")
    tmp = [S.sb([128, NTL], F32, "atmp%d" % i) for i in range(2)]
    PSS = bank(S, "PSSA")
    for t in range(n_tiles):
        sl = slice(t * NTL, (t + 1) * NTL)
        x = xt[t % 2]
        S.dma('sp', x[:], io['xT'][:, :, sl].rearrange("k p n -> p k n"), W=[x])
        u = ub[t % 2]
        norm_mod(S, x, Aa[:], modn[:, 0:8], [u], sq, ones_f, PSS, lnt, rst, tmp, [Aa, modn])
        S.dma('sp', io['uT_out'][:, :, sl].rearrange("k p n -> p k n"), u[:], R=[u])
    S.end_phase()


CONST_SPECS = {'c_ident_b': ([128, 128], BF16), 'c_ident_f': ([128, 128], F32), 'c_tri_b': ([128, 128], BF16),
               'c_E65': ([65, 64], F32)}


def _declare(nc, specs, kind):
    io = {}
    for name, (shape, dt) in specs.items():
        io[name] = nc.dram_tensor(name, list(shape), dt, kind=kind).ap()
    return io


def build_M():
    nc = bass.Bass("TRN2", target_bir_lowering=False)
    io = _declare(nc, {'c_col': ([128, 8], F32), 'w_ada': ([1024, 6144], F32), 'b_ada': ([1, 6144], F32)}, "ExternalInput")
    io.update(_declare(nc, {'mod_out': ([1, 6144], F32)}, "ExternalOutput"))
    with ExitStack() as st:
        S = Sched(nc, st)
        emit_mod(S, io)
        S.finish_all()
    return nc


A_IN = {'xT': ([8, 128, TOK], F32), 'modn': ([128, 16], F32), 'attn_norm_col': ([128, 8], F32)}


def build_A():
    nc = bass.Bass("TRN2", target_bir_lowering=False)
    io = _declare(nc, dict(A_IN, **CONST_SPECS), "ExternalInput")
    io.update(_declare(nc, {'uT_out': ([8, 128, TOK], BF16)}, "ExternalOutput"))
    with ExitStack() as st:
        S = Sched(nc, st)
        c = load_consts(S, io)
        emit_A(S, io, c)
        S.finish_all()
    return nc


B_IN = {'uT': ([8, 128, S_LEN], BF16), 'WB': ([1024, 1186], F32), 'wuq': ([256, 192], F32), 'wukv': ([128, 256], F32),
        'mla_ncol': ([128, 3], F32), 'mla_grow': ([1, 384], F32), 'c_rope': ([128, 4096], F32),
        'c_Bd': ([128, 128], F32), 'c_M4': ([128, 512], BF16), 'dil_gcol': ([128, 2], F32),
        'ml_conv': ([64, 10], F32), 'ml_gate': ([1, 2], F32), 'ml_hg': ([1, 128], F32)}


def build_B():
    nc = bass.Bass("TRN2", target_bir_lowering=False)
    io = _declare(nc, dict(B_IN, **CONST_SPECS), "ExternalInput")
    io.update(_declare(nc, {'yaT': ([128, S_LEN], BF16), 'ybT': ([128, S_LEN], BF16), 'ycT': ([128, S_LEN], BF16)}, "ExternalOutput"))
    with ExitStack() as st:
        S = Sched(nc, st)
        c = load_consts(S, io)
        emit_mlstm(S, io, c)
        emit_dil(S, io, c)
        emit_mla(S, io, c)
        S.finish_all()
    return nc


C_IN = {'xT': ([8, 128, TOK], F32), 'uT_loc': ([8, 128, TOK], BF16), 'yT': ([3, 4, 128, TOK], BF16),
        'w_gate': ([1024, 3072], F32), 'w_br': ([3, 512, 1024], F32), 'w_out': ([1024, 1024], F32),
        'w_router': ([1024, 20], F32), 'b_router': ([1, 20], F32),
        'w_eg': ([16, 1024, 256], F32), 'w_eu': ([16, 1024, 256], F32), 'w_ed': ([16, 256, 1024], F32),
        'modc': ([128, 48], F32), 'modn': ([128, 16], F32), 'ffn_norm_col': ([128, 8], F32), 'attn_norm_col': ([128, 8], F32),
        'c_Sel': ([16, 2048], F32)}


def build_C():
    nc = bass.Bass("TRN2", target_bir_lowering=False)
    io = _declare(nc, dict(C_IN, **CONST_SPECS), "ExternalInput")
    io.update(_declare(nc, {'xT_out': ([8, 128, TOK], F32), 'uT_out': ([8, 128, TOK], BF16)}, "ExternalOutput"))
    io.update(_declare(nc, {'mgT': ([8, 128, TOK], BF16), 'x1T': ([8, 128, TOK], F32), 'hfT': ([8, 128, TOK], BF16),
                            'cwT': ([16, TOK], F32)}, "Internal"))
    with ExitStack() as st:
        S = Sched(nc, st)
        c = load_consts(S, io)
        emit_C1a(S, io, c)
        emit_C1b(S, io, c)
        emit_C2(S, io, c, last_layer=False)
        S.finish_all()
    return nc


def col8(v):
    return np.ascontiguousarray(np.asarray(v, np.float32).reshape(8, 128).T)


def _run(nc, in_maps):
    res = run_bass_kernel_spmd(nc, in_maps, core_ids=list(range(8)))
    return res.results


def kernel(**inp):
    inp = {k: np.asarray(v) for k, v in inp.items()}
    x = inp['x'].astype(np.float32, copy=False)
    cst = host_consts()
    cst.update(host_consts_dil())
    sel = np.zeros((16, 16, 128), np.float32)
    for e in range(16):
        sel[e, e, :] = 1.0
    cst['c_Sel'] = np.ascontiguousarray(sel.reshape(16, 2048))
    base_c = {k: cst[k] for k in CONST_SPECS}

    ncM = build_M()
    maps = []
    for cidx in range(8):
        l, b = cidx // 2, cidx % 2
        maps.append({'c_col': col8(inp['c'][b]), 'w_ada': np.ascontiguousarray(inp['w_ada'][l]),
                     'b_ada': np.ascontiguousarray(inp['b_ada'][l][None, :])})
    r = _run(ncM, maps)
    mod = np.zeros((DEPTH, NB, 6, 1024), np.float32)
    for cidx in range(8):
        mod[cidx // 2, cidx % 2] = r[cidx]['mod_out'].reshape(6, 1024)

    def modcols(l, b, rows):
        return np.ascontiguousarray(np.concatenate([col8(mod[l, b, i]) for i in rows], axis=1))

    xT = []
    for cidx in range(8):
        b, j = cidx // 4, cidx % 4
        xT.append(np.ascontiguousarray(x[b, j * TOK:(j + 1) * TOK, :].T.reshape(8, 128, TOK)))
    ncA = build_A()
    maps = []
    for cidx in range(8):
        b = cidx // 4
        m = dict(base_c)
        m.update({'xT': xT[cidx], 'modn': modcols(0, b, (0, 1)), 'attn_norm_col': col8(inp['attn_norm'][0])})
        maps.append(m)
    r = _run(ncA, maps)
    uT_loc = [r[cidx]['uT_out'] for cidx in range(8)]

    ncB = build_B()
    ncC = build_C()
    for l in range(DEPTH):
        uT_full = [np.ascontiguousarray(np.concatenate([uT_loc[b * 4 + j] for j in range(4)], axis=2)) for b in range(NB)]
        maps = []
        for cidx in range(8):
            b, j = cidx // 4, cidx % 4
            m = dict(base_c)
            m.update(prep_B_weights(inp, l, j))
            m.update(prep_dil_vecs(inp, l))
            m.update(prep_mlstm_vecs(inp, l, j))
            m.update({'uT': uT_full[b], 'c_rope': cst['c_rope'], 'c_Bd': cst['c_Bd'], 'c_M4': cst['c_M4']})
            maps.append(m)
        rB = _run(ncB, maps)
        ln = min(l + 1, DEPTH - 1)
        wC = {'w_gate': np.ascontiguousarray(inp['w_in'][l][:, 3496:6568]),
              'w_br': np.ascontiguousarray(np.stack([inp['w_branch_a'][l], inp['w_branch_b'][l], inp['w_branch_c'][l]])),
              'w_out': np.ascontiguousarray(inp['w_out'][l]),
              'w_router': np.ascontiguousarray(np.concatenate([inp['w_router_group'][l], inp['w_router_expert'][l]], axis=1)),
              'b_router': np.ascontiguousarray(np.concatenate([inp['b_router_group'][l], inp['b_router_expert'][l]])[None, :]),
              'w_eg': np.ascontiguousarray(inp['w_exp_gate'][l]), 'w_eu': np.ascontiguousarray(inp['w_exp_up'][l]),
              'w_ed': np.ascontiguousarray(inp['w_exp_down'][l]),
              'ffn_norm_col': col8(inp['ffn_norm'][l]), 'attn_norm_col': col8(inp['attn_norm'][ln]), 'c_Sel': cst['c_Sel']}
        maps = []
        for cidx in range(8):
            b, j = cidx // 4, cidx % 4
            sl = slice(j * TOK, (j + 1) * TOK)
            yT = np.stack([np.stack([rB[b * 4 + jj][nm][:, sl] for jj in range(4)]) for nm in ('yaT', 'ybT', 'ycT')])
            m = dict(base_c)
            m.update(wC)
            m.update({'xT': xT[cidx], 'uT_loc': uT_loc[cidx], 'yT': np.ascontiguousarray(yT),
                      'modc': modcols(l, b, range(6)), 'modn': modcols(ln, b, (0, 1))})
            maps.append(m)
        rC = _run(ncC, maps)
        xT = [rC[cidx]['xT_out'] for cidx in range(8)]
        uT_loc = [rC[cidx]['uT_out'] for cidx in range(8)]

    out = np.empty((NB, S_LEN, D), np.float32)
    for cidx in range(8):
        b, j = cidx // 4, cidx % 4
        out[b, j * TOK:(j + 1) * TOK, :] = xT[cidx].reshape(D, TOK).T
    return out
```

```python
import numpy as np
import ml_dtypes
from contextlib import ExitStack
import concourse.bass as bass
import concourse.mybir as mybir
from concourse.bass_utils import run_bass_kernel_spmd

F32 = mybir.dt.float32
BF16 = mybir.dt.bfloat16
AF = mybir.ActivationFunctionType
ALU = mybir.AluOpType
AX = mybir.AxisListType
NPBF = ml_dtypes.bfloat16

D = 1024
S_LEN = 16384
NB = 2
DEPTH = 4
EPS = 1e-6
TOK = 4096
NT = 512


class T:
    __slots__ = ("ap", "name", "lw", "rs", "dsem", "dcnt")

    def __init__(self, ap, name):
        self.ap = ap
        self.name = name
        self.lw = None
        self.rs = {}
        self.dsem = None
        self.dcnt = 0

    def __getitem__(self, k):
        return self.ap[k]


class Sched:
    SEM_MAX = 30000

    def __init__(self, nc, stack):
        self.nc = nc
        self.root = stack
        self.eng = {'pe': nc.tensor, 'act': nc.scalar, 'dve': nc.vector, 'pool': nc.gpsimd, 'sp': nc.sync}
        self.sem = {}
        self.cnt = {}
        self.nsem = 0
        for e in self.eng:
            self._newsem(e)
        self.waited = {e: {} for e in self.eng}
        self.phase = None
        self.phase_tiles = []
        self.dma_pool = []
        self.all_dma = {}
        self.cc_sem = None
        self.ninst = 0

    def _newsem(self, e):
        self.nsem += 1
        self.sem[e] = self.root.enter_context(self.nc.semaphore("s_%s_%d" % (e, self.nsem)))
        self.cnt[e] = 0

    def begin_phase(self):
        self.phase = ExitStack()
        self.phase_tiles = []

    def end_phase(self):
        deps = []
        for t in self.phase_tiles:
            if t.lw is not None:
                deps.append(t.lw)
            deps.extend(t.rs.values())
        self._wait('sp', deps, True)
        self.sp_mark()
        self.barrier()
        for t in self.phase_tiles:
            if t.dsem is not None:
                self.dma_pool.append((t.dsem, t.dcnt))
        self.phase.close()
        self.phase = None
        self.phase_tiles = []

    def barrier(self):
        tags = [(self.sem[e], self.cnt[e], e) for e in self.eng if self.cnt[e] > 0]
        for e in self.eng:
            self._wait(e, tags, False)

    def sp_mark(self):
        ins = self.eng['sp'].sem_inc(self.sem['sp'], 1)
        self.cnt['sp'] += 1

    def collective(self, src_ap, dst_ap, deps, groups=((0, 1, 2, 3), (4, 5, 6, 7))):
        if self.cc_sem is None:
            self.cc_sem = self.root.enter_context(self.nc.semaphore("cc_sem"))
            self.cc_cnt = 0
        self._wait('pool', list(deps), True)
        ins = self.eng['pool'].collective_compute("AllGather", ALU.bypass, replica_groups=[list(g) for g in groups], ins=[src_ap], outs=[dst_ap])
        self.cc_cnt += 16
        ins.then_inc(self.cc_sem, 16)
        tag = (self.cc_sem, self.cc_cnt, 'dma')
        self.all_dma[id(self.cc_sem)] = tag
        self._wait('sp', [tag], True)
        return tag

    def sb(self, shape, dt, name):
        st = self.phase if self.phase is not None else self.root
        self.nsem += 1
        t = T(st.enter_context(self.nc.sbuf_tensor("sb_%s_%d" % (name, self.nsem), list(shape), dt)), name)
        if self.phase is not None:
            self.phase_tiles.append(t)
        return t

    def ps(self, shape, dt, name):
        st = self.phase if self.phase is not None else self.root
        self.nsem += 1
        t = T(st.enter_context(self.nc.psum_tensor("ps_%s_%d" % (name, self.nsem), list(shape), dt)), name)
        if self.phase is not None:
            self.phase_tiles.append(t)
        return t

    def sub(self, ap, name):
        t = T(ap, name)
        if self.phase is not None:
            self.phase_tiles.append(t)
        return t

    def _wait(self, e, deps, is_dma):
        w = self.waited[e]
        for (sem, val, de) in deps:
            if de == e and not is_dma and e == 'pe':
                continue
            key = id(sem)
            if w.get(key, 0) >= val:
                continue
            self.eng[e].wait_ge(sem, val)
            w[key] = val

    def _deps(self, R, W):
        deps = []
        for t in R:
            if t.lw is not None:
                deps.append(t.lw)
        for t in W:
            if t.lw is not None:
                deps.append(t.lw)
            deps.extend(t.rs.values())
        return deps

    def _mark(self, tag, R, W):
        sem = tag[0]
        for t in W:
            t.lw = tag
            t.rs = {}
        for t in R:
            t.rs[id(sem)] = tag

    def op(self, e, fn, R=(), W=()):
        self._wait(e, self._deps(R, W), False)
        if self.cnt[e] >= self.SEM_MAX:
            self._newsem(e)
        ins = fn(self.eng[e])
        self.cnt[e] += 1
        self.ninst += 1
        ins.then_inc(self.sem[e], 1)
        self._mark((self.sem[e], self.cnt[e], e), R, W)
        return ins

    def dma(self, q, out_ap, in_ap, R=(), W=(), owner=None):
        if q == 'pool':
            q = 'sp'
        self._wait(q, self._deps(R, W), True)
        if owner is None:
            owner = (list(W) + list(R))[0]
        if owner.dsem is None or owner.dcnt >= self.SEM_MAX:
            if owner.dsem is None and self.dma_pool:
                owner.dsem, owner.dcnt = self.dma_pool.pop()
            else:
                owner.dsem = self.root.enter_context(self.nc.semaphore("d_%d" % self.nsem))
                self.nsem += 1
                owner.dcnt = 0
        ins = self.eng[q].dma_start(out=out_ap, in_=in_ap)
        owner.dcnt += 16
        self.ninst += 1
        ins.then_inc(owner.dsem, 16)
        tag = (owner.dsem, owner.dcnt, 'dma')
        self.all_dma[id(owner.dsem)] = tag
        self._mark(tag, R, W)
        return tag

    def finish_all(self):
        self._wait('sp', list(self.all_dma.values()), True)
        self.barrier()

    def finish(self, tiles):
        deps = []
        for t in tiles:
            if t.lw is not None:
                deps.append(t.lw)
            deps.extend(t.rs.values())
        self._wait('sp', deps, True)


def bank(S, name, dt=F32):
    return S.ps([128, 512 if dt == F32 else 1024], dt, name)


def rsqrt_mean(S, out_ap, in_ap, n, tmp_ap, R, W):
    S.op('act', lambda a: a.activation(tmp_ap, in_ap, AF.Ln, scale=1.0 / n, bias=S.eps_col[0:tmp_ap.shape[0], 0:1]), R=R + [S.eps_t], W=W)
    S.op('act', lambda a: a.activation(out_ap, tmp_ap, AF.Exp, scale=-0.5), R=W, W=W)


def load_consts(S, io):
    c = {}
    c['ident_b'] = S.sb([128, 128], BF16, "ident_b")
    c['ident_f'] = S.sb([128, 128], F32, "ident_f")
    c['tri_b'] = S.sb([128, 128], BF16, "tri_b")
    c['E65'] = S.sb([65, 64], F32, "E65")
    c['ones_f'] = S.sb([128, 128], F32, "ones_f")
    S.eps_t = S.sb([128, 1], F32, "eps_t")
    S.eps_col = S.eps_t.ap
    S.dma('sp', c['ident_b'][:], io['c_ident_b'], W=[c['ident_b']])
    S.dma('sp', c['ident_f'][:], io['c_ident_f'], W=[c['ident_f']])
    S.dma('sp', c['tri_b'][:], io['c_tri_b'], W=[c['tri_b']])
    S.dma('sp', c['E65'][:], io['c_E65'], W=[c['E65']])
    S.op('pool', lambda g: g.memset(c['ones_f'][:], 1.0), W=[c['ones_f']])
    S.op('pool', lambda g: g.memset(S.eps_t[:], EPS), W=[S.eps_t])
    return c


def emit_mla(S, io, c, n_tiles=32):
    S.begin_phase()
    uT = io['uT']
    ident_b, tri_b, E65 = c['ident_b'], c['tri_b'], c['E65']
    wl_f = S.sb([128, 8, 416], F32, "wl_f")
    S.dma('sp', wl_f[:], io['WB'][:, 0:416].rearrange("(k p) n -> p k n", p=128), W=[wl_f])
    wl = S.sb([128, 8, 416], BF16, "wl")
    S.op('pool', lambda g: g.tensor_copy(wl[:], wl_f[:]), R=[wl_f], W=[wl])
    wuq_f = S.sb([128, 2, 192], F32, "wuq_f")
    S.dma('sp', wuq_f[:], io['wuq'].rearrange("(k p) n -> p k n", p=128), W=[wuq_f])
    wukv_f = S.sb([128, 256], F32, "wukv_f")
    S.dma('sp', wukv_f[:], io['wukv'], W=[wukv_f])
    ncol = S.sb([128, 3], F32, "ncol")
    S.dma('sp', ncol[:], io['mla_ncol'], W=[ncol])
    wuq = S.sb([128, 2, 192], BF16, "wuq")
    wukv = S.sb([128, 256], BF16, "wukv")
    for k in range(2):
        S.op('dve', lambda v: v.tensor_scalar(wuq[:, k, :], wuq_f[:, k, :], ncol[:, k:k + 1], None, ALU.mult), R=[wuq_f, ncol], W=[wuq])
    S.op('dve', lambda v: v.tensor_scalar(wukv[:], wukv_f[:], ncol[:, 2:3], None, ALU.mult), R=[wukv_f, ncol], W=[wukv])
    g4 = S.sb([128, 384], F32, "g4")
    S.dma('sp', g4[:], io['mla_grow'].partition_broadcast(128), W=[g4])
    S.op('dve', lambda v: v.tensor_scalar(g4[:, 0:192], g4[:, 0:192], 96 ** -0.5, None, ALU.mult), R=[g4], W=[g4])
    g4v = g4[:].rearrange("p (a b) -> p a b", a=4)
    cs = S.sb([128, 128 * 32], F32, "cs")
    S.dma('sp', cs[:], io['c_rope'], W=[cs])
    csv = cs[:].rearrange("p (t c) -> p t c", c=32)
    KT = S.sb([96, 128, 2, 128], BF16, "KT")
    VA = S.sb([128, 128, 2, 65], BF16, "VA")
    KT_t = [S.sub(KT[:, 4 * i:4 * i + 4, :, :], "KT%d" % i) for i in range(32)]
    VA_t = [S.sub(VA[:, 4 * i:4 * i + 4, :, :], "VA%d" % i) for i in range(32)]
    S.op('pool', lambda g: g.memset(VA[:, :, :, 64:65], 1.0), W=VA_t)
    QT = [S.sb([96, 2, 512], BF16, "QT%d" % i) for i in range(2)]
    uts = [S.sb([128, 8, 512], BF16, "ut%d" % i) for i in range(2)]
    p_lat = bank(S, "p_lat")
    p_trq = bank(S, "p_trq", BF16)
    p_tr = p_trq
    p_qkT = p_trq
    p_qkv = bank(S, "p_qkv")
    p_s = [bank(S, "p_s%d" % i) for i in range(3)]
    p_o0 = bank(S, "p_o0")
    p_o = [p_o0, p_o0]
    p_den = bank(S, "p_den")
    trv = p_trq[:, 0:512].rearrange("p (a b) -> p a b", b=128)
    qkTv = p_trq[:, 512:1024].rearrange("p (a b) -> p a b", b=128)
    R2 = 2
    junk = [S.sb([128, 256], F32, "junk%d" % i) for i in range(R2)]
    ss = [S.sb([128, 2], F32, "ss%d" % i) for i in range(R2)]
    sst = [S.sb([128, 2], F32, "sst%d" % i) for i in range(R2)]
    rstd = [S.sb([128, 2], F32, "rstd%d" % i) for i in range(R2)]
    cn = [S.sb([128, 384], BF16, "cn%d" % i) for i in range(R2)]
    cnT = [S.sb([128, 3, 128], BF16, "cnT%d" % i) for i in range(R2)]
    qk = [S.sb([128, 4, 96], F32, "qk%d" % i) for i in range(R2)]
    sq = [S.sb([128, 4, 96], F32, "sq%d" % i) for i in range(R2)]
    ss4 = [S.sb([128, 4], F32, "ss4%d" % i) for i in range(R2)]
    ss4t = [S.sb([128, 4], F32, "ss4t%d" % i) for i in range(R2)]
    rs4 = [S.sb([128, 4], F32, "rs4%d" % i) for i in range(R2)]
    qkn = [S.sb([128, 4, 96], F32, "qkn%d" % i) for i in range(R2)]
    rt = [[S.sb([128, 4, 16], F32, "rt%d_%d" % (j, i)) for j in range(4)] for i in range(R2)]
    qkr = [S.sb([128, 4, 96], BF16, "qkr%d" % i) for i in range(R2)]
    pts = [S.sb([128, 512], BF16, "pt%d" % i) for i in range(3)]
    o_sb = [S.sb([65, 512], F32, "o_sb%d" % i) for i in range(2)]
    rden = [S.sb([64, 512], F32, "rden%d" % i) for i in range(2)]
    yts = [S.sb([128, 512], BF16, "yt%d" % i) for i in range(2)]

    def load_u(i):
        S.dma('sp', uts[i % 2][:], uT[:, :, i * 512:(i + 1) * 512].rearrange("k p n -> p k n"), W=[uts[i % 2]])

    import os
    STOP = int(os.environ.get('DBG_STOP', '99'))

    def proj_sub(i, s):
        ut = uts[i % 2]
        r = (i * 4 + s) % R2
        blk = i * 4 + s
        for k in range(8):
            S.op('pe', lambda p: p.matmul(p_lat[:, 0:416], ut[:, k, s * 128:(s + 1) * 128], wl[:, k, :], start=(k == 0), stop=(k == 7)), R=[ut, wl], W=[p_lat])
        yield
        S.op('act', lambda a: a.activation(junk[r][:, 0:256], p_lat[:, 0:256], AF.Square, accum_out=ss[r][:, 0:1]), R=[p_lat], W=[junk[r], ss[r]])
        S.op('act', lambda a: a.activation(junk[r][:, 0:128], p_lat[:, 256:384], AF.Square, accum_out=ss[r][:, 1:2]), R=[p_lat], W=[junk[r], ss[r]])
        yield
        S.op('act', lambda a: a.activation(sst[r][:, 0:1], ss[r][:, 0:1], AF.Ln, scale=1.0 / 256, bias=S.eps_col[:, 0:1]), R=[ss[r], S.eps_t], W=[sst[r]])
        S.op('act', lambda a: a.activation(sst[r][:, 1:2], ss[r][:, 1:2], AF.Ln, scale=1.0 / 128, bias=S.eps_col[:, 0:1]), R=[ss[r], S.eps_t], W=[sst[r]])
        S.op('act', lambda a: a.activation(rstd[r][:], sst[r][:], AF.Exp, scale=-0.5), R=[sst[r]], W=[rstd[r]])
        yield
        S.op('act', lambda a: a.activation(cn[r][:, 0:256], p_lat[:, 0:256], AF.Copy, scale=rstd[r][:, 0:1]), R=[p_lat, rstd[r]], W=[cn[r]])
        S.op('dve', lambda v: v.tensor_scalar(cn[r][:, 256:384], p_lat[:, 256:384], rstd[r][:, 1:2], None, ALU.mult), R=[p_lat, rstd[r]], W=[cn[r]])
        yield
        for h in range(2):
            S.op('dve', lambda v: v.tensor_copy(qk[r][:, 2 + h, 64:96], p_lat[:, 384:416]), R=[p_lat], W=[qk[r]])
        yield
        for j in range(3):
            S.op('pe', lambda p: p.transpose(trv[:, j, :], cn[r][:, j * 128:(j + 1) * 128], ident_b[:]), R=[cn[r], ident_b], W=[p_tr])
        yield
        S.op('dve', lambda v: v.tensor_copy(cnT[r][:], trv[:, 0:3, :]), R=[p_tr], W=[cnT[r]])
        yield
        S.op('pe', lambda p: p.matmul(p_qkv[:, 0:192], cnT[r][:, 0, :], wuq[:, 0, :], start=True, stop=False), R=[cnT[r], wuq], W=[p_qkv])
        S.op('pe', lambda p: p.matmul(p_qkv[:, 0:192], cnT[r][:, 1, :], wuq[:, 1, :], start=False, stop=False), R=[cnT[r], wuq], W=[p_qkv])
        S.op('pe', lambda p: p.matmul(p_qkv[:, 192:448], cnT[r][:, 2, :], wukv[:], start=False, stop=True), R=[cnT[r], wukv], W=[p_qkv])
        yield
        kvv = p_qkv[:, 192:448].rearrange("p (a b) -> p a b", a=2)
        S.op('act', lambda a: a.activation(qk[r][:, 0:2, :], p_qkv[:, 0:192].rearrange("p (a b) -> p a b", a=2), AF.Copy), R=[p_qkv], W=[qk[r]])
        S.op('dve', lambda v: v.tensor_copy(qk[r][:, 2:4, 0:64], kvv[:, :, 0:64]), R=[p_qkv], W=[qk[r]])
        S.op('act', lambda a: a.activation(VA[:, blk, :, 0:64], kvv[:, :, 64:128], AF.Copy), R=[p_qkv], W=[VA_t[i]])
        yield
        S.op('dve', lambda v: v.tensor_tensor(sq[r][:], qk[r][:], qk[r][:], ALU.mult), R=[qk[r]], W=[sq[r]])
        S.op('dve', lambda v: v.tensor_reduce(ss4[r][:], sq[r][:], AX.X, ALU.add), R=[sq[r]], W=[ss4[r]])
        yield
        S.op('act', lambda a: a.activation(ss4t[r][:], ss4[r][:], AF.Ln, scale=1.0 / 96, bias=S.eps_col[:, 0:1]), R=[ss4[r], S.eps_t], W=[ss4t[r]])
        S.op('act', lambda a: a.activation(rs4[r][:], ss4t[r][:], AF.Exp, scale=-0.5), R=[ss4t[r]], W=[rs4[r]])
        yield
        S.op('pool', lambda g: g.tensor_tensor(sq[r][:], qk[r][:], g4v, ALU.mult), R=[qk[r], g4], W=[sq[r]])
        for sl in range(4):
            S.op('dve', lambda v: v.tensor_scalar(qkn[r][:, sl, :], sq[r][:, sl, :], rs4[r][:, sl:sl + 1], None, ALU.mult), R=[sq[r], rs4[r]], W=[qkn[r]])
        cosb = csv[:, blk:blk + 1, 0:16].to_broadcast([128, 4, 16])
        sinb = csv[:, blk:blk + 1, 16:32].to_broadcast([128, 4, 16])
        x1 = qkn[r][:, :, 64:80]
        x2 = qkn[r][:, :, 80:96]
        t1, t2, t3, t4 = rt[r]
        S.op('pool', lambda g: g.tensor_copy(qkr[r][:, :, 0:64], qkn[r][:, :, 0:64]), R=[qkn[r]], W=[qkr[r]])
        S.op('dve', lambda v: v.tensor_tensor(t1[:], x1, cosb, ALU.mult), R=[qkn[r], cs], W=[t1])
        S.op('pool', lambda g: g.tensor_tensor(t2[:], x2, sinb, ALU.mult), R=[qkn[r], cs], W=[t2])
        S.op('pool', lambda g: g.tensor_tensor(t3[:], x1, sinb, ALU.mult), R=[qkn[r], cs], W=[t3])
        S.op('dve', lambda v: v.tensor_tensor(t4[:], x2, cosb, ALU.mult), R=[qkn[r], cs], W=[t4])
        yield
        S.op('dve', lambda v: v.tensor_tensor(qkr[r][:, :, 64:80], t1[:], t2[:], ALU.subtract), R=[t1, t2], W=[qkr[r]])
        S.op('pool', lambda g: g.tensor_tensor(qkr[r][:, :, 80:96], t3[:], t4[:], ALU.add), R=[t3, t4], W=[qkr[r]])
        yield
        for sl in range(4):
            S.op('pe', lambda p: p.transpose(qkTv[0:96, sl, :], qkr[r][:, sl, :], ident_b[:]), R=[qkr[r], ident_b], W=[p_qkT])
        yield
        S.op('act', lambda a: a.activation(QT[i % 2][:, :, s * 128:(s + 1) * 128], qkTv[0:96, 0:2, :], AF.Copy), R=[p_qkT], W=[QT[i % 2]])
        S.op('act', lambda a: a.activation(KT[:, blk, :, :], qkTv[0:96, 2:4, :], AF.Copy), R=[p_qkT], W=[KT_t[i]])

    cnt = [0]

    def attention(i, fillers):
        nblk = 4 * i + 4
        qt = QT[i % 2]
        yt = yts[i % 2]
        items = [(h, kb) for h in range(2) for kb in range(nblk)]
        NST = 60
        done_st = [0]

        def advance(idx):
            if fillers is None:
                return
            want = min(NST, ((idx + 1) * NST + len(items) - 1) // len(items))
            while done_st[0] < want:
                try:
                    next(fillers)
                except StopIteration:
                    done_st[0] = NST
                    return
                done_st[0] += 1

        def qk(idx):
            h, kb = items[idx]
            d = kb - 4 * i
            q0 = max(d, 0) * 128
            n = cnt[0] + idx
            sT = p_s[n % 3]
            S.op('pe', lambda p: p.matmul(sT[:, q0:512], KT[:, kb, h, :], qt[:, h, q0:512], start=True, stop=True), R=[KT_t[kb // 4], qt], W=[sT])

        qk(0)
        if len(items) > 1:
            qk(1)
        for idx, (h, kb) in enumerate(items):
            d = kb - 4 * i
            q0 = max(d, 0) * 128
            n = cnt[0] + idx
            sT = p_s[n % 3]
            pt = pts[n % 3]
            po = p_o[h]
            if idx + 2 < len(items):
                qk(idx + 2)
            S.op('act', lambda a: a.activation(pt[:, q0:512], sT[:, q0:512], AF.Exp), R=[sT], W=[pt])
            if d >= 0:
                S.op('pool', lambda g: g.tensor_tensor(pt[:, q0:q0 + 128], pt[:, q0:q0 + 128], tri_b[:], ALU.mult), R=[pt, tri_b], W=[pt])
            S.op('pe', lambda p: p.matmul(po[0:65, q0:512], VA[:, kb, h, :], pt[:, q0:512], start=(kb == 0), stop=(kb == nblk - 1)), R=[VA_t[kb // 4], pt], W=[po])
            advance(idx)
            if kb == nblk - 1:
                osb = o_sb[h]
                S.op('act', lambda a: a.activation(osb[:], po[0:65, :], AF.Copy), R=[po], W=[osb])
                S.op('pe', lambda p: p.matmul(p_den[0:64, :], E65[:], osb[:], start=True, stop=True), R=[E65, osb], W=[p_den])
                S.op('dve', lambda v: v.reciprocal(rden[h][:], p_den[0:64, :]), R=[p_den], W=[rden[h]])
                S.op('dve', lambda v: v.tensor_tensor(yt[h * 64:(h + 1) * 64, :], osb[0:64, :], rden[h][:], ALU.mult), R=[osb, rden[h]], W=[yt])
        cnt[0] += len(items)
        if fillers is not None:
            for _ in fillers:
                pass
        S.dma('sp', io['yaT'][:, i * 512:(i + 1) * 512], yt[:], R=[yt])

    import os
    lvl = int(os.environ.get("DBG_LVL", "9"))
    load_u(0)
    if n_tiles > 1:
        load_u(1)
    def proj_gen(i):
        for s_ in range(4):
            yield from proj_sub(i, s_)

    if lvl >= 1:
        for _ in proj_gen(0):
            pass
    if lvl < 2:
        n_tiles = 0
        S.dma('pool', io['yaT'][:, 0:512], uts[0][:, 0, :], R=[uts[0]])
    for i in range(n_tiles):
        fillers = proj_gen(i + 1) if i + 1 < n_tiles else None
        attention(i, fillers)
        if i + 2 < n_tiles:
            load_u(i + 2)
    S.end_phase()


def host_consts():
    c = {}
    c['c_ident_b'] = np.eye(128, dtype=np.float32).astype(NPBF)
    c['c_ident_f'] = np.eye(128, dtype=np.float32)
    p = np.arange(128)[:, None]
    f = np.arange(128)[None, :]
    c['c_tri_b'] = (p <= f).astype(np.float32).astype(NPBF)
    e = np.zeros((65, 64), np.float32)
    e[64, :] = 1.0
    c['c_E65'] = e
    half = 16
    inv = (np.float32(10000.0) ** (-np.arange(half, dtype=np.float32) / np.float32(half))).astype(np.float32)
    pos = np.arange(S_LEN, dtype=np.float32)
    ang = (pos[:, None] * inv[None, :]).astype(np.float32)
    tab = np.concatenate([np.cos(ang), np.sin(ang)], axis=1).astype(np.float32)
    c['c_rope'] = np.ascontiguousarray(tab.reshape(128, 128, 32).transpose(1, 0, 2).reshape(128, 128 * 32))
    return c


B_CONST_KEYS = ['c_ident_b', 'c_ident_f', 'c_tri_b', 'c_E65', 'c_rope']


def prep_B_weights(inp, l, j):
    w_in = inp['w_in'][l]
    hq = slice(416 + j * 64, 416 + (j + 1) * 64)
    hk = slice(416 + 256 + j * 64, 416 + 256 + (j + 1) * 64)
    hv = slice(928 + j * 128, 928 + (j + 1) * 128)
    ho = slice(1440 + j * 128, 1440 + (j + 1) * 128)
    hi = slice(1952 + j, 1953 + j)
    hf = slice(1956 + j, 1957 + j)
    dq = slice(1960 + j * 128, 1960 + (j + 1) * 128)
    dk = slice(2472 + j * 128, 2472 + (j + 1) * 128)
    dv = slice(2984 + j * 128, 2984 + (j + 1) * 128)
    WB = np.concatenate([w_in[:, 0:416], w_in[:, hq], w_in[:, hk], w_in[:, hv], w_in[:, ho], w_in[:, hi], w_in[:, hf],
                         w_in[:, dq], w_in[:, dk], w_in[:, dv]], axis=1)
    d = {'WB': np.ascontiguousarray(WB)}
    d['wuq'] = np.ascontiguousarray(inp['mla_w_uq'][l][:, j * 192:(j + 1) * 192])
    d['wukv'] = np.ascontiguousarray(inp['mla_w_ukv'][l][:, j * 256:(j + 1) * 256])
    qn = inp['mla_q_norm'][l].reshape(2, 128).T
    kvn = inp['mla_kv_norm'][l].reshape(1, 128).T
    d['mla_ncol'] = np.ascontiguousarray(np.concatenate([qn, kvn], axis=1))
    qg = inp['mla_q_gain'][l]
    kg = inp['mla_k_gain'][l]
    d['mla_grow'] = np.ascontiguousarray(np.concatenate([qg, qg, kg, kg])[None, :])
    return d


DIL_R = (1, 4, 16)


def sst_(c0, r, n=128):
    return slice(c0, c0 + (n - 1) * r + 1, r)


def emit_dil(S, io, c, n_sb=8):
    S.begin_phase()
    uT = io['uT']
    ident_b, E65 = c['ident_b'], c['E65']
    C0 = 802
    wd_f = S.sb([128, 8, 384], F32, "wd_f")
    S.dma('sp', wd_f[:], io['WB'][:, C0:C0 + 384].rearrange("(k p) n -> p k n", p=128), W=[wd_f])
    wd = S.sb([128, 8, 384], BF16, "wd")
    S.op('pool', lambda g: g.tensor_copy(wd[:], wd_f[:]), R=[wd_f], W=[wd])
    gcol = S.sb([128, 2], F32, "gcol")
    S.dma('sp', gcol[:], io['dil_gcol'], W=[gcol])
    S.op('dve', lambda v: v.tensor_scalar(gcol[:, 0:1], gcol[:, 0:1], 64 ** -0.5, None, ALU.mult), R=[gcol], W=[gcol])
    Bd = S.sb([128, 128], F32, "Bd")
    S.dma('sp', Bd[:], io['c_Bd'], W=[Bd])
    M4 = S.sb([128, 512], BF16, "M4")
    S.dma('sp', M4[:], io['c_M4'], W=[M4])
    uts = [S.sb([128, 8, 512], BF16, "dut%d" % i) for i in range(2)]
    KTd = [S.sb([128, 2048], BF16, "KTd%d" % i) for i in range(2)]
    QTd = [S.sb([128, 2048], BF16, "QTd%d" % i) for i in range(2)]
    VTd = [S.sb([128, 2048], BF16, "VTd%d" % i) for i in range(2)]
    Vr = [[S.sb([128, 16, 2, 65], BF16, "Vr%d_%d" % (p, ri)) for ri in range(3)] for p in range(2)]
    for p in range(2):
        for ri in range(3):
            S.op('pool', lambda g: g.memset(Vr[p][ri][:, :, :, 64:65], 1.0), W=[Vr[p][ri]])
    P0 = bank(S, "dP0")
    P1 = bank(S, "dP1")
    p_tr = bank(S, "dp_tr", BF16)
    p_sc = bank(S, "dp_sc")
    p_acc = [bank(S, "dp_acc%d" % i) for i in range(4)]
    trv = p_tr[:].rearrange("p (a b) -> p a b", b=128)
    raw = [S.sb([128, 512], F32, "draw%d" % i) for i in range(2)]
    sqt = [S.sb([128, 512], F32, "dsq%d" % i) for i in range(2)]
    lnt = [S.sb([128, 512], F32, "dln%d" % i) for i in range(2)]
    rst = [S.sb([128, 512], F32, "drs%d" % i) for i in range(2)]
    pts = [S.sb([128, 512], BF16, "dpt%d" % i) for i in range(3)]
    o_sb = [S.sb([65, 512], F32, "do_sb%d" % i) for i in range(2)]
    rden = [S.sb([64, 512], F32, "drden%d" % i) for i in range(2)]
    yts = [S.sb([128, 2048], BF16, "dyt%d" % i) for i in range(2)]
    cnt = [0, 0]

    def load_u(t):
        S.dma('sp', uts[t % 2][:], uT[:, :, t * 512:(t + 1) * 512].rearrange("k p n -> p k n"), W=[uts[t % 2]])

    def proj_tile(sb, tt):
        t = sb * 4 + tt
        ut = uts[t % 2]
        par = sb % 2
        cols = slice(tt * 512, (tt + 1) * 512)
        for which in range(3):
            for k in range(8):
                S.op('pe', lambda p: p.matmul(P0[:, :], wd[:, k, which * 128:(which + 1) * 128], ut[:, k, :], start=(k == 0), stop=(k == 7)), R=[wd, ut], W=[P0])
            if which == 2:
                S.op('act', lambda a: a.activation(VTd[par][:, cols], P0[:, :], AF.Copy), R=[P0], W=[VTd[par]])
                continue
            x = cnt[1] % 2
            cnt[1] += 1
            S.op('act', lambda a: a.activation(raw[x][:], P0[:, :], AF.Copy), R=[P0], W=[raw[x]])
            S.op('act', lambda a: a.activation(sqt[x][:], P0[:, :], AF.Square), R=[P0], W=[sqt[x]])
            S.op('pe', lambda p: p.matmul(P1[:, :], Bd[:], sqt[x][:], start=True, stop=True), R=[Bd, sqt[x]], W=[P1])
            S.op('act', lambda a: a.activation(lnt[x][:], P1[:, :], AF.Ln, scale=1.0 / 64, bias=S.eps_col[:, 0:1]), R=[P1, S.eps_t], W=[lnt[x]])
            S.op('act', lambda a: a.activation(rst[x][:], lnt[x][:], AF.Exp, scale=-0.5), R=[lnt[x]], W=[rst[x]])
            dst = QTd[par] if which == 0 else KTd[par]
            S.op('dve', lambda v: v.scalar_tensor_tensor(dst[:, cols], raw[x][:], gcol[:, which:which + 1], rst[x][:], ALU.mult, ALU.mult), R=[raw[x], gcol, rst[x]], W=[dst])

    def vtrans(sb):
        par = sb % 2
        for ri, r in enumerate(DIL_R):
            for b in range(16):
                n, rho = divmod(b, r)
                c0 = n * 128 * r + rho
                S.op('pe', lambda p: p.transpose(trv[:, b % 4, :], VTd[par][:, sst_(c0, r)], ident_b[:]), R=[VTd[par], ident_b], W=[p_tr])
                if b % 4 == 3:
                    S.op('act', lambda a: a.activation(Vr[par][ri][:, b - 3:b + 1, :, 0:64], trv[:, 0:4, :].rearrange("p a (h d) -> p a h d", h=2), AF.Copy), R=[p_tr], W=[Vr[par][ri]])

    def attention(sb, h):
        par = sb % 2
        hp = slice(h * 64, (h + 1) * 64)
        blocks = []
        for ri, r in enumerate(DIL_R):
            for b in range(16):
                n, rho = divmod(b, r)
                c0 = n * 128 * r + rho
                cur = (par, b, c0)
                if n > 0:
                    prev = (par, b - r, c0 - 128 * r)
                elif sb > 0:
                    nb = 16 // r - 1
                    prev = (1 - par, nb * r + rho, nb * 128 * r + rho)
                else:
                    prev = None
                blocks.append((ri, r, b, c0, cur, prev))
        pairs = [blocks[i:i + 2] for i in range(0, len(blocks), 2)]
        pv_all = []
        for pi, pair in enumerate(pairs):
            for u, (ri, r, b, c0, cur, prev) in enumerate(pair):
                for part, kb in enumerate((cur, prev)):
                    if kb is None:
                        continue
                    base = u * 256 + part * 128
                    lhs = Vr[kb[0]][ri][:, kb[1], h, :]
                    if r == 1:
                        pv_all.append((pi, c0 // 512, slice(c0 % 512, c0 % 512 + 128), lhs, slice(base, base + 128), Vr[kb[0]][ri]))
                    elif r == 4:
                        pv_all.append((pi, c0 // 512, sst_(c0 % 512, 4), lhs, slice(base, base + 128), Vr[kb[0]][ri]))
                    else:
                        for jb in range(4):
                            pv_all.append((pi, jb, sst_(c0, 16, 32), lhs, slice(base + 32 * jb, base + 32 * jb + 32), Vr[kb[0]][ri]))
        first = {}
        last = {}
        for idx, op in enumerate(pv_all):
            first.setdefault(op[1], idx)
            last[op[1]] = idx
        scb = [p_sc, P1]

        def qk_pair(pi):
            psc = scb[pi % 2]
            nmm = 0
            for u, (ri, r, b, c0, cur, prev) in enumerate(pairs[pi]):
                qap = QTd[par][hp, sst_(c0, r)]
                for part, kb in enumerate((cur, prev)):
                    if kb is None:
                        kb = cur
                    kap = KTd[kb[0]][hp, sst_(kb[2], r)]
                    base = u * 256 + part * 128
                    S.op('pe', lambda p: p.matmul(psc[:, base:base + 128], kap, qap, start=(nmm == 0), stop=(nmm == 3)), R=[KTd[kb[0]], QTd[par]], W=[psc])
                    nmm += 1

        idx = 0
        qk_pair(0)
        for pi, pair in enumerate(pairs):
            pt = pts[cnt[0] % 3]
            cnt[0] += 1
            psc = scb[pi % 2]
            if pi + 1 < len(pairs):
                qk_pair(pi + 1)
            S.op('act', lambda a: a.activation(pt[:], psc[:, :], AF.Exp), R=[psc], W=[pt])
            S.op('dve', lambda v: v.tensor_tensor(pt[:], pt[:], M4[:], ALU.mult), R=[pt, M4], W=[pt])
            while idx < len(pv_all) and pv_all[idx][0] == pi:
                _, bk, osl, lhs, psl, vt = pv_all[idx]
                S.op('pe', lambda p: p.matmul(p_acc[bk][0:65, osl], lhs, pt[:, psl], start=(first[bk] == idx), stop=(last[bk] == idx)), R=[vt, pt], W=[p_acc[bk]])
                idx += 1
        yt = yts[sb % 2]
        for jb in range(4):
            osb = o_sb[jb % 2]
            rd = rden[jb % 2]
            S.op('act', lambda a: a.activation(osb[:], p_acc[jb][0:65, :], AF.Copy), R=[p_acc[jb]], W=[osb])
            S.op('pe', lambda p: p.matmul(P1[0:64, :], E65[:], osb[:], start=True, stop=True), R=[E65, osb], W=[P1])
            S.op('dve', lambda v: v.reciprocal(rd[:], P1[0:64, :]), R=[P1], W=[rd])
            S.op('dve', lambda v: v.tensor_tensor(yt[h * 64:(h + 1) * 64, jb * 512:(jb + 1) * 512], osb[0:64, :], rd[:], ALU.mult), R=[osb, rd], W=[yt])

    load_u(0)
    load_u(1)
    for sb in range(n_sb):
        for tt in range(4):
            proj_tile(sb, tt)
            if sb * 4 + tt + 2 < n_sb * 4:
                load_u(sb * 4 + tt + 2)
        vtrans(sb)
        for h in range(2):
            attention(sb, h)
        S.dma('sp', io['ycT'][:, sb * 2048:(sb + 1) * 2048], yts[sb % 2][:], R=[yts[sb % 2]])
    S.end_phase()


def host_consts_dil():
    c = {}
    bd = np.zeros((128, 128), np.float32)
    bd[0:64, 0:64] = 1.0
    bd[64:128, 64:128] = 1.0
    c['c_Bd'] = bd
    p = np.arange(128)[:, None]
    f = np.arange(128)[None, :]
    mc = (p <= f).astype(np.float32)
    mp = (p >= f).astype(np.float32)
    c['c_M4'] = np.concatenate([mc, mp, mc, mp], axis=1).astype(NPBF)
    return c


def prep_dil_vecs(inp, l):
    return {'dil_gcol': np.ascontiguousarray(np.stack([np.tile(inp['dil_q_gain'][l], 2), np.tile(inp['dil_k_gain'][l], 2)], axis=1))}


def emit_mlstm(S, io, c, n_tiles=32):
    S.begin_phase()
    uT = io['uT']
    ident_b, tri_b = c['ident_b'], c['tri_b']
    wm_f = S.sb([128, 8, 386], F32, "wm_f")
    S.dma('sp', wm_f[:], io['WB'][:, 416:802].rearrange("(k p) n -> p k n", p=128), W=[wm_f])
    wm = S.sb([128, 8, 386], BF16, "wm")
    S.op('pool', lambda g: g.tensor_copy(wm[:], wm_f[:]), R=[wm_f], W=[wm])
    cw = S.sb([64, 10], F32, "cw")
    S.dma('sp', cw[:], io['ml_conv'], W=[cw])
    gb = S.sb([1, 2], F32, "gb")
    S.dma('sp', gb[:], io['ml_gate'], W=[gb])
    nbf = S.sb([1, 1], F32, "nbf")
    S.op('dve', lambda v: v.tensor_scalar(nbf[:], gb[0:1, 1:2], -1.0, None, ALU.mult), R=[gb], W=[nbf])
    hg = S.sb([128, 128], F32, "hg")
    S.dma('sp', hg[:], io['ml_hg'].partition_broadcast(128), W=[hg])
    one = S.sb([1, 1], F32, "one")
    S.op('pool', lambda g: g.memset(one[:], 1.0), W=[one])
    ones_r = S.sb([1, 128], F32, "ones_r")
    zeros_r = S.sb([1, 128], F32, "zeros_r")
    S.op('pool', lambda g: g.memset(ones_r[:], 1.0), W=[ones_r])
    S.op('pool', lambda g: g.memset(zeros_r[:], 0.0), W=[zeros_r])
    uts = [S.sb([128, 8, 512], BF16, "mut%d" % i) for i in range(2)]
    xq = [[S.sb([64, 515], F32, "xq%d_%d" % (w, i)) for i in range(2)] for w in range(2)]
    for w in range(2):
        S.op('pool', lambda g: g.memset(xq[w][0][:, 0:3], 0.0), W=[xq[w][0]])
    cv = [S.sb([64, 512], F32, "cv%d" % i) for i in range(2)]
    ex = [S.sb([64, 512], F32, "ex%d" % i) for i in range(2)]
    qkT = [[S.sb([64, 512], BF16, "qkT%d_%d" % (w, i)) for i in range(2)] for w in range(2)]
    Brow = [S.sb([1, 128], F32, "Brow%d" % i) for i in range(2)]
    Grow = [S.sb([1, 128], F32, "Grow%d" % i) for i in range(2)]
    zero1 = S.sb([1, 1], F32, "zero1")
    S.op('pool', lambda g: g.memset(zero1[:], 0.0), W=[zero1])
    t1 = [S.sb([1, 128], F32, "mt1_%d" % i) for i in range(2)]
    t2 = [S.sb([1, 128], F32, "mt2_%d" % i) for i in range(2)]
    arow = [S.sb([1, 128], F32, "arow%d" % i) for i in range(2)]
    bg = [S.sb([1, 128], F32, "bg%d" % i) for i in range(2)]
    ngp = [S.sb([1, 1], F32, "ngp%d" % i) for i in range(2)]
    rows3 = [S.sb([1, 3, 128], F32, "rows3_%d" % i) for i in range(2)]
    cols = [S.sb([128, 4], F32, "cols%d" % i) for i in range(3)]
    ones_c = S.sb([128, 4], F32, "ones_c")
    S.op('pool', lambda g: g.memset(ones_c[:], 1.0), W=[ones_c])
    Vp = [S.sb([128, 129], BF16, "Vp%d" % i) for i in range(2)]
    so = [S.sb([128, 128], F32, "so%d" % i) for i in range(3)]
    ktok = [S.sb([128, 64], BF16, "ktok%d" % i) for i in range(2)]
    scm = [S.sb([128, 128], BF16, "scm%d" % i) for i in range(2)]
    Dst = S.sb([64, 129], F32, "Dst")
    Cb = S.sb([64, 129], BF16, "Cb")
    S.op('pool', lambda g: g.memset(Dst[:], 0.0), W=[Dst])
    sm = [[S.sb([128, 1], F32, "sm%d_%d" % (j, i)) for j in range(6)] for i in range(2)]
    hh = [S.sb([128, 128], F32, "hh%d" % i) for i in range(2)]
    hj = [S.sb([128, 128], F32, "hj%d" % i) for i in range(2)]
    y1 = [S.sb([128, 128], F32, "y1_%d" % i) for i in range(2)]
    y2 = [S.sb([128, 128], BF16, "y2_%d" % i) for i in range(2)]
    ybt = [S.sb([128, 512], BF16, "ybt%d" % i) for i in range(2)]
    P_qk = bank(S, "mP_qk")
    P_vo = [P_qk]
    P_gc = bank(S, "mP_gc")
    P_s = bank(S, "mP_s")
    P_u = bank(S, "mP_u")
    P_h = [bank(S, "mP_h%d" % i) for i in range(2)]
    p_trk = bank(S, "mp_trk", BF16)
    p_try = bank(S, "mp_try", BF16)

    def load_u(t):
        S.dma('sp', uts[t % 2][:], uT[:, :, t * 512:(t + 1) * 512].rearrange("k p n -> p k n"), W=[uts[t % 2]])

    def qk_tile(t):
        ut = uts[t % 2]
        for w in range(2):
            xb = xq[w][t % 2]
            for k in range(8):
                S.op('pe', lambda p: p.matmul(P_qk[0:64, :], wm[:, k, w * 64:(w + 1) * 64], ut[:, k, :], start=(k == 0), stop=(k == 7)), R=[wm, ut], W=[P_qk])
            if t > 0:
                S.op('pool', lambda g: g.tensor_copy(xb[:, 0:3], xq[w][(t - 1) % 2][:, 512:515]), R=[xq[w][(t - 1) % 2]], W=[xb])
            S.op('act', lambda a: a.activation(xb[:, 3:515], P_qk[0:64, :], AF.Copy), R=[P_qk], W=[xb])
            o = w * 5
            cvt = cv[w]
            S.op('dve', lambda v: v.tensor_scalar(cvt[:], xb[:, 3:515], cw[:, o + 3:o + 4], cw[:, o + 4:o + 5], ALU.mult, ALU.add), R=[xb, cw], W=[cvt])
            for j in (2, 1, 0):
                S.op('dve', lambda v: v.scalar_tensor_tensor(cvt[:], xb[:, j:j + 512], cw[:, o + j:o + j + 1], cvt[:], ALU.mult, ALU.add), R=[xb, cw, cvt], W=[cvt])
            ext = ex[w]
            S.op('act', lambda a: a.activation(ext[:], cvt[:], AF.Exp, scale=-1.0), R=[cvt], W=[ext])
            S.op('dve', lambda v: v.tensor_scalar_add(ext[:], ext[:], 1.0), R=[ext], W=[ext])
            S.op('dve', lambda v: v.reciprocal(ext[:], ext[:]), R=[ext], W=[ext])
            S.op('dve', lambda v: v.scalar_tensor_tensor(qkT[w][t % 2][:], cvt[:], (0.125 if w == 0 else 1.0), ext[:], ALU.mult, ALU.mult), R=[cvt, ext], W=[qkT[w][t % 2]])

    def gates_a(g):
        t, cc = divmod(g, 4)
        ut = uts[t % 2]
        x = g % 2
        csl = slice(cc * 128, (cc + 1) * 128)
        for w in range(2):
            for k in range(8):
                S.op('pe', lambda p: p.matmul(P_gc[0:1, w * 128:(w + 1) * 128], wm[:, k, 384 + w:385 + w], ut[:, k, csl], start=(k == 0), stop=(k == 7)), R=[wm, ut], W=[P_gc])
        yield
        S.op('act', lambda a: a.activation(t1[x][:], P_gc[0:1, 128:256], AF.Exp, scale=-1.0, bias=nbf[0:1, 0:1]), R=[P_gc, nbf], W=[t1[x]])
        S.op('act', lambda a: a.activation(t2[x][:], t1[x][:], AF.Ln, bias=one[0:1, 0:1]), R=[t1[x], one], W=[t2[x]])
        yield
        bprev = Brow[1 - x][0:1, 127:128] if g > 0 else zero1[0:1, 0:1]
        gprev = Grow[1 - x][0:1, 127:128] if g > 0 else zero1[0:1, 0:1]
        prevB = [Brow[1 - x]] if g > 0 else [zero1]
        prevG = [Grow[1 - x]] if g > 0 else [zero1]
        S.op('dve', lambda v: v.tensor_tensor_scan(Brow[x][:], ones_r[:], t2[x][:], bprev, ALU.mult, ALU.subtract), R=[ones_r, t2[x]] + prevB, W=[Brow[x]])
        S.op('dve', lambda v: v.scalar_tensor_tensor(arow[x][:], P_gc[0:1, 0:128], gb[0:1, 0:1], Brow[x][:], ALU.add, ALU.subtract), R=[P_gc, gb, Brow[x]], W=[arow[x]])
        S.op('dve', lambda v: v.tensor_tensor_scan(Grow[x][:], zeros_r[:], arow[x][:], gprev, ALU.add, ALU.max), R=[zeros_r, arow[x]] + prevG, W=[Grow[x]])
        S.op('dve', lambda v: v.tensor_scalar(ngp[x][:], gprev, -1.0, None, ALU.mult), R=prevG, W=[ngp[x]])
        S.op('dve', lambda v: v.tensor_tensor(bg[x][:], Brow[x][:], Grow[x][:], ALU.add), R=[Brow[x], Grow[x]], W=[bg[x]])
        yield
        S.op('act', lambda a: a.activation(rows3[x][0:1, 0, :], arow[x][:], AF.Exp, bias=ngp[x][0:1, 0:1]), R=[arow[x], ngp[x]], W=[rows3[x]])
        S.op('act', lambda a: a.activation(rows3[x][0:1, 1, :], Grow[x][:], AF.Exp, scale=-1.0, bias=gprev), R=[Grow[x]] + prevG, W=[rows3[x]])
        S.op('act', lambda a: a.activation(rows3[x][0:1, 2, :], bg[x][:], AF.Exp, scale=-1.0), R=[bg[x]], W=[rows3[x]])
        yield
        cl = cols[g % 3]
        for j in range(3):
            S.op('pe', lambda p: p.matmul(P_gc[:, 256 + j:257 + j], rows3[x][0:1, j, :], one[0:1, 0:1], start=(j == 0), stop=False), R=[rows3[x], one], W=[P_gc])
        S.op('pe', lambda p: p.matmul(P_gc[:, 259:260], rows3[x][0:1, 1, 127:128].to_broadcast([1, 128]), one[0:1, 0:1], start=False, stop=True), R=[rows3[x], one], W=[P_gc])
        yield
        S.op('act', lambda a: a.activation(cl[:], P_gc[:, 256:260], AF.Copy), R=[P_gc], W=[cl])
        yield

    def stageA(g):
        t, cc = divmod(g, 4)
        if cc == 0:
            qk_tile(t)
            yield
        yield from gates_a(g)
        ut = uts[t % 2]
        x = g % 2
        x3 = g % 3
        csl = slice(cc * 128, (cc + 1) * 128)
        cl = cols[g % 3]
        qT = qkT[0][t % 2]
        kT = qkT[1][t % 2]
        pvo = P_vo[0]
        for k in range(8):
            S.op('pe', lambda p: p.matmul(pvo[:, 0:256], ut[:, k, csl], wm[:, k, 128:384], start=(k == 0), stop=(k == 7)), R=[ut, wm], W=[pvo])
        S.op('pe', lambda p: p.transpose(p_trk[:, 0:64], kT[:, csl], ident_b[0:64, 0:64]), R=[kT, ident_b], W=[p_trk])
        S.op('pe', lambda p: p.matmul(P_s[:, 0:128], kT[:, csl], qT[:, csl], start=True, stop=True), R=[kT, qT], W=[P_s])
        yield
        S.op('dve', lambda v: v.tensor_scalar(Vp[x][:, 0:128], pvo[:, 0:128], cl[:, 0:1], None, ALU.mult), R=[pvo, cl], W=[Vp[x]])
        S.op('act', lambda a: a.activation(Vp[x][:, 128:129], cl[:, 0:1], AF.Copy), R=[cl], W=[Vp[x]])
        S.op('act', lambda a: a.activation(so[x3][:], pvo[:, 128:256], AF.Exp, scale=-1.0), R=[pvo], W=[so[x3]])
        S.op('act', lambda a: a.activation(ktok[x][:], p_trk[:, 0:64], AF.Copy), R=[p_trk], W=[ktok[x]])
        S.op('dve', lambda v: v.tensor_tensor(scm[x][:], P_s[:, 0:128], tri_b[:], ALU.mult), R=[P_s, tri_b], W=[scm[x]])
        yield
        S.op('dve', lambda v: v.tensor_scalar_add(so[x3][:], so[x3][:], 1.0), R=[so[x3]], W=[so[x3]])
        S.op('dve', lambda v: v.reciprocal(so[x3][:], so[x3][:]), R=[so[x3]], W=[so[x3]])
        yield

    def stageB(g):
        t, cc = divmod(g, 4)
        x = g % 2
        csl = slice(cc * 128, (cc + 1) * 128)
        clp = cols[(g - 1) % 3] if g > 0 else ones_c
        qT = qkT[0][t % 2]
        ph = P_h[x]
        S.op('dve', lambda v: v.tensor_scalar(Cb[:], Dst[:], clp[0:64, 3:4], None, ALU.mult), R=[Dst, clp], W=[Cb])
        S.op('pe', lambda p: p.matmul(P_u[0:64, 0:129], ktok[x][:], Vp[x][:], start=True, stop=True), R=[ktok[x], Vp[x]], W=[P_u])
        yield
        S.op('pe', lambda p: p.matmul(ph[:, 0:129], qT[:, csl], Cb[:], start=True, stop=False), R=[qT, Cb], W=[ph])
        S.op('pe', lambda p: p.matmul(ph[:, 0:129], scm[x][:], Vp[x][:], start=False, stop=True), R=[scm[x], Vp[x]], W=[ph])
        S.op('dve', lambda v: v.scalar_tensor_tensor(Dst[:], Dst[:], clp[0:64, 3:4], P_u[0:64, 0:129], ALU.mult, ALU.add), R=[Dst, clp, P_u], W=[Dst])
        yield

    def stageC(g):
        t, cc = divmod(g, 4)
        x = g % 2
        x3 = g % 3
        csl = slice(cc * 128, (cc + 1) * 128)
        cl = cols[g % 3]
        ph = P_h[x]
        ta, tb, rr, r2, ssq, rs = sm[x]
        S.op('act', lambda a: a.activation(ta[:], ph[:, 128:129], AF.Abs), R=[ph], W=[ta])
        yield
        S.op('dve', lambda v: v.scalar_tensor_tensor(tb[:], ta[:], cl[:, 1:2], cl[:, 2:3], ALU.mult, ALU.max), R=[ta, cl], W=[tb])
        S.op('dve', lambda v: v.reciprocal(rr[:], tb[:]), R=[tb], W=[rr])
        S.op('dve', lambda v: v.tensor_tensor(r2[:], rr[:], cl[:, 1:2], ALU.mult), R=[rr, cl], W=[r2])
        S.op('dve', lambda v: v.tensor_scalar(hh[x][:], ph[:, 0:128], r2[:, 0:1], None, ALU.mult), R=[ph, r2], W=[hh[x]])
        yield
        S.op('act', lambda a: a.activation(hj[x][:], hh[x][:], AF.Square, accum_out=ssq[:, 0:1]), R=[hh[x]], W=[hj[x], ssq])
        S.op('act', lambda a: a.activation(ssq[:], ssq[:], AF.Ln, scale=1.0 / 128, bias=S.eps_col[:, 0:1]), R=[ssq, S.eps_t], W=[ssq])
        S.op('act', lambda a: a.activation(rs[:], ssq[:], AF.Exp, scale=-0.5), R=[ssq], W=[rs])
        yield
        S.op('dve', lambda v: v.scalar_tensor_tensor(y1[x][:], hh[x][:], rs[:, 0:1], hg[:], ALU.mult, ALU.mult), R=[hh[x], rs, hg], W=[y1[x]])
        S.op('dve', lambda v: v.tensor_tensor(y2[x][:], y1[x][:], so[x3][:], ALU.mult), R=[y1[x], so[x3]], W=[y2[x]])
        yield
        S.op('pe', lambda p: p.transpose(p_try[:, 0:128], y2[x][:], ident_b[:]), R=[y2[x], ident_b], W=[p_try])
        yield
        S.op('act', lambda a: a.activation(ybt[t % 2][:, csl], p_try[:, 0:128], AF.Copy), R=[p_try], W=[ybt[t % 2]])
        if cc == 3:
            S.dma('sp', io['ybT'][:, t * 512:(t + 1) * 512], ybt[t % 2][:], R=[ybt[t % 2]])
        yield

    n_ch = n_tiles * 4
    load_u(0)
    if n_tiles > 1:
        load_u(1)
    for _ in stageA(0):
        pass
    for it in range(n_ch + 1):
        gens = []
        if it + 1 < n_ch:
            gens.append(stageA(it + 1))
        if it < n_ch:
            gens.append(stageB(it))
        if it >= 1:
            gens.append(stageC(it - 1))
        while gens:
            for gnr in list(gens):
                try:
                    next(gnr)
                except StopIteration:
                    gens.remove(gnr)
        t, cc = divmod(it, 4)
        if it < n_ch and cc == 3 and t + 2 < n_tiles:
            load_u(t + 2)
    S.end_phase()


def prep_mlstm_vecs(inp, l, j):
    cwq = inp['mlstm_conv_w'][l][:, j * 64:(j + 1) * 64].T
    cbq = inp['mlstm_conv_b'][l][j * 64:(j + 1) * 64][:, None]
    cwk = inp['mlstm_conv_w'][l][:, 256 + j * 64:256 + (j + 1) * 64].T
    cbk = inp['mlstm_conv_b'][l][256 + j * 64:256 + (j + 1) * 64][:, None]
    d = {'ml_conv': np.ascontiguousarray(np.concatenate([cwq, cbq, cwk, cbk], axis=1))}
    d['ml_gate'] = np.ascontiguousarray(np.array([[inp['mlstm_b_i'][l][j], inp['mlstm_b_f'][l][j]]], np.float32))
    d['ml_hg'] = np.ascontiguousarray(inp['mlstm_head_gain'][l][j * 128:(j + 1) * 128][None, :])
    return d


def emit_mod(S, io):
    S.begin_phase()
    cc = S.sb([128, 8], F32, "cc")
    S.dma('sp', cc[:], io['c_col'], W=[cc])
    ca = S.sb([128, 8], F32, "ca")
    S.op('act', lambda a: a.activation(ca[:], cc[:], AF.Silu), R=[cc], W=[ca])
    brow = S.sb([1, 6144], F32, "brow")
    S.dma('sp', brow[:], io['b_ada'], W=[brow])
    orow = S.sb([1, 6144], F32, "orow")
    wt = [S.sb([128, 8, 512], F32, "wada%d" % i) for i in range(2)]
    pm = [bank(S, "pm%d" % i) for i in range(2)]
    for gi in range(12):
        w = wt[gi % 2]
        S.dma('sp', w[:], io['w_ada'][:, gi * 512:(gi + 1) * 512].rearrange("(k p) n -> p k n", p=128), W=[w])
        p = pm[gi % 2]
        for k in range(8):
            S.op('pe', lambda pe: pe.matmul(p[0:1, :], ca[:, k:k + 1], w[:, k, :], start=(k == 0), stop=(k == 7)), R=[ca, w], W=[p])
        S.op('dve', lambda v: v.tensor_tensor(orow[0:1, gi * 512:(gi + 1) * 512], p[0:1, :], brow[0:1, gi * 512:(gi + 1) * 512], ALU.add), R=[p, brow], W=[orow])
    S.dma('sp', io['mod_out'], orow[:], R=[orow])
    S.end_phase()


def norm_mod(S, xt, Acol, Bcol, outs, sq, ones_f, pss, lnt, rst, tmp, R_extra):
    n = xt.ap.shape[2]
    S.op('pool', lambda g: g.tensor_tensor(sq[:], xt[:], xt[:], ALU.mult), R=[xt], W=[sq])
    for k in range(8):
        S.op('pe', lambda p: p.matmul(pss[:, 0:n], ones_f[:], sq[:, k, :], start=(k == 0), stop=(k == 7)), R=[ones_f, sq], W=[pss])
    S.op('act', lambda a: a.activation(lnt[:], pss[:, 0:n], AF.Ln, scale=1.0 / D, bias=S.eps_col[:, 0:1]), R=[pss, S.eps_t], W=[lnt])
    S.op('act', lambda a: a.activation(rst[:], lnt[:], AF.Exp, scale=-0.5), R=[lnt], W=[rst])
    for k in range(8):
        t = tmp[k % len(tmp)]
        S.op('dve', lambda v: v.scalar_tensor_tensor(t[:], xt[:, k, :], Acol[:, k:k + 1], rst[:], ALU.mult, ALU.mult), R=[xt, rst] + R_extra, W=[t])
        for o in outs:
            S.op('act', lambda a: a.activation(o[:, k, :], t[:], AF.Identity, bias=Bcol[:, k:k + 1]), R=[t] + R_extra, W=[o])


def scol(S, name, ap_dram, shape):
    t = S.sb(list(shape), F32, name)
    S.dma('sp', t[:], ap_dram, W=[t])
    return t


def emit_C1a(S, io, c, n_tiles=8):
    S.begin_phase()
    NTL = 512
    Wg = S.sb([128, 8, 3072], BF16, "Wg")
    Wbr = [S.sb([128, 4, 1024], BF16, "Wbr%d" % i) for i in range(3)]
    stg = [S.sb([128, 8, 512], F32, "stgA%d" % i) for i in range(2)]
    for gi in range(6):
        st = stg[gi % 2]
        S.dma('sp', st[:], io['w_gate'][:, gi * 512:(gi + 1) * 512].rearrange("(k p) n -> p k n", p=128), W=[st])
        S.op('pool', lambda g: g.tensor_copy(Wg[:, :, gi * 512:(gi + 1) * 512], st[:]), R=[st], W=[Wg])
    for br in range(3):
        for hh_ in range(2):
            st = stg[(br * 2 + hh_) % 2]
            stv = st[:].rearrange("p (a k) n -> p a k n", a=2)[:, 0, :, :]
            S.dma('sp', stv, io['w_br'][br, :, hh_ * 512:(hh_ + 1) * 512].rearrange("(k p) n -> p k n", p=128), W=[st])
            S.op('pool', lambda g: g.tensor_copy(Wbr[br][:, :, hh_ * 512:(hh_ + 1) * 512], stv), R=[st], W=[Wbr[br]])
    uts = [S.sb([128, 8, NTL], BF16, "cut%d" % i) for i in range(2)]
    yts = [[S.sb([128, 4, NTL], BF16, "cyt%d_%d" % (br, i)) for i in range(2)] for br in range(3)]
    mgs = [S.sb([128, 8, NTL], BF16, "mg%d" % i) for i in range(2)]
    eg = [S.sb([128, NTL], F32, "eg%d" % i) for i in range(3)]
    mm = [S.sb([128, NTL], F32, "mm%d" % i) for i in range(2)]
    tt = [S.sb([128, NTL], F32, "tt%d" % i) for i in range(2)]
    PG = [bank(S, "PG%d" % i) for i in range(3)]
    PB = [bank(S, "PB%d" % i) for i in range(3)]

    def load(t):
        sl = slice(t * NTL, (t + 1) * NTL)
        S.dma('sp', uts[t % 2][:], io['uT_loc'][:, :, sl].rearrange("k p n -> p k n"), W=[uts[t % 2]])
        for br in range(3):
            S.dma('sp', yts[br][t % 2][:], io['yT'][br, :, :, sl].rearrange("k p n -> p k n"), W=[yts[br][t % 2]])

    load(0)
    for t in range(n_tiles):
        if t + 1 < n_tiles:
            load(t + 1)
        ut = uts[t % 2]
        mg = mgs[t % 2]
        for dc in range(8):
            for br in range(3):
                for k in range(8):
                    S.op('pe', lambda p: p.matmul(PG[br][:, :], Wg[:, k, br * 1024 + dc * 128:br * 1024 + (dc + 1) * 128], ut[:, k, :], start=(k == 0), stop=(k == 7)), R=[Wg, ut], W=[PG[br]])
                S.op('act', lambda a: a.activation(eg[br][:], PG[br][:, :], AF.Sigmoid), R=[PG[br]], W=[eg[br]])
            for br in range(3):
                yt = yts[br][t % 2]
                for k in range(4):
                    S.op('pe', lambda p: p.matmul(PB[br][:, :], Wbr[br][:, k, dc * 128:(dc + 1) * 128], yt[:, k, :], start=(k == 0), stop=(k == 3)), R=[Wbr[br], yt], W=[PB[br]])
            m = mm[dc % 2]
            S.op('dve', lambda v: v.tensor_tensor(m[:], PB[0][:, :], eg[0][:], ALU.mult), R=[PB[0], eg[0]], W=[m])
            for br in (1, 2):
                t1 = tt[br % 2]
                S.op('dve', lambda v: v.tensor_tensor(t1[:], PB[br][:, :], eg[br][:], ALU.mult), R=[PB[br], eg[br]], W=[t1])
                if br == 1:
                    S.op('pool', lambda g: g.tensor_tensor(m[:], m[:], t1[:], ALU.add), R=[m, t1], W=[m])
                else:
                    S.op('pool', lambda g: g.tensor_tensor(mg[:, dc, :], m[:], t1[:], ALU.add), R=[m, t1], W=[mg])
        S.dma('sp', io['mgT'][:, :, t * NTL:(t + 1) * NTL].rearrange("k p n -> p k n"), mg[:], R=[mg])
    S.end_phase()


def emit_C1b(S, io, c, n_tiles=8):
    S.begin_phase()
    NTL = 512
    ones_f, ident_f = c['ones_f'], c['ident_f']
    Wo = S.sb([128, 8, 1024], BF16, "Wo")
    stg = [S.sb([128, 8, 512], F32, "stgB%d" % i) for i in range(2)]
    for gi in range(2):
        st = stg[gi]
        S.dma('sp', st[:], io['w_out'][:, gi * 512:(gi + 1) * 512].rearrange("(k p) n -> p k n", p=128), W=[st])
        S.op('pool', lambda g: g.tensor_copy(Wo[:, :, gi * 512:(gi + 1) * 512], st[:]), R=[st], W=[Wo])
    Wr = S.sb([128, 8, 20], F32, "Wr")
    S.dma('sp', Wr[:], io['w_router'].rearrange("(k p) n -> p k n", p=128), W=[Wr])
    rb = S.sb([128, 20], F32, "rb")
    S.dma('sp', rb[:], io['b_router'].partition_broadcast(128), W=[rb])
    modc = scol(S, "modc", io['modc'], [128, 48])
    fng = scol(S, "fng", io['ffn_norm_col'], [128, 8])
    Af = S.sb([128, 8], F32, "Af")
    S.op('dve', lambda v: v.scalar_tensor_tensor(Af[:], modc[:, 32:40], 1.0, fng[:], ALU.add, ALU.mult), R=[modc, fng], W=[Af])
    xts = [S.sb([128, 8, NTL], F32, "xt%d" % i) for i in range(2)]
    mgs = [S.sb([128, 8, NTL], BF16, "bmg%d" % i) for i in range(2)]
    sq = S.sb([128, 8, NTL], F32, "bsq")
    hf32 = S.sb([128, 8, NTL], F32, "hf32")
    hfb = [S.sb([128, 8, NTL], BF16, "hfb%d" % i) for i in range(2)]
    lnt = S.sb([128, NTL], F32, "blnt")
    rst = S.sb([128, NTL], F32, "brst")
    tmp = [S.sb([128, NTL], F32, "btmp%d" % i) for i in range(2)]
    cwT = [S.sb([16, NTL], F32, "cwT%d" % i) for i in range(2)]
    PO = [bank(S, "PO%d" % i) for i in range(2)]
    PSS = bank(S, "PSS")
    PR = bank(S, "PR")
    PT = bank(S, "PT")
    def rt(nm, shp):
        return [S.sb(shp, F32, "%s%d" % (nm, i)) for i in range(2)]
    lgb = rt("lgb", [128, 20]); gmax = rt("gmax", [128, 1]); ngm = rt("ngm", [128, 1]); oh = rt("oh", [128, 4])
    egj = rt("egj", [128, 4]); sume = rt("sume", [128, 1]); psel = rt("psel", [128, 1]); m1 = rt("m1", [128, 4])
    is1 = rt("is1", [128, 4, 4]); E2 = rt("E2", [128, 4, 4]); m2 = rt("m2", [128, 4]); sel = rt("sel", [128, 4, 4])
    exx = rt("exx", [128, 4, 4]); den = rt("den", [128, 4]); fac = rt("fac", [128, 4]); cw = rt("cw", [128, 4, 4])

    def load(t):
        sl = slice(t * NTL, (t + 1) * NTL)
        S.dma('sp', xts[t % 2][:], io['xT'][:, :, sl].rearrange("k p n -> p k n"), W=[xts[t % 2]])
        S.dma('sp', mgs[t % 2][:], io['mgT'][:, :, sl].rearrange("k p n -> p k n"), W=[mgs[t % 2]])

    def bc4(ap):
        return ap.unsqueeze(2).to_broadcast([128, 4, 4])

    load(0)
    for t in range(n_tiles):
        if t + 1 < n_tiles:
            load(t + 1)
        xt = xts[t % 2]
        mg = mgs[t % 2]
        sl = slice(t * NTL, (t + 1) * NTL)
        for dc in range(8):
            po = PO[dc % 2]
            for k in range(8):
                S.op('pe', lambda p: p.matmul(po[:, :], Wo[:, k, dc * 128:(dc + 1) * 128], mg[:, k, :], start=(k == 0), stop=(k == 7)), R=[Wo, mg], W=[po])
            S.op('dve', lambda v: v.scalar_tensor_tensor(xt[:, dc, :], po[:, :], modc[:, 16 + dc:17 + dc], xt[:, dc, :], ALU.mult, ALU.add), R=[po, modc, xt], W=[xt])
        S.dma('sp', io['x1T'][:, :, sl].rearrange("k p n -> p k n"), xt[:], R=[xt])
        hb = hfb[t % 2]
        norm_mod(S, xt, Af[:], modc[:, 24:32], [hb, hf32], sq, ones_f, PSS, lnt, rst, tmp, [Af, modc])
        S.dma('sp', io['hfT'][:, :, sl].rearrange("k p n -> p k n"), hb[:], R=[hb])
        ct = cwT[t % 2]
        for s_ in range(4):
            x = s_ % 2
            for k in range(8):
                S.op('pe', lambda p: p.matmul(PR[:, 0:20], hf32[:, k, s_ * 128:(s_ + 1) * 128], Wr[:, k, :], start=(k == 0), stop=(k == 7)), R=[hf32, Wr], W=[PR])
            S.op('dve', lambda v: v.tensor_tensor(lgb[x][:], PR[:, 0:20], rb[:], ALU.add), R=[PR, rb], W=[lgb[x]])
            G = lgb[x][:, 0:4]
            E = lgb[x][:, 4:20].rearrange("p (g e) -> p g e", g=4)
            S.op('dve', lambda v: v.tensor_reduce(gmax[x][:], G, AX.X, ALU.max), R=[lgb[x]], W=[gmax[x]])
            S.op('dve', lambda v: v.tensor_scalar(ngm[x][:], gmax[x][:], -1.0, None, ALU.mult), R=[gmax[x]], W=[ngm[x]])
            S.op('dve', lambda v: v.tensor_scalar(oh[x][:], G, gmax[x][:, 0:1], None, ALU.is_equal), R=[lgb[x], gmax[x]], W=[oh[x]])
            S.op('act', lambda a: a.activation(egj[x][:], G, AF.Exp, bias=ngm[x][:, 0:1], accum_out=sume[x][:, 0:1]), R=[lgb[x], ngm[x]], W=[egj[x], sume[x]])
            S.op('dve', lambda v: v.reciprocal(psel[x][:], sume[x][:]), R=[sume[x]], W=[psel[x]])
            S.op('dve', lambda v: v.tensor_reduce(m1[x][:], E, AX.X, ALU.max), R=[lgb[x]], W=[m1[x]])
            S.op('dve', lambda v: v.tensor_tensor(is1[x][:], E, bc4(m1[x][:]), ALU.is_equal), R=[lgb[x], m1[x]], W=[is1[x]])
            S.op('dve', lambda v: v.scalar_tensor_tensor(E2[x][:], is1[x][:], -1e30, E, ALU.mult, ALU.add), R=[is1[x], lgb[x]], W=[E2[x]])
            S.op('dve', lambda v: v.tensor_reduce(m2[x][:], E2[x][:], AX.X, ALU.max), R=[E2[x]], W=[m2[x]])
            S.op('dve', lambda v: v.tensor_tensor(sel[x][:], E, bc4(m2[x][:]), ALU.is_ge), R=[lgb[x], m2[x]], W=[sel[x]])
            S.op('dve', lambda v: v.tensor_tensor(exx[x][:], E, bc4(m1[x][:]), ALU.subtract), R=[lgb[x], m1[x]], W=[exx[x]])
            S.op('act', lambda a: a.activation(exx[x][:], exx[x][:], AF.Exp), R=[exx[x]], W=[exx[x]])
            S.op('dve', lambda v: v.tensor_tensor(exx[x][:], exx[x][:], sel[x][:], ALU.mult), R=[exx[x], sel[x]], W=[exx[x]])
            S.op('dve', lambda v: v.tensor_reduce(den[x][:], exx[x][:], AX.X, ALU.add), R=[exx[x]], W=[den[x]])
            S.op('dve', lambda v: v.reciprocal(den[x][:], den[x][:]), R=[den[x]], W=[den[x]])
            S.op('dve', lambda v: v.tensor_tensor(fac[x][:], den[x][:], oh[x][:], ALU.mult), R=[den[x], oh[x]], W=[fac[x]])
            S.op('dve', lambda v: v.tensor_scalar(fac[x][:], fac[x][:], psel[x][:, 0:1], None, ALU.mult), R=[fac[x], psel[x]], W=[fac[x]])
            S.op('dve', lambda v: v.tensor_tensor(cw[x][:], exx[x][:], bc4(fac[x][:]), ALU.mult), R=[exx[x], fac[x]], W=[cw[x]])
            S.op('pe', lambda p: p.transpose(PT[0:16, 0:128], cw[x][:].rearrange("p g e -> p (g e)"), ident_f[:]), R=[cw[x], ident_f], W=[PT])
            S.op('act', lambda a: a.activation(ct[:, s_ * 128:(s_ + 1) * 128], PT[0:16, 0:128], AF.Copy), R=[PT], W=[ct])
        S.dma('sp', io['cwT'][:, sl], ct[:], R=[ct])
    S.end_phase()


def emit_C2(S, io, c, last_layer, n_half=2):
    S.begin_phase()
    NTL = 512
    NE = 256
    HT = 2048
    ones_f = c['ones_f']
    modc = scol(S, "modc2", io['modc'], [128, 48])
    Sel = S.sb([16, 16 * 128], F32, "Sel")
    S.dma('sp', Sel[:], io['c_Sel'], W=[Sel])
    if not last_layer:
        modn = scol(S, "modn", io['modn'], [128, 16])
        ang = scol(S, "ang", io['attn_norm_col'], [128, 8])
        Aa = S.sb([128, 8], F32, "Aa")
        S.op('dve', lambda v: v.scalar_tensor_tensor(Aa[:], modn[:, 8:16], 1.0, ang[:], ALU.add, ALU.mult), R=[modn, ang], W=[Aa])
    hf = S.sb([128, 8, HT], BF16, "hfh")
    cwt = S.sb([16, HT], F32, "cwh")
    acc = S.sb([128, 8, HT], F32, "macc")
    acc_t = [[S.sub(acc[:, dc, tl * NTL:(tl + 1) * NTL], "acc%d_%d" % (dc, tl)) for tl in range(4)] for dc in range(8)]
    stg = [S.sb([128, 8, 256], F32, "stgC%d" % i) for i in range(2)]
    Wge = [S.sb([128, 8, 256], BF16, "Wge%d" % i) for i in range(2)]
    Wue = [S.sb([128, 8, 256], BF16, "Wue%d" % i) for i in range(2)]
    Wde = [S.sb([128, 2, 1024], BF16, "Wde%d" % i) for i in range(2)]
    cwb = [S.sb([128, NTL], F32, "cwb%d" % i) for i in range(2)]
    sg = [S.sb([128, NTL], F32, "sg%d" % i) for i in range(2)]
    h1 = [S.sb([128, NTL], F32, "h1_%d" % i) for i in range(2)]
    pt_ = [S.sb([128, NTL], F32, "ptl%d" % i) for i in range(2)]
    hid = [S.sb([128, 2, NTL], BF16, "hid%d" % i) for i in range(2)]
    xt = S.sb([128, 8, NE], F32, "c2xt")
    sq = S.sb([128, 8, NE], F32, "c2sq")
    ub = S.sb([128, 8, NE], BF16, "c2ub")
    lnt = S.sb([128, NE], F32, "c2ln")
    rst = S.sb([128, NE], F32, "c2rs")
    tmp = [S.sb([128, NE], F32, "c2tmp%d" % i) for i in range(2)]
    PGU = [bank(S, "PGU%d" % i) for i in range(4)]
    PD = [bank(S, "PD%d" % i) for i in range(2)]
    PCW = bank(S, "PCW")
    PSS = bank(S, "PSS2")
    nst = [0]

    def load_expert(e, slot):
        for (dst, src) in ((Wge[slot], io['w_eg'][e]), (Wue[slot], io['w_eu'][e])):
            st = stg[nst[0] % 2]
            nst[0] += 1
            S.dma('sp', st[:], src.rearrange("(k p) n -> p k n", p=128), W=[st])
            S.op('pool', lambda g: g.tensor_copy(dst[:], st[:]), R=[st], W=[dst])
        st = stg[nst[0] % 2]
        nst[0] += 1
        stv = st[:].rearrange("p (a k) n -> p a (k n)", a=2)
        S.dma('sp', stv, io['w_ed'][e].rearrange("(k p) n -> p k n", p=128), W=[st])
        S.op('pool', lambda g: g.tensor_copy(Wde[slot][:], stv), R=[st], W=[Wde[slot]])

    nx = [0]
    for half in range(n_half):
        h0 = half * HT
        S.dma('sp', hf[:], io['hfT'][:, :, h0:h0 + HT].rearrange("k p n -> p k n"), W=[hf])
        S.dma('sp', cwt[:], io['cwT'][:, h0:h0 + HT], W=[cwt])
        load_expert(0, 0)
        items = [(e, tl) for e in range(16) for tl in range(4)]
        bufs = {}

        def gu(n):
            e, tl = items[n]
            slot = e % 2
            tsl = slice(tl * NTL, (tl + 1) * NTL)
            hd = hid[n % 2]
            cb = cwb[n % 2]
            bufs[n] = hd
            S.op('pe', lambda p: p.matmul(PCW[:, :], Sel[:, e * 128:(e + 1) * 128], cwt[:, tsl], start=True, stop=True), R=[Sel, cwt], W=[PCW])
            S.op('act', lambda a: a.activation(cb[:], PCW[:, :], AF.Copy), R=[PCW], W=[cb])
            for fc in range(2):
                pg = PGU[fc * 2]
                pu = PGU[fc * 2 + 1]
                for k in range(8):
                    S.op('pe', lambda p: p.matmul(pg[:, :], Wge[slot][:, k, fc * 128:(fc + 1) * 128], hf[:, k, tsl], start=(k == 0), stop=(k == 7)), R=[Wge[slot], hf], W=[pg])
                for k in range(8):
                    S.op('pe', lambda p: p.matmul(pu[:, :], Wue[slot][:, k, fc * 128:(fc + 1) * 128], hf[:, k, tsl], start=(k == 0), stop=(k == 7)), R=[Wue[slot], hf], W=[pu])
                s1 = sg[fc]
                S.op('act', lambda a: a.activation(s1[:], pg[:, :], AF.Silu), R=[pg], W=[s1])
                S.op('dve', lambda v: v.tensor_tensor(h1[fc][:], pu[:, :], s1[:], ALU.mult), R=[pu, s1], W=[h1[fc]])
                S.op('pool', lambda g: g.tensor_tensor(hd[:, fc, :], h1[fc][:], cb[:], ALU.mult), R=[h1[fc], cb], W=[hd])

        def down(n):
            e, tl = items[n]
            slot = e % 2
            tsl = slice(tl * NTL, (tl + 1) * NTL)
            hd = bufs.pop(n)
            for dc in range(8):
                pd = PD[dc % 2]
                for fc in range(2):
                    S.op('pe', lambda p: p.matmul(pd[:, :], Wde[slot][:, fc, dc * 128:(dc + 1) * 128], hd[:, fc, :], start=(fc == 0), stop=(fc == 1)), R=[Wde[slot], hd], W=[pd])
                at = acc_t[dc][tl]
                if e == 0:
                    S.op('act', lambda a: a.activation(acc[:, dc, tsl], pd[:, :], AF.Copy), R=[pd], W=[at])
                else:
                    S.op('dve', lambda v: v.tensor_tensor(acc[:, dc, tsl], pd[:, :], acc[:, dc, tsl], ALU.add), R=[pd, at], W=[at])

        load_expert(1, 1)
        gu(0)
        for n in range(len(items)):
            if n + 1 < len(items):
                gu(n + 1)
            down(n)
            e_, tl_ = items[n]
            if tl_ == 3 and e_ + 2 < 16:
                load_expert(e_ + 2, e_ % 2)
        for tl in range(HT // NE):
            gsl = slice(h0 + tl * NE, h0 + (tl + 1) * NE)
            tsl = slice(tl * NE, (tl + 1) * NE)
            ats = [acc_t[dc][(tl * NE) // NTL] for dc in range(8)]
            S.dma('sp', xt[:], io['x1T'][:, :, gsl].rearrange("k p n -> p k n"), W=[xt])
            for dc in range(8):
                S.op('dve', lambda v: v.scalar_tensor_tensor(xt[:, dc, :], acc[:, dc, tsl], modc[:, 40 + dc:41 + dc], xt[:, dc, :], ALU.mult, ALU.add), R=[ats[dc], modc, xt], W=[xt])
            S.dma('sp', io['xT_out'][:, :, gsl].rearrange("k p n -> p k n"), xt[:], R=[xt])
            if not last_layer:
                norm_mod(S, xt, Aa[:], modn[:, 0:8], [ub], sq, ones_f, PSS, lnt, rst, tmp, [Aa, modn])
                S.dma('sp', io['uT_out'][:, :, gsl].rearrange("k p n -> p k n"), ub[:], R=[ub])
    S.end_phase()


def emit_A(S, io, c, n_tiles=8):
    S.begin_phase()
    NTL = 512
    ones_f = c['ones_f']
    modn = scol(S, "modnA", io['modn'], [128, 16])
    ang = scol(S, "angA", io['attn_norm_col'], [128, 8])
    Aa = S.sb([128, 8], F32, "AaA")
    S.op('dve', lambda v: v.scalar_tensor_tensor(Aa[:], modn[:, 8:16], 1.0, ang[:], ALU.add, ALU.mult), R=[modn, ang], W=[Aa])
    xt = [S.sb([128, 8, NTL], F32, "axt%d" % i) for i in range(2)]
    ub = [S.sb([128, 8, NTL], BF16, "aub%d" % i) for i in range(2)]
    sq = S.sb([128, 8, NTL], F32, "asq")
    lnt = S.sb([128, NTL], F32, "aln")
    rst = S.sb([128, NTL], F32, "ars")
    tmp = [S.sb([128, NTL], F32, "atmp%d" % i) for i in range(2)]
    PSS = bank(S, "PSSA")
    for t in range(n_tiles):
        sl = slice(t * NTL, (t + 1) * NTL)
        x = xt[t % 2]
        S.dma('sp', x[:], io['xT'][:, :, sl].rearrange("k p n -> p k n"), W=[x])
        u = ub[t % 2]
        norm_mod(S, x, Aa[:], modn[:, 0:8], [u], sq, ones_f, PSS, lnt, rst, tmp, [Aa, modn])
        S.dma('sp', io['uT_out'][:, :, sl].rearrange("k p n -> p k n"), u[:], R=[u])
    S.end_phase()


CONST_SPECS = {'c_ident_b': ([128, 128], BF16), 'c_ident_f': ([128, 128], F32), 'c_tri_b': ([128, 128], BF16),
               'c_E65': ([65, 64], F32)}


def _declare(nc, specs, kind):
    io = {}
    for name, (shape, dt) in specs.items():
        io[name] = nc.dram_tensor(name, list(shape), dt, kind=kind).ap()
    return io


def build_M():
    nc = bass.Bass("TRN2", target_bir_lowering=False)
    io = _declare(nc, {'c_col': ([128, 8], F32), 'w_ada': ([1024, 6144], F32), 'b_ada': ([1, 6144], F32)}, "ExternalInput")
    io.update(_declare(nc, {'mod_out': ([1, 6144], F32)}, "ExternalOutput"))
    with ExitStack() as st:
        S = Sched(nc, st)
        emit_mod(S, io)
        S.finish_all()
    return nc


A_IN = {'xT': ([8, 128, TOK], F32), 'modn': ([128, 16], F32), 'attn_norm_col': ([128, 8], F32)}


def build_A():
    nc = bass.Bass("TRN2", target_bir_lowering=False)
    io = _declare(nc, dict(A_IN, **CONST_SPECS), "ExternalInput")
    io.update(_declare(nc, {'uT_out': ([8, 128, TOK], BF16)}, "ExternalOutput"))
    with ExitStack() as st:
        S = Sched(nc, st)
        c = load_consts(S, io)
        emit_A(S, io, c)
        S.finish_all()
    return nc


B_IN = {'uT': ([8, 128, S_LEN], BF16), 'WB': ([1024, 1186], F32), 'wuq': ([256, 192], F32), 'wukv': ([128, 256], F32),
        'mla_ncol': ([128, 3], F32), 'mla_grow': ([1, 384], F32), 'c_rope': ([128, 4096], F32),
        'c_Bd': ([128, 128], F32), 'c_M4': ([128, 512], BF16), 'dil_gcol': ([128, 2], F32),
        'ml_conv': ([64, 10], F32), 'ml_gate': ([1, 2], F32), 'ml_hg': ([1, 128], F32)}


def build_B():
    nc = bass.Bass("TRN2", target_bir_lowering=False)
    io = _declare(nc, dict(B_IN, **CONST_SPECS), "ExternalInput")
    io.update(_declare(nc, {'yaT': ([128, S_LEN], BF16), 'ybT': ([128, S_LEN], BF16), 'ycT': ([128, S_LEN], BF16)}, "ExternalOutput"))
    with ExitStack() as st:
        S = Sched(nc, st)
        c = load_consts(S, io)
        emit_mlstm(S, io, c)
        emit_dil(S, io, c)
        emit_mla(S, io, c)
        S.finish_all()
    return nc


C_IN = {'xT': ([8, 128, TOK], F32), 'uT_loc': ([8, 128, TOK], BF16), 'yT': ([3, 4, 128, TOK], BF16),
        'w_gate': ([1024, 3072], F32), 'w_br': ([3, 512, 1024], F32), 'w_out': ([1024, 1024], F32),
        'w_router': ([1024, 20], F32), 'b_router': ([1, 20], F32),
        'w_eg': ([16, 1024, 256], F32), 'w_eu': ([16, 1024, 256], F32), 'w_ed': ([16, 256, 1024], F32),
        'modc': ([128, 48], F32), 'modn': ([128, 16], F32), 'ffn_norm_col': ([128, 8], F32), 'attn_norm_col': ([128, 8], F32),
        'c_Sel': ([16, 2048], F32)}


def build_C():
    nc = bass.Bass("TRN2", target_bir_lowering=False)
    io = _declare(nc, dict(C_IN, **CONST_SPECS), "ExternalInput")
    io.update(_declare(nc, {'xT_out': ([8, 128, TOK], F32), 'uT_out': ([8, 128, TOK], BF16)}, "ExternalOutput"))
    io.update(_declare(nc, {'mgT': ([8, 128, TOK], BF16), 'x1T': ([8, 128, TOK], F32), 'hfT': ([8, 128, TOK], BF16),
                            'cwT': ([16, TOK], F32)}, "Internal"))
    with ExitStack() as st:
        S = Sched(nc, st)
        c = load_consts(S, io)
        emit_C1a(S, io, c)
        emit_C1b(S, io, c)
        emit_C2(S, io, c, last_layer=False)
        S.finish_all()
    return nc


def col8(v):
    return np.ascontiguousarray(np.asarray(v, np.float32).reshape(8, 128).T)


def _run(nc, in_maps):
    res = run_bass_kernel_spmd(nc, in_maps, core_ids=list(range(8)))
    return res.results


def kernel(**inp):
    inp = {k: np.asarray(v) for k, v in inp.items()}
    x = inp['x'].astype(np.float32, copy=False)
    cst = host_consts()
    cst.update(host_consts_dil())
    sel = np.zeros((16, 16, 128), np.float32)
    for e in range(16):
        sel[e, e, :] = 1.0
    cst['c_Sel'] = np.ascontiguousarray(sel.reshape(16, 2048))
    base_c = {k: cst[k] for k in CONST_SPECS}

    ncM = build_M()
    maps = []
    for cidx in range(8):
        l, b = cidx // 2, cidx % 2
        maps.append({'c_col': col8(inp['c'][b]), 'w_ada': np.ascontiguousarray(inp['w_ada'][l]),
                     'b_ada': np.ascontiguousarray(inp['b_ada'][l][None, :])})
    r = _run(ncM, maps)
    mod = np.zeros((DEPTH, NB, 6, 1024), np.float32)
    for cidx in range(8):
        mod[cidx // 2, cidx % 2] = r[cidx]['mod_out'].reshape(6, 1024)

    def modcols(l, b, rows):
        return np.ascontiguousarray(np.concatenate([col8(mod[l, b, i]) for i in rows], axis=1))

    xT = []
    for cidx in range(8):
        b, j = cidx // 4, cidx % 4
        xT.append(np.ascontiguousarray(x[b, j * TOK:(j + 1) * TOK, :].T.reshape(8, 128, TOK)))
    ncA = build_A()
    maps = []
    for cidx in range(8):
        b = cidx // 4
        m = dict(base_c)
        m.update({'xT': xT[cidx], 'modn': modcols(0, b, (0, 1)), 'attn_norm_col': col8(inp['attn_norm'][0])})
        maps.append(m)
    r = _run(ncA, maps)
    uT_loc = [r[cidx]['uT_out'] for cidx in range(8)]

    ncB = build_B()
    ncC = build_C()
    for l in range(DEPTH):
        uT_full = [np.ascontiguousarray(np.concatenate([uT_loc[b * 4 + j] for j in range(4)], axis=2)) for b in range(NB)]
        maps = []
        for cidx in range(8):
            b, j = cidx // 4, cidx % 4
            m = dict(base_c)
            m.update(prep_B_weights(inp, l, j))
            m.update(prep_dil_vecs(inp, l))
            m.update(prep_mlstm_vecs(inp, l, j))
            m.update({'uT': uT_full[b], 'c_rope': cst['c_rope'], 'c_Bd': cst['c_Bd'], 'c_M4': cst['c_M4']})
            maps.append(m)
        rB = _run(ncB, maps)
        ln = min(l + 1, DEPTH - 1)
        wC = {'w_gate': np.ascontiguousarray(inp['w_in'][l][:, 3496:6568]),
              'w_br': np.ascontiguousarray(np.stack([inp['w_branch_a'][l], inp['w_branch_b'][l], inp['w_branch_c'][l]])),
              'w_out': np.ascontiguousarray(inp['w_out'][l]),
              'w_router': np.ascontiguousarray(np.concatenate([inp['w_router_group'][l], inp['w_router_expert'][l]], axis=1)),
              'b_router': np.ascontiguousarray(np.concatenate([inp['b_router_group'][l], inp['b_router_expert'][l]])[None, :]),
              'w_eg': np.ascontiguousarray(inp['w_exp_gate'][l]), 'w_eu': np.ascontiguousarray(inp['w_exp_up'][l]),
              'w_ed': np.ascontiguousarray(inp['w_exp_down'][l]),
              'ffn_norm_col': col8(inp['ffn_norm'][l]), 'attn_norm_col': col8(inp['attn_norm'][ln]), 'c_Sel': cst['c_Sel']}
        maps = []
        for cidx in range(8):
            b, j = cidx // 4, cidx % 4
            sl = slice(j * TOK, (j + 1) * TOK)
            yT = np.stack([np.stack([rB[b * 4 + jj][nm][:, sl] for jj in range(4)]) for nm in ('yaT', 'ybT', 'ycT')])
            m = dict(base_c)
            m.update(wC)
            m.update({'xT': xT[cidx], 'uT_loc': uT_loc[cidx], 'yT': np.ascontiguousarray(yT),
                      'modc': modcols(l, b, range(6)), 'modn': modcols(ln, b, (0, 1))})
            maps.append(m)
        rC = _run(ncC, maps)
        xT = [rC[cidx]['xT_out'] for cidx in range(8)]
        uT_loc = [rC[cidx]['uT_out'] for cidx in range(8)]

    out = np.empty((NB, S_LEN, D), np.float32)
    for cidx in range(8):
        b, j = cidx // 4, cidx % 4
        out[b, j * TOK:(j + 1) * TOK, :] = xT[cidx].reshape(D, TOK).T
    return out
```

```python
import numpy as np
import ml_dtypes
from contextlib import ExitStack
import concourse.bass as bass
import concourse.mybir as mybir
from concourse.bass_utils import run_bass_kernel_spmd

F32 = mybir.dt.float32
BF16 = mybir.dt.bfloat16
AF = mybir.ActivationFunctionType
ALU = mybir.AluOpType
AX = mybir.AxisListType
NPBF = ml_dtypes.bfloat16

D = 1024
S_LEN = 16384
NB = 2
DEPTH = 4
EPS = 1e-6
TOK = 4096
NT = 512


class T:
    __slots__ = ("ap", "name", "lw", "rs", "dsem", "dcnt")

    def __init__(self, ap, name):
        self.ap = ap
        self.name = name
        self.lw = None
        self.rs = {}
        self.dsem = None
        self.dcnt = 0

    def __getitem__(self, k):
        return self.ap[k]


class Sched:
    SEM_MAX = 30000

    def __init__(self, nc, stack):
        self.nc = nc
        self.root = stack
        self.eng = {'pe': nc.tensor, 'act': nc.scalar, 'dve': nc.vector, 'pool': nc.gpsimd, 'sp': nc.sync}
        self.sem = {}
        self.cnt = {}
        self.nsem = 0
        for e in self.eng:
            self._newsem(e)
        self.waited = {e: {} for e in self.eng}
        self.phase = None
        self.phase_tiles = []
        self.dma_pool = []
        self.all_dma = {}
        self.cc_sem = None
        self.ninst = 0

    def _newsem(self, e):
        self.nsem += 1
        self.sem[e] = self.root.enter_context(self.nc.semaphore("s_%s_%d" % (e, self.nsem)))
        self.cnt[e] = 0

    def begin_phase(self):
        self.phase = ExitStack()
        self.phase_tiles = []

    def end_phase(self):
        deps = []
        for t in self.phase_tiles:
            if t.lw is not None:
                deps.append(t.lw)
            deps.extend(t.rs.values())
        self._wait('sp', deps, True)
        self.sp_mark()
        self.barrier()
        for t in self.phase_tiles:
            if t.dsem is not None:
                self.dma_pool.append((t.dsem, t.dcnt))
        self.phase.close()
        self.phase = None
        self.phase_tiles = []

    def barrier(self):
        tags = [(self.sem[e], self.cnt[e], e) for e in self.eng if self.cnt[e] > 0]
        for e in self.eng:
            self._wait(e, tags, False)

    def sp_mark(self):
        ins = self.eng['sp'].sem_inc(self.sem['sp'], 1)
        self.cnt['sp'] += 1

    def collective(self, src_ap, dst_ap, deps, groups=((0, 1, 2, 3), (4, 5, 6, 7))):
        if self.cc_sem is None:
            self.cc_sem = self.root.enter_context(self.nc.semaphore("cc_sem"))
            self.cc_cnt = 0
        self._wait('pool', list(deps), True)
        ins = self.eng['pool'].collective_compute("AllGather", ALU.bypass, replica_groups=[list(g) for g in groups], ins=[src_ap], outs=[dst_ap])
        self.cc_cnt += 16
        ins.then_inc(self.cc_sem, 16)
        tag = (self.cc_sem, self.cc_cnt, 'dma')
        self.all_dma[id(self.cc_sem)] = tag
        self._wait('sp', [tag], True)
        return tag

    def sb(self, shape, dt, name):
        st = self.phase if self.phase is not None else self.root
        self.nsem += 1
        t = T(st.enter_context(self.nc.sbuf_tensor("sb_%s_%d" % (name, self.nsem), list(shape), dt)), name)
        if self.phase is not None:
            self.phase_tiles.append(t)
        return t

    def ps(self, shape, dt, name):
        st = self.phase if self.phase is not None else self.root
        self.nsem += 1
        t = T(st.enter_context(self.nc.psum_tensor("ps_%s_%d" % (name, self.nsem), list(shape), dt)), name)
        if self.phase is not None:
            self.phase_tiles.append(t)
        return t

    def sub(self, ap, name):
        t = T(ap, name)
        if self.phase is not None:
            self.phase_tiles.append(t)
        return t

    def _wait(self, e, deps, is_dma):
        w = self.waited[e]
        for (sem, val, de) in deps:
            if de == e and not is_dma and e == 'pe':
                continue
            key = id(sem)
            if w.get(key, 0) >= val:
                continue
            self.eng[e].wait_ge(sem, val)
            w[key] = val

    def _deps(self, R, W):
        deps = []
        for t in R:
            if t.lw is not None:
                deps.append(t.lw)
        for t in W:
            if t.lw is not None:
                deps.append(t.lw)
            deps.extend(t.rs.values())
        return deps

    def _mark(self, tag, R, W):
        sem = tag[0]
        for t in W:
            t.lw = tag
            t.rs = {}
        for t in R:
            t.rs[id(sem)] = tag

    def op(self, e, fn, R=(), W=()):
        self._wait(e, self._deps(R, W), False)
        if self.cnt[e] >= self.SEM_MAX:
            self._newsem(e)
        ins = fn(self.eng[e])
        self.cnt[e] += 1
        self.ninst += 1
        ins.then_inc(self.sem[e], 1)
        self._mark((self.sem[e], self.cnt[e], e), R, W)
        return ins

    def dma(self, q, out_ap, in_ap, R=(), W=(), owner=None):
        if q == 'pool':
            q = 'sp'
        self._wait(q, self._deps(R, W), True)
        if owner is None:
            owner = (list(W) + list(R))[0]
        if owner.dsem is None or owner.dcnt >= self.SEM_MAX:
            if owner.dsem is None and self.dma_pool:
                owner.dsem, owner.dcnt = self.dma_pool.pop()
            else:
                owner.dsem = self.root.enter_context(self.nc.semaphore("d_%d" % self.nsem))
                self.nsem += 1
                owner.dcnt = 0
        ins = self.eng[q].dma_start(out=out_ap, in_=in_ap)
        owner.dcnt += 16
        self.ninst += 1
        ins.then_inc(owner.dsem, 16)
        tag = (owner.dsem, owner.dcnt, 'dma')
        self.all_dma[id(owner.dsem)] = tag
        self._mark(tag, R, W)
        return tag

    def finish_all(self):
        self._wait('sp', list(self.all_dma.values()), True)
        self.barrier()

    def finish(self, tiles):
        deps = []
        for t in tiles:
            if t.lw is not None:
                deps.append(t.lw)
            deps.extend(t.rs.values())
        self._wait('sp', deps, True)


def bank(S, name, dt=F32):
    return S.ps([128, 512 if dt == F32 else 1024], dt, name)


def rsqrt_mean(S, out_ap, in_ap, n, tmp_ap, R, W):
    S.op('act', lambda a: a.activation(tmp_ap, in_ap, AF.Ln, scale=1.0 / n, bias=S.eps_col[0:tmp_ap.shape[0], 0:1]), R=R + [S.eps_t], W=W)
    S.op('act', lambda a: a.activation(out_ap, tmp_ap, AF.Exp, scale=-0.5), R=W, W=W)


def load_consts(S, io):
    c = {}
    c['ident_b'] = S.sb([128, 128], BF16, "ident_b")
    c['ident_f'] = S.sb([128, 128], F32, "ident_f")
    c['tri_b'] = S.sb([128, 128], BF16, "tri_b")
    c['E65'] = S.sb([65, 64], F32, "E65")
    c['ones_f'] = S.sb([128, 128], F32, "ones_f")
    S.eps_t = S.sb([128, 1], F32, "eps_t")
    S.eps_col = S.eps_t.ap
    S.dma('sp', c['ident_b'][:], io['c_ident_b'], W=[c['ident_b']])
    S.dma('sp', c['ident_f'][:], io['c_ident_f'], W=[c['ident_f']])
    S.dma('sp', c['tri_b'][:], io['c_tri_b'], W=[c['tri_b']])
    S.dma('sp', c['E65'][:], io['c_E65'], W=[c['E65']])
    S.op('pool', lambda g: g.memset(c['ones_f'][:], 1.0), W=[c['ones_f']])
    S.op('pool', lambda g: g.memset(S.eps_t[:], EPS), W=[S.eps_t])
    return c


def emit_mla(S, io, c, n_tiles=32):
    S.begin_phase()
    uT = io['uT']
    ident_b, tri_b, E65 = c['ident_b'], c['tri_b'], c['E65']
    wl_f = S.sb([128, 8, 416], F32, "wl_f")
    S.dma('sp', wl_f[:], io['WB'][:, 0:416].rearrange("(k p) n -> p k n", p=128), W=[wl_f])
    wl = S.sb([128, 8, 416], BF16, "wl")
    S.op('pool', lambda g: g.tensor_copy(wl[:], wl_f[:]), R=[wl_f], W=[wl])
    wuq_f = S.sb([128, 2, 192], F32, "wuq_f")
    S.dma('sp', wuq_f[:], io['wuq'].rearrange("(k p) n -> p k n", p=128), W=[wuq_f])
    wukv_f = S.sb([128, 256], F32, "wukv_f")
    S.dma('sp', wukv_f[:], io['wukv'], W=[wukv_f])
    ncol = S.sb([128, 3], F32, "ncol")
    S.dma('sp', ncol[:], io['mla_ncol'], W=[ncol])
    wuq = S.sb([128, 2, 192], BF16, "wuq")
    wukv = S.sb([128, 256], BF16, "wukv")
    for k in range(2):
        S.op('dve', lambda v: v.tensor_scalar(wuq[:, k, :], wuq_f[:, k, :], ncol[:, k:k + 1], None, ALU.mult), R=[wuq_f, ncol], W=[wuq])
    S.op('dve', lambda v: v.tensor_scalar(wukv[:], wukv_f[:], ncol[:, 2:3], None, ALU.mult), R=[wukv_f, ncol], W=[wukv])
    g4 = S.sb([128, 384], F32, "g4")
    S.dma('sp', g4[:], io['mla_grow'].partition_broadcast(128), W=[g4])
    S.op('dve', lambda v: v.tensor_scalar(g4[:, 0:192], g4[:, 0:192], 96 ** -0.5, None, ALU.mult), R=[g4], W=[g4])
    g4v = g4[:].rearrange("p (a b) -> p a b", a=4)
    cs = S.sb([128, 128 * 32], F32, "cs")
    S.dma('sp', cs[:], io['c_rope'], W=[cs])
    csv = cs[:].rearrange("p (t c) -> p t c", c=32)
    KT = S.sb([96, 128, 2, 128], BF16, "KT")
    VA = S.sb([128, 128, 2, 65], BF16, "VA")
    KT_t = [S.sub(KT[:, 4 * i:4 * i + 4, :, :], "KT%d" % i) for i in range(32)]
    VA_t = [S.sub(VA[:, 4 * i:4 * i + 4, :, :], "VA%d" % i) for i in range(32)]
    S.op('pool', lambda g: g.memset(VA[:, :, :, 64:65], 1.0), W=VA_t)
    QT = [S.sb([96, 2, 512], BF16, "QT%d" % i) for i in range(2)]
    uts = [S.sb([128, 8, 512], BF16, "ut%d" % i) for i in range(2)]
    p_lat = bank(S, "p_lat")
    p_trq = bank(S, "p_trq", BF16)
    p_tr = p_trq
    p_qkT = p_trq
    p_qkv = bank(S, "p_qkv")
    p_s = [bank(S, "p_s%d" % i) for i in range(3)]
    p_o0 = bank(S, "p_o0")
    p_o = [p_o0, p_o0]
    p_den = bank(S, "p_den")
    trv = p_trq[:, 0:512].rearrange("p (a b) -> p a b", b=128)
    qkTv = p_trq[:, 512:1024].rearrange("p (a b) -> p a b", b=128)
    R2 = 2
    junk = [S.sb([128, 256], F32, "junk%d" % i) for i in range(R2)]
    ss = [S.sb([128, 2], F32, "ss%d" % i) for i in range(R2)]
    sst = [S.sb([128, 2], F32, "sst%d" % i) for i in range(R2)]
    rstd = [S.sb([128, 2], F32, "rstd%d" % i) for i in range(R2)]
    cn = [S.sb([128, 384], BF16, "cn%d" % i) for i in range(R2)]
    cnT = [S.sb([128, 3, 128], BF16, "cnT%d" % i) for i in range(R2)]
    qk = [S.sb([128, 4, 96], F32, "qk%d" % i) for i in range(R2)]
    sq = [S.sb([128, 4, 96], F32, "sq%d" % i) for i in range(R2)]
    ss4 = [S.sb([128, 4], F32, "ss4%d" % i) for i in range(R2)]
    ss4t = [S.sb([128, 4], F32, "ss4t%d" % i) for i in range(R2)]
    rs4 = [S.sb([128, 4], F32, "rs4%d" % i) for i in range(R2)]
    qkn = [S.sb([128, 4, 96], F32, "qkn%d" % i) for i in range(R2)]
    rt = [[S.sb([128, 4, 16], F32, "rt%d_%d" % (j, i)) for j in range(4)] for i in range(R2)]
    qkr = [S.sb([128, 4, 96], BF16, "qkr%d" % i) for i in range(R2)]
    pts = [S.sb([128, 512], BF16, "pt%d" % i) for i in range(3)]
    o_sb = [S.sb([65, 512], F32, "o_sb%d" % i) for i in range(2)]
    rden = [S.sb([64, 512], F32, "rden%d" % i) for i in range(2)]
    yts = [S.sb([128, 512], BF16, "yt%d" % i) for i in range(2)]

    def load_u(i):
        S.dma('sp', uts[i % 2][:], uT[:, :, i * 512:(i + 1) * 512].rearrange("k p n -> p k n"), W=[uts[i % 2]])

    import os
    STOP = int(os.environ.get('DBG_STOP', '99'))

    def proj_sub(i, s):
        ut = uts[i % 2]
        r = (i * 4 + s) % R2
        blk = i * 4 + s
        for k in range(8):
            S.op('pe', lambda p: p.matmul(p_lat[:, 0:416], ut[:, k, s * 128:(s + 1) * 128], wl[:, k, :], start=(k == 0), stop=(k == 7)), R=[ut, wl], W=[p_lat])
        yield
        S.op('act', lambda a: a.activation(junk[r][:, 0:256], p_lat[:, 0:256], AF.Square, accum_out=ss[r][:, 0:1]), R=[p_lat], W=[junk[r], ss[r]])
        S.op('act', lambda a: a.activation(junk[r][:, 0:128], p_lat[:, 256:384], AF.Square, accum_out=ss[r][:, 1:2]), R=[p_lat], W=[junk[r], ss[r]])
        yield
        S.op('act', lambda a: a.activation(sst[r][:, 0:1], ss[r][:, 0:1], AF.Ln, scale=1.0 / 256, bias=S.eps_col[:, 0:1]), R=[ss[r], S.eps_t], W=[sst[r]])
        S.op('act', lambda a: a.activation(sst[r][:, 1:2], ss[r][:, 1:2], AF.Ln, scale=1.0 / 128, bias=S.eps_col[:, 0:1]), R=[ss[r], S.eps_t], W=[sst[r]])
        S.op('act', lambda a: a.activation(rstd[r][:], sst[r][:], AF.Exp, scale=-0.5), R=[sst[r]], W=[rstd[r]])
        yield
        S.op('dve', lambda v: v.tensor_scalar(cn[r][:, 0:256], p_lat[:, 0:256], rstd[r][:, 0:1], None, ALU.mult), R=[p_lat, rstd[r]], W=[cn[r]])
        S.op('dve', lambda v: v.tensor_scalar(cn[r][:, 256:384], p_lat[:, 256:384], rstd[r][:, 1:2], None, ALU.mult), R=[p_lat, rstd[r]], W=[cn[r]])
        yield
        for h in range(2):
            S.op('dve', lambda v: v.tensor_copy(qk[r][:, 2 + h, 64:96], p_lat[:, 384:416]), R=[p_lat], W=[qk[r]])
        yield
        for j in range(3):
            S.op('pe', lambda p: p.transpose(trv[:, j, :], cn[r][:, j * 128:(j + 1) * 128], ident_b[:]), R=[cn[r], ident_b], W=[p_tr])
        yield
        S.op('dve', lambda v: v.tensor_copy(cnT[r][:], trv[:, 0:3, :]), R=[p_tr], W=[cnT[r]])
        yield
        S.op('pe', lambda p: p.matmul(p_qkv[:, 0:192], cnT[r][:, 0, :], wuq[:, 0, :], start=True, stop=False), R=[cnT[r], wuq], W=[p_qkv])
        S.op('pe', lambda p: p.matmul(p_qkv[:, 0:192], cnT[r][:, 1, :], wuq[:, 1, :], start=False, stop=False), R=[cnT[r], wuq], W=[p_qkv])
        S.op('pe', lambda p: p.matmul(p_qkv[:, 192:448], cnT[r][:, 2, :], wukv[:], start=False, stop=True), R=[cnT[r], wukv], W=[p_qkv])
        yield
        kvv = p_qkv[:, 192:448].rearrange("p (a b) -> p a b", a=2)
        S.op('act', lambda a: a.activation(qk[r][:, 0:2, :], p_qkv[:, 0:192].rearrange("p (a b) -> p a b", a=2), AF.Copy), R=[p_qkv], W=[qk[r]])
        S.op('dve', lambda v: v.tensor_copy(qk[r][:, 2:4, 0:64], kvv[:, :, 0:64]), R=[p_qkv], W=[qk[r]])
        S.op('act', lambda a: a.activation(VA[:, blk, :, 0:64], kvv[:, :, 64:128], AF.Copy), R=[p_qkv], W=[VA_t[i]])
        yield
        S.op('dve', lambda v: v.tensor_tensor(sq[r][:], qk[r][:], qk[r][:], ALU.mult), R=[qk[r]], W=[sq[r]])
        S.op('dve', lambda v: v.tensor_reduce(ss4[r][:], sq[r][:], AX.X, ALU.add), R=[sq[r]], W=[ss4[r]])
        yield
        S.op('act', lambda a: a.activation(ss4t[r][:], ss4[r][:], AF.Ln, scale=1.0 / 96, bias=S.eps_col[:, 0:1]), R=[ss4[r], S.eps_t], W=[ss4t[r]])
        S.op('act', lambda a: a.activation(rs4[r][:], ss4t[r][:], AF.Exp, scale=-0.5), R=[ss4t[r]], W=[rs4[r]])
        yield
        S.op('pool', lambda g: g.tensor_tensor(sq[r][:], qk[r][:], g4v, ALU.mult), R=[qk[r], g4], W=[sq[r]])
        for sl in range(4):
            S.op('dve', lambda v: v.tensor_scalar(qkn[r][:, sl, :], sq[r][:, sl, :], rs4[r][:, sl:sl + 1], None, ALU.mult), R=[sq[r], rs4[r]], W=[qkn[r]])
        cosb = csv[:, blk:blk + 1, 0:16].to_broadcast([128, 4, 16])
        sinb = csv[:, blk:blk + 1, 16:32].to_broadcast([128, 4, 16])
        x1 = qkn[r][:, :, 64:80]
        x2 = qkn[r][:, :, 80:96]
        t1, t2, t3, t4 = rt[r]
        S.op('pool', lambda g: g.tensor_copy(qkr[r][:, :, 0:64], qkn[r][:, :, 0:64]), R=[qkn[r]], W=[qkr[r]])
        S.op('dve', lambda v: v.tensor_tensor(t1[:], x1, cosb, ALU.mult), R=[qkn[r], cs], W=[t1])
        S.op('pool', lambda g: g.tensor_tensor(t2[:], x2, sinb, ALU.mult), R=[qkn[r], cs], W=[t2])
        S.op('pool', lambda g: g.tensor_tensor(t3[:], x1, sinb, ALU.mult), R=[qkn[r], cs], W=[t3])
        S.op('dve', lambda v: v.tensor_tensor(t4[:], x2, cosb, ALU.mult), R=[qkn[r], cs], W=[t4])
        yield
        S.op('dve', lambda v: v.tensor_tensor(qkr[r][:, :, 64:80], t1[:], t2[:], ALU.subtract), R=[t1, t2], W=[qkr[r]])
        S.op('pool', lambda g: g.tensor_tensor(qkr[r][:, :, 80:96], t3[:], t4[:], ALU.add), R=[t3, t4], W=[qkr[r]])
        yield
        for sl in range(4):
            S.op('pe', lambda p: p.transpose(qkTv[0:96, sl, :], qkr[r][:, sl, :], ident_b[:]), R=[qkr[r], ident_b], W=[p_qkT])
        yield
        S.op('act', lambda a: a.activation(QT[i % 2][:, :, s * 128:(s + 1) * 128], qkTv[0:96, 0:2, :], AF.Copy), R=[p_qkT], W=[QT[i % 2]])
        S.op('act', lambda a: a.activation(KT[:, blk, :, :], qkTv[0:96, 2:4, :], AF.Copy), R=[p_qkT], W=[KT_t[i]])

    cnt = [0]

    def attention(i, fillers):
        nblk = 4 * i + 4
        qt = QT[i % 2]
        yt = yts[i % 2]
        items = [(h, kb) for h in range(2) for kb in range(nblk)]
        NST = 60
        done_st = [0]

        def advance(idx):
            if fillers is None:
                return
            want = min(NST, ((idx + 1) * NST + len(items) - 1) // len(items))
            while done_st[0] < want:
                try:
                    next(fillers)
                except StopIteration:
                    done_st[0] = NST
                    return
                done_st[0] += 1

        def qk(idx):
            h, kb = items[idx]
            d = kb - 4 * i
            q0 = max(d, 0) * 128
            n = cnt[0] + idx
            sT = p_s[n % 3]
            S.op('pe', lambda p: p.matmul(sT[:, q0:512], KT[:, kb, h, :], qt[:, h, q0:512], start=True, stop=True), R=[KT_t[kb // 4], qt], W=[sT])

        qk(0)
        if len(items) > 1:
            qk(1)
        for idx, (h, kb) in enumerate(items):
            d = kb - 4 * i
            q0 = max(d, 0) * 128
            n = cnt[0] + idx
            sT = p_s[n % 3]
            pt = pts[n % 3]
            po = p_o[h]
            if idx + 2 < len(items):
                qk(idx + 2)
            S.op('act', lambda a: a.activation(pt[:, q0:512], sT[:, q0:512], AF.Exp), R=[sT], W=[pt])
            if d >= 0:
                S.op('pool', lambda g: g.tensor_tensor(pt[:, q0:q0 + 128], pt[:, q0:q0 + 128], tri_b[:], ALU.mult), R=[pt, tri_b], W=[pt])
            S.op('pe', lambda p: p.matmul(po[0:65, q0:512], VA[:, kb, h, :], pt[:, q0:512], start=(kb == 0), stop=(kb == nblk - 1)), R=[VA_t[kb // 4], pt], W=[po])
            advance(idx)
            if kb == nblk - 1:
                osb = o_sb[h]
                S.op('act', lambda a: a.activation(osb[:], po[0:65, :], AF.Copy), R=[po], W=[osb])
                S.op('pe', lambda p: p.matmul(p_den[0:64, :], E65[:], osb[:], start=True, stop=True), R=[E65, osb], W=[p_den])
                S.op('dve', lambda v: v.reciprocal(rden[h][:], p_den[0:64, :]), R=[p_den], W=[rden[h]])
                S.op('dve', lambda v: v.tensor_tensor(yt[h * 64:(h + 1) * 64, :], osb[0:64, :], rden[h][:], ALU.mult), R=[osb, rden[h]], W=[yt])
        cnt[0] += len(items)
        if fillers is not None:
            for _ in fillers:
                pass
        S.dma('sp', io['yaT'][:, i * 512:(i + 1) * 512], yt[:], R=[yt])

    import os
    lvl = int(os.environ.get("DBG_LVL", "9"))
    load_u(0)
    if n_tiles > 1:
        load_u(1)
    def proj_gen(i):
        for s_ in range(4):
            yield from proj_sub(i, s_)

    if lvl >= 1:
        for _ in proj_gen(0):
            pass
    if lvl < 2:
        n_tiles = 0
        S.dma('pool', io['yaT'][:, 0:512], uts[0][:, 0, :], R=[uts[0]])
    for i in range(n_tiles):
        fillers = proj_gen(i + 1) if i + 1 < n_tiles else None
        attention(i, fillers)
        if i + 2 < n_tiles:
            load_u(i + 2)
    S.end_phase()


def host_consts():
    c = {}
    c['c_ident_b'] = np.eye(128, dtype=np.float32).astype(NPBF)
    c['c_ident_f'] = np.eye(128, dtype=np.float32)
    p = np.arange(128)[:, None]
    f = np.arange(128)[None, :]
    c['c_tri_b'] = (p <= f).astype(np.float32).astype(NPBF)
    e = np.zeros((65, 64), np.float32)
    e[64, :] = 1.0
    c['c_E65'] = e
    half = 16
    inv = (np.float32(10000.0) ** (-np.arange(half, dtype=np.float32) / np.float32(half))).astype(np.float32)
    pos = np.arange(S_LEN, dtype=np.float32)
    ang = (pos[:, None] * inv[None, :]).astype(np.float32)
    tab = np.concatenate([np.cos(ang), np.sin(ang)], axis=1).astype(np.float32)
    c['c_rope'] = np.ascontiguousarray(tab.reshape(128, 128, 32).transpose(1, 0, 2).reshape(128, 128 * 32))
    return c


B_CONST_KEYS = ['c_ident_b', 'c_ident_f', 'c_tri_b', 'c_E65', 'c_rope']


def prep_B_weights(inp, l, j):
    w_in = inp['w_in'][l]
    hq = slice(416 + j * 64, 416 + (j + 1) * 64)
    hk = slice(416 + 256 + j * 64, 416 + 256 + (j + 1) * 64)
    hv = slice(928 + j * 128, 928 + (j + 1) * 128)
    ho = slice(1440 + j * 128, 1440 + (j + 1) * 128)
    hi = slice(1952 + j, 1953 + j)
    hf = slice(1956 + j, 1957 + j)
    dq = slice(1960 + j * 128, 1960 + (j + 1) * 128)
    dk = slice(2472 + j * 128, 2472 + (j + 1) * 128)
    dv = slice(2984 + j * 128, 2984 + (j + 1) * 128)
    WB = np.concatenate([w_in[:, 0:416], w_in[:, hq], w_in[:, hk], w_in[:, hv], w_in[:, ho], w_in[:, hi], w_in[:, hf],
                         w_in[:, dq], w_in[:, dk], w_in[:, dv]], axis=1)
    d = {'WB': np.ascontiguousarray(WB)}
    d['wuq'] = np.ascontiguousarray(inp['mla_w_uq'][l][:, j * 192:(j + 1) * 192])
    d['wukv'] = np.ascontiguousarray(inp['mla_w_ukv'][l][:, j * 256:(j + 1) * 256])
    qn = inp['mla_q_norm'][l].reshape(2, 128).T
    kvn = inp['mla_kv_norm'][l].reshape(1, 128).T
    d['mla_ncol'] = np.ascontiguousarray(np.concatenate([qn, kvn], axis=1))
    qg = inp['mla_q_gain'][l]
    kg = inp['mla_k_gain'][l]
    d['mla_grow'] = np.ascontiguousarray(np.concatenate([qg, qg, kg, kg])[None, :])
    return d


DIL_R = (1, 4, 16)


def sst_(c0, r, n=128):
    return slice(c0, c0 + (n - 1) * r + 1, r)


def emit_dil(S, io, c, n_sb=8):
    S.begin_phase()
    uT = io['uT']
    ident_b, E65 = c['ident_b'], c['E65']
    C0 = 802
    wd_f = S.sb([128, 8, 384], F32, "wd_f")
    S.dma('sp', wd_f[:], io['WB'][:, C0:C0 + 384].rearrange("(k p) n -> p k n", p=128), W=[wd_f])
    wd = S.sb([128, 8, 384], BF16, "wd")
    S.op('pool', lambda g: g.tensor_copy(wd[:], wd_f[:]), R=[wd_f], W=[wd])
    gcol = S.sb([128, 2], F32, "gcol")
    S.dma('sp', gcol[:], io['dil_gcol'], W=[gcol])
    S.op('dve', lambda v: v.tensor_scalar(gcol[:, 0:1], gcol[:, 0:1], 64 ** -0.5, None, ALU.mult), R=[gcol], W=[gcol])
    Bd = S.sb([128, 128], F32, "Bd")
    S.dma('sp', Bd[:], io['c_Bd'], W=[Bd])
    M4 = S.sb([128, 512], BF16, "M4")
    S.dma('sp', M4[:], io['c_M4'], W=[M4])
    uts = [S.sb([128, 8, 512], BF16, "dut%d" % i) for i in range(2)]
    KTd = [S.sb([128, 2048], BF16, "KTd%d" % i) for i in range(2)]
    QTd = [S.sb([128, 2048], BF16, "QTd%d" % i) for i in range(2)]
    VTd = [S.sb([128, 2048], BF16, "VTd%d" % i) for i in range(2)]
    Vr = [[S.sb([128, 16, 2, 65], BF16, "Vr%d_%d" % (p, ri)) for ri in range(3)] for p in range(2)]
    for p in range(2):
        for ri in range(3):
            S.op('pool', lambda g: g.memset(Vr[p][ri][:, :, :, 64:65], 1.0), W=[Vr[p][ri]])
    P0 = bank(S, "dP0")
    P1 = bank(S, "dP1")
    p_tr = bank(S, "dp_tr", BF16)
    p_sc = bank(S, "dp_sc")
    p_acc = [bank(S, "dp_acc%d" % i) for i in range(4)]
    trv = p_tr[:].rearrange("p (a b) -> p a b", b=128)
    raw = [S.sb([128, 512], F32, "draw%d" % i) for i in range(2)]
    sqt = [S.sb([128, 512], F32, "dsq%d" % i) for i in range(2)]
    lnt = [S.sb([128, 512], F32, "dln%d" % i) for i in range(2)]
    rst = [S.sb([128, 512], F32, "drs%d" % i) for i in range(2)]
    pts = [S.sb([128, 512], BF16, "dpt%d" % i) for i in range(3)]
    o_sb = [S.sb([65, 512], F32, "do_sb%d" % i) for i in range(2)]
    rden = [S.sb([64, 512], F32, "drden%d" % i) for i in range(2)]
    yts = [S.sb([128, 2048], BF16, "dyt%d" % i) for i in range(2)]
    cnt = [0, 0]

    def load_u(t):
        S.dma('sp', uts[t % 2][:], uT[:, :, t * 512:(t + 1) * 512].rearrange("k p n -> p k n"), W=[uts[t % 2]])

    def proj_tile(sb, tt):
        t = sb * 4 + tt
        ut = uts[t % 2]
        par = sb % 2
        cols = slice(tt * 512, (tt + 1) * 512)
        for which in range(3):
            for k in range(8):
                S.op('pe', lambda p: p.matmul(P0[:, :], wd[:, k, which * 128:(which + 1) * 128], ut[:, k, :], start=(k == 0), stop=(k == 7)), R=[wd, ut], W=[P0])
            if which == 2:
                S.op('act', lambda a: a.activation(VTd[par][:, cols], P0[:, :], AF.Copy), R=[P0], W=[VTd[par]])
                continue
            x = cnt[1] % 2
            cnt[1] += 1
            S.op('act', lambda a: a.activation(raw[x][:], P0[:, :], AF.Copy), R=[P0], W=[raw[x]])
            S.op('act', lambda a: a.activation(sqt[x][:], P0[:, :], AF.Square), R=[P0], W=[sqt[x]])
            S.op('pe', lambda p: p.matmul(P1[:, :], Bd[:], sqt[x][:], start=True, stop=True), R=[Bd, sqt[x]], W=[P1])
            S.op('act', lambda a: a.activation(lnt[x][:], P1[:, :], AF.Ln, scale=1.0 / 64, bias=S.eps_col[:, 0:1]), R=[P1, S.eps_t], W=[lnt[x]])
            S.op('act', lambda a: a.activation(rst[x][:], lnt[x][:], AF.Exp, scale=-0.5), R=[lnt[x]], W=[rst[x]])
            dst = QTd[par] if which == 0 else KTd[par]
            S.op('dve', lambda v: v.scalar_tensor_tensor(dst[:, cols], raw[x][:], gcol[:, which:which + 1], rst[x][:], ALU.mult, ALU.mult), R=[raw[x], gcol, rst[x]], W=[dst])

    def vtrans(sb):
        par = sb % 2
        for ri, r in enumerate(DIL_R):
            for b in range(16):
                n, rho = divmod(b, r)
                c0 = n * 128 * r + rho
                S.op('pe', lambda p: p.transpose(trv[:, b % 4, :], VTd[par][:, sst_(c0, r)], ident_b[:]), R=[VTd[par], ident_b], W=[p_tr])
                if b % 4 == 3:
                    S.op('act', lambda a: a.activation(Vr[par][ri][:, b - 3:b + 1, :, 0:64], trv[:, 0:4, :].rearrange("p a (h d) -> p a h d", h=2), AF.Copy), R=[p_tr], W=[Vr[par][ri]])

    def attention(sb, h):
        par = sb % 2
        hp = slice(h * 64, (h + 1) * 64)
        blocks = []
        for ri, r in enumerate(DIL_R):
            for b in range(16):
                n, rho = divmod(b, r)
                c0 = n * 128 * r + rho
                cur = (par, b, c0)
                if n > 0:
                    prev = (par, b - r, c0 - 128 * r)
                elif sb > 0:
                    nb = 16 // r - 1
                    prev = (1 - par, nb * r + rho, nb * 128 * r + rho)
                else:
                    prev = None
                blocks.append((ri, r, b, c0, cur, prev))
        pairs = [blocks[i:i + 2] for i in range(0, len(blocks), 2)]
        pv_all = []
        for pi, pair in enumerate(pairs):
            for u, (ri, r, b, c0, cur, prev) in enumerate(pair):
                for part, kb in enumerate((cur, prev)):
                    if kb is None:
                        continue
                    base = u * 256 + part * 128
                    lhs = Vr[kb[0]][ri][:, kb[1], h, :]
                    if r == 1:
                        pv_all.append((pi, c0 // 512, slice(c0 % 512, c0 % 512 + 128), lhs, slice(base, base + 128), Vr[kb[0]][ri]))
                    elif r == 4:
                        pv_all.append((pi, c0 // 512, sst_(c0 % 512, 4), lhs, slice(base, base + 128), Vr[kb[0]][ri]))
                    else:
                        for jb in range(4):
                            pv_all.append((pi, jb, sst_(c0, 16, 32), lhs, slice(base + 32 * jb, base + 32 * jb + 32), Vr[kb[0]][ri]))
        first = {}
        last = {}
        for idx, op in enumerate(pv_all):
            first.setdefault(op[1], idx)
            last[op[1]] = idx
        scb = [p_sc, P1]

        def qk_pair(pi):
            psc = scb[pi % 2]
            nmm = 0
            for u, (ri, r, b, c0, cur, prev) in enumerate(pairs[pi]):
                qap = QTd[par][hp, sst_(c0, r)]
                for part, kb in enumerate((cur, prev)):
                    if kb is None:
                        kb = cur
                    kap = KTd[kb[0]][hp, sst_(kb[2], r)]
                    base = u * 256 + part * 128
                    S.op('pe', lambda p: p.matmul(psc[:, base:base + 128], kap, qap, start=(nmm == 0), stop=(nmm == 3)), R=[KTd[kb[0]], QTd[par]], W=[psc])
                    nmm += 1

        idx = 0
        qk_pair(0)
        for pi, pair in enumerate(pairs):
            pt = pts[cnt[0] % 3]
            cnt[0] += 1
            psc = scb[pi % 2]
            if pi + 1 < len(pairs):
                qk_pair(pi + 1)
            S.op('act', lambda a: a.activation(pt[:], psc[:, :], AF.Exp), R=[psc], W=[pt])
            S.op('dve', lambda v: v.tensor_tensor(pt[:], pt[:], M4[:], ALU.mult), R=[pt, M4], W=[pt])
            while idx < len(pv_all) and pv_all[idx][0] == pi:
                _, bk, osl, lhs, psl, vt = pv_all[idx]
                S.op('pe', lambda p: p.matmul(p_acc[bk][0:65, osl], lhs, pt[:, psl], start=(first[bk] == idx), stop=(last[bk] == idx)), R=[vt, pt], W=[p_acc[bk]])
                idx += 1
        yt = yts[sb % 2]
        for jb in range(4):
            osb = o_sb[jb % 2]
            rd = rden[jb % 2]
            S.op('act', lambda a: a.activation(osb[:], p_acc[jb][0:65, :], AF.Copy), R=[p_acc[jb]], W=[osb])
            S.op('pe', lambda p: p.matmul(P1[0:64, :], E65[:], osb[:], start=True, stop=True), R=[E65, osb], W=[P1])
            S.op('dve', lambda v: v.reciprocal(rd[:], P1[0:64, :]), R=[P1], W=[rd])
            S.op('dve', lambda v: v.tensor_tensor(yt[h * 64:(h + 1) * 64, jb * 512:(jb + 1) * 512], osb[0:64, :], rd[:], ALU.mult), R=[osb, rd], W=[yt])

    load_u(0)
    load_u(1)
    for sb in range(n_sb):
        for tt in range(4):
            proj_tile(sb, tt)
            if sb * 4 + tt + 2 < n_sb * 4:
                load_u(sb * 4 + tt + 2)
        vtrans(sb)
        for h in range(2):
            attention(sb, h)
        S.dma('sp', io['ycT'][:, sb * 2048:(sb + 1) * 2048], yts[sb % 2][:], R=[yts[sb % 2]])
    S.end_phase()


def host_consts_dil():
    c = {}
    bd = np.zeros((128, 128), np.float32)
    bd[0:64, 0:64] = 1.0
    bd[64:128, 64:128] = 1.0
    c['c_Bd'] = bd
    p = np.arange(128)[:, None]
    f = np.arange(128)[None, :]
    mc = (p <= f).astype(np.float32)
    mp = (p >= f).astype(np.float32)
    c['c_M4'] = np.concatenate([mc, mp, mc, mp], axis=1).astype(NPBF)
    return c


def prep_dil_vecs(inp, l):
    return {'dil_gcol': np.ascontiguousarray(np.stack([np.tile(inp['dil_q_gain'][l], 2), np.tile(inp['dil_k_gain'][l], 2)], axis=1))}


def emit_mlstm(S, io, c, n_tiles=32):
    S.begin_phase()
    uT = io['uT']
    ident_b, tri_b = c['ident_b'], c['tri_b']
    wm_f = S.sb([128, 8, 386], F32, "wm_f")
    S.dma('sp', wm_f[:], io['WB'][:, 416:802].rearrange("(k p) n -> p k n", p=128), W=[wm_f])
    wm = S.sb([128, 8, 386], BF16, "wm")
    S.op('pool', lambda g: g.tensor_copy(wm[:], wm_f[:]), R=[wm_f], W=[wm])
    cw = S.sb([64, 10], F32, "cw")
    S.dma('sp', cw[:], io['ml_conv'], W=[cw])
    gb = S.sb([1, 2], F32, "gb")
    S.dma('sp', gb[:], io['ml_gate'], W=[gb])
    nbf = S.sb([1, 1], F32, "nbf")
    S.op('dve', lambda v: v.tensor_scalar(nbf[:], gb[0:1, 1:2], -1.0, None, ALU.mult), R=[gb], W=[nbf])
    hg = S.sb([128, 128], F32, "hg")
    S.dma('sp', hg[:], io['ml_hg'].partition_broadcast(128), W=[hg])
    one = S.sb([1, 1], F32, "one")
    S.op('pool', lambda g: g.memset(one[:], 1.0), W=[one])
    ones_r = S.sb([1, 128], F32, "ones_r")
    zeros_r = S.sb([1, 128], F32, "zeros_r")
    S.op('pool', lambda g: g.memset(ones_r[:], 1.0), W=[ones_r])
    S.op('pool', lambda g: g.memset(zeros_r[:], 0.0), W=[zeros_r])
    uts = [S.sb([128, 8, 512], BF16, "mut%d" % i) for i in range(2)]
    xq = [[S.sb([64, 515], F32, "xq%d_%d" % (w, i)) for i in range(2)] for w in range(2)]
    for w in range(2):
        S.op('pool', lambda g: g.memset(xq[w][0][:, 0:3], 0.0), W=[xq[w][0]])
    cv = [S.sb([64, 512], F32, "cv%d" % i) for i in range(2)]
    ex = [S.sb([64, 512], F32, "ex%d" % i) for i in range(2)]
    qkT = [[S.sb([64, 512], BF16, "qkT%d_%d" % (w, i)) for i in range(2)] for w in range(2)]
    Brow = [S.sb([1, 128], F32, "Brow%d" % i) for i in range(2)]
    Grow = [S.sb([1, 128], F32, "Grow%d" % i) for i in range(2)]
    zero1 = S.sb([1, 1], F32, "zero1")
    S.op('pool', lambda g: g.memset(zero1[:], 0.0), W=[zero1])
    t1 = [S.sb([1, 128], F32, "mt1_%d" % i) for i in range(2)]
    t2 = [S.sb([1, 128], F32, "mt2_%d" % i) for i in range(2)]
    arow = [S.sb([1, 128], F32, "arow%d" % i) for i in range(2)]
    bg = [S.sb([1, 128], F32, "bg%d" % i) for i in range(2)]
    ngp = [S.sb([1, 1], F32, "ngp%d" % i) for i in range(2)]
    rows3 = [S.sb([1, 3, 128], F32, "rows3_%d" % i) for i in range(2)]
    cols = [S.sb([128, 4], F32, "cols%d" % i) for i in range(5)]
    ones_c = S.sb([128, 4], F32, "ones_c")
    S.op('pool', lambda g: g.memset(ones_c[:], 1.0), W=[ones_c])
    Vp = [S.sb([128, 129], BF16, "Vp%d" % i) for i in range(2)]
    so = [S.sb([128, 128], F32, "so%d" % i) for i in range(3)]
    ktok = [S.sb([128, 64], BF16, "ktok%d" % i) for i in range(2)]
    scm = [S.sb([128, 128], BF16, "scm%d" % i) for i in range(2)]
    Dst = S.sb([64, 129], F32, "Dst")
    Cb = S.sb([64, 129], BF16, "Cb")
    S.op('pool', lambda g: g.memset(Dst[:], 0.0), W=[Dst])
    sm = [[S.sb([128, 1], F32, "sm%d_%d" % (j, i)) for j in range(6)] for i in range(2)]
    hh = [S.sb([128, 128], F32, "hh%d" % i) for i in range(2)]
    hj = [S.sb([128, 128], F32, "hj%d" % i) for i in range(2)]
    y1 = [S.sb([128, 128], F32, "y1_%d" % i) for i in range(2)]
    y2 = [S.sb([128, 128], BF16, "y2_%d" % i) for i in range(2)]
    ybt = [S.sb([128, 512], BF16, "ybt%d" % i) for i in range(2)]
    P_qk = bank(S, "mP_qk")
    P_vo = [P_qk]
    P_gc = bank(S, "mP_gc")
    P_s = bank(S, "mP_s")
    P_u = bank(S, "mP_u")
    P_h = [bank(S, "mP_h%d" % i) for i in range(2)]
    p_trb = bank(S, "mp_trb", BF16)
    p_trk = p_trb
    p_try = p_trb
    P_gr = bank(S, "mP_gr")

    def load_u(t):
        S.dma('sp', uts[t % 2][:], uT[:, :, t * 512:(t + 1) * 512].rearrange("k p n -> p k n"), W=[uts[t % 2]])

    def qk_tile(t):
        ut = uts[t % 2]
        for w in range(2):
            xb = xq[w][t % 2]
            for k in range(8):
                S.op('pe', lambda p: p.matmul(P_qk[0:64, :], wm[:, k, w * 64:(w + 1) * 64], ut[:, k, :], start=(k == 0), stop=(k == 7)), R=[wm, ut], W=[P_qk])
            if t > 0:
                S.op('pool', lambda g: g.tensor_copy(xb[:, 0:3], xq[w][(t - 1) % 2][:, 512:515]), R=[xq[w][(t - 1) % 2]], W=[xb])
            S.op('act', lambda a: a.activation(xb[:, 3:515], P_qk[0:64, :], AF.Copy), R=[P_qk], W=[xb])
            o = w * 5
            cvt = cv[w]
            S.op('dve', lambda v: v.tensor_scalar(cvt[:], xb[:, 3:515], cw[:, o + 3:o + 4], cw[:, o + 4:o + 5], ALU.mult, ALU.add), R=[xb, cw], W=[cvt])
            for j in (2, 1, 0):
                S.op('dve', lambda v: v.scalar_tensor_tensor(cvt[:], xb[:, j:j + 512], cw[:, o + j:o + j + 1], cvt[:], ALU.mult, ALU.add), R=[xb, cw, cvt], W=[cvt])
            ext = ex[w]
            S.op('act', lambda a: a.activation(ext[:], cvt[:], AF.Exp, scale=-1.0), R=[cvt], W=[ext])
            S.op('dve', lambda v: v.tensor_scalar_add(ext[:], ext[:], 1.0), R=[ext], W=[ext])
            S.op('dve', lambda v: v.reciprocal(ext[:], ext[:]), R=[ext], W=[ext])
            S.op('dve', lambda v: v.scalar_tensor_tensor(qkT[w][t % 2][:], cvt[:], (0.125 if w == 0 else 1.0), ext[:], ALU.mult, ALU.mult), R=[cvt, ext], W=[qkT[w][t % 2]])

    def gates_a(g):
        t, cc = divmod(g, 4)
        ut = uts[t % 2]
        x = g % 2
        csl = slice(cc * 128, (cc + 1) * 128)
        if g % 2 == 0:
            hsl = slice(cc * 128, cc * 128 + 256)
            for w in range(2):
                for k in range(8):
                    S.op('pe', lambda p: p.matmul(P_gr[0:1, w * 256:(w + 1) * 256], wm[:, k, 384 + w:385 + w], ut[:, k, hsl], start=(k == 0), stop=(k == 7)), R=[wm, ut], W=[P_gr])
            yield
        go = (g % 2) * 128
        S.op('act', lambda a: a.activation(t1[x][:], P_gr[0:1, 256 + go:256 + go + 128], AF.Exp, scale=-1.0, bias=nbf[0:1, 0:1]), R=[P_gr, nbf], W=[t1[x]])
        S.op('act', lambda a: a.activation(t2[x][:], t1[x][:], AF.Ln, bias=one[0:1, 0:1]), R=[t1[x], one], W=[t2[x]])
        yield
        bprev = Brow[1 - x][0:1, 127:128] if g > 0 else zero1[0:1, 0:1]
        gprev = Grow[1 - x][0:1, 127:128] if g > 0 else zero1[0:1, 0:1]
        prevB = [Brow[1 - x]] if g > 0 else [zero1]
        prevG = [Grow[1 - x]] if g > 0 else [zero1]
        S.op('dve', lambda v: v.tensor_tensor_scan(Brow[x][:], ones_r[:], t2[x][:], bprev, ALU.mult, ALU.subtract), R=[ones_r, t2[x]] + prevB, W=[Brow[x]])
        S.op('dve', lambda v: v.scalar_tensor_tensor(arow[x][:], P_gr[0:1, go:go + 128], gb[0:1, 0:1], Brow[x][:], ALU.add, ALU.subtract), R=[P_gr, gb, Brow[x]], W=[arow[x]])
        S.op('dve', lambda v: v.tensor_tensor_scan(Grow[x][:], zeros_r[:], arow[x][:], gprev, ALU.add, ALU.max), R=[zeros_r, arow[x]] + prevG, W=[Grow[x]])
        S.op('dve', lambda v: v.tensor_scalar(ngp[x][:], gprev, -1.0, None, ALU.mult), R=prevG, W=[ngp[x]])
        S.op('dve', lambda v: v.tensor_tensor(bg[x][:], Brow[x][:], Grow[x][:], ALU.add), R=[Brow[x], Grow[x]], W=[bg[x]])
        yield
        S.op('act', lambda a: a.activation(rows3[x][0:1, 0, :], arow[x][:], AF.Exp, bias=ngp[x][0:1, 0:1]), R=[arow[x], ngp[x]], W=[rows3[x]])
        S.op('act', lambda a: a.activation(rows3[x][0:1, 1, :], Grow[x][:], AF.Exp, scale=-1.0, bias=gprev), R=[Grow[x]] + prevG, W=[rows3[x]])
        S.op('act', lambda a: a.activation(rows3[x][0:1, 2, :], bg[x][:], AF.Exp, scale=-1.0), R=[bg[x]], W=[rows3[x]])
        yield
        cl = cols[g % 5]
        for j in range(3):
            S.op('pe', lambda p: p.matmul(P_gc[:, 256 + j:257 + j], rows3[x][0:1, j, :], one[0:1, 0:1], start=(j == 0), stop=False), R=[rows3[x], one], W=[P_gc])
        S.op('pe', lambda p: p.matmul(P_gc[:, 259:260], rows3[x][0:1, 1, 127:128].to_broadcast([1, 128]), one[0:1, 0:1], start=False, stop=True), R=[rows3[x], one], W=[P_gc])
        yield
        S.op('act', lambda a: a.activation(cl[:], P_gc[:, 256:260], AF.Copy), R=[P_gc], W=[cl])
        yield

    def stageA(g):
        t, cc = divmod(g, 4)
        if cc == 0:
            qk_tile(t)
            yield
        ut = uts[t % 2]
        x = g % 2
        x3 = g % 3
        csl = slice(cc * 128, (cc + 1) * 128)
        cl = cols[g % 5]
        qT = qkT[0][t % 2]
        kT = qkT[1][t % 2]
        pvo = P_vo[0]
        for k in range(8):
            S.op('pe', lambda p: p.matmul(pvo[:, 0:256], ut[:, k, csl], wm[:, k, 128:384], start=(k == 0), stop=(k == 7)), R=[ut, wm], W=[pvo])
        S.op('pe', lambda p: p.transpose(p_trb[:, 0:64], kT[:, csl], ident_b[0:64, 0:64]), R=[kT, ident_b], W=[p_trb])
        S.op('pe', lambda p: p.matmul(P_s[:, 0:128], kT[:, csl], qT[:, csl], start=True, stop=True), R=[kT, qT], W=[P_s])
        yield
        S.op('dve', lambda v: v.tensor_scalar(Vp[x][:, 0:128], pvo[:, 0:128], cl[:, 0:1], None, ALU.mult), R=[pvo, cl], W=[Vp[x]])
        S.op('act', lambda a: a.activation(Vp[x][:, 128:129], cl[:, 0:1], AF.Copy), R=[cl], W=[Vp[x]])
        S.op('act', lambda a: a.activation(so[x3][:], pvo[:, 128:256], AF.Exp, scale=-1.0), R=[pvo], W=[so[x3]])
        S.op('act', lambda a: a.activation(ktok[x][:], p_trb[:, 0:64], AF.Copy), R=[p_trb], W=[ktok[x]])
        S.op('dve', lambda v: v.tensor_tensor(scm[x][:], P_s[:, 0:128], tri_b[:], ALU.mult), R=[P_s, tri_b], W=[scm[x]])
        yield
        S.op('dve', lambda v: v.tensor_scalar_add(so[x3][:], so[x3][:], 1.0), R=[so[x3]], W=[so[x3]])
        S.op('dve', lambda v: v.reciprocal(so[x3][:], so[x3][:]), R=[so[x3]], W=[so[x3]])
        yield

    def stageB(g):
        t, cc = divmod(g, 4)
        x = g % 2
        csl = slice(cc * 128, (cc + 1) * 128)
        clp = cols[(g - 1) % 5] if g > 0 else ones_c
        qT = qkT[0][t % 2]
        ph = P_h[x]
        S.op('dve', lambda v: v.tensor_scalar(Cb[:], Dst[:], clp[0:64, 3:4], None, ALU.mult), R=[Dst, clp], W=[Cb])
        S.op('pe', lambda p: p.matmul(P_u[0:64, 0:129], ktok[x][:], Vp[x][:], start=True, stop=True), R=[ktok[x], Vp[x]], W=[P_u])
        yield
        S.op('pe', lambda p: p.matmul(ph[:, 0:129], qT[:, csl], Cb[:], start=True, stop=False), R=[qT, Cb], W=[ph])
        S.op('pe', lambda p: p.matmul(ph[:, 0:129], scm[x][:], Vp[x][:], start=False, stop=True), R=[scm[x], Vp[x]], W=[ph])
        S.op('dve', lambda v: v.scalar_tensor_tensor(Dst[:], Dst[:], clp[0:64, 3:4], P_u[0:64, 0:129], ALU.mult, ALU.add), R=[Dst, clp, P_u], W=[Dst])
        yield

    def stageC(g):
        t, cc = divmod(g, 4)
        x = g % 2
        x3 = g % 3
        csl = slice(cc * 128, (cc + 1) * 128)
        cl = cols[g % 5]
        ph = P_h[x]
        ta, tb, rr, r2, ssq, rs = sm[x]
        S.op('act', lambda a: a.activation(ta[:], ph[:, 128:129], AF.Abs), R=[ph], W=[ta])
        yield
        S.op('dve', lambda v: v.scalar_tensor_tensor(tb[:], ta[:], cl[:, 1:2], cl[:, 2:3], ALU.mult, ALU.max), R=[ta, cl], W=[tb])
        S.op('dve', lambda v: v.reciprocal(rr[:], tb[:]), R=[tb], W=[rr])
        S.op('dve', lambda v: v.tensor_tensor(r2[:], rr[:], cl[:, 1:2], ALU.mult), R=[rr, cl], W=[r2])
        S.op('dve', lambda v: v.tensor_scalar(hh[x][:], ph[:, 0:128], r2[:, 0:1], None, ALU.mult), R=[ph, r2], W=[hh[x]])
        yield
        S.op('act', lambda a: a.activation(hj[x][:], hh[x][:], AF.Square, accum_out=ssq[:, 0:1]), R=[hh[x]], W=[hj[x], ssq])
        S.op('act', lambda a: a.activation(ssq[:], ssq[:], AF.Ln, scale=1.0 / 128, bias=S.eps_col[:, 0:1]), R=[ssq, S.eps_t], W=[ssq])
        S.op('act', lambda a: a.activation(rs[:], ssq[:], AF.Exp, scale=-0.5), R=[ssq], W=[rs])
        yield
        S.op('dve', lambda v: v.scalar_tensor_tensor(y1[x][:], hh[x][:], rs[:, 0:1], hg[:], ALU.mult, ALU.mult), R=[hh[x], rs, hg], W=[y1[x]])
        S.op('dve', lambda v: v.tensor_tensor(y2[x][:], y1[x][:], so[x3][:], ALU.mult), R=[y1[x], so[x3]], W=[y2[x]])
        yield
        S.op('pe', lambda p: p.transpose(p_trb[:, 512:640], y2[x][:], ident_b[:]), R=[y2[x], ident_b], W=[p_trb])
        yield
        S.op('act', lambda a: a.activation(ybt[t % 2][:, csl], p_trb[:, 512:640], AF.Copy), R=[p_trb], W=[ybt[t % 2]])
        if cc == 3:
            S.dma('sp', io['ybT'][:, t * 512:(t + 1) * 512], ybt[t % 2][:], R=[ybt[t % 2]])
        yield

    n_ch = n_tiles * 4
    load_u(0)
    if n_tiles > 1:
        load_u(1)
    for g0 in range(min(2, n_ch)):
        for _ in gates_a(g0):
            pass
    for _ in stageA(0):
        pass
    for it in range(n_ch + 1):
        gens = []
        if it + 2 < n_ch:
            gens.append(gates_a(it + 2))
        if it + 1 < n_ch:
            gens.append(stageA(it + 1))
        if it < n_ch:
            gens.append(stageB(it))
        if it >= 1:
            gens.append(stageC(it - 1))
        while gens:
            for gnr in list(gens):
                try:
                    next(gnr)
                except StopIteration:
                    gens.remove(gnr)
        t, cc = divmod(it, 4)
        if it < n_ch and cc == 3 and t + 2 < n_tiles:
            load_u(t + 2)
    S.end_phase()


def prep_mlstm_vecs(inp, l, j):
    cwq = inp['mlstm_conv_w'][l][:, j * 64:(j + 1) * 64].T
    cbq = inp['mlstm_conv_b'][l][j * 64:(j + 1) * 64][:, None]
    cwk = inp['mlstm_conv_w'][l][:, 256 + j * 64:256 + (j + 1) * 64].T
    cbk = inp['mlstm_conv_b'][l][256 + j * 64:256 + (j + 1) * 64][:, None]
    d = {'ml_conv': np.ascontiguousarray(np.concatenate([cwq, cbq, cwk, cbk], axis=1))}
    d['ml_gate'] = np.ascontiguousarray(np.array([[inp['mlstm_b_i'][l][j], inp['mlstm_b_f'][l][j]]], np.float32))
    d['ml_hg'] = np.ascontiguousarray(inp['mlstm_head_gain'][l][j * 128:(j + 1) * 128][None, :])
    return d


def emit_mod(S, io):
    S.begin_phase()
    cc = S.sb([128, 8], F32, "cc")
    S.dma('sp', cc[:], io['c_col'], W=[cc])
    ca = S.sb([128, 8], F32, "ca")
    S.op('act', lambda a: a.activation(ca[:], cc[:], AF.Silu), R=[cc], W=[ca])
    brow = S.sb([1, 6144], F32, "brow")
    S.dma('sp', brow[:], io['b_ada'], W=[brow])
    orow = S.sb([1, 6144], F32, "orow")
    wt = [S.sb([128, 8, 512], F32, "wada%d" % i) for i in range(2)]
    pm = [bank(S, "pm%d" % i) for i in range(2)]
    for gi in range(12):
        w = wt[gi % 2]
        S.dma('sp', w[:], io['w_ada'][:, gi * 512:(gi + 1) * 512].rearrange("(k p) n -> p k n", p=128), W=[w])
        p = pm[gi % 2]
        for k in range(8):
            S.op('pe', lambda pe: pe.matmul(p[0:1, :], ca[:, k:k + 1], w[:, k, :], start=(k == 0), stop=(k == 7)), R=[ca, w], W=[p])
        S.op('dve', lambda v: v.tensor_tensor(orow[0:1, gi * 512:(gi + 1) * 512], p[0:1, :], brow[0:1, gi * 512:(gi + 1) * 512], ALU.add), R=[p, brow], W=[orow])
    S.dma('sp', io['mod_out'], orow[:], R=[orow])
    S.end_phase()


def norm_mod(S, xt, Acol, Bcol, outs, sq, ones_f, pss, lnt, rst, tmp, R_extra):
    n = xt.ap.shape[2]
    S.op('act', lambda a: a.activation(sq[:], xt[:], AF.Square), R=[xt], W=[sq])
    for k in range(8):
        S.op('pe', lambda p: p.matmul(pss[:, 0:n], ones_f[:], sq[:, k, :], start=(k == 0), stop=(k == 7)), R=[ones_f, sq], W=[pss])
    S.op('act', lambda a: a.activation(lnt[:], pss[:, 0:n], AF.Ln, scale=1.0 / D, bias=S.eps_col[:, 0:1]), R=[pss, S.eps_t], W=[lnt])
    S.op('act', lambda a: a.activation(rst[:], lnt[:], AF.Exp, scale=-0.5), R=[lnt], W=[rst])
    for k in range(8):
        t = tmp[k % len(tmp)]
        S.op('dve', lambda v: v.scalar_tensor_tensor(t[:], xt[:, k, :], Acol[:, k:k + 1], rst[:], ALU.mult, ALU.mult), R=[xt, rst] + R_extra, W=[t])
        for o in outs:
            S.op('act', lambda a: a.activation(o[:, k, :], t[:], AF.Identity, bias=Bcol[:, k:k + 1]), R=[t] + R_extra, W=[o])


def scol(S, name, ap_dram, shape):
    t = S.sb(list(shape), F32, name)
    S.dma('sp', t[:], ap_dram, W=[t])
    return t


def emit_C1a(S, io, c, n_tiles=8):
    S.begin_phase()
    NTL = 512
    Wg = S.sb([128, 8, 3072], BF16, "Wg")
    Wbr = [S.sb([128, 4, 1024], BF16, "Wbr%d" % i) for i in range(3)]
    stg = [S.sb([128, 8, 512], F32, "stgA%d" % i) for i in range(2)]
    for gi in range(6):
        st = stg[gi % 2]
        S.dma('sp', st[:], io['w_gate'][:, gi * 512:(gi + 1) * 512].rearrange("(k p) n -> p k n", p=128), W=[st])
        S.op('pool', lambda g: g.tensor_copy(Wg[:, :, gi * 512:(gi + 1) * 512], st[:]), R=[st], W=[Wg])
    for br in range(3):
        for hh_ in range(2):
            st = stg[(br * 2 + hh_) % 2]
            stv = st[:].rearrange("p (a k) n -> p a k n", a=2)[:, 0, :, :]
            S.dma('sp', stv, io['w_br'][br, :, hh_ * 512:(hh_ + 1) * 512].rearrange("(k p) n -> p k n", p=128), W=[st])
            S.op('pool', lambda g: g.tensor_copy(Wbr[br][:, :, hh_ * 512:(hh_ + 1) * 512], stv), R=[st], W=[Wbr[br]])
    uts = [S.sb([128, 8, NTL], BF16, "cut%d" % i) for i in range(2)]
    yts = [[S.sb([128, 4, NTL], BF16, "cyt%d_%d" % (br, i)) for i in range(2)] for br in range(3)]
    mgs = [S.sb([128, 8, NTL], BF16, "mg%d" % i) for i in range(2)]
    eg = [S.sb([128, NTL], F32, "eg%d" % i) for i in range(3)]
    mm = [S.sb([128, NTL], F32, "mm%d" % i) for i in range(2)]
    tt = [S.sb([128, NTL], F32, "tt%d" % i) for i in range(2)]
    PG = [bank(S, "PG%d" % i) for i in range(3)]
    PB = [bank(S, "PB%d" % i) for i in range(3)]

    def load(t):
        sl = slice(t * NTL, (t + 1) * NTL)
        S.dma('sp', uts[t % 2][:], io['uT_loc'][:, :, sl].rearrange("k p n -> p k n"), W=[uts[t % 2]])
        for br in range(3):
            S.dma('sp', yts[br][t % 2][:], io['yT'][br, :, :, sl].rearrange("k p n -> p k n"), W=[yts[br][t % 2]])

    load(0)
    for t in range(n_tiles):
        if t + 1 < n_tiles:
            load(t + 1)
        ut = uts[t % 2]
        mg = mgs[t % 2]
        for dc in range(8):
            for br in range(3):
                for k in range(8):
                    S.op('pe', lambda p: p.matmul(PG[br][:, :], Wg[:, k, br * 1024 + dc * 128:br * 1024 + (dc + 1) * 128], ut[:, k, :], start=(k == 0), stop=(k == 7)), R=[Wg, ut], W=[PG[br]])
                S.op('act', lambda a: a.activation(eg[br][:], PG[br][:, :], AF.Sigmoid), R=[PG[br]], W=[eg[br]])
            for br in range(3):
                yt = yts[br][t % 2]
                for k in range(4):
                    S.op('pe', lambda p: p.matmul(PB[br][:, :], Wbr[br][:, k, dc * 128:(dc + 1) * 128], yt[:, k, :], start=(k == 0), stop=(k == 3)), R=[Wbr[br], yt], W=[PB[br]])
            m = mm[dc % 2]
            S.op('dve', lambda v: v.tensor_tensor(m[:], PB[0][:, :], eg[0][:], ALU.mult), R=[PB[0], eg[0]], W=[m])
            for br in (1, 2):
                t1 = tt[br % 2]
                S.op('dve', lambda v: v.tensor_tensor(t1[:], PB[br][:, :], eg[br][:], ALU.mult), R=[PB[br], eg[br]], W=[t1])
                if br == 1:
                    S.op('pool', lambda g: g.tensor_tensor(m[:], m[:], t1[:], ALU.add), R=[m, t1], W=[m])
                else:
                    S.op('pool', lambda g: g.tensor_tensor(mg[:, dc, :], m[:], t1[:], ALU.add), R=[m, t1], W=[mg])
        S.dma('sp', io['mgT'][:, :, t * NTL:(t + 1) * NTL].rearrange("k p n -> p k n"), mg[:], R=[mg])
    S.end_phase()


def emit_C1b(S, io, c, n_tiles=8):
    S.begin_phase()
    NTL = 512
    ones_f, ident_f = c['ones_f'], c['ident_f']
    Wo = S.sb([128, 8, 1024], BF16, "Wo")
    stg = [S.sb([128, 8, 512], F32, "stgB%d" % i) for i in range(2)]
    for gi in range(2):
        st = stg[gi]
        S.dma('sp', st[:], io['w_out'][:, gi * 512:(gi + 1) * 512].rearrange("(k p) n -> p k n", p=128), W=[st])
        S.op('pool', lambda g: g.tensor_copy(Wo[:, :, gi * 512:(gi + 1) * 512], st[:]), R=[st], W=[Wo])
    Wr = S.sb([128, 8, 20], F32, "Wr")
    S.dma('sp', Wr[:], io['w_router'].rearrange("(k p) n -> p k n", p=128), W=[Wr])
    rb = S.sb([128, 20], F32, "rb")
    S.dma('sp', rb[:], io['b_router'].partition_broadcast(128), W=[rb])
    modc = scol(S, "modc", io['modc'], [128, 48])
    fng = scol(S, "fng", io['ffn_norm_col'], [128, 8])
    Af = S.sb([128, 8], F32, "Af")
    S.op('dve', lambda v: v.scalar_tensor_tensor(Af[:], modc[:, 32:40], 1.0, fng[:], ALU.add, ALU.mult), R=[modc, fng], W=[Af])
    xts = [S.sb([128, 8, NTL], F32, "xt%d" % i) for i in range(2)]
    mgs = [S.sb([128, 8, NTL], BF16, "bmg%d" % i) for i in range(2)]
    sq = S.sb([128, 8, NTL], F32, "bsq")
    hf32 = S.sb([128, 8, NTL], F32, "hf32")
    hfb = [S.sb([128, 8, NTL], BF16, "hfb%d" % i) for i in range(2)]
    lnt = S.sb([128, NTL], F32, "blnt")
    rst = S.sb([128, NTL], F32, "brst")
    tmp = [S.sb([128, NTL], F32, "btmp%d" % i) for i in range(2)]
    cwT = [S.sb([16, NTL], F32, "cwT%d" % i) for i in range(2)]
    PO = [bank(S, "PO%d" % i) for i in range(2)]
    PSS = bank(S, "PSS")
    PR = bank(S, "PR")
    PT = bank(S, "PT")
    def rt(nm, shp):
        return [S.sb(shp, F32, "%s%d" % (nm, i)) for i in range(2)]
    rb4 = S.sb([128, 4, 20], F32, "rb4")
    for s_ in range(4):
        S.op('pool', lambda g: g.tensor_copy(rb4[:, s_, :], rb[:]), R=[rb], W=[rb4])
    lgb = rt("lgb", [128, 4, 20]); gmax = rt("gmax", [128, 4]); oh = rt("oh", [128, 4, 4]); gs = rt("gs", [128, 4, 4])
    sume = rt("sume", [128, 4]); psel = rt("psel", [128, 4]); m1 = rt("m1", [128, 4, 4])
    is1 = rt("is1", [128, 4, 4, 4]); E2 = rt("E2", [128, 4, 4, 4]); m2 = rt("m2", [128, 4, 4]); sel = rt("sel", [128, 4, 4, 4])
    exx = rt("exx", [128, 4, 4, 4]); den = rt("den", [128, 4, 4]); fac = rt("fac", [128, 4, 4]); cw = rt("cw", [128, 4, 4, 4])

    def load(t):
        sl = slice(t * NTL, (t + 1) * NTL)
        S.dma('sp', xts[t % 2][:], io['xT'][:, :, sl].rearrange("k p n -> p k n"), W=[xts[t % 2]])
        S.dma('sp', mgs[t % 2][:], io['mgT'][:, :, sl].rearrange("k p n -> p k n"), W=[mgs[t % 2]])

    def bc4(ap):
        return ap.unsqueeze(2).to_broadcast([128, 4, 4])

    def stage1(t):
        xt = xts[t % 2]
        mg = mgs[t % 2]
        sl = slice(t * NTL, (t + 1) * NTL)
        for dc in range(8):
            po = PO[dc % 2]
            for k in range(8):
                S.op('pe', lambda p: p.matmul(po[:, :], Wo[:, k, dc * 128:(dc + 1) * 128], mg[:, k, :], start=(k == 0), stop=(k == 7)), R=[Wo, mg], W=[po])
            S.op('dve', lambda v: v.scalar_tensor_tensor(xt[:, dc, :], po[:, :], modc[:, 16 + dc:17 + dc], xt[:, dc, :], ALU.mult, ALU.add), R=[po, modc, xt], W=[xt])
        S.dma('sp', io['x1T'][:, :, sl].rearrange("k p n -> p k n"), xt[:], R=[xt])

    def stage2(t):
        xt = xts[t % 2]
        sl = slice(t * NTL, (t + 1) * NTL)
        hb = hfb[t % 2]
        norm_mod(S, xt, Af[:], modc[:, 24:32], [hb, hf32], sq, ones_f, PSS, lnt, rst, tmp, [Af, modc])
        S.dma('sp', io['hfT'][:, :, sl].rearrange("k p n -> p k n"), hb[:], R=[hb])
        ct = cwT[t % 2]
        x = t % 2
        for s_ in range(4):
            for k in range(8):
                S.op('pe', lambda p: p.matmul(PR[:, s_ * 20:(s_ + 1) * 20], hf32[:, k, s_ * 128:(s_ + 1) * 128], Wr[:, k, :], start=(k == 0), stop=(k == 7)), R=[hf32, Wr], W=[PR])
        S.op('dve', lambda v: v.tensor_tensor(lgb[x][:], PR[:, 0:80].rearrange("p (s n) -> p s n", s=4), rb4[:], ALU.add), R=[PR, rb4], W=[lgb[x]])
        G = lgb[x][:, :, 0:4]
        E = lgb[x][:, :, 4:20].rearrange("p s (g e) -> p s g e", g=4)

        def b3(ap):
            return ap.unsqueeze(2).to_broadcast([128, 4, 4])

        def b4(ap):
            return ap.unsqueeze(3).to_broadcast([128, 4, 4, 4])

        S.op('dve', lambda v: v.tensor_reduce(gmax[x][:], G, AX.X, ALU.max), R=[lgb[x]], W=[gmax[x]])
        S.op('dve', lambda v: v.tensor_tensor(oh[x][:], G, b3(gmax[x][:]), ALU.is_equal), R=[lgb[x], gmax[x]], W=[oh[x]])
        S.op('dve', lambda v: v.tensor_tensor(gs[x][:], G, b3(gmax[x][:]), ALU.subtract), R=[lgb[x], gmax[x]], W=[gs[x]])
        S.op('act', lambda a: a.activation(gs[x][:], gs[x][:], AF.Exp), R=[gs[x]], W=[gs[x]])
        S.op('dve', lambda v: v.tensor_reduce(sume[x][:], gs[x][:], AX.X, ALU.add), R=[gs[x]], W=[sume[x]])
        S.op('dve', lambda v: v.reciprocal(psel[x][:], sume[x][:]), R=[sume[x]], W=[psel[x]])
        S.op('dve', lambda v: v.tensor_reduce(m1[x][:], E, AX.X, ALU.max), R=[lgb[x]], W=[m1[x]])
        S.op('dve', lambda v: v.tensor_tensor(is1[x][:], E, b4(m1[x][:]), ALU.is_equal), R=[lgb[x], m1[x]], W=[is1[x]])
        S.op('dve', lambda v: v.scalar_tensor_tensor(E2[x][:], is1[x][:], -1e30, E, ALU.mult, ALU.add), R=[is1[x], lgb[x]], W=[E2[x]])
        S.op('dve', lambda v: v.tensor_reduce(m2[x][:], E2[x][:], AX.X, ALU.max), R=[E2[x]], W=[m2[x]])
        S.op('dve', lambda v: v.tensor_tensor(sel[x][:], E, b4(m2[x][:]), ALU.is_ge), R=[lgb[x], m2[x]], W=[sel[x]])
        S.op('dve', lambda v: v.tensor_tensor(exx[x][:], E, b4(m1[x][:]), ALU.subtract), R=[lgb[x], m1[x]], W=[exx[x]])
        S.op('act', lambda a: a.activation(exx[x][:], exx[x][:], AF.Exp), R=[exx[x]], W=[exx[x]])
        S.op('dve', lambda v: v.tensor_tensor(exx[x][:], exx[x][:], sel[x][:], ALU.mult), R=[exx[x], sel[x]], W=[exx[x]])
        S.op('dve', lambda v: v.tensor_reduce(den[x][:], exx[x][:], AX.X, ALU.add), R=[exx[x]], W=[den[x]])
        S.op('dve', lambda v: v.reciprocal(den[x][:], den[x][:]), R=[den[x]], W=[den[x]])
        S.op('dve', lambda v: v.tensor_tensor(fac[x][:], den[x][:], oh[x][:], ALU.mult), R=[den[x], oh[x]], W=[fac[x]])
        S.op('dve', lambda v: v.tensor_tensor(fac[x][:], fac[x][:], b3(psel[x][:]), ALU.mult), R=[fac[x], psel[x]], W=[fac[x]])
        S.op('dve', lambda v: v.tensor_tensor(cw[x][:], exx[x][:], b4(fac[x][:]), ALU.mult), R=[exx[x], fac[x]], W=[cw[x]])
        for s_ in range(4):
            S.op('pe', lambda p: p.transpose(PT[0:16, s_ * 128:(s_ + 1) * 128], cw[x][:, s_, :, :].rearrange("p g e -> p (g e)"), ident_f[:]), R=[cw[x], ident_f], W=[PT])
        S.op('act', lambda a: a.activation(ct[:], PT[0:16, :], AF.Copy), R=[PT], W=[ct])
        S.dma('sp', io['cwT'][:, sl], ct[:], R=[ct])

    load(0)
    if n_tiles > 1:
        load(1)
    stage1(0)
    for t in range(n_tiles):
        if t + 1 < n_tiles:
            stage1(t + 1)
        stage2(t)
        if t + 2 < n_tiles:
            load(t + 2)
    S.end_phase()


def emit_C2(S, io, c, last_layer, n_half=2):
    S.begin_phase()
    NTL = 512
    NE = 256
    HT = 2048
    ones_f = c['ones_f']
    modc = scol(S, "modc2", io['modc'], [128, 48])
    Sel = S.sb([16, 16 * 128], F32, "Sel")
    S.dma('sp', Sel[:], io['c_Sel'], W=[Sel])
    if not last_layer:
        modn = scol(S, "modn", io['modn'], [128, 16])
        ang = scol(S, "ang", io['attn_norm_col'], [128, 8])
        Aa = S.sb([128, 8], F32, "Aa")
        S.op('dve', lambda v: v.scalar_tensor_tensor(Aa[:], modn[:, 8:16], 1.0, ang[:], ALU.add, ALU.mult), R=[modn, ang], W=[Aa])
    hf = S.sb([128, 8, HT], BF16, "hfh")
    cwt = S.sb([16, HT], F32, "cwh")
    acc = S.sb([128, 8, HT], F32, "macc")
    acc_t = [[S.sub(acc[:, dc, tl * NTL:(tl + 1) * NTL], "acc%d_%d" % (dc, tl)) for tl in range(4)] for dc in range(8)]
    stg = [S.sb([128, 8, 256], F32, "stgC%d" % i) for i in range(2)]
    Wge = [S.sb([128, 8, 256], BF16, "Wge%d" % i) for i in range(2)]
    Wue = [S.sb([128, 8, 256], BF16, "Wue%d" % i) for i in range(2)]
    Wde = [S.sb([128, 2, 1024], BF16, "Wde%d" % i) for i in range(2)]
    cwb = [S.sb([128, NTL], F32, "cwb%d" % i) for i in range(2)]
    sg = [S.sb([128, NTL], F32, "sg%d" % i) for i in range(2)]
    h1 = [S.sb([128, NTL], F32, "h1_%d" % i) for i in range(2)]
    pt_ = [S.sb([128, NTL], F32, "ptl%d" % i) for i in range(2)]
    hid = [S.sb([128, 2, NTL], BF16, "hid%d" % i) for i in range(2)]
    xt = S.sb([128, 8, NE], F32, "c2xt")
    sq = S.sb([128, 8, NE], F32, "c2sq")
    ub = S.sb([128, 8, NE], BF16, "c2ub")
    lnt = S.sb([128, NE], F32, "c2ln")
    rst = S.sb([128, NE], F32, "c2rs")
    tmp = [S.sb([128, NE], F32, "c2tmp%d" % i) for i in range(2)]
    PGU = [bank(S, "PGU%d" % i) for i in range(4)]
    PD = [bank(S, "PD%d" % i) for i in range(2)]
    PCW = bank(S, "PCW")
    PSS = bank(S, "PSS2")
    nst = [0]

    def load_expert(e, slot):
        for (dst, src) in ((Wge[slot], io['w_eg'][e]), (Wue[slot], io['w_eu'][e])):
            st = stg[nst[0] % 2]
            nst[0] += 1
            S.dma('sp', st[:], src.rearrange("(k p) n -> p k n", p=128), W=[st])
            S.op('pool', lambda g: g.tensor_copy(dst[:], st[:]), R=[st], W=[dst])
        st = stg[nst[0] % 2]
        nst[0] += 1
        stv = st[:].rearrange("p (a k) n -> p a (k n)", a=2)
        S.dma('sp', stv, io['w_ed'][e].rearrange("(k p) n -> p k n", p=128), W=[st])
        S.op('pool', lambda g: g.tensor_copy(Wde[slot][:], stv), R=[st], W=[Wde[slot]])

    nx = [0]

    def epi(half, tl):
        h0 = half * HT
        gsl = slice(h0 + tl * NE, h0 + (tl + 1) * NE)
        tsl = slice(tl * NE, (tl + 1) * NE)
        ats = [acc_t[dc][(tl * NE) // NTL] for dc in range(8)]
        S.dma('sp', xt[:], io['x1T'][:, :, gsl].rearrange("k p n -> p k n"), W=[xt])
        for dc in range(8):
            S.op('dve', lambda v: v.scalar_tensor_tensor(xt[:, dc, :], acc[:, dc, tsl], modc[:, 40 + dc:41 + dc], xt[:, dc, :], ALU.mult, ALU.add), R=[ats[dc], modc, xt], W=[xt])
        S.dma('sp', io['xT_out'][:, :, gsl].rearrange("k p n -> p k n"), xt[:], R=[xt])
        if not last_layer:
            norm_mod(S, xt, Aa[:], modn[:, 0:8], [ub], sq, ones_f, PSS, lnt, rst, tmp, [Aa, modn])
            S.dma('sp', io['uT_out'][:, :, gsl].rearrange("k p n -> p k n"), ub[:], R=[ub])

    for half in range(n_half):
        h0 = half * HT
        S.dma('sp', hf[:], io['hfT'][:, :, h0:h0 + HT].rearrange("k p n -> p k n"), W=[hf])
        S.dma('sp', cwt[:], io['cwT'][:, h0:h0 + HT], W=[cwt])
        load_expert(0, 0)
        items = [(e, tl) for e in range(16) for tl in range(4)]
        bufs = {}

        def gu(n):
            e, tl = items[n]
            slot = e % 2
            tsl = slice(tl * NTL, (tl + 1) * NTL)
            hd = hid[n % 2]
            cb = cwb[n % 2]
            bufs[n] = hd
            S.op('pe', lambda p: p.matmul(PCW[:, :], Sel[:, e * 128:(e + 1) * 128], cwt[:, tsl], start=True, stop=True), R=[Sel, cwt], W=[PCW])
            S.op('act', lambda a: a.activation(cb[:], PCW[:, :], AF.Copy), R=[PCW], W=[cb])
            for fc in range(2):
                pg = PGU[fc * 2]
                pu = PGU[fc * 2 + 1]
                for k in range(8):
                    S.op('pe', lambda p: p.matmul(pg[:, :], Wge[slot][:, k, fc * 128:(fc + 1) * 128], hf[:, k, tsl], start=(k == 0), stop=(k == 7)), R=[Wge[slot], hf], W=[pg])
                for k in range(8):
                    S.op('pe', lambda p: p.matmul(pu[:, :], Wue[slot][:, k, fc * 128:(fc + 1) * 128], hf[:, k, tsl], start=(k == 0), stop=(k == 7)), R=[Wue[slot], hf], W=[pu])
                s1 = sg[fc]
                S.op('act', lambda a: a.activation(s1[:], pg[:, :], AF.Silu), R=[pg], W=[s1])
                S.op('dve', lambda v: v.tensor_tensor(h1[fc][:], pu[:, :], s1[:], ALU.mult), R=[pu, s1], W=[h1[fc]])
                S.op('pool', lambda g: g.tensor_tensor(hd[:, fc, :], h1[fc][:], cb[:], ALU.mult), R=[h1[fc], cb], W=[hd])

        def down(n):
            e, tl = items[n]
            slot = e % 2
            tsl = slice(tl * NTL, (tl + 1) * NTL)
            hd = bufs.pop(n)
            for dc in range(8):
                pd = PD[dc % 2]
                for fc in range(2):
                    S.op('pe', lambda p: p.matmul(pd[:, :], Wde[slot][:, fc, dc * 128:(dc + 1) * 128], hd[:, fc, :], start=(fc == 0), stop=(fc == 1)), R=[Wde[slot], hd], W=[pd])
                at = acc_t[dc][tl]
                if e == 0:
                    S.op('act', lambda a: a.activation(acc[:, dc, tsl], pd[:, :], AF.Copy), R=[pd], W=[at])
                else:
                    S.op('dve', lambda v: v.tensor_tensor(acc[:, dc, tsl], pd[:, :], acc[:, dc, tsl], ALU.add), R=[pd, at], W=[at])

        load_expert(1, 1)
        gu(0)
        for n in range(len(items)):
            if n + 1 < len(items):
                gu(n + 1)
            e_, tl_ = items[n]
            if half > 0 and e_ == 0:
                epi(half - 1, 2 * tl_)
                epi(half - 1, 2 * tl_ + 1)
            down(n)
            if tl_ == 3 and e_ + 2 < 16:
                load_expert(e_ + 2, e_ % 2)
        if half == n_half - 1:
            for tlE in range(HT // NE):
                epi(half, tlE)
    S.end_phase()


def emit_A(S, io, c, n_tiles=8):
    S.begin_phase()
    NTL = 512
    ones_f = c['ones_f']
    modn = scol(S, "modnA", io['modn'], [128, 16])
    ang = scol(S, "angA", io['attn_norm_col'], [128, 8])
    Aa = S.sb([128, 8], F32, "AaA")
    S.op('dve', lambda v: v.scalar_tensor_tensor(Aa[:], modn[:, 8:16], 1.0, ang[:], ALU.add, ALU.mult), R=[modn, ang], W=[Aa])
    xt = [S.sb([128, 8, NTL], F32, "axt%d" % i) for i in range(2)]
    ub = [S.sb([128, 8, NTL], BF16, "aub%d" % i) for i in range(2)]
    sq = S.sb([128, 8, NTL], F32, "asq")
    lnt = S.sb([128, NTL], F32, "aln")
    rst = S.sb([128, NTL], F32, "ars")
    tmp = [S.sb([128, NTL], F32, "atmp%d" % i) for i in range(2)]
    PSS = bank(S, "PSSA")
    for t in range(n_tiles):
        sl = slice(t * NTL, (t + 1) * NTL)
        x = xt[t % 2]
        S.dma('sp', x[:], io['xT'][:, :, sl].rearrange("k p n -> p k n"), W=[x])
        u = ub[t % 2]
        norm_mod(S, x, Aa[:], modn[:, 0:8], [u], sq, ones_f, PSS, lnt, rst, tmp, [Aa, modn])
        S.dma('sp', io['uT_out'][:, :, sl].rearrange("k p n -> p k n"), u[:], R=[u])
    S.end_phase()


CONST_SPECS = {'c_ident_b': ([128, 128], BF16), 'c_ident_f': ([128, 128], F32), 'c_tri_b': ([128, 128], BF16),
               'c_E65': ([65, 64], F32)}


def _declare(nc, specs, kind):
    io = {}
    for name, (shape, dt) in specs.items():
        io[name] = nc.dram_tensor(name, list(shape), dt, kind=kind).ap()
    return io


def build_M():
    nc = bass.Bass("TRN2", target_bir_lowering=False)
    io = _declare(nc, {'c_col': ([128, 8], F32), 'w_ada': ([1024, 6144], F32), 'b_ada': ([1, 6144], F32)}, "ExternalInput")
    io.update(_declare(nc, {'mod_out': ([1, 6144], F32)}, "ExternalOutput"))
    with ExitStack() as st:
        S = Sched(nc, st)
        emit_mod(S, io)
        S.finish_all()
    return nc


A_IN = {'xT': ([8, 128, TOK], F32), 'modn': ([128, 16], F32), 'attn_norm_col': ([128, 8], F32)}


def build_A():
    nc = bass.Bass("TRN2", target_bir_lowering=False)
    io = _declare(nc, dict(A_IN, **CONST_SPECS), "ExternalInput")
    io.update(_declare(nc, {'uT_out': ([8, 128, TOK], BF16)}, "ExternalOutput"))
    with ExitStack() as st:
        S = Sched(nc, st)
        c = load_consts(S, io)
        emit_A(S, io, c)
        S.finish_all()
    return nc


B_IN = {'uT': ([8, 128, S_LEN], BF16), 'WB': ([1024, 1186], F32), 'wuq': ([256, 192], F32), 'wukv': ([128, 256], F32),
        'mla_ncol': ([128, 3], F32), 'mla_grow': ([1, 384], F32), 'c_rope': ([128, 4096], F32),
        'c_Bd': ([128, 128], F32), 'c_M4': ([128, 512], BF16), 'dil_gcol': ([128, 2], F32),
        'ml_conv': ([64, 10], F32), 'ml_gate': ([1, 2], F32), 'ml_hg': ([1, 128], F32)}


def build_B():
    nc = bass.Bass("TRN2", target_bir_lowering=False)
    io = _declare(nc, dict(B_IN, **CONST_SPECS), "ExternalInput")
    io.update(_declare(nc, {'yaT': ([128, S_LEN], BF16), 'ybT': ([128, S_LEN], BF16), 'ycT': ([128, S_LEN], BF16)}, "ExternalOutput"))
    with ExitStack() as st:
        S = Sched(nc, st)
        c = load_consts(S, io)
        emit_mlstm(S, io, c)
        emit_dil(S, io, c)
        emit_mla(S, io, c)
        S.finish_all()
    return nc


C_IN = {'xT': ([8, 128, TOK], F32), 'uT_loc': ([8, 128, TOK], BF16), 'yT': ([3, 4, 128, TOK], BF16),
        'w_gate': ([1024, 3072], F32), 'w_br': ([3, 512, 1024], F32), 'w_out': ([1024, 1024], F32),
        'w_router': ([1024, 20], F32), 'b_router': ([1, 20], F32),
        'w_eg': ([16, 1024, 256], F32), 'w_eu': ([16, 1024, 256], F32), 'w_ed': ([16, 256, 1024], F32),
        'modc': ([128, 48], F32), 'modn': ([128, 16], F32), 'ffn_norm_col': ([128, 8], F32), 'attn_norm_col': ([128, 8], F32),
        'c_Sel': ([16, 2048], F32)}


def build_C():
    nc = bass.Bass("TRN2", target_bir_lowering=False)
    io = _declare(nc, dict(C_IN, **CONST_SPECS), "ExternalInput")
    io.update(_declare(nc, {'xT_out': ([8, 128, TOK], F32), 'uT_out': ([8, 128, TOK], BF16)}, "ExternalOutput"))
    io.update(_declare(nc, {'mgT': ([8, 128, TOK], BF16), 'x1T': ([8, 128, TOK], F32), 'hfT': ([8, 128, TOK], BF16),
                            'cwT': ([16, TOK], F32)}, "Internal"))
    with ExitStack() as st:
        S = Sched(nc, st)
        c = load_consts(S, io)
        emit_C1a(S, io, c)
        emit_C1b(S, io, c)
        emit_C2(S, io, c, last_layer=False)
        S.finish_all()
    return nc


def col8(v):
    return np.ascontiguousarray(np.asarray(v, np.float32).reshape(8, 128).T)


def _run(nc, in_maps):
    res = run_bass_kernel_spmd(nc, in_maps, core_ids=list(range(8)))
    return res.results


def kernel(**inp):
    inp = {k: np.asarray(v) for k, v in inp.items()}
    x = inp['x'].astype(np.float32, copy=False)
    cst = host_consts()
    cst.update(host_consts_dil())
    sel = np.zeros((16, 16, 128), np.float32)
    for e in range(16):
        sel[e, e, :] = 1.0
    cst['c_Sel'] = np.ascontiguousarray(sel.reshape(16, 2048))
    base_c = {k: cst[k] for k in CONST_SPECS}

    ncM = build_M()
    maps = []
    for cidx in range(8):
        l, b = cidx // 2, cidx % 2
        maps.append({'c_col': col8(inp['c'][b]), 'w_ada': np.ascontiguousarray(inp['w_ada'][l]),
                     'b_ada': np.ascontiguousarray(inp['b_ada'][l][None, :])})
    r = _run(ncM, maps)
    mod = np.zeros((DEPTH, NB, 6, 1024), np.float32)
    for cidx in range(8):
        mod[cidx // 2, cidx % 2] = r[cidx]['mod_out'].reshape(6, 1024)

    def modcols(l, b, rows):
        return np.ascontiguousarray(np.concatenate([col8(mod[l, b, i]) for i in rows], axis=1))

    xT = []
    for cidx in range(8):
        b, j = cidx // 4, cidx % 4
        xT.append(np.ascontiguousarray(x[b, j * TOK:(j + 1) * TOK, :].T.reshape(8, 128, TOK)))
    ncA = build_A()
    maps = []
    for cidx in range(8):
        b = cidx // 4
        m = dict(base_c)
        m.update({'xT': xT[cidx], 'modn': modcols(0, b, (0, 1)), 'attn_norm_col': col8(inp['attn_norm'][0])})
        maps.append(m)
    r = _run(ncA, maps)
    uT_loc = [r[cidx]['uT_out'] for cidx in range(8)]

    ncB = build_B()
    ncC = build_C()
    for l in range(DEPTH):
        uT_full = [np.ascontiguousarray(np.concatenate([uT_loc[b * 4 + j] for j in range(4)], axis=2)) for b in range(NB)]
        maps = []
        for cidx in range(8):
            b, j = cidx // 4, cidx % 4
            m = dict(base_c)
            m.update(prep_B_weights(inp, l, j))
            m.update(prep_dil_vecs(inp, l))
            m.update(prep_mlstm_vecs(inp, l, j))
            m.update({'uT': uT_full[b], 'c_rope': cst['c_rope'], 'c_Bd': cst['c_Bd'], 'c_M4': cst['c_M4']})
            maps.append(m)
        rB = _run(ncB, maps)
        ln = min(l + 1, DEPTH - 1)
        wC = {'w_gate': np.ascontiguousarray(inp['w_in'][l][:, 3496:6568]),
              'w_br': np.ascontiguousarray(np.stack([inp['w_branch_a'][l], inp['w_branch_b'][l], inp['w_branch_c'][l]])),
              'w_out': np.ascontiguousarray(inp['w_out'][l]),
              'w_router': np.ascontiguousarray(np.concatenate([inp['w_router_group'][l], inp['w_router_expert'][l]], axis=1)),
              'b_router': np.ascontiguousarray(np.concatenate([inp['b_router_group'][l], inp['b_router_expert'][l]])[None, :]),
              'w_eg': np.ascontiguousarray(inp['w_exp_gate'][l]), 'w_eu': np.ascontiguousarray(inp['w_exp_up'][l]),
              'w_ed': np.ascontiguousarray(inp['w_exp_down'][l]),
              'ffn_norm_col': col8(inp['ffn_norm'][l]), 'attn_norm_col': col8(inp['attn_norm'][ln]), 'c_Sel': cst['c_Sel']}
        maps = []
        for cidx in range(8):
            b, j = cidx // 4, cidx % 4
            sl = slice(j * TOK, (j + 1) * TOK)
            yT = np.stack([np.stack([rB[b * 4 + jj][nm][:, sl] for jj in range(4)]) for nm in ('yaT', 'ybT', 'ycT')])
            m = dict(base_c)
            m.update(wC)
            m.update({'xT': xT[cidx], 'uT_loc': uT_loc[cidx], 'yT': np.ascontiguousarray(yT),
                      'modc': modcols(l, b, range(6)), 'modn': modcols(ln, b, (0, 1))})
            maps.append(m)
        rC = _run(ncC, maps)
        xT = [rC[cidx]['xT_out'] for cidx in range(8)]
        uT_loc = [rC[cidx]['uT_out'] for cidx in range(8)]

    out = np.empty((NB, S_LEN, D), np.float32)
    for cidx in range(8):
        b, j = cidx // 4, cidx % 4
        out[b, j * TOK:(j + 1) * TOK, :] = xT[cidx].reshape(D, TOK).T
    return out
```

```python
import numpy as np
import ml_dtypes
from contextlib import ExitStack
import concourse.bass as bass
import concourse.mybir as mybir
from concourse.bass_utils import run_bass_kernel_spmd

F32 = mybir.dt.float32
BF16 = mybir.dt.bfloat16
AF = mybir.ActivationFunctionType
ALU = mybir.AluOpType
AX = mybir.AxisListType
NPBF = ml_dtypes.bfloat16

D = 1024
S_LEN = 16384
NB = 2
DEPTH = 4
EPS = 1e-6
TOK = 4096
NT = 512


class T:
    __slots__ = ("ap", "name", "lw", "rs", "dsem", "dcnt")

    def __init__(self, ap, name):
        self.ap = ap
        self.name = name
        self.lw = None
        self.rs = {}
        self.dsem = None
        self.dcnt = 0

    def __getitem__(self, k):
        return self.ap[k]


class Sched:
    SEM_MAX = 30000

    def __init__(self, nc, stack):
        self.nc = nc
        self.root = stack
        self.eng = {'pe': nc.tensor, 'act': nc.scalar, 'dve': nc.vector, 'pool': nc.gpsimd, 'sp': nc.sync}
        self.sem = {}
        self.cnt = {}
        self.nsem = 0
        for e in self.eng:
            self._newsem(e)
        self.waited = {e: {} for e in self.eng}
        self.phase = None
        self.phase_tiles = []
        self.dma_pool = []
        self.all_dma = {}
        self.cc_sem = None
        self.ninst = 0

    def _newsem(self, e):
        self.nsem += 1
        self.sem[e] = self.root.enter_context(self.nc.semaphore("s_%s_%d" % (e, self.nsem)))
        self.cnt[e] = 0

    def begin_phase(self):
        self.phase = ExitStack()
        self.phase_tiles = []

    def end_phase(self):
        deps = []
        for t in self.phase_tiles:
            if t.lw is not None:
                deps.append(t.lw)
            deps.extend(t.rs.values())
        self._wait('sp', deps, True)
        self.sp_mark()
        self.barrier()
        for t in self.phase_tiles:
            if t.dsem is not None:
                self.dma_pool.append((t.dsem, t.dcnt))
        self.phase.close()
        self.phase = None
        self.phase_tiles = []

    def barrier(self):
        tags = [(self.sem[e], self.cnt[e], e) for e in self.eng if self.cnt[e] > 0]
        for e in self.eng:
            self._wait(e, tags, False)

    def sp_mark(self):
        ins = self.eng['sp'].sem_inc(self.sem['sp'], 1)
        self.cnt['sp'] += 1

    def collective(self, src_ap, dst_ap, deps, groups=((0, 1, 2, 3), (4, 5, 6, 7))):
        if self.cc_sem is None:
            self.cc_sem = self.root.enter_context(self.nc.semaphore("cc_sem"))
            self.cc_cnt = 0
        self._wait('pool', list(deps), True)
        ins = self.eng['pool'].collective_compute("AllGather", ALU.bypass, replica_groups=[list(g) for g in groups], ins=[src_ap], outs=[dst_ap])
        self.cc_cnt += 16
        ins.then_inc(self.cc_sem, 16)
        tag = (self.cc_sem, self.cc_cnt, 'dma')
        self.all_dma[id(self.cc_sem)] = tag
        self._wait('sp', [tag], True)
        return tag

    def sb(self, shape, dt, name):
        st = self.phase if self.phase is not None else self.root
        self.nsem += 1
        t = T(st.enter_context(self.nc.sbuf_tensor("sb_%s_%d" % (name, self.nsem), list(shape), dt)), name)
        if self.phase is not None:
            self.phase_tiles.append(t)
        return t

    def ps(self, shape, dt, name):
        st = self.phase if self.phase is not None else self.root
        self.nsem += 1
        t = T(st.enter_context(self.nc.psum_tensor("ps_%s_%d" % (name, self.nsem), list(shape), dt)), name)
        if self.phase is not None:
            self.phase_tiles.append(t)
        return t

    def sub(self, ap, name):
        t = T(ap, name)
        if self.phase is not None:
            self.phase_tiles.append(t)
        return t

    def _wait(self, e, deps, is_dma):
        w = self.waited[e]
        for (sem, val, de) in deps:
            if de == e and not is_dma and e == 'pe':
                continue
            key = id(sem)
            if w.get(key, 0) >= val:
                continue
            self.eng[e].wait_ge(sem, val)
            w[key] = val

    def _deps(self, R, W):
        deps = []
        for t in R:
            if t.lw is not None:
                deps.append(t.lw)
        for t in W:
            if t.lw is not None:
                deps.append(t.lw)
            deps.extend(t.rs.values())
        return deps

    def _mark(self, tag, R, W):
        sem = tag[0]
        for t in W:
            t.lw = tag
            t.rs = {}
        for t in R:
            t.rs[id(sem)] = tag

    def op(self, e, fn, R=(), W=()):
        self._wait(e, self._deps(R, W), False)
        if self.cnt[e] >= self.SEM_MAX:
            self._newsem(e)
        ins = fn(self.eng[e])
        self.cnt[e] += 1
        self.ninst += 1
        ins.then_inc(self.sem[e], 1)
        self._mark((self.sem[e], self.cnt[e], e), R, W)
        return ins

    def dma(self, q, out_ap, in_ap, R=(), W=(), owner=None):
        if q == 'pool':
            q = 'sp'
        self._wait(q, self._deps(R, W), True)
        if owner is None:
            owner = (list(W) + list(R))[0]
        if owner.dsem is None or owner.dcnt >= self.SEM_MAX:
            if owner.dsem is None and self.dma_pool:
                owner.dsem, owner.dcnt = self.dma_pool.pop()
            else:
                owner.dsem = self.root.enter_context(self.nc.semaphore("d_%d" % self.nsem))
                self.nsem += 1
                owner.dcnt = 0
        ins = self.eng[q].dma_start(out=out_ap, in_=in_ap)
        owner.dcnt += 16
        self.ninst += 1
        ins.then_inc(owner.dsem, 16)
        tag = (owner.dsem, owner.dcnt, 'dma')
        self.all_dma[id(owner.dsem)] = tag
        self._mark(tag, R, W)
        return tag

    def finish_all(self):
        self._wait('sp', list(self.all_dma.values()), True)
        self.barrier()

    def finish(self, tiles):
        deps = []
        for t in tiles:
            if t.lw is not None:
                deps.append(t.lw)
            deps.extend(t.rs.values())
        self._wait('sp', deps, True)


def bank(S, name, dt=F32):
    return S.ps([128, 512 if dt == F32 else 1024], dt, name)


def rsqrt_mean(S, out_ap, in_ap, n, tmp_ap, R, W):
    S.op('act', lambda a: a.activation(tmp_ap, in_ap, AF.Ln, scale=1.0 / n, bias=S.eps_col[0:tmp_ap.shape[0], 0:1]), R=R + [S.eps_t], W=W)
    S.op('act', lambda a: a.activation(out_ap, tmp_ap, AF.Exp, scale=-0.5), R=W, W=W)


def load_consts(S, io):
    c = {}
    c['ident_b'] = S.sb([128, 128], BF16, "ident_b")
    c['ident_f'] = S.sb([128, 128], F32, "ident_f")
    c['tri_b'] = S.sb([128, 128], BF16, "tri_b")
    c['E65'] = S.sb([65, 64], F32, "E65")
    c['ones_f'] = S.sb([128, 128], F32, "ones_f")
    S.eps_t = S.sb([128, 1], F32, "eps_t")
    S.eps_col = S.eps_t.ap
    S.dma('sp', c['ident_b'][:], io['c_ident_b'], W=[c['ident_b']])
    S.dma('sp', c['ident_f'][:], io['c_ident_f'], W=[c['ident_f']])
    S.dma('sp', c['tri_b'][:], io['c_tri_b'], W=[c['tri_b']])
    S.dma('sp', c['E65'][:], io['c_E65'], W=[c['E65']])
    S.op('pool', lambda g: g.memset(c['ones_f'][:], 1.0), W=[c['ones_f']])
    S.op('pool', lambda g: g.memset(S.eps_t[:], EPS), W=[S.eps_t])
    return c


def emit_mla(S, io, c, n_tiles=32):
    S.begin_phase()
    uT = io['uT']
    ident_b, tri_b, E65 = c['ident_b'], c['tri_b'], c['E65']
    wl_f = S.sb([128, 8, 416], F32, "wl_f")
    S.dma('sp', wl_f[:], io['WB'][:, 0:416].rearrange("(k p) n -> p k n", p=128), W=[wl_f])
    wl = S.sb([128, 8, 416], BF16, "wl")
    S.op('pool', lambda g: g.tensor_copy(wl[:], wl_f[:]), R=[wl_f], W=[wl])
    wuq_f = S.sb([128, 2, 192], F32, "wuq_f")
    S.dma('sp', wuq_f[:], io['wuq'].rearrange("(k p) n -> p k n", p=128), W=[wuq_f])
    wukv_f = S.sb([128, 256], F32, "wukv_f")
    S.dma('sp', wukv_f[:], io['wukv'], W=[wukv_f])
    ncol = S.sb([128, 3], F32, "ncol")
    S.dma('sp', ncol[:], io['mla_ncol'], W=[ncol])
    wuq = S.sb([128, 2, 192], BF16, "wuq")
    wukv = S.sb([128, 256], BF16, "wukv")
    for k in range(2):
        S.op('dve', lambda v: v.tensor_scalar(wuq[:, k, :], wuq_f[:, k, :], ncol[:, k:k + 1], None, ALU.mult), R=[wuq_f, ncol], W=[wuq])
    S.op('dve', lambda v: v.tensor_scalar(wukv[:], wukv_f[:], ncol[:, 2:3], None, ALU.mult), R=[wukv_f, ncol], W=[wukv])
    g4 = S.sb([128, 384], F32, "g4")
    S.dma('sp', g4[:], io['mla_grow'].partition_broadcast(128), W=[g4])
    S.op('dve', lambda v: v.tensor_scalar(g4[:, 0:192], g4[:, 0:192], 96 ** -0.5, None, ALU.mult), R=[g4], W=[g4])
    g4v = g4[:].rearrange("p (a b) -> p a b", a=4)
    cs = S.sb([128, 128 * 32], F32, "cs")
    S.dma('sp', cs[:], io['c_rope'], W=[cs])
    csv = cs[:].rearrange("p (t c) -> p t c", c=32)
    KT = S.sb([96, 128, 2, 128], BF16, "KT")
    VA = S.sb([128, 128, 2, 65], BF16, "VA")
    KT_t = [S.sub(KT[:, 4 * i:4 * i + 4, :, :], "KT%d" % i) for i in range(32)]
    VA_t = [S.sub(VA[:, 4 * i:4 * i + 4, :, :], "VA%d" % i) for i in range(32)]
    S.op('pool', lambda g: g.memset(VA[:, :, :, 64:65], 1.0), W=VA_t)
    QT = [S.sb([96, 2, 512], BF16, "QT%d" % i) for i in range(2)]
    uts = [S.sb([128, 8, 512], BF16, "ut%d" % i) for i in range(2)]
    p_lat = bank(S, "p_lat")
    p_trq = bank(S, "p_trq", BF16)
    p_tr = p_trq
    p_qkT = p_trq
    p_qkv = bank(S, "p_qkv")
    p_s = [bank(S, "p_s%d" % i) for i in range(3)]
    p_o0 = bank(S, "p_o0")
    p_o = [p_o0, p_o0]
    p_den = bank(S, "p_den")
    trv = p_trq[:, 0:512].rearrange("p (a b) -> p a b", b=128)
    qkTv = p_trq[:, 512:1024].rearrange("p (a b) -> p a b", b=128)
    R2 = 2
    junk = [S.sb([128, 256], F32, "junk%d" % i) for i in range(R2)]
    ss = [S.sb([128, 2], F32, "ss%d" % i) for i in range(R2)]
    sst = [S.sb([128, 2], F32, "sst%d" % i) for i in range(R2)]
    rstd = [S.sb([128, 2], F32, "rstd%d" % i) for i in range(R2)]
    cn = [S.sb([128, 384], BF16, "cn%d" % i) for i in range(R2)]
    cnT = [S.sb([128, 3, 128], BF16, "cnT%d" % i) for i in range(R2)]
    qk = [S.sb([128, 4, 96], F32, "qk%d" % i) for i in range(R2)]
    sq = [S.sb([128, 4, 96], F32, "sq%d" % i) for i in range(R2)]
    ss4 = [S.sb([128, 4], F32, "ss4%d" % i) for i in range(R2)]
    ss4t = [S.sb([128, 4], F32, "ss4t%d" % i) for i in range(R2)]
    rs4 = [S.sb([128, 4], F32, "rs4%d" % i) for i in range(R2)]
    qkn = [S.sb([128, 4, 96], F32, "qkn%d" % i) for i in range(R2)]
    rt = [[S.sb([128, 4, 16], F32, "rt%d_%d" % (j, i)) for j in range(4)] for i in range(R2)]
    qkr = [S.sb([128, 4, 96], BF16, "qkr%d" % i) for i in range(R2)]
    pts = [S.sb([128, 512], BF16, "pt%d" % i) for i in range(3)]
    o_sb = [S.sb([65, 512], F32, "o_sb%d" % i) for i in range(2)]
    rden = [S.sb([64, 512], F32, "rden%d" % i) for i in range(2)]
    yts = [S.sb([128, 512], BF16, "yt%d" % i) for i in range(2)]

    def load_u(i):
        S.dma('sp', uts[i % 2][:], uT[:, :, i * 512:(i + 1) * 512].rearrange("k p n -> p k n"), W=[uts[i % 2]])

    import os
    STOP = int(os.environ.get('DBG_STOP', '99'))

    def proj_sub(i, s):
        ut = uts[i % 2]
        r = (i * 4 + s) % R2
        blk = i * 4 + s
        for k in range(8):
            S.op('pe', lambda p: p.matmul(p_lat[:, 0:416], ut[:, k, s * 128:(s + 1) * 128], wl[:, k, :], start=(k == 0), stop=(k == 7)), R=[ut, wl], W=[p_lat])
        yield
        S.op('act', lambda a: a.activation(junk[r][:, 0:256], p_lat[:, 0:256], AF.Square, accum_out=ss[r][:, 0:1]), R=[p_lat], W=[junk[r], ss[r]])
        S.op('act', lambda a: a.activation(junk[r][:, 0:128], p_lat[:, 256:384], AF.Square, accum_out=ss[r][:, 1:2]), R=[p_lat], W=[junk[r], ss[r]])
        yield
        S.op('act', lambda a: a.activation(sst[r][:, 0:1], ss[r][:, 0:1], AF.Ln, scale=1.0 / 256, bias=S.eps_col[:, 0:1]), R=[ss[r], S.eps_t], W=[sst[r]])
        S.op('act', lambda a: a.activation(sst[r][:, 1:2], ss[r][:, 1:2], AF.Ln, scale=1.0 / 128, bias=S.eps_col[:, 0:1]), R=[ss[r], S.eps_t], W=[sst[r]])
        S.op('act', lambda a: a.activation(rstd[r][:], sst[r][:], AF.Exp, scale=-0.5), R=[sst[r]], W=[rstd[r]])
        yield
        S.op('dve', lambda v: v.tensor_scalar(cn[r][:, 0:256], p_lat[:, 0:256], rstd[r][:, 0:1], None, ALU.mult), R=[p_lat, rstd[r]], W=[cn[r]])
        S.op('dve', lambda v: v.tensor_scalar(cn[r][:, 256:384], p_lat[:, 256:384], rstd[r][:, 1:2], None, ALU.mult), R=[p_lat, rstd[r]], W=[cn[r]])
        yield
        for h in range(2):
            S.op('dve', lambda v: v.tensor_copy(qk[r][:, 2 + h, 64:96], p_lat[:, 384:416]), R=[p_lat], W=[qk[r]])
        yield
        for j in range(3):
            S.op('pe', lambda p: p.transpose(trv[:, j, :], cn[r][:, j * 128:(j + 1) * 128], ident_b[:]), R=[cn[r], ident_b], W=[p_tr])
        yield
        S.op('dve', lambda v: v.tensor_copy(cnT[r][:], trv[:, 0:3, :]), R=[p_tr], W=[cnT[r]])
        yield
        S.op('pe', lambda p: p.matmul(p_qkv[:, 0:192], cnT[r][:, 0, :], wuq[:, 0, :], start=True, stop=False), R=[cnT[r], wuq], W=[p_qkv])
        S.op('pe', lambda p: p.matmul(p_qkv[:, 0:192], cnT[r][:, 1, :], wuq[:, 1, :], start=False, stop=False), R=[cnT[r], wuq], W=[p_qkv])
        S.op('pe', lambda p: p.matmul(p_qkv[:, 192:448], cnT[r][:, 2, :], wukv[:], start=False, stop=True), R=[cnT[r], wukv], W=[p_qkv])
        yield
        kvv = p_qkv[:, 192:448].rearrange("p (a b) -> p a b", a=2)
        S.op('act', lambda a: a.activation(qk[r][:, 0:2, :], p_qkv[:, 0:192].rearrange("p (a b) -> p a b", a=2), AF.Copy), R=[p_qkv], W=[qk[r]])
        S.op('dve', lambda v: v.tensor_copy(qk[r][:, 2:4, 0:64], kvv[:, :, 0:64]), R=[p_qkv], W=[qk[r]])
        S.op('act', lambda a: a.activation(VA[:, blk, :, 0:64], kvv[:, :, 64:128], AF.Copy), R=[p_qkv], W=[VA_t[i]])
        yield
        S.op('dve', lambda v: v.tensor_tensor(sq[r][:], qk[r][:], qk[r][:], ALU.mult), R=[qk[r]], W=[sq[r]])
        S.op('dve', lambda v: v.tensor_reduce(ss4[r][:], sq[r][:], AX.X, ALU.add), R=[sq[r]], W=[ss4[r]])
        yield
        S.op('act', lambda a: a.activation(ss4t[r][:], ss4[r][:], AF.Ln, scale=1.0 / 96, bias=S.eps_col[:, 0:1]), R=[ss4[r], S.eps_t], W=[ss4t[r]])
        S.op('act', lambda a: a.activation(rs4[r][:], ss4t[r][:], AF.Exp, scale=-0.5), R=[ss4t[r]], W=[rs4[r]])
        yield
        S.op('pool', lambda g: g.tensor_tensor(sq[r][:], qk[r][:], g4v, ALU.mult), R=[qk[r], g4], W=[sq[r]])
        for sl in range(4):
            S.op('dve', lambda v: v.tensor_scalar(qkn[r][:, sl, :], sq[r][:, sl, :], rs4[r][:, sl:sl + 1], None, ALU.mult), R=[sq[r], rs4[r]], W=[qkn[r]])
        cosb = csv[:, blk:blk + 1, 0:16].to_broadcast([128, 4, 16])
        sinb = csv[:, blk:blk + 1, 16:32].to_broadcast([128, 4, 16])
        x1 = qkn[r][:, :, 64:80]
        x2 = qkn[r][:, :, 80:96]
        t1, t2, t3, t4 = rt[r]
        S.op('pool', lambda g: g.tensor_copy(qkr[r][:, :, 0:64], qkn[r][:, :, 0:64]), R=[qkn[r]], W=[qkr[r]])
        S.op('dve', lambda v: v.tensor_tensor(t1[:], x1, cosb, ALU.mult), R=[qkn[r], cs], W=[t1])
        S.op('pool', lambda g: g.tensor_tensor(t2[:], x2, sinb, ALU.mult), R=[qkn[r], cs], W=[t2])
        S.op('pool', lambda g: g.tensor_tensor(t3[:], x1, sinb, ALU.mult), R=[qkn[r], cs], W=[t3])
        S.op('dve', lambda v: v.tensor_tensor(t4[:], x2, cosb, ALU.mult), R=[qkn[r], cs], W=[t4])
        yield
        S.op('dve', lambda v: v.tensor_tensor(qkr[r][:, :, 64:80], t1[:], t2[:], ALU.subtract), R=[t1, t2], W=[qkr[r]])
        S.op('pool', lambda g: g.tensor_tensor(qkr[r][:, :, 80:96], t3[:], t4[:], ALU.add), R=[t3, t4], W=[qkr[r]])
        yield
        for sl in range(4):
            S.op('pe', lambda p: p.transpose(qkTv[0:96, sl, :], qkr[r][:, sl, :], ident_b[:]), R=[qkr[r], ident_b], W=[p_qkT])
        yield
        S.op('act', lambda a: a.activation(QT[i % 2][:, :, s * 128:(s + 1) * 128], qkTv[0:96, 0:2, :], AF.Copy), R=[p_qkT], W=[QT[i % 2]])
        S.op('act', lambda a: a.activation(KT[:, blk, :, :], qkTv[0:96, 2:4, :], AF.Copy), R=[p_qkT], W=[KT_t[i]])

    cnt = [0]

    def attention(i, fillers):
        nblk = 4 * i + 4
        qt = QT[i % 2]
        yt = yts[i % 2]
        items = [(h, kb) for h in range(2) for kb in range(nblk)]
        NST = 60
        done_st = [0]

        def advance(idx):
            if fillers is None:
                return
            want = min(NST, ((idx + 1) * NST + len(items) - 1) // len(items))
            while done_st[0] < want:
                try:
                    next(fillers)
                except StopIteration:
                    done_st[0] = NST
                    return
                done_st[0] += 1

        def qk(idx):
            h, kb = items[idx]
            d = kb - 4 * i
            q0 = max(d, 0) * 128
            n = cnt[0] + idx
            sT = p_s[n % 3]
            S.op('pe', lambda p: p.matmul(sT[:, q0:512], KT[:, kb, h, :], qt[:, h, q0:512], start=True, stop=True), R=[KT_t[kb // 4], qt], W=[sT])

        qk(0)
        if len(items) > 1:
            qk(1)
        for idx, (h, kb) in enumerate(items):
            d = kb - 4 * i
            q0 = max(d, 0) * 128
            n = cnt[0] + idx
            sT = p_s[n % 3]
            pt = pts[n % 3]
            po = p_o[h]
            if idx + 2 < len(items):
                qk(idx + 2)
            S.op('act', lambda a: a.activation(pt[:, q0:512], sT[:, q0:512], AF.Exp), R=[sT], W=[pt])
            if d >= 0:
                S.op('pool', lambda g: g.tensor_tensor(pt[:, q0:q0 + 128], pt[:, q0:q0 + 128], tri_b[:], ALU.mult), R=[pt, tri_b], W=[pt])
            S.op('pe', lambda p: p.matmul(po[0:65, q0:512], VA[:, kb, h, :], pt[:, q0:512], start=(kb == 0), stop=(kb == nblk - 1)), R=[VA_t[kb // 4], pt], W=[po])
            advance(idx)
            if kb == nblk - 1:
                osb = o_sb[h]
                S.op('act', lambda a: a.activation(osb[:], po[0:65, :], AF.Copy), R=[po], W=[osb])
                S.op('pe', lambda p: p.matmul(p_den[0:64, :], E65[:], osb[:], start=True, stop=True), R=[E65, osb], W=[p_den])
                S.op('dve', lambda v: v.reciprocal(rden[h][:], p_den[0:64, :]), R=[p_den], W=[rden[h]])
                S.op('dve', lambda v: v.tensor_tensor(yt[h * 64:(h + 1) * 64, :], osb[0:64, :], rden[h][:], ALU.mult), R=[osb, rden[h]], W=[yt])
        cnt[0] += len(items)
        if fillers is not None:
            for _ in fillers:
                pass
        S.dma('sp', io['yaT'][:, i * 512:(i + 1) * 512], yt[:], R=[yt])

    import os
    lvl = int(os.environ.get("DBG_LVL", "9"))
    load_u(0)
    if n_tiles > 1:
        load_u(1)
    def proj_gen(i):
        for s_ in range(4):
            yield from proj_sub(i, s_)

    if lvl >= 1:
        for _ in proj_gen(0):
            pass
    if lvl < 2:
        n_tiles = 0
        S.dma('pool', io['yaT'][:, 0:512], uts[0][:, 0, :], R=[uts[0]])
    for i in range(n_tiles):
        fillers = proj_gen(i + 1) if i + 1 < n_tiles else None
        attention(i, fillers)
        if i + 2 < n_tiles:
            load_u(i + 2)
    S.end_phase()


def host_consts():
    c = {}
    c['c_ident_b'] = np.eye(128, dtype=np.float32).astype(NPBF)
    c['c_ident_f'] = np.eye(128, dtype=np.float32)
    p = np.arange(128)[:, None]
    f = np.arange(128)[None, :]
    c['c_tri_b'] = (p <= f).astype(np.float32).astype(NPBF)
    e = np.zeros((65, 64), np.float32)
    e[64, :] = 1.0
    c['c_E65'] = e
    half = 16
    inv = (np.float32(10000.0) ** (-np.arange(half, dtype=np.float32) / np.float32(half))).astype(np.float32)
    pos = np.arange(S_LEN, dtype=np.float32)
    ang = (pos[:, None] * inv[None, :]).astype(np.float32)
    tab = np.concatenate([np.cos(ang), np.sin(ang)], axis=1).astype(np.float32)
    c['c_rope'] = np.ascontiguousarray(tab.reshape(128, 128, 32).transpose(1, 0, 2).reshape(128, 128 * 32))
    return c


B_CONST_KEYS = ['c_ident_b', 'c_ident_f', 'c_tri_b', 'c_E65', 'c_rope']


def prep_B_weights(inp, l, j):
    w_in = inp['w_in'][l]
    hq = slice(416 + j * 64, 416 + (j + 1) * 64)
    hk = slice(416 + 256 + j * 64, 416 + 256 + (j + 1) * 64)
    hv = slice(928 + j * 128, 928 + (j + 1) * 128)
    ho = slice(1440 + j * 128, 1440 + (j + 1) * 128)
    hi = slice(1952 + j, 1953 + j)
    hf = slice(1956 + j, 1957 + j)
    dq = slice(1960 + j * 128, 1960 + (j + 1) * 128)
    dk = slice(2472 + j * 128, 2472 + (j + 1) * 128)
    dv = slice(2984 + j * 128, 2984 + (j + 1) * 128)
    WB = np.concatenate([w_in[:, 0:416], w_in[:, hq], w_in[:, hk], w_in[:, hv], w_in[:, ho], w_in[:, hi], w_in[:, hf],
                         w_in[:, dq], w_in[:, dk], w_in[:, dv]], axis=1)
    d = {'WB': np.ascontiguousarray(WB)}
    d['wuq'] = np.ascontiguousarray(inp['mla_w_uq'][l][:, j * 192:(j + 1) * 192])
    d['wukv'] = np.ascontiguousarray(inp['mla_w_ukv'][l][:, j * 256:(j + 1) * 256])
    qn = inp['mla_q_norm'][l].reshape(2, 128).T
    kvn = inp['mla_kv_norm'][l].reshape(1, 128).T
    d['mla_ncol'] = np.ascontiguousarray(np.concatenate([qn, kvn], axis=1))
    qg = inp['mla_q_gain'][l]
    kg = inp['mla_k_gain'][l]
    d['mla_grow'] = np.ascontiguousarray(np.concatenate([qg, qg, kg, kg])[None, :])
    return d


DIL_R = (1, 4, 16)


def sst_(c0, r, n=128):
    return slice(c0, c0 + (n - 1) * r + 1, r)


def emit_dil(S, io, c, n_sb=8):
    S.begin_phase()
    uT = io['uT']
    ident_b, E65 = c['ident_b'], c['E65']
    C0 = 802
    wd_f = S.sb([128, 8, 384], F32, "wd_f")
    S.dma('sp', wd_f[:], io['WB'][:, C0:C0 + 384].rearrange("(k p) n -> p k n", p=128), W=[wd_f])
    wd = S.sb([128, 8, 384], BF16, "wd")
    S.op('pool', lambda g: g.tensor_copy(wd[:], wd_f[:]), R=[wd_f], W=[wd])
    gcol = S.sb([128, 2], F32, "gcol")
    S.dma('sp', gcol[:], io['dil_gcol'], W=[gcol])
    S.op('dve', lambda v: v.tensor_scalar(gcol[:, 0:1], gcol[:, 0:1], 64 ** -0.5, None, ALU.mult), R=[gcol], W=[gcol])
    Bd = S.sb([128, 128], F32, "Bd")
    S.dma('sp', Bd[:], io['c_Bd'], W=[Bd])
    M4 = S.sb([128, 512], BF16, "M4")
    S.dma('sp', M4[:], io['c_M4'], W=[M4])
    uts = [S.sb([128, 8, 512], BF16, "dut%d" % i) for i in range(2)]
    KTd = [S.sb([128, 2048], BF16, "KTd%d" % i) for i in range(2)]
    QTd = [S.sb([128, 2048], BF16, "QTd%d" % i) for i in range(2)]
    VTd = [S.sb([128, 2048], BF16, "VTd%d" % i) for i in range(2)]
    Vr = [[S.sb([128, 16, 2, 65], BF16, "Vr%d_%d" % (p, ri)) for ri in range(3)] for p in range(2)]
    for p in range(2):
        for ri in range(3):
            S.op('pool', lambda g: g.memset(Vr[p][ri][:, :, :, 64:65], 1.0), W=[Vr[p][ri]])
    P0 = bank(S, "dP0")
    P1 = bank(S, "dP1")
    p_tr = bank(S, "dp_tr", BF16)
    p_sc = bank(S, "dp_sc")
    p_acc = [bank(S, "dp_acc%d" % i) for i in range(4)]
    trv = p_tr[:].rearrange("p (a b) -> p a b", b=128)
    raw = [S.sb([128, 512], F32, "draw%d" % i) for i in range(2)]
    sqt = [S.sb([128, 512], F32, "dsq%d" % i) for i in range(2)]
    lnt = [S.sb([128, 512], F32, "dln%d" % i) for i in range(2)]
    rst = [S.sb([128, 512], F32, "drs%d" % i) for i in range(2)]
    pts = [S.sb([128, 512], BF16, "dpt%d" % i) for i in range(3)]
    o_sb = [S.sb([65, 512], F32, "do_sb%d" % i) for i in range(2)]
    rden = [S.sb([64, 512], F32, "drden%d" % i) for i in range(2)]
    yts = [S.sb([128, 2048], BF16, "dyt%d" % i) for i in range(2)]
    cnt = [0, 0]

    def load_u(t):
        S.dma('sp', uts[t % 2][:], uT[:, :, t * 512:(t + 1) * 512].rearrange("k p n -> p k n"), W=[uts[t % 2]])

    def proj_tile(sb, tt):
        t = sb * 4 + tt
        ut = uts[t % 2]
        par = sb % 2
        cols = slice(tt * 512, (tt + 1) * 512)
        for which in range(3):
            for k in range(8):
                S.op('pe', lambda p: p.matmul(P0[:, :], wd[:, k, which * 128:(which + 1) * 128], ut[:, k, :], start=(k == 0), stop=(k == 7)), R=[wd, ut], W=[P0])
            if which == 2:
                S.op('act', lambda a: a.activation(VTd[par][:, cols], P0[:, :], AF.Copy), R=[P0], W=[VTd[par]])
                continue
            x = cnt[1] % 2
            cnt[1] += 1
            S.op('act', lambda a: a.activation(raw[x][:], P0[:, :], AF.Copy), R=[P0], W=[raw[x]])
            S.op('act', lambda a: a.activation(sqt[x][:], P0[:, :], AF.Square), R=[P0], W=[sqt[x]])
            S.op('pe', lambda p: p.matmul(P1[:, :], Bd[:], sqt[x][:], start=True, stop=True), R=[Bd, sqt[x]], W=[P1])
            S.op('act', lambda a: a.activation(lnt[x][:], P1[:, :], AF.Ln, scale=1.0 / 64, bias=S.eps_col[:, 0:1]), R=[P1, S.eps_t], W=[lnt[x]])
            S.op('act', lambda a: a.activation(rst[x][:], lnt[x][:], AF.Exp, scale=-0.5), R=[lnt[x]], W=[rst[x]])
            dst = QTd[par] if which == 0 else KTd[par]
            S.op('dve', lambda v: v.scalar_tensor_tensor(dst[:, cols], raw[x][:], gcol[:, which:which + 1], rst[x][:], ALU.mult, ALU.mult), R=[raw[x], gcol, rst[x]], W=[dst])

    def vtrans(sb):
        par = sb % 2
        for ri, r in enumerate(DIL_R):
            for b in range(16):
                n, rho = divmod(b, r)
                c0 = n * 128 * r + rho
                S.op('pe', lambda p: p.transpose(trv[:, b % 4, :], VTd[par][:, sst_(c0, r)], ident_b[:]), R=[VTd[par], ident_b], W=[p_tr])
                if b % 4 == 3:
                    S.op('act', lambda a: a.activation(Vr[par][ri][:, b - 3:b + 1, :, 0:64], trv[:, 0:4, :].rearrange("p a (h d) -> p a h d", h=2), AF.Copy), R=[p_tr], W=[Vr[par][ri]])

    def attention(sb, h):
        par = sb % 2
        hp = slice(h * 64, (h + 1) * 64)
        blocks = []
        for ri, r in enumerate(DIL_R):
            for b in range(16):
                n, rho = divmod(b, r)
                c0 = n * 128 * r + rho
                cur = (par, b, c0)
                if n > 0:
                    prev = (par, b - r, c0 - 128 * r)
                elif sb > 0:
                    nb = 16 // r - 1
                    prev = (1 - par, nb * r + rho, nb * 128 * r + rho)
                else:
                    prev = None
                blocks.append((ri, r, b, c0, cur, prev))
        pairs = [blocks[i:i + 2] for i in range(0, len(blocks), 2)]
        pv_all = []
        for pi, pair in enumerate(pairs):
            for u, (ri, r, b, c0, cur, prev) in enumerate(pair):
                for part, kb in enumerate((cur, prev)):
                    if kb is None:
                        continue
                    base = u * 256 + part * 128
                    lhs = Vr[kb[0]][ri][:, kb[1], h, :]
                    if r == 1:
                        pv_all.append((pi, c0 // 512, slice(c0 % 512, c0 % 512 + 128), lhs, slice(base, base + 128), Vr[kb[0]][ri]))
                    elif r == 4:
                        pv_all.append((pi, c0 // 512, sst_(c0 % 512, 4), lhs, slice(base, base + 128), Vr[kb[0]][ri]))
                    else:
                        for jb in range(4):
                            pv_all.append((pi, jb, sst_(c0, 16, 32), lhs, slice(base + 32 * jb, base + 32 * jb + 32), Vr[kb[0]][ri]))
        first = {}
        last = {}
        for idx, op in enumerate(pv_all):
            first.setdefault(op[1], idx)
            last[op[1]] = idx
        scb = [p_sc, P1]

        def qk_pair(pi):
            psc = scb[pi % 2]
            nmm = 0
            for u, (ri, r, b, c0, cur, prev) in enumerate(pairs[pi]):
                qap = QTd[par][hp, sst_(c0, r)]
                for part, kb in enumerate((cur, prev)):
                    if kb is None:
                        kb = cur
                    kap = KTd[kb[0]][hp, sst_(kb[2], r)]
                    base = u * 256 + part * 128
                    S.op('pe', lambda p: p.matmul(psc[:, base:base + 128], kap, qap, start=(nmm == 0), stop=(nmm == 3)), R=[KTd[kb[0]], QTd[par]], W=[psc])
                    nmm += 1

        idx = 0
        qk_pair(0)
        for pi, pair in enumerate(pairs):
            pt = pts[cnt[0] % 3]
            cnt[0] += 1
            psc = scb[pi % 2]
            if pi + 1 < len(pairs):
                qk_pair(pi + 1)
            S.op('act', lambda a: a.activation(pt[:], psc[:, :], AF.Exp), R=[psc], W=[pt])
            S.op('dve', lambda v: v.tensor_tensor(pt[:], pt[:], M4[:], ALU.mult), R=[pt, M4], W=[pt])
            while idx < len(pv_all) and pv_all[idx][0] == pi:
                _, bk, osl, lhs, psl, vt = pv_all[idx]
                S.op('pe', lambda p: p.matmul(p_acc[bk][0:65, osl], lhs, pt[:, psl], start=(first[bk] == idx), stop=(last[bk] == idx)), R=[vt, pt], W=[p_acc[bk]])
                idx += 1
        yt = yts[sb % 2]
        for jb in range(4):
            osb = o_sb[jb % 2]
            rd = rden[jb % 2]
            S.op('act', lambda a: a.activation(osb[:], p_acc[jb][0:65, :], AF.Copy), R=[p_acc[jb]], W=[osb])
            S.op('pe', lambda p: p.matmul(P1[0:64, :], E65[:], osb[:], start=True, stop=True), R=[E65, osb], W=[P1])
            S.op('dve', lambda v: v.reciprocal(rd[:], P1[0:64, :]), R=[P1], W=[rd])
            S.op('dve', lambda v: v.tensor_tensor(yt[h * 64:(h + 1) * 64, jb * 512:(jb + 1) * 512], osb[0:64, :], rd[:], ALU.mult), R=[osb, rd], W=[yt])

    load_u(0)
    load_u(1)
    for sb in range(n_sb):
        for tt in range(4):
            proj_tile(sb, tt)
            if sb * 4 + tt + 2 < n_sb * 4:
                load_u(sb * 4 + tt + 2)
        vtrans(sb)
        for h in range(2):
            attention(sb, h)
        S.dma('sp', io['ycT'][:, sb * 2048:(sb + 1) * 2048], yts[sb % 2][:], R=[yts[sb % 2]])
    S.end_phase()


def host_consts_dil():
    c = {}
    bd = np.zeros((128, 128), np.float32)
    bd[0:64, 0:64] = 1.0
    bd[64:128, 64:128] = 1.0
    c['c_Bd'] = bd
    p = np.arange(128)[:, None]
    f = np.arange(128)[None, :]
    mc = (p <= f).astype(np.float32)
    mp = (p >= f).astype(np.float32)
    c['c_M4'] = np.concatenate([mc, mp, mc, mp], axis=1).astype(NPBF)
    return c


def prep_dil_vecs(inp, l):
    return {'dil_gcol': np.ascontiguousarray(np.stack([np.tile(inp['dil_q_gain'][l], 2), np.tile(inp['dil_k_gain'][l], 2)], axis=1))}


def emit_mlstm(S, io, c, n_tiles=32):
    S.begin_phase()
    uT = io['uT']
    ident_b, tri_b = c['ident_b'], c['tri_b']
    wm_f = S.sb([128, 8, 386], F32, "wm_f")
    S.dma('sp', wm_f[:], io['WB'][:, 416:802].rearrange("(k p) n -> p k n", p=128), W=[wm_f])
    wm = S.sb([128, 8, 386], BF16, "wm")
    S.op('pool', lambda g: g.tensor_copy(wm[:], wm_f[:]), R=[wm_f], W=[wm])
    cw = S.sb([64, 10], F32, "cw")
    S.dma('sp', cw[:], io['ml_conv'], W=[cw])
    gb = S.sb([1, 2], F32, "gb")
    S.dma('sp', gb[:], io['ml_gate'], W=[gb])
    nbf = S.sb([1, 1], F32, "nbf")
    S.op('dve', lambda v: v.tensor_scalar(nbf[:], gb[0:1, 1:2], -1.0, None, ALU.mult), R=[gb], W=[nbf])
    hg = S.sb([128, 128], F32, "hg")
    S.dma('sp', hg[:], io['ml_hg'].partition_broadcast(128), W=[hg])
    one = S.sb([1, 1], F32, "one")
    S.op('pool', lambda g: g.memset(one[:], 1.0), W=[one])
    ones_r = S.sb([1, 128], F32, "ones_r")
    zeros_r = S.sb([1, 128], F32, "zeros_r")
    S.op('pool', lambda g: g.memset(ones_r[:], 1.0), W=[ones_r])
    S.op('pool', lambda g: g.memset(zeros_r[:], 0.0), W=[zeros_r])
    uts = [S.sb([128, 8, 512], BF16, "mut%d" % i) for i in range(2)]
    xq = [[S.sb([64, 515], F32, "xq%d_%d" % (w, i)) for i in range(2)] for w in range(2)]
    for w in range(2):
        S.op('pool', lambda g: g.memset(xq[w][0][:, 0:3], 0.0), W=[xq[w][0]])
    cv = [S.sb([64, 512], F32, "cv%d" % i) for i in range(2)]
    ex = [S.sb([64, 512], F32, "ex%d" % i) for i in range(2)]
    qkT = [[S.sb([64, 512], BF16, "qkT%d_%d" % (w, i)) for i in range(2)] for w in range(2)]
    Brow = [S.sb([1, 128], F32, "Brow%d" % i) for i in range(2)]
    Grow = [S.sb([1, 128], F32, "Grow%d" % i) for i in range(2)]
    zero1 = S.sb([1, 1], F32, "zero1")
    S.op('pool', lambda g: g.memset(zero1[:], 0.0), W=[zero1])
    t1 = [S.sb([1, 128], F32, "mt1_%d" % i) for i in range(2)]
    t2 = [S.sb([1, 128], F32, "mt2_%d" % i) for i in range(2)]
    arow = [S.sb([1, 128], F32, "arow%d" % i) for i in range(2)]
    bg = [S.sb([1, 128], F32, "bg%d" % i) for i in range(2)]
    ngp = [S.sb([1, 1], F32, "ngp%d" % i) for i in range(2)]
    rows3 = [S.sb([1, 3, 128], F32, "rows3_%d" % i) for i in range(2)]
    cols = [S.sb([128, 4], F32, "cols%d" % i) for i in range(5)]
    ones_c = S.sb([128, 4], F32, "ones_c")
    S.op('pool', lambda g: g.memset(ones_c[:], 1.0), W=[ones_c])
    Vp = [S.sb([128, 129], BF16, "Vp%d" % i) for i in range(2)]
    so = [S.sb([128, 128], F32, "so%d" % i) for i in range(3)]
    ktok = [S.sb([128, 64], BF16, "ktok%d" % i) for i in range(2)]
    scm = [S.sb([128, 128], BF16, "scm%d" % i) for i in range(2)]
    Dst = S.sb([64, 129], F32, "Dst")
    Cb = S.sb([64, 129], BF16, "Cb")
    S.op('pool', lambda g: g.memset(Dst[:], 0.0), W=[Dst])
    sm = [[S.sb([128, 1], F32, "sm%d_%d" % (j, i)) for j in range(6)] for i in range(2)]
    hh = [S.sb([128, 128], F32, "hh%d" % i) for i in range(2)]
    hj = [S.sb([128, 128], F32, "hj%d" % i) for i in range(2)]
    y1 = [S.sb([128, 128], F32, "y1_%d" % i) for i in range(2)]
    y2 = [S.sb([128, 128], BF16, "y2_%d" % i) for i in range(2)]
    ybt = [S.sb([128, 512], BF16, "ybt%d" % i) for i in range(2)]
    P_qk = bank(S, "mP_qk")
    P_vo = [P_qk]
    P_gc = bank(S, "mP_gc")
    P_s = bank(S, "mP_s")
    P_u = bank(S, "mP_u")
    P_h = [bank(S, "mP_h%d" % i) for i in range(2)]
    p_trb = bank(S, "mp_trb", BF16)
    p_trk = p_trb
    p_try = p_trb
    P_gr = bank(S, "mP_gr")

    def load_u(t):
        S.dma('sp', uts[t % 2][:], uT[:, :, t * 512:(t + 1) * 512].rearrange("k p n -> p k n"), W=[uts[t % 2]])

    def qk_tile(t):
        ut = uts[t % 2]
        for w in range(2):
            xb = xq[w][t % 2]
            for k in range(8):
                S.op('pe', lambda p: p.matmul(P_qk[0:64, :], wm[:, k, w * 64:(w + 1) * 64], ut[:, k, :], start=(k == 0), stop=(k == 7)), R=[wm, ut], W=[P_qk])
            if t > 0:
                S.op('pool', lambda g: g.tensor_copy(xb[:, 0:3], xq[w][(t - 1) % 2][:, 512:515]), R=[xq[w][(t - 1) % 2]], W=[xb])
            S.op('act', lambda a: a.activation(xb[:, 3:515], P_qk[0:64, :], AF.Copy), R=[P_qk], W=[xb])
            o = w * 5
            cvt = cv[w]
            S.op('dve', lambda v: v.tensor_scalar(cvt[:], xb[:, 3:515], cw[:, o + 3:o + 4], cw[:, o + 4:o + 5], ALU.mult, ALU.add), R=[xb, cw], W=[cvt])
            for j in (2, 1, 0):
                S.op('dve', lambda v: v.scalar_tensor_tensor(cvt[:], xb[:, j:j + 512], cw[:, o + j:o + j + 1], cvt[:], ALU.mult, ALU.add), R=[xb, cw, cvt], W=[cvt])
            ext = ex[w]
            S.op('act', lambda a: a.activation(ext[:], cvt[:], AF.Exp, scale=-1.0), R=[cvt], W=[ext])
            S.op('dve', lambda v: v.tensor_scalar_add(ext[:], ext[:], 1.0), R=[ext], W=[ext])
            S.op('dve', lambda v: v.reciprocal(ext[:], ext[:]), R=[ext], W=[ext])
            S.op('dve', lambda v: v.scalar_tensor_tensor(qkT[w][t % 2][:], cvt[:], (0.125 if w == 0 else 1.0), ext[:], ALU.mult, ALU.mult), R=[cvt, ext], W=[qkT[w][t % 2]])

    def gates_a(g):
        t, cc = divmod(g, 4)
        ut = uts[t % 2]
        x = g % 2
        csl = slice(cc * 128, (cc + 1) * 128)
        if g % 2 == 0:
            hsl = slice(cc * 128, cc * 128 + 256)
            for w in range(2):
                for k in range(8):
                    S.op('pe', lambda p: p.matmul(P_gr[0:1, w * 256:(w + 1) * 256], wm[:, k, 384 + w:385 + w], ut[:, k, hsl], start=(k == 0), stop=(k == 7)), R=[wm, ut], W=[P_gr])
            yield
        go = (g % 2) * 128
        S.op('act', lambda a: a.activation(t1[x][:], P_gr[0:1, 256 + go:256 + go + 128], AF.Exp, scale=-1.0, bias=nbf[0:1, 0:1]), R=[P_gr, nbf], W=[t1[x]])
        S.op('act', lambda a: a.activation(t2[x][:], t1[x][:], AF.Ln, bias=one[0:1, 0:1]), R=[t1[x], one], W=[t2[x]])
        yield
        bprev = Brow[1 - x][0:1, 127:128] if g > 0 else zero1[0:1, 0:1]
        gprev = Grow[1 - x][0:1, 127:128] if g > 0 else zero1[0:1, 0:1]
        prevB = [Brow[1 - x]] if g > 0 else [zero1]
        prevG = [Grow[1 - x]] if g > 0 else [zero1]
        S.op('dve', lambda v: v.tensor_tensor_scan(Brow[x][:], ones_r[:], t2[x][:], bprev, ALU.mult, ALU.subtract), R=[ones_r, t2[x]] + prevB, W=[Brow[x]])
        S.op('dve', lambda v: v.scalar_tensor_tensor(arow[x][:], P_gr[0:1, go:go + 128], gb[0:1, 0:1], Brow[x][:], ALU.add, ALU.subtract), R=[P_gr, gb, Brow[x]], W=[arow[x]])
        S.op('dve', lambda v: v.tensor_tensor_scan(Grow[x][:], zeros_r[:], arow[x][:], gprev, ALU.add, ALU.max), R=[zeros_r, arow[x]] + prevG, W=[Grow[x]])
        S.op('dve', lambda v: v.tensor_scalar(ngp[x][:], gprev, -1.0, None, ALU.mult), R=prevG, W=[ngp[x]])
        S.op('dve', lambda v: v.tensor_tensor(bg[x][:], Brow[x][:], Grow[x][:], ALU.add), R=[Brow[x], Grow[x]], W=[bg[x]])
        yield
        S.op('act', lambda a: a.activation(rows3[x][0:1, 0, :], arow[x][:], AF.Exp, bias=ngp[x][0:1, 0:1]), R=[arow[x], ngp[x]], W=[rows3[x]])
        S.op('act', lambda a: a.activation(rows3[x][0:1, 1, :], Grow[x][:], AF.Exp, scale=-1.0, bias=gprev), R=[Grow[x]] + prevG, W=[rows3[x]])
        S.op('act', lambda a: a.activation(rows3[x][0:1, 2, :], bg[x][:], AF.Exp, scale=-1.0), R=[bg[x]], W=[rows3[x]])
        yield
        cl = cols[g % 5]
        for j in range(3):
            S.op('pe', lambda p: p.matmul(P_gc[:, 256 + j:257 + j], rows3[x][0:1, j, :], one[0:1, 0:1], start=(j == 0), stop=False), R=[rows3[x], one], W=[P_gc])
        S.op('pe', lambda p: p.matmul(P_gc[:, 259:260], rows3[x][0:1, 1, 127:128].to_broadcast([1, 128]), one[0:1, 0:1], start=False, stop=True), R=[rows3[x], one], W=[P_gc])
        yield
        S.op('act', lambda a: a.activation(cl[:], P_gc[:, 256:260], AF.Copy), R=[P_gc], W=[cl])
        yield

    def stageA(g):
        t, cc = divmod(g, 4)
        if cc == 0:
            qk_tile(t)
            yield
        ut = uts[t % 2]
        x = g % 2
        x3 = g % 3
        csl = slice(cc * 128, (cc + 1) * 128)
        cl = cols[g % 5]
        qT = qkT[0][t % 2]
        kT = qkT[1][t % 2]
        pvo = P_vo[0]
        for k in range(8):
            S.op('pe', lambda p: p.matmul(pvo[:, 0:256], ut[:, k, csl], wm[:, k, 128:384], start=(k == 0), stop=(k == 7)), R=[ut, wm], W=[pvo])
        S.op('pe', lambda p: p.transpose(p_trb[:, 0:64], kT[:, csl], ident_b[0:64, 0:64]), R=[kT, ident_b], W=[p_trb])
        S.op('pe', lambda p: p.matmul(P_s[:, 0:128], kT[:, csl], qT[:, csl], start=True, stop=True), R=[kT, qT], W=[P_s])
        yield
        S.op('dve', lambda v: v.tensor_scalar(Vp[x][:, 0:128], pvo[:, 0:128], cl[:, 0:1], None, ALU.mult), R=[pvo, cl], W=[Vp[x]])
        S.op('act', lambda a: a.activation(Vp[x][:, 128:129], cl[:, 0:1], AF.Copy), R=[cl], W=[Vp[x]])
        S.op('act', lambda a: a.activation(so[x3][:], pvo[:, 128:256], AF.Exp, scale=-1.0), R=[pvo], W=[so[x3]])
        S.op('act', lambda a: a.activation(ktok[x][:], p_trb[:, 0:64], AF.Copy), R=[p_trb], W=[ktok[x]])
        S.op('dve', lambda v: v.tensor_tensor(scm[x][:], P_s[:, 0:128], tri_b[:], ALU.mult), R=[P_s, tri_b], W=[scm[x]])
        yield
        S.op('dve', lambda v: v.tensor_scalar_add(so[x3][:], so[x3][:], 1.0), R=[so[x3]], W=[so[x3]])
        S.op('dve', lambda v: v.reciprocal(so[x3][:], so[x3][:]), R=[so[x3]], W=[so[x3]])
        yield

    def stageB(g):
        t, cc = divmod(g, 4)
        x = g % 2
        csl = slice(cc * 128, (cc + 1) * 128)
        clp = cols[(g - 1) % 5] if g > 0 else ones_c
        qT = qkT[0][t % 2]
        ph = P_h[x]
        S.op('dve', lambda v: v.tensor_scalar(Cb[:], Dst[:], clp[0:64, 3:4], None, ALU.mult), R=[Dst, clp], W=[Cb])
        S.op('pe', lambda p: p.matmul(P_u[0:64, 0:129], ktok[x][:], Vp[x][:], start=True, stop=True), R=[ktok[x], Vp[x]], W=[P_u])
        yield
        S.op('pe', lambda p: p.matmul(ph[:, 0:129], qT[:, csl], Cb[:], start=True, stop=False), R=[qT, Cb], W=[ph])
        S.op('pe', lambda p: p.matmul(ph[:, 0:129], scm[x][:], Vp[x][:], start=False, stop=True), R=[scm[x], Vp[x]], W=[ph])
        S.op('dve', lambda v: v.scalar_tensor_tensor(Dst[:], Dst[:], clp[0:64, 3:4], P_u[0:64, 0:129], ALU.mult, ALU.add), R=[Dst, clp, P_u], W=[Dst])
        yield

    def stageC(g):
        t, cc = divmod(g, 4)
        x = g % 2
        x3 = g % 3
        csl = slice(cc * 128, (cc + 1) * 128)
        cl = cols[g % 5]
        ph = P_h[x]
        ta, tb, rr, r2, ssq, rs = sm[x]
        S.op('act', lambda a: a.activation(ta[:], ph[:, 128:129], AF.Abs), R=[ph], W=[ta])
        yield
        S.op('dve', lambda v: v.scalar_tensor_tensor(tb[:], ta[:], cl[:, 1:2], cl[:, 2:3], ALU.mult, ALU.max), R=[ta, cl], W=[tb])
        S.op('dve', lambda v: v.reciprocal(rr[:], tb[:]), R=[tb], W=[rr])
        S.op('dve', lambda v: v.tensor_tensor(r2[:], rr[:], cl[:, 1:2], ALU.mult), R=[rr, cl], W=[r2])
        S.op('dve', lambda v: v.tensor_scalar(hh[x][:], ph[:, 0:128], r2[:, 0:1], None, ALU.mult), R=[ph, r2], W=[hh[x]])
        yield
        S.op('act', lambda a: a.activation(hj[x][:], hh[x][:], AF.Square, accum_out=ssq[:, 0:1]), R=[hh[x]], W=[hj[x], ssq])
        S.op('act', lambda a: a.activation(ssq[:], ssq[:], AF.Ln, scale=1.0 / 128, bias=S.eps_col[:, 0:1]), R=[ssq, S.eps_t], W=[ssq])
        S.op('act', lambda a: a.activation(rs[:], ssq[:], AF.Exp, scale=-0.5), R=[ssq], W=[rs])
        yield
        S.op('dve', lambda v: v.scalar_tensor_tensor(y1[x][:], hh[x][:], rs[:, 0:1], hg[:], ALU.mult, ALU.mult), R=[hh[x], rs, hg], W=[y1[x]])
        S.op('dve', lambda v: v.tensor_tensor(y2[x][:], y1[x][:], so[x3][:], ALU.mult), R=[y1[x], so[x3]], W=[y2[x]])
        yield
        S.op('pe', lambda p: p.transpose(p_trb[:, 512:640], y2[x][:], ident_b[:]), R=[y2[x], ident_b], W=[p_trb])
        yield
        S.op('act', lambda a: a.activation(ybt[t % 2][:, csl], p_trb[:, 512:640], AF.Copy), R=[p_trb], W=[ybt[t % 2]])
        if cc == 3:
            S.dma('sp', io['ybT'][:, t * 512:(t + 1) * 512], ybt[t % 2][:], R=[ybt[t % 2]])
        yield

    n_ch = n_tiles * 4
    load_u(0)
    if n_tiles > 1:
        load_u(1)
    for g0 in range(min(2, n_ch)):
        for _ in gates_a(g0):
            pass
    for _ in stageA(0):
        pass
    for it in range(n_ch + 1):
        gens = []
        if it + 2 < n_ch:
            gens.append(gates_a(it + 2))
        if it + 1 < n_ch:
            gens.append(stageA(it + 1))
        if it < n_ch:
            gens.append(stageB(it))
        if it >= 1:
            gens.append(stageC(it - 1))
        while gens:
            for gnr in list(gens):
                try:
                    next(gnr)
                except StopIteration:
                    gens.remove(gnr)
        t, cc = divmod(it, 4)
        if it < n_ch and cc == 3 and t + 2 < n_tiles:
            load_u(t + 2)
    S.end_phase()


def prep_mlstm_vecs(inp, l, j):
    cwq = inp['mlstm_conv_w'][l][:, j * 64:(j + 1) * 64].T
    cbq = inp['mlstm_conv_b'][l][j * 64:(j + 1) * 64][:, None]
    cwk = inp['mlstm_conv_w'][l][:, 256 + j * 64:256 + (j + 1) * 64].T
    cbk = inp['mlstm_conv_b'][l][256 + j * 64:256 + (j + 1) * 64][:, None]
    d = {'ml_conv': np.ascontiguousarray(np.concatenate([cwq, cbq, cwk, cbk], axis=1))}
    d['ml_gate'] = np.ascontiguousarray(np.array([[inp['mlstm_b_i'][l][j], inp['mlstm_b_f'][l][j]]], np.float32))
    d['ml_hg'] = np.ascontiguousarray(inp['mlstm_head_gain'][l][j * 128:(j + 1) * 128][None, :])
    return d


def emit_mod(S, io):
    S.begin_phase()
    cc = S.sb([128, 8], F32, "cc")
    S.dma('sp', cc[:], io['c_col'], W=[cc])
    ca = S.sb([128, 8], F32, "ca")
    S.op('act', lambda a: a.activation(ca[:], cc[:], AF.Silu), R=[cc], W=[ca])
    brow = S.sb([1, 6144], F32, "brow")
    S.dma('sp', brow[:], io['b_ada'], W=[brow])
    orow = S.sb([1, 6144], F32, "orow")
    wt = [S.sb([128, 8, 512], F32, "wada%d" % i) for i in range(2)]
    pm = [bank(S, "pm%d" % i) for i in range(2)]
    for gi in range(12):
        w = wt[gi % 2]
        S.dma('sp', w[:], io['w_ada'][:, gi * 512:(gi + 1) * 512].rearrange("(k p) n -> p k n", p=128), W=[w])
        p = pm[gi % 2]
        for k in range(8):
            S.op('pe', lambda pe: pe.matmul(p[0:1, :], ca[:, k:k + 1], w[:, k, :], start=(k == 0), stop=(k == 7)), R=[ca, w], W=[p])
        S.op('dve', lambda v: v.tensor_tensor(orow[0:1, gi * 512:(gi + 1) * 512], p[0:1, :], brow[0:1, gi * 512:(gi + 1) * 512], ALU.add), R=[p, brow], W=[orow])
    S.dma('sp', io['mod_out'], orow[:], R=[orow])
    S.end_phase()


def norm_mod(S, xt, Acol, Bcol, outs, sq, ones_f, pss, lnt, rst, tmp, R_extra, do_sq=True):
    n = xt.ap.shape[2]
    if do_sq:
        S.op('act', lambda a: a.activation(sq[:], xt[:], AF.Square), R=[xt], W=[sq])
    for k in range(8):
        S.op('pe', lambda p: p.matmul(pss[:, 0:n], ones_f[:], sq[:, k, :], start=(k == 0), stop=(k == 7)), R=[ones_f, sq], W=[pss])
    S.op('act', lambda a: a.activation(lnt[:], pss[:, 0:n], AF.Ln, scale=1.0 / D, bias=S.eps_col[:, 0:1]), R=[pss, S.eps_t], W=[lnt])
    S.op('act', lambda a: a.activation(rst[:], lnt[:], AF.Exp, scale=-0.5), R=[lnt], W=[rst])
    for k in range(8):
        t = tmp[k % len(tmp)]
        S.op('dve', lambda v: v.scalar_tensor_tensor(t[:], xt[:, k, :], Acol[:, k:k + 1], rst[:], ALU.mult, ALU.mult), R=[xt, rst] + R_extra, W=[t])
        for o in outs:
            S.op('act', lambda a: a.activation(o[:, k, :], t[:], AF.Identity, bias=Bcol[:, k:k + 1]), R=[t] + R_extra, W=[o])


def scol(S, name, ap_dram, shape):
    t = S.sb(list(shape), F32, name)
    S.dma('sp', t[:], ap_dram, W=[t])
    return t


def emit_C1a(S, io, c, n_tiles=8):
    S.begin_phase()
    NTL = 512
    Wg = S.sb([128, 8, 3072], BF16, "Wg")
    Wbr = [S.sb([128, 4, 1024], BF16, "Wbr%d" % i) for i in range(3)]
    stg = [S.sb([128, 8, 512], F32, "stgA%d" % i) for i in range(2)]
    for gi in range(6):
        st = stg[gi % 2]
        S.dma('sp', st[:], io['w_gate'][:, gi * 512:(gi + 1) * 512].rearrange("(k p) n -> p k n", p=128), W=[st])
        S.op('pool', lambda g: g.tensor_copy(Wg[:, :, gi * 512:(gi + 1) * 512], st[:]), R=[st], W=[Wg])
    for br in range(3):
        for hh_ in range(2):
            st = stg[(br * 2 + hh_) % 2]
            stv = st[:].rearrange("p (a k) n -> p a k n", a=2)[:, 0, :, :]
            S.dma('sp', stv, io['w_br'][br, :, hh_ * 512:(hh_ + 1) * 512].rearrange("(k p) n -> p k n", p=128), W=[st])
            S.op('pool', lambda g: g.tensor_copy(Wbr[br][:, :, hh_ * 512:(hh_ + 1) * 512], stv), R=[st], W=[Wbr[br]])
    uts = [S.sb([128, 8, NTL], BF16, "cut%d" % i) for i in range(2)]
    yts = [[S.sb([128, 4, NTL], BF16, "cyt%d_%d" % (br, i)) for i in range(2)] for br in range(3)]
    mgs = [S.sb([128, 8, NTL], BF16, "mg%d" % i) for i in range(2)]
    eg = [S.sb([128, NTL], F32, "eg%d" % i) for i in range(3)]
    mm = [S.sb([128, NTL], F32, "mm%d" % i) for i in range(2)]
    tt = [S.sb([128, NTL], F32, "tt%d" % i) for i in range(2)]
    PG = [bank(S, "PG%d" % i) for i in range(3)]
    PB = [bank(S, "PB%d" % i) for i in range(3)]

    def load(t):
        sl = slice(t * NTL, (t + 1) * NTL)
        S.dma('sp', uts[t % 2][:], io['uT_loc'][:, :, sl].rearrange("k p n -> p k n"), W=[uts[t % 2]])
        for br in range(3):
            S.dma('sp', yts[br][t % 2][:], io['yT'][br, :, :, sl].rearrange("k p n -> p k n"), W=[yts[br][t % 2]])

    load(0)
    for t in range(n_tiles):
        if t + 1 < n_tiles:
            load(t + 1)
        ut = uts[t % 2]
        mg = mgs[t % 2]
        for dc in range(8):
            for br in range(3):
                for k in range(8):
                    S.op('pe', lambda p: p.matmul(PG[br][:, :], Wg[:, k, br * 1024 + dc * 128:br * 1024 + (dc + 1) * 128], ut[:, k, :], start=(k == 0), stop=(k == 7)), R=[Wg, ut], W=[PG[br]])
                S.op('act', lambda a: a.activation(eg[br][:], PG[br][:, :], AF.Sigmoid), R=[PG[br]], W=[eg[br]])
            for br in range(3):
                yt = yts[br][t % 2]
                for k in range(4):
                    S.op('pe', lambda p: p.matmul(PB[br][:, :], Wbr[br][:, k, dc * 128:(dc + 1) * 128], yt[:, k, :], start=(k == 0), stop=(k == 3)), R=[Wbr[br], yt], W=[PB[br]])
            m = mm[dc % 2]
            S.op('dve', lambda v: v.tensor_tensor(m[:], PB[0][:, :], eg[0][:], ALU.mult), R=[PB[0], eg[0]], W=[m])
            for br in (1, 2):
                t1 = tt[br % 2]
                S.op('dve', lambda v: v.tensor_tensor(t1[:], PB[br][:, :], eg[br][:], ALU.mult), R=[PB[br], eg[br]], W=[t1])
                if br == 1:
                    S.op('pool', lambda g: g.tensor_tensor(m[:], m[:], t1[:], ALU.add), R=[m, t1], W=[m])
                else:
                    S.op('pool', lambda g: g.tensor_tensor(mg[:, dc, :], m[:], t1[:], ALU.add), R=[m, t1], W=[mg])
        S.dma('sp', io['mgT'][:, :, t * NTL:(t + 1) * NTL].rearrange("k p n -> p k n"), mg[:], R=[mg])
    S.end_phase()


def emit_C1b(S, io, c, n_tiles=8):
    S.begin_phase()
    NTL = 512
    ones_f, ident_f = c['ones_f'], c['ident_f']
    Wo = S.sb([128, 8, 1024], BF16, "Wo")
    stg = [S.sb([128, 8, 512], F32, "stgB%d" % i) for i in range(2)]
    for gi in range(2):
        st = stg[gi]
        S.dma('sp', st[:], io['w_out'][:, gi * 512:(gi + 1) * 512].rearrange("(k p) n -> p k n", p=128), W=[st])
        S.op('pool', lambda g: g.tensor_copy(Wo[:, :, gi * 512:(gi + 1) * 512], st[:]), R=[st], W=[Wo])
    Wr = S.sb([128, 8, 20], F32, "Wr")
    S.dma('sp', Wr[:], io['w_router'].rearrange("(k p) n -> p k n", p=128), W=[Wr])
    rb = S.sb([128, 20], F32, "rb")
    S.dma('sp', rb[:], io['b_router'].partition_broadcast(128), W=[rb])
    modc = scol(S, "modc", io['modc'], [128, 48])
    fng = scol(S, "fng", io['ffn_norm_col'], [128, 8])
    Af = S.sb([128, 8], F32, "Af")
    S.op('dve', lambda v: v.scalar_tensor_tensor(Af[:], modc[:, 32:40], 1.0, fng[:], ALU.add, ALU.mult), R=[modc, fng], W=[Af])
    xts = [S.sb([128, 8, NTL], F32, "xt%d" % i) for i in range(2)]
    mgs = [S.sb([128, 8, NTL], BF16, "bmg%d" % i) for i in range(2)]
    sq = S.sb([128, 8, NTL], F32, "bsq")
    hf32s = [S.sb([128, 8, NTL], F32, "hf32_%d" % i) for i in range(2)]
    hfb = [S.sb([128, 8, NTL], BF16, "hfb%d" % i) for i in range(2)]
    lnt = S.sb([128, NTL], F32, "blnt")
    rst = S.sb([128, NTL], F32, "brst")
    tmp = [S.sb([128, NTL], F32, "btmp%d" % i) for i in range(2)]
    cwT = [S.sb([16, NTL], F32, "cwT%d" % i) for i in range(2)]
    PO = [bank(S, "PO%d" % i) for i in range(2)]
    PSS = bank(S, "PSS")
    PR = bank(S, "PR")
    PT = bank(S, "PT")
    def rt(nm, shp):
        return [S.sb(shp, F32, "%s%d" % (nm, i)) for i in range(2)]
    rb4 = S.sb([128, 4, 20], F32, "rb4")
    for s_ in range(4):
        S.op('pool', lambda g: g.tensor_copy(rb4[:, s_, :], rb[:]), R=[rb], W=[rb4])
    lgb = rt("lgb", [128, 4, 20]); gmax = rt("gmax", [128, 4]); oh = rt("oh", [128, 4, 4]); gs = rt("gs", [128, 4, 4])
    sume = rt("sume", [128, 4]); psel = rt("psel", [128, 4]); m1 = rt("m1", [128, 4, 4])
    is1 = rt("is1", [128, 4, 4, 4]); E2 = rt("E2", [128, 4, 4, 4]); m2 = rt("m2", [128, 4, 4]); sel = rt("sel", [128, 4, 4, 4])
    exx = rt("exx", [128, 4, 4, 4]); den = rt("den", [128, 4, 4]); fac = rt("fac", [128, 4, 4]); cw = rt("cw", [128, 4, 4, 4])

    def load(t):
        sl = slice(t * NTL, (t + 1) * NTL)
        S.dma('sp', xts[t % 2][:], io['xT'][:, :, sl].rearrange("k p n -> p k n"), W=[xts[t % 2]])
        S.dma('sp', mgs[t % 2][:], io['mgT'][:, :, sl].rearrange("k p n -> p k n"), W=[mgs[t % 2]])

    def bc4(ap):
        return ap.unsqueeze(2).to_broadcast([128, 4, 4])

    def stage1(t):
        xt = xts[t % 2]
        mg = mgs[t % 2]
        sl = slice(t * NTL, (t + 1) * NTL)
        for dc in range(8):
            po = PO[dc % 2]
            for k in range(8):
                S.op('pe', lambda p: p.matmul(po[:, :], Wo[:, k, dc * 128:(dc + 1) * 128], mg[:, k, :], start=(k == 0), stop=(k == 7)), R=[Wo, mg], W=[po])
            S.op('dve', lambda v: v.scalar_tensor_tensor(xt[:, dc, :], po[:, :], modc[:, 16 + dc:17 + dc], xt[:, dc, :], ALU.mult, ALU.add), R=[po, modc, xt], W=[xt])
        S.dma('sp', io['x1T'][:, :, sl].rearrange("k p n -> p k n"), xt[:], R=[xt])

    def stage2(t):
        xt = xts[t % 2]
        sl = slice(t * NTL, (t + 1) * NTL)
        hb = hfb[t % 2]
        hf32 = hf32s[t % 2]
        norm_mod(S, xt, Af[:], modc[:, 24:32], [hb, hf32], sq, ones_f, PSS, lnt, rst, tmp, [Af, modc])
        S.dma('sp', io['hfT'][:, :, sl].rearrange("k p n -> p k n"), hb[:], R=[hb])

    def stage2b(t):
        sl = slice(t * NTL, (t + 1) * NTL)
        hf32 = hf32s[t % 2]
        ct = cwT[t % 2]
        x = t % 2
        for s_ in range(4):
            for k in range(8):
                S.op('pe', lambda p: p.matmul(PR[:, s_ * 20:(s_ + 1) * 20], hf32[:, k, s_ * 128:(s_ + 1) * 128], Wr[:, k, :], start=(k == 0), stop=(k == 7)), R=[hf32, Wr], W=[PR])
        S.op('dve', lambda v: v.tensor_tensor(lgb[x][:], PR[:, 0:80].rearrange("p (s n) -> p s n", s=4), rb4[:], ALU.add), R=[PR, rb4], W=[lgb[x]])
        G = lgb[x][:, :, 0:4]
        E = lgb[x][:, :, 4:20].rearrange("p s (g e) -> p s g e", g=4)

        def b3(ap):
            return ap.unsqueeze(2).to_broadcast([128, 4, 4])

        def b4(ap):
            return ap.unsqueeze(3).to_broadcast([128, 4, 4, 4])

        S.op('dve', lambda v: v.tensor_reduce(gmax[x][:], G, AX.X, ALU.max), R=[lgb[x]], W=[gmax[x]])
        S.op('dve', lambda v: v.tensor_tensor(oh[x][:], G, b3(gmax[x][:]), ALU.is_equal), R=[lgb[x], gmax[x]], W=[oh[x]])
        S.op('dve', lambda v: v.tensor_tensor(gs[x][:], G, b3(gmax[x][:]), ALU.subtract), R=[lgb[x], gmax[x]], W=[gs[x]])
        S.op('act', lambda a: a.activation(gs[x][:], gs[x][:], AF.Exp), R=[gs[x]], W=[gs[x]])
        S.op('dve', lambda v: v.tensor_reduce(sume[x][:], gs[x][:], AX.X, ALU.add), R=[gs[x]], W=[sume[x]])
        S.op('dve', lambda v: v.reciprocal(psel[x][:], sume[x][:]), R=[sume[x]], W=[psel[x]])
        S.op('dve', lambda v: v.tensor_reduce(m1[x][:], E, AX.X, ALU.max), R=[lgb[x]], W=[m1[x]])
        S.op('dve', lambda v: v.tensor_tensor(is1[x][:], E, b4(m1[x][:]), ALU.is_equal), R=[lgb[x], m1[x]], W=[is1[x]])
        S.op('dve', lambda v: v.scalar_tensor_tensor(E2[x][:], is1[x][:], -1e30, E, ALU.mult, ALU.add), R=[is1[x], lgb[x]], W=[E2[x]])
        S.op('dve', lambda v: v.tensor_reduce(m2[x][:], E2[x][:], AX.X, ALU.max), R=[E2[x]], W=[m2[x]])
        S.op('dve', lambda v: v.tensor_tensor(sel[x][:], E, b4(m2[x][:]), ALU.is_ge), R=[lgb[x], m2[x]], W=[sel[x]])
        S.op('dve', lambda v: v.tensor_tensor(exx[x][:], E, b4(m1[x][:]), ALU.subtract), R=[lgb[x], m1[x]], W=[exx[x]])
        S.op('act', lambda a: a.activation(exx[x][:], exx[x][:], AF.Exp), R=[exx[x]], W=[exx[x]])
        S.op('dve', lambda v: v.tensor_tensor(exx[x][:], exx[x][:], sel[x][:], ALU.mult), R=[exx[x], sel[x]], W=[exx[x]])
        S.op('dve', lambda v: v.tensor_reduce(den[x][:], exx[x][:], AX.X, ALU.add), R=[exx[x]], W=[den[x]])
        S.op('dve', lambda v: v.reciprocal(den[x][:], den[x][:]), R=[den[x]], W=[den[x]])
        S.op('dve', lambda v: v.tensor_tensor(fac[x][:], den[x][:], oh[x][:], ALU.mult), R=[den[x], oh[x]], W=[fac[x]])
        S.op('dve', lambda v: v.tensor_tensor(fac[x][:], fac[x][:], b3(psel[x][:]), ALU.mult), R=[fac[x], psel[x]], W=[fac[x]])
        S.op('dve', lambda v: v.tensor_tensor(cw[x][:], exx[x][:], b4(fac[x][:]), ALU.mult), R=[exx[x], fac[x]], W=[cw[x]])
        for s_ in range(4):
            S.op('pe', lambda p: p.transpose(PT[0:16, s_ * 128:(s_ + 1) * 128], cw[x][:, s_, :, :].rearrange("p g e -> p (g e)"), ident_f[:]), R=[cw[x], ident_f], W=[PT])
        S.op('act', lambda a: a.activation(ct[:], PT[0:16, :], AF.Copy), R=[PT], W=[ct])
        S.dma('sp', io['cwT'][:, sl], ct[:], R=[ct])

    load(0)
    if n_tiles > 1:
        load(1)
    stage1(0)
    for t in range(n_tiles + 1):
        if t + 1 < n_tiles:
            stage1(t + 1)
        if t < n_tiles:
            stage2(t)
        if t >= 1:
            stage2b(t - 1)
        if t + 2 < n_tiles:
            load(t + 2)
    S.end_phase()


def emit_C2(S, io, c, last_layer, n_half=2):
    S.begin_phase()
    NTL = 512
    NE = 256
    HT = 2048
    ones_f = c['ones_f']
    modc = scol(S, "modc2", io['modc'], [128, 48])
    ident_f = c['ident_f']
    if not last_layer:
        modn = scol(S, "modn", io['modn'], [128, 16])
        ang = scol(S, "ang", io['attn_norm_col'], [128, 8])
        Aa = S.sb([128, 8], F32, "Aa")
        S.op('dve', lambda v: v.scalar_tensor_tensor(Aa[:], modn[:, 8:16], 1.0, ang[:], ALU.add, ALU.mult), R=[modn, ang], W=[Aa])
    hf = S.sb([128, 8, HT], BF16, "hfh")
    cwt = S.sb([16, HT], F32, "cwh")
    acc = S.sb([128, 8, HT], F32, "macc")
    acc_t = [[S.sub(acc[:, dc, tl * NTL:(tl + 1) * NTL], "acc%d_%d" % (dc, tl)) for tl in range(4)] for dc in range(8)]
    stg = [S.sb([128, 8, 256], F32, "stgC%d" % i) for i in range(2)]
    Wge = [S.sb([128, 8, 256], BF16, "Wge%d" % i) for i in range(2)]
    Wue = [S.sb([128, 8, 256], BF16, "Wue%d" % i) for i in range(2)]
    Wde = [S.sb([128, 2, 1024], BF16, "Wde%d" % i) for i in range(2)]
    cwb = [S.sb([128, NTL], F32, "cwb%d" % i) for i in range(2)]
    sg = [S.sb([128, NTL], F32, "sg%d" % i) for i in range(2)]
    h1 = [S.sb([128, NTL], F32, "h1_%d" % i) for i in range(2)]
    hid = [S.sb([128, 2, NTL], BF16, "hid%d" % i) for i in range(2)]
    xts = [S.sb([128, 8, NE], F32, "c2xt%d" % i) for i in range(2)]
    sqs = [S.sb([128, 8, NE], F32, "c2sq%d" % i) for i in range(2)]
    ub = S.sb([128, 8, NE], BF16, "c2ub")
    lnt = S.sb([128, NE], F32, "c2ln")
    rst = S.sb([128, NE], F32, "c2rs")
    tmp = [S.sb([128, NE], F32, "c2tmp%d" % i) for i in range(2)]
    PGU = [bank(S, "PGU%d" % i) for i in range(4)]
    PD = [bank(S, "PD%d" % i) for i in range(2)]
    PCW = bank(S, "PCW")
    PSS = bank(S, "PSS2")
    nst = [0]

    def load_expert(e, slot):
        for (dst, src) in ((Wge[slot], io['w_eg'][e]), (Wue[slot], io['w_eu'][e])):
            st = stg[nst[0] % 2]
            nst[0] += 1
            S.dma('sp', st[:], src.rearrange("(k p) n -> p k n", p=128), W=[st])
            S.op('pool', lambda g: g.tensor_copy(dst[:], st[:]), R=[st], W=[dst])
        st = stg[nst[0] % 2]
        nst[0] += 1
        stv = st[:].rearrange("p (a k) n -> p a (k n)", a=2)
        S.dma('sp', stv, io['w_ed'][e].rearrange("(k p) n -> p k n", p=128), W=[st])
        S.op('pool', lambda g: g.tensor_copy(Wde[slot][:], stv), R=[st], W=[Wde[slot]])

    nx = [0]

    def epi_res(half, tl):
        h0 = half * HT
        gsl = slice(h0 + tl * NE, h0 + (tl + 1) * NE)
        tsl = slice(tl * NE, (tl + 1) * NE)
        xt = xts[tl % 2]
        ats = [acc_t[dc][(tl * NE) // NTL] for dc in range(8)]
        S.dma('sp', xt[:], io['x1T'][:, :, gsl].rearrange("k p n -> p k n"), W=[xt])
        for dc in range(8):
            S.op('dve', lambda v: v.scalar_tensor_tensor(xt[:, dc, :], acc[:, dc, tsl], modc[:, 40 + dc:41 + dc], xt[:, dc, :], ALU.mult, ALU.add), R=[ats[dc], modc, xt], W=[xt])
        S.dma('sp', io['xT_out'][:, :, gsl].rearrange("k p n -> p k n"), xt[:], R=[xt])
        if not last_layer:
            S.op('act', lambda a: a.activation(sqs[tl % 2][:], xt[:], AF.Square), R=[xt], W=[sqs[tl % 2]])

    def epi_norm(half, tl):
        if last_layer:
            return
        h0 = half * HT
        gsl = slice(h0 + tl * NE, h0 + (tl + 1) * NE)
        xt = xts[tl % 2]
        norm_mod(S, xt, Aa[:], modn[:, 0:8], [ub], sqs[tl % 2], ones_f, PSS, lnt, rst, tmp, [Aa, modn], do_sq=False)
        S.dma('sp', io['uT_out'][:, :, gsl].rearrange("k p n -> p k n"), ub[:], R=[ub])

    for half in range(n_half):
        h0 = half * HT
        S.dma('sp', hf[:], io['hfT'][:, :, h0:h0 + HT].rearrange("k p n -> p k n"), W=[hf])
        S.dma('sp', cwt[:], io['cwT'][:, h0:h0 + HT], W=[cwt])
        if half == 0:
            load_expert(0, 0)
            load_expert(1, 1)
        items = [(e, tl) for e in range(16) for tl in range(4)]
        bufs = {}

        def gu(n):
            e, tl = items[n]
            slot = e % 2
            tsl = slice(tl * NTL, (tl + 1) * NTL)
            hd = hid[n % 2]
            cb = cwb[n % 2]
            bufs[n] = hd
            S.op('pe', lambda p: p.matmul(PCW[:, :], ident_f[0:16, e:e + 1].to_broadcast([16, 128]), cwt[:, tsl], start=True, stop=True), R=[ident_f, cwt], W=[PCW])
            S.op('act', lambda a: a.activation(cb[:], PCW[:, :], AF.Copy), R=[PCW], W=[cb])
            for fc in range(2):
                pg = PGU[fc * 2]
                pu = PGU[fc * 2 + 1]
                for k in range(8):
                    S.op('pe', lambda p: p.matmul(pg[:, :], Wge[slot][:, k, fc * 128:(fc + 1) * 128], hf[:, k, tsl], start=(k == 0), stop=(k == 7)), R=[Wge[slot], hf], W=[pg])
                for k in range(8):
                    S.op('pe', lambda p: p.matmul(pu[:, :], Wue[slot][:, k, fc * 128:(fc + 1) * 128], hf[:, k, tsl], start=(k == 0), stop=(k == 7)), R=[Wue[slot], hf], W=[pu])
                s1 = sg[fc]
                S.op('act', lambda a: a.activation(s1[:], pg[:, :], AF.Silu), R=[pg], W=[s1])
                S.op('dve', lambda v: v.tensor_tensor(h1[fc][:], pu[:, :], s1[:], ALU.mult), R=[pu, s1], W=[h1[fc]])
                S.op('pool', lambda g: g.tensor_tensor(hd[:, fc, :], h1[fc][:], cb[:], ALU.mult), R=[h1[fc], cb], W=[hd])

        def down(n):
            e, tl = items[n]
            slot = e % 2
            tsl = slice(tl * NTL, (tl + 1) * NTL)
            hd = bufs.pop(n)
            for dc in range(8):
                pd = PD[dc % 2]
                for fc in range(2):
                    S.op('pe', lambda p: p.matmul(pd[:, :], Wde[slot][:, fc, dc * 128:(dc + 1) * 128], hd[:, fc, :], start=(fc == 0), stop=(fc == 1)), R=[Wde[slot], hd], W=[pd])
                at = acc_t[dc][tl]
                if e == 0:
                    S.op('act', lambda a: a.activation(acc[:, dc, tsl], pd[:, :], AF.Copy), R=[pd], W=[at])
                else:
                    S.op('dve', lambda v: v.tensor_tensor(acc[:, dc, tsl], pd[:, :], acc[:, dc, tsl], ALU.add), R=[pd, at], W=[at])

        gu(0)
        for n in range(len(items)):
            if n + 1 < len(items):
                gu(n + 1)
            e_, tl_ = items[n]
            if half > 0 and e_ == 0:
                epi_res(half - 1, 2 * tl_)
                epi_res(half - 1, 2 * tl_ + 1)
            down(n)
            if half > 0 and e_ == 0:
                epi_norm(half - 1, 2 * tl_)
                epi_norm(half - 1, 2 * tl_ + 1)
            if tl_ == 3 and (e_ + 2 < 16 or half + 1 < n_half):
                load_expert((e_ + 2) % 16, e_ % 2)
        if half == n_half - 1:
            for tlE in range(0, HT // NE, 2):
                epi_res(half, tlE)
                epi_res(half, tlE + 1)
                epi_norm(half, tlE)
                epi_norm(half, tlE + 1)
    S.end_phase()


def emit_A(S, io, c, n_tiles=8):
    S.begin_phase()
    NTL = 512
    ones_f = c['ones_f']
    modn = scol(S, "modnA", io['modn'], [128, 16])
    ang = scol(S, "angA", io['attn_norm_col'], [128, 8])
    Aa = S.sb([128, 8], F32, "AaA")
    S.op('dve', lambda v: v.scalar_tensor_tensor(Aa[:], modn[:, 8:16], 1.0, ang[:], ALU.add, ALU.mult), R=[modn, ang], W=[Aa])
    xt = [S.sb([128, 8, NTL], F32, "axt%d" % i) for i in range(2)]
    ub = [S.sb([128, 8, NTL], BF16, "aub%d" % i) for i in range(2)]
    sq = S.sb([128, 8, NTL], F32, "asq")
    lnt = S.sb([128, NTL], F32, "aln")
    rst = S.sb([128, NTL], F32, "ars")
    tmp = [S.sb([128, NTL], F32, "atmp%d" % i) for i in range(2)]
    PSS = bank(S, "PSSA")
    for t in range(n_tiles):
        sl = slice(t * NTL, (t + 1) * NTL)
        x = xt[t % 2]
        S.dma('sp', x[:], io['xT'][:, :, sl].rearrange("k p n -> p k n"), W=[x])
        u = ub[t % 2]
        norm_mod(S, x, Aa[:], modn[:, 0:8], [u], sq, ones_f, PSS, lnt, rst, tmp, [Aa, modn])
        S.dma('sp', io['uT_out'][:, :, sl].rearrange("k p n -> p k n"), u[:], R=[u])
    S.end_phase()


CONST_SPECS = {'c_ident_b': ([128, 128], BF16), 'c_ident_f': ([128, 128], F32), 'c_tri_b': ([128, 128], BF16),
               'c_E65': ([65, 64], F32)}


def _declare(nc, specs, kind):
    io = {}
    for name, (shape, dt) in specs.items():
        io[name] = nc.dram_tensor(name, list(shape), dt, kind=kind).ap()
    return io


def build_M():
    nc = bass.Bass("TRN2", target_bir_lowering=False)
    io = _declare(nc, {'c_col': ([128, 8], F32), 'w_ada': ([1024, 6144], F32), 'b_ada': ([1, 6144], F32)}, "ExternalInput")
    io.update(_declare(nc, {'mod_out': ([1, 6144], F32)}, "ExternalOutput"))
    with ExitStack() as st:
        S = Sched(nc, st)
        emit_mod(S, io)
        S.finish_all()
    return nc


A_IN = {'xT': ([8, 128, TOK], F32), 'modn': ([128, 16], F32), 'attn_norm_col': ([128, 8], F32)}


def build_A():
    nc = bass.Bass("TRN2", target_bir_lowering=False)
    io = _declare(nc, dict(A_IN, **CONST_SPECS), "ExternalInput")
    io.update(_declare(nc, {'uT_out': ([8, 128, TOK], BF16)}, "ExternalOutput"))
    with ExitStack() as st:
        S = Sched(nc, st)
        c = load_consts(S, io)
        emit_A(S, io, c)
        S.finish_all()
    return nc


B_IN = {'uT': ([8, 128, S_LEN], BF16), 'WB': ([1024, 1186], F32), 'wuq': ([256, 192], F32), 'wukv': ([128, 256], F32),
        'mla_ncol': ([128, 3], F32), 'mla_grow': ([1, 384], F32), 'c_rope': ([128, 4096], F32),
        'c_Bd': ([128, 128], F32), 'c_M4': ([128, 512], BF16), 'dil_gcol': ([128, 2], F32),
        'ml_conv': ([64, 10], F32), 'ml_gate': ([1, 2], F32), 'ml_hg': ([1, 128], F32)}


def build_B():
    nc = bass.Bass("TRN2", target_bir_lowering=False)
    io = _declare(nc, dict(B_IN, **CONST_SPECS), "ExternalInput")
    io.update(_declare(nc, {'yaT': ([128, S_LEN], BF16), 'ybT': ([128, S_LEN], BF16), 'ycT': ([128, S_LEN], BF16)}, "ExternalOutput"))
    with ExitStack() as st:
        S = Sched(nc, st)
        c = load_consts(S, io)
        emit_mlstm(S, io, c)
        emit_dil(S, io, c)
        emit_mla(S, io, c)
        S.finish_all()
    return nc


C_IN = {'xT': ([8, 128, TOK], F32), 'uT_loc': ([8, 128, TOK], BF16), 'yT': ([3, 4, 128, TOK], BF16),
        'w_gate': ([1024, 3072], F32), 'w_br': ([3, 512, 1024], F32), 'w_out': ([1024, 1024], F32),
        'w_router': ([1024, 20], F32), 'b_router': ([1, 20], F32),
        'w_eg': ([16, 1024, 256], F32), 'w_eu': ([16, 1024, 256], F32), 'w_ed': ([16, 256, 1024], F32),
        'modc': ([128, 48], F32), 'modn': ([128, 16], F32), 'ffn_norm_col': ([128, 8], F32), 'attn_norm_col': ([128, 8], F32),
        'c_Sel': ([16, 2048], F32)}


def build_C():
    nc = bass.Bass("TRN2", target_bir_lowering=False)
    io = _declare(nc, dict(C_IN, **CONST_SPECS), "ExternalInput")
    io.update(_declare(nc, {'xT_out': ([8, 128, TOK], F32), 'uT_out': ([8, 128, TOK], BF16)}, "ExternalOutput"))
    io.update(_declare(nc, {'mgT': ([8, 128, TOK], BF16), 'x1T': ([8, 128, TOK], F32), 'hfT': ([8, 128, TOK], BF16),
                            'cwT': ([16, TOK], F32)}, "Internal"))
    with ExitStack() as st:
        S = Sched(nc, st)
        c = load_consts(S, io)
        emit_C1a(S, io, c)
        emit_C1b(S, io, c)
        emit_C2(S, io, c, last_layer=False)
        S.finish_all()
    return nc


def col8(v):
    return np.ascontiguousarray(np.asarray(v, np.float32).reshape(8, 128).T)


def _run(nc, in_maps):
    res = run_bass_kernel_spmd(nc, in_maps, core_ids=list(range(8)))
    return res.results


def kernel(**inp):
    inp = {k: np.asarray(v) for k, v in inp.items()}
    x = inp['x'].astype(np.float32, copy=False)
    cst = host_consts()
    cst.update(host_consts_dil())
    sel = np.zeros((16, 16, 128), np.float32)
    for e in range(16):
        sel[e, e, :] = 1.0
    cst['c_Sel'] = np.ascontiguousarray(sel.reshape(16, 2048))
    base_c = {k: cst[k] for k in CONST_SPECS}

    ncM = build_M()
    maps = []
    for cidx in range(8):
        l, b = cidx // 2, cidx % 2
        maps.append({'c_col': col8(inp['c'][b]), 'w_ada': np.ascontiguousarray(inp['w_ada'][l]),
                     'b_ada': np.ascontiguousarray(inp['b_ada'][l][None, :])})
    r = _run(ncM, maps)
    mod = np.zeros((DEPTH, NB, 6, 1024), np.float32)
    for cidx in range(8):
        mod[cidx // 2, cidx % 2] = r[cidx]['mod_out'].reshape(6, 1024)

    def modcols(l, b, rows):
        return np.ascontiguousarray(np.concatenate([col8(mod[l, b, i]) for i in rows], axis=1))

    xT = []
    for cidx in range(8):
        b, j = cidx // 4, cidx % 4
        xT.append(np.ascontiguousarray(x[b, j * TOK:(j + 1) * TOK, :].T.reshape(8, 128, TOK)))
    ncA = build_A()
    maps = []
    for cidx in range(8):
        b = cidx // 4
        m = dict(base_c)
        m.update({'xT': xT[cidx], 'modn': modcols(0, b, (0, 1)), 'attn_norm_col': col8(inp['attn_norm'][0])})
        maps.append(m)
    r = _run(ncA, maps)
    uT_loc = [r[cidx]['uT_out'] for cidx in range(8)]

    ncB = build_B()
    ncC = build_C()
    for l in range(DEPTH):
        uT_full = [np.ascontiguousarray(np.concatenate([uT_loc[b * 4 + j] for j in range(4)], axis=2)) for b in range(NB)]
        maps = []
        for cidx in range(8):
            b, j = cidx // 4, cidx % 4
            m = dict(base_c)
            m.update(prep_B_weights(inp, l, j))
            m.update(prep_dil_vecs(inp, l))
            m.update(prep_mlstm_vecs(inp, l, j))
            m.update({'uT': uT_full[b], 'c_rope': cst['c_rope'], 'c_Bd': cst['c_Bd'], 'c_M4': cst['c_M4']})
            maps.append(m)
        rB = _run(ncB, maps)
        ln = min(l + 1, DEPTH - 1)
        wC = {'w_gate': np.ascontiguousarray(inp['w_in'][l][:, 3496:6568]),
              'w_br': np.ascontiguousarray(np.stack([inp['w_branch_a'][l], inp['w_branch_b'][l], inp['w_branch_c'][l]])),
              'w_out': np.ascontiguousarray(inp['w_out'][l]),
              'w_router': np.ascontiguousarray(np.concatenate([inp['w_router_group'][l], inp['w_router_expert'][l]], axis=1)),
              'b_router': np.ascontiguousarray(np.concatenate([inp['b_router_group'][l], inp['b_router_expert'][l]])[None, :]),
              'w_eg': np.ascontiguousarray(inp['w_exp_gate'][l]), 'w_eu': np.ascontiguousarray(inp['w_exp_up'][l]),
              'w_ed': np.ascontiguousarray(inp['w_exp_down'][l]),
              'ffn_norm_col': col8(inp['ffn_norm'][l]), 'attn_norm_col': col8(inp['attn_norm'][ln]), 'c_Sel': cst['c_Sel']}
        maps = []
        for cidx in range(8):
            b, j = cidx // 4, cidx % 4
            sl = slice(j * TOK, (j + 1) * TOK)
            yT = np.stack([np.stack([rB[b * 4 + jj][nm][:, sl] for jj in range(4)]) for nm in ('yaT', 'ybT', 'ycT')])
            m = dict(base_c)
            m.update(wC)
            m.update({'xT': xT[cidx], 'uT_loc': uT_loc[cidx], 'yT': np.ascontiguousarray(yT),
                      'modc': modcols(l, b, range(6)), 'modn': modcols(ln, b, (0, 1))})
            maps.append(m)
        rC = _run(ncC, maps)
        xT = [rC[cidx]['xT_out'] for cidx in range(8)]
        uT_loc = [rC[cidx]['uT_out'] for cidx in range(8)]

    out = np.empty((NB, S_LEN, D), np.float32)
    for cidx in range(8):
        b, j = cidx // 4, cidx % 4
        out[b, j * TOK:(j + 1) * TOK, :] = xT[cidx].reshape(D, TOK).T
    return out
```

```python
import numpy as np
import ml_dtypes
from contextlib import ExitStack
import concourse.bass as bass
import concourse.mybir as mybir
from concourse.bass_utils import run_bass_kernel_spmd

F32 = mybir.dt.float32
BF16 = mybir.dt.bfloat16
AF = mybir.ActivationFunctionType
ALU = mybir.AluOpType
AX = mybir.AxisListType
NPBF = ml_dtypes.bfloat16

D = 1024
S_LEN = 16384
NB = 2
DEPTH = 4
EPS = 1e-6
TOK = 4096
NT = 512


class T:
    __slots__ = ("ap", "name", "lw", "rs", "dsem", "dcnt")

    def __init__(self, ap, name):
        self.ap = ap
        self.name = name
        self.lw = None
        self.rs = {}
        self.dsem = None
        self.dcnt = 0

    def __getitem__(self, k):
        return self.ap[k]


class Sched:
    SEM_MAX = 30000

    def __init__(self, nc, stack):
        self.nc = nc
        self.root = stack
        self.eng = {'pe': nc.tensor, 'act': nc.scalar, 'dve': nc.vector, 'pool': nc.gpsimd, 'sp': nc.sync}
        self.sem = {}
        self.cnt = {}
        self.nsem = 0
        for e in self.eng:
            self._newsem(e)
        self.waited = {e: {} for e in self.eng}
        self.phase = None
        self.phase_tiles = []
        self.dma_pool = []
        self.all_dma = {}
        self.cc_sem = None
        self.ninst = 0

    def _newsem(self, e):
        self.nsem += 1
        self.sem[e] = self.root.enter_context(self.nc.semaphore("s_%s_%d" % (e, self.nsem)))
        self.cnt[e] = 0

    def begin_phase(self):
        self.phase = ExitStack()
        self.phase_tiles = []

    def end_phase(self):
        deps = []
        for t in self.phase_tiles:
            if t.lw is not None:
                deps.append(t.lw)
            deps.extend(t.rs.values())
        self._wait('sp', deps, True)
        self.sp_mark()
        self.barrier()
        for t in self.phase_tiles:
            if t.dsem is not None:
                self.dma_pool.append((t.dsem, t.dcnt))
        self.phase.close()
        self.phase = None
        self.phase_tiles = []

    def barrier(self):
        tags = [(self.sem[e], self.cnt[e], e) for e in self.eng if self.cnt[e] > 0]
        for e in self.eng:
            self._wait(e, tags, False)

    def sp_mark(self):
        ins = self.eng['sp'].sem_inc(self.sem['sp'], 1)
        self.cnt['sp'] += 1

    def collective(self, src_ap, dst_ap, deps, groups=((0, 1, 2, 3), (4, 5, 6, 7))):
        if self.cc_sem is None:
            self.cc_sem = self.root.enter_context(self.nc.semaphore("cc_sem"))
            self.cc_cnt = 0
        self._wait('pool', list(deps), True)
        ins = self.eng['pool'].collective_compute("AllGather", ALU.bypass, replica_groups=[list(g) for g in groups], ins=[src_ap], outs=[dst_ap])
        self.cc_cnt += 16
        ins.then_inc(self.cc_sem, 16)
        tag = (self.cc_sem, self.cc_cnt, 'dma')
        self.all_dma[id(self.cc_sem)] = tag
        self._wait('sp', [tag], True)
        return tag

    def sb(self, shape, dt, name):
        st = self.phase if self.phase is not None else self.root
        self.nsem += 1
        t = T(st.enter_context(self.nc.sbuf_tensor("sb_%s_%d" % (name, self.nsem), list(shape), dt)), name)
        if self.phase is not None:
            self.phase_tiles.append(t)
        return t

    def ps(self, shape, dt, name):
        st = self.phase if self.phase is not None else self.root
        self.nsem += 1
        t = T(st.enter_context(self.nc.psum_tensor("ps_%s_%d" % (name, self.nsem), list(shape), dt)), name)
        if self.phase is not None:
            self.phase_tiles.append(t)
        return t

    def sub(self, ap, name):
        t = T(ap, name)
        if self.phase is not None:
            self.phase_tiles.append(t)
        return t

    def _wait(self, e, deps, is_dma):
        w = self.waited[e]
        for (sem, val, de) in deps:
            if de == e and not is_dma and e == 'pe':
                continue
            key = id(sem)
            if w.get(key, 0) >= val:
                continue
            self.eng[e].wait_ge(sem, val)
            w[key] = val

    def _deps(self, R, W):
        deps = []
        for t in R:
            if t.lw is not None:
                deps.append(t.lw)
        for t in W:
            if t.lw is not None:
                deps.append(t.lw)
            deps.extend(t.rs.values())
        return deps

    def _mark(self, tag, R, W):
        sem = tag[0]
        for t in W:
            t.lw = tag
            t.rs = {}
        for t in R:
            t.rs[id(sem)] = tag

    def op(self, e, fn, R=(), W=()):
        self._wait(e, self._deps(R, W), False)
        if self.cnt[e] >= self.SEM_MAX:
            self._newsem(e)
        ins = fn(self.eng[e])
        self.cnt[e] += 1
        self.ninst += 1
        ins.then_inc(self.sem[e], 1)
        self._mark((self.sem[e], self.cnt[e], e), R, W)
        return ins

    def dma(self, q, out_ap, in_ap, R=(), W=(), owner=None):
        if q == 'pool':
            q = 'sp'
        self._wait(q, self._deps(R, W), True)
        if owner is None:
            owner = (list(W) + list(R))[0]
        if owner.dsem is None or owner.dcnt >= self.SEM_MAX:
            if owner.dsem is None and self.dma_pool:
                owner.dsem, owner.dcnt = self.dma_pool.pop()
            else:
                owner.dsem = self.root.enter_context(self.nc.semaphore("d_%d" % self.nsem))
                self.nsem += 1
                owner.dcnt = 0
        ins = self.eng[q].dma_start(out=out_ap, in_=in_ap)
        owner.dcnt += 16
        self.ninst += 1
        ins.then_inc(owner.dsem, 16)
        tag = (owner.dsem, owner.dcnt, 'dma')
        self.all_dma[id(owner.dsem)] = tag
        self._mark(tag, R, W)
        return tag

    def finish_all(self):
        self._wait('sp', list(self.all_dma.values()), True)
        self.barrier()

    def finish(self, tiles):
        deps = []
        for t in tiles:
            if t.lw is not None:
                deps.append(t.lw)
            deps.extend(t.rs.values())
        self._wait('sp', deps, True)


def bank(S, name, dt=F32):
    return S.ps([128, 512 if dt == F32 else 1024], dt, name)


def rsqrt_mean(S, out_ap, in_ap, n, tmp_ap, R, W):
    S.op('act', lambda a: a.activation(tmp_ap, in_ap, AF.Ln, scale=1.0 / n, bias=S.eps_col[0:tmp_ap.shape[0], 0:1]), R=R + [S.eps_t], W=W)
    S.op('act', lambda a: a.activation(out_ap, tmp_ap, AF.Exp, scale=-0.5), R=W, W=W)


def load_consts(S, io):
    c = {}
    c['ident_b'] = S.sb([128, 128], BF16, "ident_b")
    c['ident_f'] = S.sb([128, 128], F32, "ident_f")
    c['tri_b'] = S.sb([128, 128], BF16, "tri_b")
    c['E65'] = S.sb([65, 64], F32, "E65")
    c['ones_f'] = S.sb([128, 128], F32, "ones_f")
    S.eps_t = S.sb([128, 1], F32, "eps_t")
    S.eps_col = S.eps_t.ap
    S.dma('sp', c['ident_b'][:], io['c_ident_b'], W=[c['ident_b']])
    S.dma('sp', c['ident_f'][:], io['c_ident_f'], W=[c['ident_f']])
    S.dma('sp', c['tri_b'][:], io['c_tri_b'], W=[c['tri_b']])
    S.dma('sp', c['E65'][:], io['c_E65'], W=[c['E65']])
    S.op('pool', lambda g: g.memset(c['ones_f'][:], 1.0), W=[c['ones_f']])
    S.op('pool', lambda g: g.memset(S.eps_t[:], EPS), W=[S.eps_t])
    return c


def emit_mla(S, io, c, n_tiles=32):
    S.begin_phase()
    uT = io['uT']
    ident_b, tri_b, E65 = c['ident_b'], c['tri_b'], c['E65']
    wl_f = S.sb([128, 8, 416], F32, "wl_f")
    S.dma('sp', wl_f[:], io['WB'][:, 0:416].rearrange("(k p) n -> p k n", p=128), W=[wl_f])
    wl = S.sb([128, 8, 416], BF16, "wl")
    S.op('pool', lambda g: g.tensor_copy(wl[:], wl_f[:]), R=[wl_f], W=[wl])
    wuq_f = S.sb([128, 2, 192], F32, "wuq_f")
    S.dma('sp', wuq_f[:], io['wuq'].rearrange("(k p) n -> p k n", p=128), W=[wuq_f])
    wukv_f = S.sb([128, 256], F32, "wukv_f")
    S.dma('sp', wukv_f[:], io['wukv'], W=[wukv_f])
    ncol = S.sb([128, 3], F32, "ncol")
    S.dma('sp', ncol[:], io['mla_ncol'], W=[ncol])
    wuq = S.sb([128, 2, 192], BF16, "wuq")
    wukv = S.sb([128, 256], BF16, "wukv")
    for k in range(2):
        S.op('dve', lambda v: v.tensor_scalar(wuq[:, k, :], wuq_f[:, k, :], ncol[:, k:k + 1], None, ALU.mult), R=[wuq_f, ncol], W=[wuq])
    S.op('dve', lambda v: v.tensor_scalar(wukv[:], wukv_f[:], ncol[:, 2:3], None, ALU.mult), R=[wukv_f, ncol], W=[wukv])
    g4 = S.sb([128, 384], F32, "g4")
    S.dma('sp', g4[:], io['mla_grow'].partition_broadcast(128), W=[g4])
    S.op('dve', lambda v: v.tensor_scalar(g4[:, 0:192], g4[:, 0:192], 96 ** -0.5, None, ALU.mult), R=[g4], W=[g4])
    g4v = g4[:].rearrange("p (a b) -> p a b", a=4)
    cs = S.sb([128, 128 * 32], F32, "cs")
    S.dma('sp', cs[:], io['c_rope'], W=[cs])
    csv = cs[:].rearrange("p (t c) -> p t c", c=32)
    KT = S.sb([96, 128, 2, 128], BF16, "KT")
    VA = S.sb([128, 128, 2, 65], BF16, "VA")
    KT_t = [S.sub(KT[:, 4 * i:4 * i + 4, :, :], "KT%d" % i) for i in range(32)]
    VA_t = [S.sub(VA[:, 4 * i:4 * i + 4, :, :], "VA%d" % i) for i in range(32)]
    S.op('pool', lambda g: g.memset(VA[:, :, :, 64:65], 1.0), W=VA_t)
    QT = [S.sb([96, 2, 512], BF16, "QT%d" % i) for i in range(2)]
    uts = [S.sb([128, 8, 512], BF16, "ut%d" % i) for i in range(2)]
    p_lat = bank(S, "p_lat")
    p_trq = bank(S, "p_trq", BF16)
    p_tr = p_trq
    p_qkT = p_trq
    p_qkv = bank(S, "p_qkv")
    p_s = [bank(S, "p_s%d" % i) for i in range(3)]
    p_o0 = bank(S, "p_o0")
    p_o = [p_o0, p_o0]
    p_den = bank(S, "p_den")
    trv = p_trq[:, 0:512].rearrange("p (a b) -> p a b", b=128)
    qkTv = p_trq[:, 512:1024].rearrange("p (a b) -> p a b", b=128)
    R2 = 2
    junk = [S.sb([128, 256], F32, "junk%d" % i) for i in range(R2)]
    ss = [S.sb([128, 2], F32, "ss%d" % i) for i in range(R2)]
    sst = [S.sb([128, 2], F32, "sst%d" % i) for i in range(R2)]
    rstd = [S.sb([128, 2], F32, "rstd%d" % i) for i in range(R2)]
    cn = [S.sb([128, 384], BF16, "cn%d" % i) for i in range(R2)]
    cnT = [S.sb([128, 3, 128], BF16, "cnT%d" % i) for i in range(R2)]
    qk = [S.sb([128, 4, 96], F32, "qk%d" % i) for i in range(R2)]
    sq = [S.sb([128, 4, 96], F32, "sq%d" % i) for i in range(R2)]
    ss4 = [S.sb([128, 4], F32, "ss4%d" % i) for i in range(R2)]
    ss4t = [S.sb([128, 4], F32, "ss4t%d" % i) for i in range(R2)]
    rs4 = [S.sb([128, 4], F32, "rs4%d" % i) for i in range(R2)]
    qkn = [S.sb([128, 4, 96], F32, "qkn%d" % i) for i in range(R2)]
    rt = [[S.sb([128, 4, 16], F32, "rt%d_%d" % (j, i)) for j in range(4)] for i in range(R2)]
    qkr = [S.sb([128, 4, 96], BF16, "qkr%d" % i) for i in range(R2)]
    pts = [S.sb([128, 512], BF16, "pt%d" % i) for i in range(3)]
    o_sb = [S.sb([65, 512], F32, "o_sb%d" % i) for i in range(2)]
    rden = [S.sb([64, 512], F32, "rden%d" % i) for i in range(2)]
    yts = [S.sb([128, 512], BF16, "yt%d" % i) for i in range(2)]

    def load_u(i):
        S.dma('sp', uts[i % 2][:], uT[:, :, i * 512:(i + 1) * 512].rearrange("k p n -> p k n"), W=[uts[i % 2]])

    import os
    STOP = int(os.environ.get('DBG_STOP', '99'))

    def proj_sub(i, s):
        ut = uts[i % 2]
        r = (i * 4 + s) % R2
        blk = i * 4 + s
        for k in range(8):
            S.op('pe', lambda p: p.matmul(p_lat[:, 0:416], ut[:, k, s * 128:(s + 1) * 128], wl[:, k, :], start=(k == 0), stop=(k == 7)), R=[ut, wl], W=[p_lat])
        yield
        S.op('act', lambda a: a.activation(junk[r][:, 0:256], p_lat[:, 0:256], AF.Square, accum_out=ss[r][:, 0:1]), R=[p_lat], W=[junk[r], ss[r]])
        S.op('act', lambda a: a.activation(junk[r][:, 0:128], p_lat[:, 256:384], AF.Square, accum_out=ss[r][:, 1:2]), R=[p_lat], W=[junk[r], ss[r]])
        yield
        S.op('act', lambda a: a.activation(sst[r][:, 0:1], ss[r][:, 0:1], AF.Ln, scale=1.0 / 256, bias=S.eps_col[:, 0:1]), R=[ss[r], S.eps_t], W=[sst[r]])
        S.op('act', lambda a: a.activation(sst[r][:, 1:2], ss[r][:, 1:2], AF.Ln, scale=1.0 / 128, bias=S.eps_col[:, 0:1]), R=[ss[r], S.eps_t], W=[sst[r]])
        S.op('act', lambda a: a.activation(rstd[r][:], sst[r][:], AF.Exp, scale=-0.5), R=[sst[r]], W=[rstd[r]])
        yield
        S.op('dve', lambda v: v.tensor_scalar(cn[r][:, 0:256], p_lat[:, 0:256], rstd[r][:, 0:1], None, ALU.mult), R=[p_lat, rstd[r]], W=[cn[r]])
        S.op('dve', lambda v: v.tensor_scalar(cn[r][:, 256:384], p_lat[:, 256:384], rstd[r][:, 1:2], None, ALU.mult), R=[p_lat, rstd[r]], W=[cn[r]])
        yield
        for h in range(2):
            S.op('dve', lambda v: v.tensor_copy(qk[r][:, 2 + h, 64:96], p_lat[:, 384:416]), R=[p_lat], W=[qk[r]])
        yield
        for j in range(3):
            S.op('pe', lambda p: p.transpose(trv[:, j, :], cn[r][:, j * 128:(j + 1) * 128], ident_b[:]), R=[cn[r], ident_b], W=[p_tr])
        yield
        S.op('dve', lambda v: v.tensor_copy(cnT[r][:], trv[:, 0:3, :]), R=[p_tr], W=[cnT[r]])
        yield
        S.op('pe', lambda p: p.matmul(p_qkv[:, 0:192], cnT[r][:, 0, :], wuq[:, 0, :], start=True, stop=False), R=[cnT[r], wuq], W=[p_qkv])
        S.op('pe', lambda p: p.matmul(p_qkv[:, 0:192], cnT[r][:, 1, :], wuq[:, 1, :], start=False, stop=False), R=[cnT[r], wuq], W=[p_qkv])
        S.op('pe', lambda p: p.matmul(p_qkv[:, 192:448], cnT[r][:, 2, :], wukv[:], start=False, stop=True), R=[cnT[r], wukv], W=[p_qkv])
        yield
        kvv = p_qkv[:, 192:448].rearrange("p (a b) -> p a b", a=2)
        S.op('act', lambda a: a.activation(qk[r][:, 0:2, :], p_qkv[:, 0:192].rearrange("p (a b) -> p a b", a=2), AF.Copy), R=[p_qkv], W=[qk[r]])
        S.op('dve', lambda v: v.tensor_copy(qk[r][:, 2:4, 0:64], kvv[:, :, 0:64]), R=[p_qkv], W=[qk[r]])
        S.op('act', lambda a: a.activation(VA[:, blk, :, 0:64], kvv[:, :, 64:128], AF.Copy), R=[p_qkv], W=[VA_t[i]])
        yield
        S.op('dve', lambda v: v.tensor_tensor(sq[r][:], qk[r][:], qk[r][:], ALU.mult), R=[qk[r]], W=[sq[r]])
        S.op('dve', lambda v: v.tensor_reduce(ss4[r][:], sq[r][:], AX.X, ALU.add), R=[sq[r]], W=[ss4[r]])
        yield
        S.op('act', lambda a: a.activation(ss4t[r][:], ss4[r][:], AF.Ln, scale=1.0 / 96, bias=S.eps_col[:, 0:1]), R=[ss4[r], S.eps_t], W=[ss4t[r]])
        S.op('act', lambda a: a.activation(rs4[r][:], ss4t[r][:], AF.Exp, scale=-0.5), R=[ss4t[r]], W=[rs4[r]])
        yield
        S.op('pool', lambda g: g.tensor_tensor(sq[r][:], qk[r][:], g4v, ALU.mult), R=[qk[r], g4], W=[sq[r]])
        for sl in range(4):
            S.op('dve', lambda v: v.tensor_scalar(qkn[r][:, sl, :], sq[r][:, sl, :], rs4[r][:, sl:sl + 1], None, ALU.mult), R=[sq[r], rs4[r]], W=[qkn[r]])
        cosb = csv[:, blk:blk + 1, 0:16].to_broadcast([128, 4, 16])
        sinb = csv[:, blk:blk + 1, 16:32].to_broadcast([128, 4, 16])
        x1 = qkn[r][:, :, 64:80]
        x2 = qkn[r][:, :, 80:96]
        t1, t2, t3, t4 = rt[r]
        S.op('pool', lambda g: g.tensor_copy(qkr[r][:, :, 0:64], qkn[r][:, :, 0:64]), R=[qkn[r]], W=[qkr[r]])
        S.op('dve', lambda v: v.tensor_tensor(t1[:], x1, cosb, ALU.mult), R=[qkn[r], cs], W=[t1])
        S.op('pool', lambda g: g.tensor_tensor(t2[:], x2, sinb, ALU.mult), R=[qkn[r], cs], W=[t2])
        S.op('pool', lambda g: g.tensor_tensor(t3[:], x1, sinb, ALU.mult), R=[qkn[r], cs], W=[t3])
        S.op('dve', lambda v: v.tensor_tensor(t4[:], x2, cosb, ALU.mult), R=[qkn[r], cs], W=[t4])
        yield
        S.op('dve', lambda v: v.tensor_tensor(qkr[r][:, :, 64:80], t1[:], t2[:], ALU.subtract), R=[t1, t2], W=[qkr[r]])
        S.op('pool', lambda g: g.tensor_tensor(qkr[r][:, :, 80:96], t3[:], t4[:], ALU.add), R=[t3, t4], W=[qkr[r]])
        yield
        for sl in range(4):
            S.op('pe', lambda p: p.transpose(qkTv[0:96, sl, :], qkr[r][:, sl, :], ident_b[:]), R=[qkr[r], ident_b], W=[p_qkT])
        yield
        S.op('act', lambda a: a.activation(QT[i % 2][:, :, s * 128:(s + 1) * 128], qkTv[0:96, 0:2, :], AF.Copy), R=[p_qkT], W=[QT[i % 2]])
        S.op('act', lambda a: a.activation(KT[:, blk, :, :], qkTv[0:96, 2:4, :], AF.Copy), R=[p_qkT], W=[KT_t[i]])

    cnt = [0]

    def attention(i, fillers):
        nblk = 4 * i + 4
        qt = QT[i % 2]
        yt = yts[i % 2]
        items = [(h, kb) for h in range(2) for kb in range(nblk)]
        NST = 60
        done_st = [0]

        def advance(idx):
            if fillers is None:
                return
            want = min(NST, ((idx + 1) * NST + len(items) - 1) // len(items))
            while done_st[0] < want:
                try:
                    next(fillers)
                except StopIteration:
                    done_st[0] = NST
                    return
                done_st[0] += 1

        def qk(idx):
            h, kb = items[idx]
            d = kb - 4 * i
            q0 = max(d, 0) * 128
            n = cnt[0] + idx
            sT = p_s[n % 3]
            S.op('pe', lambda p: p.matmul(sT[:, q0:512], KT[:, kb, h, :], qt[:, h, q0:512], start=True, stop=True), R=[KT_t[kb // 4], qt], W=[sT])

        qk(0)
        if len(items) > 1:
            qk(1)
        for idx, (h, kb) in enumerate(items):
            d = kb - 4 * i
            q0 = max(d, 0) * 128
            n = cnt[0] + idx
            sT = p_s[n % 3]
            pt = pts[n % 3]
            po = p_o[h]
            if idx + 2 < len(items):
                qk(idx + 2)
            S.op('act', lambda a: a.activation(pt[:, q0:512], sT[:, q0:512], AF.Exp), R=[sT], W=[pt])
            if d >= 0:
                S.op('pool', lambda g: g.tensor_tensor(pt[:, q0:q0 + 128], pt[:, q0:q0 + 128], tri_b[:], ALU.mult), R=[pt, tri_b], W=[pt])
            S.op('pe', lambda p: p.matmul(po[0:65, q0:512], VA[:, kb, h, :], pt[:, q0:512], start=(kb == 0), stop=(kb == nblk - 1)), R=[VA_t[kb // 4], pt], W=[po])
            advance(idx)
            if kb == nblk - 1:
                osb = o_sb[h]
                S.op('act', lambda a: a.activation(osb[:], po[0:65, :], AF.Copy), R=[po], W=[osb])
                S.op('pe', lambda p: p.matmul(p_den[0:64, :], E65[:], osb[:], start=True, stop=True), R=[E65, osb], W=[p_den])
                S.op('dve', lambda v: v.reciprocal(rden[h][:], p_den[0:64, :]), R=[p_den], W=[rden[h]])
                S.op('dve', lambda v: v.tensor_tensor(yt[h * 64:(h + 1) * 64, :], osb[0:64, :], rden[h][:], ALU.mult), R=[osb, rden[h]], W=[yt])
        cnt[0] += len(items)
        if fillers is not None:
            for _ in fillers:
                pass
        S.dma('sp', io['yaT'][:, i * 512:(i + 1) * 512], yt[:], R=[yt])

    import os
    lvl = int(os.environ.get("DBG_LVL", "9"))
    load_u(0)
    if n_tiles > 1:
        load_u(1)
    def proj_gen(i):
        for s_ in range(4):
            yield from proj_sub(i, s_)

    if lvl >= 1:
        for _ in proj_gen(0):
            pass
    if lvl < 2:
        n_tiles = 0
        S.dma('pool', io['yaT'][:, 0:512], uts[0][:, 0, :], R=[uts[0]])
    for i in range(n_tiles):
        fillers = proj_gen(i + 1) if i + 1 < n_tiles else None
        attention(i, fillers)
        if i + 2 < n_tiles:
            load_u(i + 2)
    S.end_phase()


def host_consts():
    c = {}
    c['c_ident_b'] = np.eye(128, dtype=np.float32).astype(NPBF)
    c['c_ident_f'] = np.eye(128, dtype=np.float32)
    p = np.arange(128)[:, None]
    f = np.arange(128)[None, :]
    c['c_tri_b'] = (p <= f).astype(np.float32).astype(NPBF)
    e = np.zeros((65, 64), np.float32)
    e[64, :] = 1.0
    c['c_E65'] = e
    half = 16
    inv = (np.float32(10000.0) ** (-np.arange(half, dtype=np.float32) / np.float32(half))).astype(np.float32)
    pos = np.arange(S_LEN, dtype=np.float32)
    ang = (pos[:, None] * inv[None, :]).astype(np.float32)
    tab = np.concatenate([np.cos(ang), np.sin(ang)], axis=1).astype(np.float32)
    c['c_rope'] = np.ascontiguousarray(tab.reshape(128, 128, 32).transpose(1, 0, 2).reshape(128, 128 * 32))
    return c


B_CONST_KEYS = ['c_ident_b', 'c_ident_f', 'c_tri_b', 'c_E65', 'c_rope']


def prep_B_weights(inp, l, j):
    w_in = inp['w_in'][l]
    hq = slice(416 + j * 64, 416 + (j + 1) * 64)
    hk = slice(416 + 256 + j * 64, 416 + 256 + (j + 1) * 64)
    hv = slice(928 + j * 128, 928 + (j + 1) * 128)
    ho = slice(1440 + j * 128, 1440 + (j + 1) * 128)
    hi = slice(1952 + j, 1953 + j)
    hf = slice(1956 + j, 1957 + j)
    dq = slice(1960 + j * 128, 1960 + (j + 1) * 128)
    dk = slice(2472 + j * 128, 2472 + (j + 1) * 128)
    dv = slice(2984 + j * 128, 2984 + (j + 1) * 128)
    WB = np.concatenate([w_in[:, 0:416], w_in[:, hq], w_in[:, hk], w_in[:, hv], w_in[:, ho], w_in[:, hi], w_in[:, hf],
                         w_in[:, dq], w_in[:, dk], w_in[:, dv]], axis=1)
    d = {'WB': np.ascontiguousarray(WB)}
    d['wuq'] = np.ascontiguousarray(inp['mla_w_uq'][l][:, j * 192:(j + 1) * 192])
    d['wukv'] = np.ascontiguousarray(inp['mla_w_ukv'][l][:, j * 256:(j + 1) * 256])
    qn = inp['mla_q_norm'][l].reshape(2, 128).T
    kvn = inp['mla_kv_norm'][l].reshape(1, 128).T
    d['mla_ncol'] = np.ascontiguousarray(np.concatenate([qn, kvn], axis=1))
    qg = inp['mla_q_gain'][l]
    kg = inp['mla_k_gain'][l]
    d['mla_grow'] = np.ascontiguousarray(np.concatenate([qg, qg, kg, kg])[None, :])
    return d


DIL_R = (1, 4, 16)


def sst_(c0, r, n=128):
    return slice(c0, c0 + (n - 1) * r + 1, r)


def emit_dil(S, io, c, n_sb=8):
    S.begin_phase()
    uT = io['uT']
    ident_b, E65 = c['ident_b'], c['E65']
    C0 = 802
    wd_f = S.sb([128, 8, 384], F32, "wd_f")
    S.dma('sp', wd_f[:], io['WB'][:, C0:C0 + 384].rearrange("(k p) n -> p k n", p=128), W=[wd_f])
    wd = S.sb([128, 8, 384], BF16, "wd")
    S.op('pool', lambda g: g.tensor_copy(wd[:], wd_f[:]), R=[wd_f], W=[wd])
    gcol = S.sb([128, 2], F32, "gcol")
    S.dma('sp', gcol[:], io['dil_gcol'], W=[gcol])
    S.op('dve', lambda v: v.tensor_scalar(gcol[:, 0:1], gcol[:, 0:1], 64 ** -0.5, None, ALU.mult), R=[gcol], W=[gcol])
    Bd = S.sb([128, 128], F32, "Bd")
    S.dma('sp', Bd[:], io['c_Bd'], W=[Bd])
    M4 = S.sb([128, 512], BF16, "M4")
    S.dma('sp', M4[:], io['c_M4'], W=[M4])
    uts = [S.sb([128, 8, 512], BF16, "dut%d" % i) for i in range(2)]
    KTd = [S.sb([128, 2048], BF16, "KTd%d" % i) for i in range(2)]
    QTd = [S.sb([128, 2048], BF16, "QTd%d" % i) for i in range(2)]
    VTd = [S.sb([128, 2048], BF16, "VTd%d" % i) for i in range(2)]
    Vr = [[S.sb([128, 16, 2, 65], BF16, "Vr%d_%d" % (p, ri)) for ri in range(3)] for p in range(2)]
    for p in range(2):
        for ri in range(3):
            S.op('pool', lambda g: g.memset(Vr[p][ri][:, :, :, 64:65], 1.0), W=[Vr[p][ri]])
    P0 = bank(S, "dP0")
    P1 = bank(S, "dP1")
    p_tr = bank(S, "dp_tr", BF16)
    p_sc = bank(S, "dp_sc")
    p_acc = [bank(S, "dp_acc%d" % i) for i in range(4)]
    trv = p_tr[:].rearrange("p (a b) -> p a b", b=128)
    raw = [S.sb([128, 512], F32, "draw%d" % i) for i in range(2)]
    sqt = [S.sb([128, 512], F32, "dsq%d" % i) for i in range(2)]
    lnt = [S.sb([128, 512], F32, "dln%d" % i) for i in range(2)]
    rst = [S.sb([128, 512], F32, "drs%d" % i) for i in range(2)]
    pts = [S.sb([128, 512], BF16, "dpt%d" % i) for i in range(3)]
    o_sb = [S.sb([65, 512], F32, "do_sb%d" % i) for i in range(2)]
    rden = [S.sb([64, 512], F32, "drden%d" % i) for i in range(2)]
    yts = [S.sb([128, 2048], BF16, "dyt%d" % i) for i in range(2)]
    cnt = [0, 0]

    def load_u(t):
        S.dma('sp', uts[t % 2][:], uT[:, :, t * 512:(t + 1) * 512].rearrange("k p n -> p k n"), W=[uts[t % 2]])

    def proj_tile(sb, tt):
        t = sb * 4 + tt
        ut = uts[t % 2]
        par = sb % 2
        cols = slice(tt * 512, (tt + 1) * 512)
        for which in range(3):
            for k in range(8):
                S.op('pe', lambda p: p.matmul(P0[:, :], wd[:, k, which * 128:(which + 1) * 128], ut[:, k, :], start=(k == 0), stop=(k == 7)), R=[wd, ut], W=[P0])
            if which == 2:
                S.op('act', lambda a: a.activation(VTd[par][:, cols], P0[:, :], AF.Copy), R=[P0], W=[VTd[par]])
                continue
            x = cnt[1] % 2
            cnt[1] += 1
            S.op('act', lambda a: a.activation(raw[x][:], P0[:, :], AF.Copy), R=[P0], W=[raw[x]])
            S.op('act', lambda a: a.activation(sqt[x][:], P0[:, :], AF.Square), R=[P0], W=[sqt[x]])
            S.op('pe', lambda p: p.matmul(P1[:, :], Bd[:], sqt[x][:], start=True, stop=True), R=[Bd, sqt[x]], W=[P1])
            S.op('act', lambda a: a.activation(lnt[x][:], P1[:, :], AF.Ln, scale=1.0 / 64, bias=S.eps_col[:, 0:1]), R=[P1, S.eps_t], W=[lnt[x]])
            S.op('act', lambda a: a.activation(rst[x][:], lnt[x][:], AF.Exp, scale=-0.5), R=[lnt[x]], W=[rst[x]])
            dst = QTd[par] if which == 0 else KTd[par]
            S.op('dve', lambda v: v.scalar_tensor_tensor(dst[:, cols], raw[x][:], gcol[:, which:which + 1], rst[x][:], ALU.mult, ALU.mult), R=[raw[x], gcol, rst[x]], W=[dst])

    def vtrans(sb):
        par = sb % 2
        for ri, r in enumerate(DIL_R):
            for b in range(16):
                n, rho = divmod(b, r)
                c0 = n * 128 * r + rho
                S.op('pe', lambda p: p.transpose(trv[:, b % 4, :], VTd[par][:, sst_(c0, r)], ident_b[:]), R=[VTd[par], ident_b], W=[p_tr])
                if b % 4 == 3:
                    S.op('act', lambda a: a.activation(Vr[par][ri][:, b - 3:b + 1, :, 0:64], trv[:, 0:4, :].rearrange("p a (h d) -> p a h d", h=2), AF.Copy), R=[p_tr], W=[Vr[par][ri]])

    def attention(sb, h):
        par = sb % 2
        hp = slice(h * 64, (h + 1) * 64)
        blocks = []
        for ri, r in enumerate(DIL_R):
            for b in range(16):
                n, rho = divmod(b, r)
                c0 = n * 128 * r + rho
                cur = (par, b, c0)
                if n > 0:
                    prev = (par, b - r, c0 - 128 * r)
                elif sb > 0:
                    nb = 16 // r - 1
                    prev = (1 - par, nb * r + rho, nb * 128 * r + rho)
                else:
                    prev = None
                blocks.append((ri, r, b, c0, cur, prev))
        pairs = [blocks[i:i + 2] for i in range(0, len(blocks), 2)]
        pv_all = []
        for pi, pair in enumerate(pairs):
            for u, (ri, r, b, c0, cur, prev) in enumerate(pair):
                for part, kb in enumerate((cur, prev)):
                    if kb is None:
                        continue
                    base = u * 256 + part * 128
                    lhs = Vr[kb[0]][ri][:, kb[1], h, :]
                    if r == 1:
                        pv_all.append((pi, c0 // 512, slice(c0 % 512, c0 % 512 + 128), lhs, slice(base, base + 128), Vr[kb[0]][ri]))
                    elif r == 4:
                        pv_all.append((pi, c0 // 512, sst_(c0 % 512, 4), lhs, slice(base, base + 128), Vr[kb[0]][ri]))
                    else:
                        for jb in range(4):
                            pv_all.append((pi, jb, sst_(c0, 16, 32), lhs, slice(base + 32 * jb, base + 32 * jb + 32), Vr[kb[0]][ri]))
        first = {}
        last = {}
        for idx, op in enumerate(pv_all):
            first.setdefault(op[1], idx)
            last[op[1]] = idx
        scb = [p_sc, P1]

        def qk_pair(pi):
            psc = scb[pi % 2]
            nmm = 0
            for u, (ri, r, b, c0, cur, prev) in enumerate(pairs[pi]):
                qap = QTd[par][hp, sst_(c0, r)]
                for part, kb in enumerate((cur, prev)):
                    if kb is None:
                        kb = cur
                    kap = KTd[kb[0]][hp, sst_(kb[2], r)]
                    base = u * 256 + part * 128
                    S.op('pe', lambda p: p.matmul(psc[:, base:base + 128], kap, qap, start=(nmm == 0), stop=(nmm == 3)), R=[KTd[kb[0]], QTd[par]], W=[psc])
                    nmm += 1

        idx = 0
        qk_pair(0)
        for pi, pair in enumerate(pairs):
            pt = pts[cnt[0] % 3]
            cnt[0] += 1
            psc = scb[pi % 2]
            if pi + 1 < len(pairs):
                qk_pair(pi + 1)
            S.op('act', lambda a: a.activation(pt[:], psc[:, :], AF.Exp), R=[psc], W=[pt])
            S.op('dve', lambda v: v.tensor_tensor(pt[:], pt[:], M4[:], ALU.mult), R=[pt, M4], W=[pt])
            while idx < len(pv_all) and pv_all[idx][0] == pi:
                _, bk, osl, lhs, psl, vt = pv_all[idx]
                S.op('pe', lambda p: p.matmul(p_acc[bk][0:65, osl], lhs, pt[:, psl], start=(first[bk] == idx), stop=(last[bk] == idx)), R=[vt, pt], W=[p_acc[bk]])
                idx += 1
        yt = yts[sb % 2]
        for jb in range(4):
            osb = o_sb[jb % 2]
            rd = rden[jb % 2]
            S.op('act', lambda a: a.activation(osb[:], p_acc[jb][0:65, :], AF.Copy), R=[p_acc[jb]], W=[osb])
            S.op('pe', lambda p: p.matmul(P1[0:64, :], E65[:], osb[:], start=True, stop=True), R=[E65, osb], W=[P1])
            S.op('dve', lambda v: v.reciprocal(rd[:], P1[0:64, :]), R=[P1], W=[rd])
            S.op('dve', lambda v: v.tensor_tensor(yt[h * 64:(h + 1) * 64, jb * 512:(jb + 1) * 512], osb[0:64, :], rd[:], ALU.mult), R=[osb, rd], W=[yt])

    load_u(0)
    load_u(1)
    for sb in range(n_sb):
        for tt in range(4):
            proj_tile(sb, tt)
            if sb * 4 + tt + 2 < n_sb * 4:
                load_u(sb * 4 + tt + 2)
        vtrans(sb)
        for h in range(2):
            attention(sb, h)
        S.dma('sp', io['ycT'][:, sb * 2048:(sb + 1) * 2048], yts[sb % 2][:], R=[yts[sb % 2]])
    S.end_phase()


def host_consts_dil():
    c = {}
    bd = np.zeros((128, 128), np.float32)
    bd[0:64, 0:64] = 1.0
    bd[64:128, 64:128] = 1.0
    c['c_Bd'] = bd
    p = np.arange(128)[:, None]
    f = np.arange(128)[None, :]
    mc = (p <= f).astype(np.float32)
    mp = (p >= f).astype(np.float32)
    c['c_M4'] = np.concatenate([mc, mp, mc, mp], axis=1).astype(NPBF)
    return c


def prep_dil_vecs(inp, l):
    return {'dil_gcol': np.ascontiguousarray(np.stack([np.tile(inp['dil_q_gain'][l], 2), np.tile(inp['dil_k_gain'][l], 2)], axis=1))}


def emit_mlstm(S, io, c, n_tiles=32):
    S.begin_phase()
    uT = io['uT']
    ident_b, tri_b = c['ident_b'], c['tri_b']
    wm_f = S.sb([128, 8, 386], F32, "wm_f")
    S.dma('sp', wm_f[:], io['WB'][:, 416:802].rearrange("(k p) n -> p k n", p=128), W=[wm_f])
    wm = S.sb([128, 8, 386], BF16, "wm")
    S.op('pool', lambda g: g.tensor_copy(wm[:], wm_f[:]), R=[wm_f], W=[wm])
    cw = S.sb([64, 10], F32, "cw")
    S.dma('sp', cw[:], io['ml_conv'], W=[cw])
    gb = S.sb([1, 2], F32, "gb")
    S.dma('sp', gb[:], io['ml_gate'], W=[gb])
    nbf = S.sb([1, 1], F32, "nbf")
    S.op('dve', lambda v: v.tensor_scalar(nbf[:], gb[0:1, 1:2], -1.0, None, ALU.mult), R=[gb], W=[nbf])
    hg = S.sb([128, 128], F32, "hg")
    S.dma('sp', hg[:], io['ml_hg'].partition_broadcast(128), W=[hg])
    one = S.sb([1, 1], F32, "one")
    S.op('pool', lambda g: g.memset(one[:], 1.0), W=[one])
    ones_r = S.sb([1, 128], F32, "ones_r")
    zeros_r = S.sb([1, 128], F32, "zeros_r")
    S.op('pool', lambda g: g.memset(ones_r[:], 1.0), W=[ones_r])
    S.op('pool', lambda g: g.memset(zeros_r[:], 0.0), W=[zeros_r])
    uts = [S.sb([128, 8, 512], BF16, "mut%d" % i) for i in range(2)]
    xq = [[S.sb([64, 515], F32, "xq%d_%d" % (w, i)) for i in range(2)] for w in range(2)]
    for w in range(2):
        S.op('pool', lambda g: g.memset(xq[w][0][:, 0:3], 0.0), W=[xq[w][0]])
    cv = [S.sb([64, 512], F32, "cv%d" % i) for i in range(2)]
    ex = [S.sb([64, 512], F32, "ex%d" % i) for i in range(2)]
    qkT = [[S.sb([64, 512], BF16, "qkT%d_%d" % (w, i)) for i in range(2)] for w in range(2)]
    Brow = [S.sb([1, 128], F32, "Brow%d" % i) for i in range(2)]
    Grow = [S.sb([1, 128], F32, "Grow%d" % i) for i in range(2)]
    zero1 = S.sb([1, 1], F32, "zero1")
    S.op('pool', lambda g: g.memset(zero1[:], 0.0), W=[zero1])
    t1 = [S.sb([1, 128], F32, "mt1_%d" % i) for i in range(2)]
    t2 = [S.sb([1, 128], F32, "mt2_%d" % i) for i in range(2)]
    arow = [S.sb([1, 128], F32, "arow%d" % i) for i in range(2)]
    bg = [S.sb([1, 128], F32, "bg%d" % i) for i in range(2)]
    ngp = [S.sb([1, 1], F32, "ngp%d" % i) for i in range(2)]
    rows3 = [S.sb([1, 3, 128], F32, "rows3_%d" % i) for i in range(2)]
    cols = [S.sb([128, 4], F32, "cols%d" % i) for i in range(5)]
    ones_c = S.sb([128, 4], F32, "ones_c")
    S.op('pool', lambda g: g.memset(ones_c[:], 1.0), W=[ones_c])
    Vp = [S.sb([128, 129], BF16, "Vp%d" % i) for i in range(2)]
    so = [S.sb([128, 128], F32, "so%d" % i) for i in range(3)]
    ktok = [S.sb([128, 64], BF16, "ktok%d" % i) for i in range(2)]
    scm = [S.sb([128, 128], BF16, "scm%d" % i) for i in range(2)]
    Dst = S.sb([64, 129], F32, "Dst")
    Cb = S.sb([64, 129], BF16, "Cb")
    S.op('pool', lambda g: g.memset(Dst[:], 0.0), W=[Dst])
    sm = [[S.sb([128, 1], F32, "sm%d_%d" % (j, i)) for j in range(6)] for i in range(2)]
    hh = [S.sb([128, 128], F32, "hh%d" % i) for i in range(2)]
    hj = [S.sb([128, 128], F32, "hj%d" % i) for i in range(2)]
    y1 = [S.sb([128, 128], F32, "y1_%d" % i) for i in range(2)]
    y2 = [S.sb([128, 128], BF16, "y2_%d" % i) for i in range(2)]
    ybt = [S.sb([128, 512], BF16, "ybt%d" % i) for i in range(2)]
    P_qk = bank(S, "mP_qk")
    P_vo = [P_qk]
    P_gc = bank(S, "mP_gc")
    P_s = bank(S, "mP_s")
    P_u = bank(S, "mP_u")
    P_h = [bank(S, "mP_h%d" % i) for i in range(2)]
    p_trb = bank(S, "mp_trb", BF16)
    p_trk = p_trb
    p_try = p_trb
    P_gr = bank(S, "mP_gr")

    def load_u(t):
        S.dma('sp', uts[t % 2][:], uT[:, :, t * 512:(t + 1) * 512].rearrange("k p n -> p k n"), W=[uts[t % 2]])

    def qk_tile(t):
        ut = uts[t % 2]
        for w in range(2):
            xb = xq[w][t % 2]
            for k in range(8):
                S.op('pe', lambda p: p.matmul(P_qk[0:64, :], wm[:, k, w * 64:(w + 1) * 64], ut[:, k, :], start=(k == 0), stop=(k == 7)), R=[wm, ut], W=[P_qk])
            if t > 0:
                S.op('pool', lambda g: g.tensor_copy(xb[:, 0:3], xq[w][(t - 1) % 2][:, 512:515]), R=[xq[w][(t - 1) % 2]], W=[xb])
            S.op('act', lambda a: a.activation(xb[:, 3:515], P_qk[0:64, :], AF.Copy), R=[P_qk], W=[xb])
            o = w * 5
            cvt = cv[w]
            S.op('dve', lambda v: v.tensor_scalar(cvt[:], xb[:, 3:515], cw[:, o + 3:o + 4], cw[:, o + 4:o + 5], ALU.mult, ALU.add), R=[xb, cw], W=[cvt])
            for j in (2, 1, 0):
                S.op('dve', lambda v: v.scalar_tensor_tensor(cvt[:], xb[:, j:j + 512], cw[:, o + j:o + j + 1], cvt[:], ALU.mult, ALU.add), R=[xb, cw, cvt], W=[cvt])
            ext = ex[w]
            S.op('act', lambda a: a.activation(ext[:], cvt[:], AF.Exp, scale=-1.0), R=[cvt], W=[ext])
            S.op('act', lambda a: a.activation(ext[:], ext[:], AF.Ln, bias=ones_c[0:64, 0:1]), R=[ext, ones_c], W=[ext])
            S.op('act', lambda a: a.activation(ext[:], ext[:], AF.Exp, scale=-1.0), R=[ext], W=[ext])
            S.op('dve', lambda v: v.scalar_tensor_tensor(qkT[w][t % 2][:], cvt[:], (0.125 if w == 0 else 1.0), ext[:], ALU.mult, ALU.mult), R=[cvt, ext], W=[qkT[w][t % 2]])

    def gates_a(g):
        t, cc = divmod(g, 4)
        ut = uts[t % 2]
        x = g % 2
        csl = slice(cc * 128, (cc + 1) * 128)
        if g % 2 == 0:
            hsl = slice(cc * 128, cc * 128 + 256)
            for w in range(2):
                for k in range(8):
                    S.op('pe', lambda p: p.matmul(P_gr[0:1, w * 256:(w + 1) * 256], wm[:, k, 384 + w:385 + w], ut[:, k, hsl], start=(k == 0), stop=(k == 7)), R=[wm, ut], W=[P_gr])
            yield
        go = (g % 2) * 128
        S.op('act', lambda a: a.activation(t1[x][:], P_gr[0:1, 256 + go:256 + go + 128], AF.Exp, scale=-1.0, bias=nbf[0:1, 0:1]), R=[P_gr, nbf], W=[t1[x]])
        S.op('act', lambda a: a.activation(t2[x][:], t1[x][:], AF.Ln, bias=one[0:1, 0:1]), R=[t1[x], one], W=[t2[x]])
        yield
        bprev = Brow[1 - x][0:1, 127:128] if g > 0 else zero1[0:1, 0:1]
        gprev = Grow[1 - x][0:1, 127:128] if g > 0 else zero1[0:1, 0:1]
        prevB = [Brow[1 - x]] if g > 0 else [zero1]
        prevG = [Grow[1 - x]] if g > 0 else [zero1]
        S.op('dve', lambda v: v.tensor_tensor_scan(Brow[x][:], ones_r[:], t2[x][:], bprev, ALU.mult, ALU.subtract), R=[ones_r, t2[x]] + prevB, W=[Brow[x]])
        S.op('dve', lambda v: v.scalar_tensor_tensor(arow[x][:], P_gr[0:1, go:go + 128], gb[0:1, 0:1], Brow[x][:], ALU.add, ALU.subtract), R=[P_gr, gb, Brow[x]], W=[arow[x]])
        S.op('dve', lambda v: v.tensor_tensor_scan(Grow[x][:], zeros_r[:], arow[x][:], gprev, ALU.add, ALU.max), R=[zeros_r, arow[x]] + prevG, W=[Grow[x]])
        S.op('dve', lambda v: v.tensor_scalar(ngp[x][:], gprev, -1.0, None, ALU.mult), R=prevG, W=[ngp[x]])
        S.op('dve', lambda v: v.tensor_tensor(bg[x][:], Brow[x][:], Grow[x][:], ALU.add), R=[Brow[x], Grow[x]], W=[bg[x]])
        yield
        S.op('act', lambda a: a.activation(rows3[x][0:1, 0, :], arow[x][:], AF.Exp, bias=ngp[x][0:1, 0:1]), R=[arow[x], ngp[x]], W=[rows3[x]])
        S.op('act', lambda a: a.activation(rows3[x][0:1, 1, :], Grow[x][:], AF.Exp, scale=-1.0, bias=gprev), R=[Grow[x]] + prevG, W=[rows3[x]])
        S.op('act', lambda a: a.activation(rows3[x][0:1, 2, :], bg[x][:], AF.Exp, scale=-1.0), R=[bg[x]], W=[rows3[x]])
        yield
        cl = cols[g % 5]
        for j in range(3):
            S.op('pe', lambda p: p.matmul(P_gc[:, 256 + j:257 + j], rows3[x][0:1, j, :], one[0:1, 0:1], start=(j == 0), stop=False), R=[rows3[x], one], W=[P_gc])
        S.op('pe', lambda p: p.matmul(P_gc[:, 259:260], rows3[x][0:1, 1, 127:128].to_broadcast([1, 128]), one[0:1, 0:1], start=False, stop=True), R=[rows3[x], one], W=[P_gc])
        yield
        S.op('act', lambda a: a.activation(cl[:], P_gc[:, 256:260], AF.Copy), R=[P_gc], W=[cl])
        yield

    def stageA(g):
        t, cc = divmod(g, 4)
        if cc == 0:
            qk_tile(t)
            yield
        ut = uts[t % 2]
        x = g % 2
        x3 = g % 3
        csl = slice(cc * 128, (cc + 1) * 128)
        cl = cols[g % 5]
        qT = qkT[0][t % 2]
        kT = qkT[1][t % 2]
        pvo = P_vo[0]
        for k in range(8):
            S.op('pe', lambda p: p.matmul(pvo[:, 0:256], ut[:, k, csl], wm[:, k, 128:384], start=(k == 0), stop=(k == 7)), R=[ut, wm], W=[pvo])
        S.op('pe', lambda p: p.transpose(p_trb[:, 0:64], kT[:, csl], ident_b[0:64, 0:64]), R=[kT, ident_b], W=[p_trb])
        S.op('pe', lambda p: p.matmul(P_s[:, 0:128], kT[:, csl], qT[:, csl], start=True, stop=True), R=[kT, qT], W=[P_s])
        yield
        S.op('dve', lambda v: v.tensor_scalar(Vp[x][:, 0:128], pvo[:, 0:128], cl[:, 0:1], None, ALU.mult), R=[pvo, cl], W=[Vp[x]])
        S.op('act', lambda a: a.activation(Vp[x][:, 128:129], cl[:, 0:1], AF.Copy), R=[cl], W=[Vp[x]])
        S.op('act', lambda a: a.activation(so[x3][:], pvo[:, 128:256], AF.Exp, scale=-1.0), R=[pvo], W=[so[x3]])
        S.op('act', lambda a: a.activation(ktok[x][:], p_trb[:, 0:64], AF.Copy), R=[p_trb], W=[ktok[x]])
        S.op('dve', lambda v: v.tensor_tensor(scm[x][:], P_s[:, 0:128], tri_b[:], ALU.mult), R=[P_s, tri_b], W=[scm[x]])
        yield
        S.op('act', lambda a: a.activation(so[x3][:], so[x3][:], AF.Ln, bias=ones_c[:, 0:1]), R=[so[x3], ones_c], W=[so[x3]])
        S.op('act', lambda a: a.activation(so[x3][:], so[x3][:], AF.Exp, scale=-1.0), R=[so[x3]], W=[so[x3]])
        yield

    def stageB(g):
        t, cc = divmod(g, 4)
        x = g % 2
        csl = slice(cc * 128, (cc + 1) * 128)
        clp = cols[(g - 1) % 5] if g > 0 else ones_c
        qT = qkT[0][t % 2]
        ph = P_h[x]
        S.op('dve', lambda v: v.tensor_scalar(Cb[:], Dst[:], clp[0:64, 3:4], None, ALU.mult), R=[Dst, clp], W=[Cb])
        S.op('pe', lambda p: p.matmul(P_u[0:64, 0:129], ktok[x][:], Vp[x][:], start=True, stop=True), R=[ktok[x], Vp[x]], W=[P_u])
        yield
        S.op('pe', lambda p: p.matmul(ph[:, 0:129], qT[:, csl], Cb[:], start=True, stop=False), R=[qT, Cb], W=[ph])
        S.op('pe', lambda p: p.matmul(ph[:, 0:129], scm[x][:], Vp[x][:], start=False, stop=True), R=[scm[x], Vp[x]], W=[ph])
        S.op('dve', lambda v: v.scalar_tensor_tensor(Dst[:], Dst[:], clp[0:64, 3:4], P_u[0:64, 0:129], ALU.mult, ALU.add), R=[Dst, clp, P_u], W=[Dst])
        yield

    def stageC(g):
        t, cc = divmod(g, 4)
        x = g % 2
        x3 = g % 3
        csl = slice(cc * 128, (cc + 1) * 128)
        cl = cols[g % 5]
        ph = P_h[x]
        ta, tb, rr, r2, ssq, rs = sm[x]
        S.op('act', lambda a: a.activation(ta[:], ph[:, 128:129], AF.Abs), R=[ph], W=[ta])
        yield
        S.op('dve', lambda v: v.scalar_tensor_tensor(tb[:], ta[:], cl[:, 1:2], cl[:, 2:3], ALU.mult, ALU.max), R=[ta, cl], W=[tb])
        S.op('dve', lambda v: v.reciprocal(rr[:], tb[:]), R=[tb], W=[rr])
        S.op('dve', lambda v: v.tensor_tensor(r2[:], rr[:], cl[:, 1:2], ALU.mult), R=[rr, cl], W=[r2])
        S.op('dve', lambda v: v.tensor_scalar(hh[x][:], ph[:, 0:128], r2[:, 0:1], None, ALU.mult), R=[ph, r2], W=[hh[x]])
        yield
        S.op('act', lambda a: a.activation(hj[x][:], hh[x][:], AF.Square, accum_out=ssq[:, 0:1]), R=[hh[x]], W=[hj[x], ssq])
        S.op('act', lambda a: a.activation(ssq[:], ssq[:], AF.Ln, scale=1.0 / 128, bias=S.eps_col[:, 0:1]), R=[ssq, S.eps_t], W=[ssq])
        S.op('act', lambda a: a.activation(rs[:], ssq[:], AF.Exp, scale=-0.5), R=[ssq], W=[rs])
        yield
        S.op('dve', lambda v: v.scalar_tensor_tensor(y1[x][:], hh[x][:], rs[:, 0:1], hg[:], ALU.mult, ALU.mult), R=[hh[x], rs, hg], W=[y1[x]])
        S.op('dve', lambda v: v.tensor_tensor(y2[x][:], y1[x][:], so[x3][:], ALU.mult), R=[y1[x], so[x3]], W=[y2[x]])
        yield
        S.op('pe', lambda p: p.transpose(p_trb[:, 512:640], y2[x][:], ident_b[:]), R=[y2[x], ident_b], W=[p_trb])
        yield
        S.op('act', lambda a: a.activation(ybt[t % 2][:, csl], p_trb[:, 512:640], AF.Copy), R=[p_trb], W=[ybt[t % 2]])
        if cc == 3:
            S.dma('sp', io['ybT'][:, t * 512:(t + 1) * 512], ybt[t % 2][:], R=[ybt[t % 2]])
        yield

    n_ch = n_tiles * 4
    load_u(0)
    if n_tiles > 1:
        load_u(1)
    for g0 in range(min(2, n_ch)):
        for _ in gates_a(g0):
            pass
    for _ in stageA(0):
        pass
    for it in range(n_ch + 1):
        gens = []
        if it + 2 < n_ch:
            gens.append(gates_a(it + 2))
        if it + 1 < n_ch:
            gens.append(stageA(it + 1))
        if it < n_ch:
            gens.append(stageB(it))
        if it >= 1:
            gens.append(stageC(it - 1))
        while gens:
            for gnr in list(gens):
                try:
                    next(gnr)
                except StopIteration:
                    gens.remove(gnr)
        t, cc = divmod(it, 4)
        if it < n_ch and cc == 3 and t + 2 < n_tiles:
            load_u(t + 2)
    S.end_phase()


def prep_mlstm_vecs(inp, l, j):
    cwq = inp['mlstm_conv_w'][l][:, j * 64:(j + 1) * 64].T
    cbq = inp['mlstm_conv_b'][l][j * 64:(j + 1) * 64][:, None]
    cwk = inp['mlstm_conv_w'][l][:, 256 + j * 64:256 + (j + 1) * 64].T
    cbk = inp['mlstm_conv_b'][l][256 + j * 64:256 + (j + 1) * 64][:, None]
    d = {'ml_conv': np.ascontiguousarray(np.concatenate([cwq, cbq, cwk, cbk], axis=1))}
    d['ml_gate'] = np.ascontiguousarray(np.array([[inp['mlstm_b_i'][l][j], inp['mlstm_b_f'][l][j]]], np.float32))
    d['ml_hg'] = np.ascontiguousarray(inp['mlstm_head_gain'][l][j * 128:(j + 1) * 128][None, :])
    return d


def emit_mod(S, io):
    S.begin_phase()
    cc = S.sb([128, 8], F32, "cc")
    S.dma('sp', cc[:], io['c_col'], W=[cc])
    ca = S.sb([128, 8], F32, "ca")
    S.op('act', lambda a: a.activation(ca[:], cc[:], AF.Silu), R=[cc], W=[ca])
    brow = S.sb([1, 6144], F32, "brow")
    S.dma('sp', brow[:], io['b_ada'], W=[brow])
    orow = S.sb([1, 6144], F32, "orow")
    wt = [S.sb([128, 8, 512], F32, "wada%d" % i) for i in range(2)]
    pm = [bank(S, "pm%d" % i) for i in range(2)]
    for gi in range(12):
        w = wt[gi % 2]
        S.dma('sp', w[:], io['w_ada'][:, gi * 512:(gi + 1) * 512].rearrange("(k p) n -> p k n", p=128), W=[w])
        p = pm[gi % 2]
        for k in range(8):
            S.op('pe', lambda pe: pe.matmul(p[0:1, :], ca[:, k:k + 1], w[:, k, :], start=(k == 0), stop=(k == 7)), R=[ca, w], W=[p])
        S.op('dve', lambda v: v.tensor_tensor(orow[0:1, gi * 512:(gi + 1) * 512], p[0:1, :], brow[0:1, gi * 512:(gi + 1) * 512], ALU.add), R=[p, brow], W=[orow])
    S.dma('sp', io['mod_out'], orow[:], R=[orow])
    S.end_phase()


def norm_mod(S, xt, Acol, Bcol, outs, sq, ones_f, pss, lnt, rst, tmp, R_extra, do_sq=True):
    n = xt.ap.shape[2]
    if do_sq:
        S.op('act', lambda a: a.activation(sq[:], xt[:], AF.Square), R=[xt], W=[sq])
    for k in range(8):
        S.op('pe', lambda p: p.matmul(pss[:, 0:n], ones_f[:], sq[:, k, :], start=(k == 0), stop=(k == 7)), R=[ones_f, sq], W=[pss])
    S.op('act', lambda a: a.activation(lnt[:], pss[:, 0:n], AF.Ln, scale=1.0 / D, bias=S.eps_col[:, 0:1]), R=[pss, S.eps_t], W=[lnt])
    S.op('act', lambda a: a.activation(rst[:], lnt[:], AF.Exp, scale=-0.5), R=[lnt], W=[rst])
    for k in range(8):
        t = tmp[k % len(tmp)]
        S.op('dve', lambda v: v.scalar_tensor_tensor(t[:], xt[:, k, :], Acol[:, k:k + 1], rst[:], ALU.mult, ALU.mult), R=[xt, rst] + R_extra, W=[t])
        for o in outs:
            S.op('act', lambda a: a.activation(o[:, k, :], t[:], AF.Identity, bias=Bcol[:, k:k + 1]), R=[t] + R_extra, W=[o])


def scol(S, name, ap_dram, shape):
    t = S.sb(list(shape), F32, name)
    S.dma('sp', t[:], ap_dram, W=[t])
    return t


def emit_C1a(S, io, c, n_tiles=8):
    S.begin_phase()
    NTL = 512
    Wg = S.sb([128, 8, 3072], BF16, "Wg")
    Wbr = [S.sb([128, 4, 1024], BF16, "Wbr%d" % i) for i in range(3)]
    stg = [S.sb([128, 8, 512], F32, "stgA%d" % i) for i in range(2)]
    for gi in range(6):
        st = stg[gi % 2]
        S.dma('sp', st[:], io['w_gate'][:, gi * 512:(gi + 1) * 512].rearrange("(k p) n -> p k n", p=128), W=[st])
        S.op('pool', lambda g: g.tensor_copy(Wg[:, :, gi * 512:(gi + 1) * 512], st[:]), R=[st], W=[Wg])
    for br in range(3):
        for hh_ in range(2):
            st = stg[(br * 2 + hh_) % 2]
            stv = st[:].rearrange("p (a k) n -> p a k n", a=2)[:, 0, :, :]
            S.dma('sp', stv, io['w_br'][br, :, hh_ * 512:(hh_ + 1) * 512].rearrange("(k p) n -> p k n", p=128), W=[st])
            S.op('pool', lambda g: g.tensor_copy(Wbr[br][:, :, hh_ * 512:(hh_ + 1) * 512], stv), R=[st], W=[Wbr[br]])
    uts = [S.sb([128, 8, NTL], BF16, "cut%d" % i) for i in range(2)]
    yts = [[S.sb([128, 4, NTL], BF16, "cyt%d_%d" % (br, i)) for i in range(2)] for br in range(3)]
    mgs = [S.sb([128, 8, NTL], BF16, "mg%d" % i) for i in range(2)]
    eg = [S.sb([128, NTL], F32, "eg%d" % i) for i in range(3)]
    mm = [S.sb([128, NTL], F32, "mm%d" % i) for i in range(2)]
    tt = [S.sb([128, NTL], F32, "tt%d" % i) for i in range(2)]
    PG = [bank(S, "PG%d" % i) for i in range(3)]
    PB = [bank(S, "PB%d" % i) for i in range(3)]

    def load(t):
        sl = slice(t * NTL, (t + 1) * NTL)
        S.dma('sp', uts[t % 2][:], io['uT_loc'][:, :, sl].rearrange("k p n -> p k n"), W=[uts[t % 2]])
        for br in range(3):
            S.dma('sp', yts[br][t % 2][:], io['yT'][br, :, :, sl].rearrange("k p n -> p k n"), W=[yts[br][t % 2]])

    load(0)
    for t in range(n_tiles):
        if t + 1 < n_tiles:
            load(t + 1)
        ut = uts[t % 2]
        mg = mgs[t % 2]
        for dc in range(8):
            for br in range(3):
                for k in range(8):
                    S.op('pe', lambda p: p.matmul(PG[br][:, :], Wg[:, k, br * 1024 + dc * 128:br * 1024 + (dc + 1) * 128], ut[:, k, :], start=(k == 0), stop=(k == 7)), R=[Wg, ut], W=[PG[br]])
                S.op('act', lambda a: a.activation(eg[br][:], PG[br][:, :], AF.Sigmoid), R=[PG[br]], W=[eg[br]])
            for br in range(3):
                yt = yts[br][t % 2]
                for k in range(4):
                    S.op('pe', lambda p: p.matmul(PB[br][:, :], Wbr[br][:, k, dc * 128:(dc + 1) * 128], yt[:, k, :], start=(k == 0), stop=(k == 3)), R=[Wbr[br], yt], W=[PB[br]])
            m = mm[dc % 2]
            S.op('dve', lambda v: v.tensor_tensor(m[:], PB[0][:, :], eg[0][:], ALU.mult), R=[PB[0], eg[0]], W=[m])
            for br in (1, 2):
                t1 = tt[br % 2]
                S.op('dve', lambda v: v.tensor_tensor(t1[:], PB[br][:, :], eg[br][:], ALU.mult), R=[PB[br], eg[br]], W=[t1])
                if br == 1:
                    S.op('pool', lambda g: g.tensor_tensor(m[:], m[:], t1[:], ALU.add), R=[m, t1], W=[m])
                else:
                    S.op('pool', lambda g: g.tensor_tensor(mg[:, dc, :], m[:], t1[:], ALU.add), R=[m, t1], W=[mg])
        S.dma('sp', io['mgT'][:, :, t * NTL:(t + 1) * NTL].rearrange("k p n -> p k n"), mg[:], R=[mg])
    S.end_phase()


def emit_C1b(S, io, c, n_tiles=8):
    S.begin_phase()
    NTL = 512
    ones_f, ident_f = c['ones_f'], c['ident_f']
    Wo = S.sb([128, 8, 1024], BF16, "Wo")
    stg = [S.sb([128, 8, 512], F32, "stgB%d" % i) for i in range(2)]
    for gi in range(2):
        st = stg[gi]
        S.dma('sp', st[:], io['w_out'][:, gi * 512:(gi + 1) * 512].rearrange("(k p) n -> p k n", p=128), W=[st])
        S.op('pool', lambda g: g.tensor_copy(Wo[:, :, gi * 512:(gi + 1) * 512], st[:]), R=[st], W=[Wo])
    Wr = S.sb([128, 8, 20], F32, "Wr")
    S.dma('sp', Wr[:], io['w_router'].rearrange("(k p) n -> p k n", p=128), W=[Wr])
    rb = S.sb([128, 20], F32, "rb")
    S.dma('sp', rb[:], io['b_router'].partition_broadcast(128), W=[rb])
    modc = scol(S, "modc", io['modc'], [128, 48])
    fng = scol(S, "fng", io['ffn_norm_col'], [128, 8])
    Af = S.sb([128, 8], F32, "Af")
    S.op('dve', lambda v: v.scalar_tensor_tensor(Af[:], modc[:, 32:40], 1.0, fng[:], ALU.add, ALU.mult), R=[modc, fng], W=[Af])
    xts = [S.sb([128, 8, NTL], F32, "xt%d" % i) for i in range(2)]
    mgs = [S.sb([128, 8, NTL], BF16, "bmg%d" % i) for i in range(2)]
    sq = S.sb([128, 8, NTL], F32, "bsq")
    hf32s = [S.sb([128, 8, NTL], F32, "hf32_%d" % i) for i in range(2)]
    hfb = [S.sb([128, 8, NTL], BF16, "hfb%d" % i) for i in range(2)]
    lnt = S.sb([128, NTL], F32, "blnt")
    rst = S.sb([128, NTL], F32, "brst")
    tmp = [S.sb([128, NTL], F32, "btmp%d" % i) for i in range(2)]
    cwT = [S.sb([16, NTL], F32, "cwT%d" % i) for i in range(2)]
    PO = [bank(S, "PO%d" % i) for i in range(2)]
    PSS = bank(S, "PSS")
    PR = bank(S, "PR")
    PT = bank(S, "PT")
    def rt(nm, shp):
        return [S.sb(shp, F32, "%s%d" % (nm, i)) for i in range(2)]
    rb4 = S.sb([128, 4, 20], F32, "rb4")
    for s_ in range(4):
        S.op('pool', lambda g: g.tensor_copy(rb4[:, s_, :], rb[:]), R=[rb], W=[rb4])
    lgb = rt("lgb", [128, 4, 20]); gmax = rt("gmax", [128, 4]); oh = rt("oh", [128, 4, 4]); gs = rt("gs", [128, 4, 4])
    sume = rt("sume", [128, 4]); psel = rt("psel", [128, 4]); m1 = rt("m1", [128, 4, 4])
    is1 = rt("is1", [128, 4, 4, 4]); E2 = rt("E2", [128, 4, 4, 4]); m2 = rt("m2", [128, 4, 4]); sel = rt("sel", [128, 4, 4, 4])
    exx = rt("exx", [128, 4, 4, 4]); den = rt("den", [128, 4, 4]); fac = rt("fac", [128, 4, 4]); cw = rt("cw", [128, 4, 4, 4])

    def load(t):
        sl = slice(t * NTL, (t + 1) * NTL)
        S.dma('sp', xts[t % 2][:], io['xT'][:, :, sl].rearrange("k p n -> p k n"), W=[xts[t % 2]])
        S.dma('sp', mgs[t % 2][:], io['mgT'][:, :, sl].rearrange("k p n -> p k n"), W=[mgs[t % 2]])

    def bc4(ap):
        return ap.unsqueeze(2).to_broadcast([128, 4, 4])

    def stage1(t):
        xt = xts[t % 2]
        mg = mgs[t % 2]
        sl = slice(t * NTL, (t + 1) * NTL)
        for dc in range(8):
            po = PO[dc % 2]
            for k in range(8):
                S.op('pe', lambda p: p.matmul(po[:, :], Wo[:, k, dc * 128:(dc + 1) * 128], mg[:, k, :], start=(k == 0), stop=(k == 7)), R=[Wo, mg], W=[po])
            S.op('dve', lambda v: v.scalar_tensor_tensor(xt[:, dc, :], po[:, :], modc[:, 16 + dc:17 + dc], xt[:, dc, :], ALU.mult, ALU.add), R=[po, modc, xt], W=[xt])
        S.dma('sp', io['x1T'][:, :, sl].rearrange("k p n -> p k n"), xt[:], R=[xt])

    def stage2(t):
        xt = xts[t % 2]
        sl = slice(t * NTL, (t + 1) * NTL)
        hb = hfb[t % 2]
        hf32 = hf32s[t % 2]
        norm_mod(S, xt, Af[:], modc[:, 24:32], [hb, hf32], sq, ones_f, PSS, lnt, rst, tmp, [Af, modc])
        S.dma('sp', io['hfT'][:, :, sl].rearrange("k p n -> p k n"), hb[:], R=[hb])

    def stage2b(t):
        sl = slice(t * NTL, (t + 1) * NTL)
        hf32 = hf32s[t % 2]
        ct = cwT[t % 2]
        x = t % 2
        for s_ in range(4):
            for k in range(8):
                S.op('pe', lambda p: p.matmul(PR[:, s_ * 20:(s_ + 1) * 20], hf32[:, k, s_ * 128:(s_ + 1) * 128], Wr[:, k, :], start=(k == 0), stop=(k == 7)), R=[hf32, Wr], W=[PR])
        S.op('dve', lambda v: v.tensor_tensor(lgb[x][:], PR[:, 0:80].rearrange("p (s n) -> p s n", s=4), rb4[:], ALU.add), R=[PR, rb4], W=[lgb[x]])
        G = lgb[x][:, :, 0:4]
        E = lgb[x][:, :, 4:20].rearrange("p s (g e) -> p s g e", g=4)

        def b3(ap):
            return ap.unsqueeze(2).to_broadcast([128, 4, 4])

        def b4(ap):
            return ap.unsqueeze(3).to_broadcast([128, 4, 4, 4])

        S.op('dve', lambda v: v.tensor_reduce(gmax[x][:], G, AX.X, ALU.max), R=[lgb[x]], W=[gmax[x]])
        S.op('dve', lambda v: v.tensor_tensor(oh[x][:], G, b3(gmax[x][:]), ALU.is_equal), R=[lgb[x], gmax[x]], W=[oh[x]])
        S.op('dve', lambda v: v.tensor_tensor(gs[x][:], G, b3(gmax[x][:]), ALU.subtract), R=[lgb[x], gmax[x]], W=[gs[x]])
        S.op('act', lambda a: a.activation(gs[x][:], gs[x][:], AF.Exp), R=[gs[x]], W=[gs[x]])
        S.op('dve', lambda v: v.tensor_reduce(sume[x][:], gs[x][:], AX.X, ALU.add), R=[gs[x]], W=[sume[x]])
        S.op('dve', lambda v: v.reciprocal(psel[x][:], sume[x][:]), R=[sume[x]], W=[psel[x]])
        S.op('dve', lambda v: v.tensor_reduce(m1[x][:], E, AX.X, ALU.max), R=[lgb[x]], W=[m1[x]])
        S.op('dve', lambda v: v.tensor_tensor(is1[x][:], E, b4(m1[x][:]), ALU.is_equal), R=[lgb[x], m1[x]], W=[is1[x]])
        S.op('dve', lambda v: v.scalar_tensor_tensor(E2[x][:], is1[x][:], -1e30, E, ALU.mult, ALU.add), R=[is1[x], lgb[x]], W=[E2[x]])
        S.op('dve', lambda v: v.tensor_reduce(m2[x][:], E2[x][:], AX.X, ALU.max), R=[E2[x]], W=[m2[x]])
        S.op('dve', lambda v: v.tensor_tensor(sel[x][:], E, b4(m2[x][:]), ALU.is_ge), R=[lgb[x], m2[x]], W=[sel[x]])
        S.op('dve', lambda v: v.tensor_tensor(exx[x][:], E, b4(m1[x][:]), ALU.subtract), R=[lgb[x], m1[x]], W=[exx[x]])
        S.op('act', lambda a: a.activation(exx[x][:], exx[x][:], AF.Exp), R=[exx[x]], W=[exx[x]])
        S.op('dve', lambda v: v.tensor_tensor(exx[x][:], exx[x][:], sel[x][:], ALU.mult), R=[exx[x], sel[x]], W=[exx[x]])
        S.op('dve', lambda v: v.tensor_reduce(den[x][:], exx[x][:], AX.X, ALU.add), R=[exx[x]], W=[den[x]])
        S.op('dve', lambda v: v.reciprocal(den[x][:], den[x][:]), R=[den[x]], W=[den[x]])
        S.op('dve', lambda v: v.tensor_tensor(fac[x][:], den[x][:], oh[x][:], ALU.mult), R=[den[x], oh[x]], W=[fac[x]])
        S.op('dve', lambda v: v.tensor_tensor(fac[x][:], fac[x][:], b3(psel[x][:]), ALU.mult), R=[fac[x], psel[x]], W=[fac[x]])
        S.op('dve', lambda v: v.tensor_tensor(cw[x][:], exx[x][:], b4(fac[x][:]), ALU.mult), R=[exx[x], fac[x]], W=[cw[x]])
        for s_ in range(4):
            S.op('pe', lambda p: p.transpose(PT[0:16, s_ * 128:(s_ + 1) * 128], cw[x][:, s_, :, :].rearrange("p g e -> p (g e)"), ident_f[:]), R=[cw[x], ident_f], W=[PT])
        S.op('act', lambda a: a.activation(ct[:], PT[0:16, :], AF.Copy), R=[PT], W=[ct])
        S.dma('sp', io['cwT'][:, sl], ct[:], R=[ct])

    load(0)
    if n_tiles > 1:
        load(1)
    stage1(0)
    for t in range(n_tiles + 1):
        if t + 1 < n_tiles:
            stage1(t + 1)
        if t < n_tiles:
            stage2(t)
        if t >= 1:
            stage2b(t - 1)
        if t + 2 < n_tiles:
            load(t + 2)
    S.end_phase()


def emit_C2(S, io, c, last_layer, n_half=2):
    S.begin_phase()
    NTL = 512
    NE = 256
    HT = 2048
    ones_f = c['ones_f']
    modc = scol(S, "modc2", io['modc'], [128, 48])
    ident_f = c['ident_f']
    if not last_layer:
        modn = scol(S, "modn", io['modn'], [128, 16])
        ang = scol(S, "ang", io['attn_norm_col'], [128, 8])
        Aa = S.sb([128, 8], F32, "Aa")
        S.op('dve', lambda v: v.scalar_tensor_tensor(Aa[:], modn[:, 8:16], 1.0, ang[:], ALU.add, ALU.mult), R=[modn, ang], W=[Aa])
    hf = S.sb([128, 8, HT], BF16, "hfh")
    cwt = S.sb([16, HT], F32, "cwh")
    acc = S.sb([128, 8, HT], F32, "macc")
    acc_t = [[S.sub(acc[:, dc, tl * NTL:(tl + 1) * NTL], "acc%d_%d" % (dc, tl)) for tl in range(4)] for dc in range(8)]
    stg = [S.sb([128, 8, 256], F32, "stgC%d" % i) for i in range(2)]
    Wge = [S.sb([128, 8, 256], BF16, "Wge%d" % i) for i in range(2)]
    Wue = [S.sb([128, 8, 256], BF16, "Wue%d" % i) for i in range(2)]
    Wde = [S.sb([128, 2, 1024], BF16, "Wde%d" % i) for i in range(2)]
    cwb = [S.sb([128, NTL], F32, "cwb%d" % i) for i in range(2)]
    sg = [S.sb([128, NTL], F32, "sg%d" % i) for i in range(2)]
    h1 = [S.sb([128, NTL], F32, "h1_%d" % i) for i in range(2)]
    hid = [S.sb([128, 2, NTL], BF16, "hid%d" % i) for i in range(2)]
    xts = [S.sb([128, 8, NE], F32, "c2xt%d" % i) for i in range(2)]
    sqs = [S.sb([128, 8, NE], F32, "c2sq%d" % i) for i in range(2)]
    ub = S.sb([128, 8, NE], BF16, "c2ub")
    lnt = S.sb([128, NE], F32, "c2ln")
    rst = S.sb([128, NE], F32, "c2rs")
    tmp = [S.sb([128, NE], F32, "c2tmp%d" % i) for i in range(2)]
    PGU = [bank(S, "PGU%d" % i) for i in range(4)]
    PD = [bank(S, "PD%d" % i) for i in range(2)]
    PCW = bank(S, "PCW")
    PSS = bank(S, "PSS2")
    nst = [0]

    def load_expert(e, slot):
        for (dst, src) in ((Wge[slot], io['w_eg'][e]), (Wue[slot], io['w_eu'][e])):
            st = stg[nst[0] % 2]
            nst[0] += 1
            S.dma('sp', st[:], src.rearrange("(k p) n -> p k n", p=128), W=[st])
            S.op('pool', lambda g: g.tensor_copy(dst[:], st[:]), R=[st], W=[dst])
        st = stg[nst[0] % 2]
        nst[0] += 1
        stv = st[:].rearrange("p (a k) n -> p a (k n)", a=2)
        S.dma('sp', stv, io['w_ed'][e].rearrange("(k p) n -> p k n", p=128), W=[st])
        S.op('pool', lambda g: g.tensor_copy(Wde[slot][:], stv), R=[st], W=[Wde[slot]])

    nx = [0]

    def epi_res(half, tl):
        h0 = half * HT
        gsl = slice(h0 + tl * NE, h0 + (tl + 1) * NE)
        tsl = slice(tl * NE, (tl + 1) * NE)
        xt = xts[tl % 2]
        ats = [acc_t[dc][(tl * NE) // NTL] for dc in range(8)]
        S.dma('sp', xt[:], io['x1T'][:, :, gsl].rearrange("k p n -> p k n"), W=[xt])
        for dc in range(8):
            S.op('dve', lambda v: v.scalar_tensor_tensor(xt[:, dc, :], acc[:, dc, tsl], modc[:, 40 + dc:41 + dc], xt[:, dc, :], ALU.mult, ALU.add), R=[ats[dc], modc, xt], W=[xt])
        S.dma('sp', io['xT_out'][:, :, gsl].rearrange("k p n -> p k n"), xt[:], R=[xt])
        if not last_layer:
            S.op('act', lambda a: a.activation(sqs[tl % 2][:], xt[:], AF.Square), R=[xt], W=[sqs[tl % 2]])

    def epi_norm(half, tl):
        if last_layer:
            return
        h0 = half * HT
        gsl = slice(h0 + tl * NE, h0 + (tl + 1) * NE)
        xt = xts[tl % 2]
        norm_mod(S, xt, Aa[:], modn[:, 0:8], [ub], sqs[tl % 2], ones_f, PSS, lnt, rst, tmp, [Aa, modn], do_sq=False)
        S.dma('sp', io['uT_out'][:, :, gsl].rearrange("k p n -> p k n"), ub[:], R=[ub])

    for half in range(n_half):
        h0 = half * HT
        S.dma('sp', hf[:], io['hfT'][:, :, h0:h0 + HT].rearrange("k p n -> p k n"), W=[hf])
        S.dma('sp', cwt[:], io['cwT'][:, h0:h0 + HT], W=[cwt])
        if half == 0:
            load_expert(0, 0)
            load_expert(1, 1)
        items = [(e, tl) for e in range(16) for tl in range(4)]
        bufs = {}

        def gu(n):
            e, tl = items[n]
            slot = e % 2
            tsl = slice(tl * NTL, (tl + 1) * NTL)
            hd = hid[n % 2]
            cb = cwb[n % 2]
            bufs[n] = hd
            S.op('pe', lambda p: p.matmul(PCW[:, :], ident_f[0:16, e:e + 1].to_broadcast([16, 128]), cwt[:, tsl], start=True, stop=True), R=[ident_f, cwt], W=[PCW])
            S.op('act', lambda a: a.activation(cb[:], PCW[:, :], AF.Copy), R=[PCW], W=[cb])
            for fc in range(2):
                pg = PGU[fc * 2]
                pu = PGU[fc * 2 + 1]
                for k in range(8):
                    S.op('pe', lambda p: p.matmul(pg[:, :], Wge[slot][:, k, fc * 128:(fc + 1) * 128], hf[:, k, tsl], start=(k == 0), stop=(k == 7)), R=[Wge[slot], hf], W=[pg])
                for k in range(8):
                    S.op('pe', lambda p: p.matmul(pu[:, :], Wue[slot][:, k, fc * 128:(fc + 1) * 128], hf[:, k, tsl], start=(k == 0), stop=(k == 7)), R=[Wue[slot], hf], W=[pu])
                s1 = sg[fc]
                S.op('act', lambda a: a.activation(s1[:], pg[:, :], AF.Silu), R=[pg], W=[s1])
                S.op('dve', lambda v: v.tensor_tensor(h1[fc][:], pu[:, :], s1[:], ALU.mult), R=[pu, s1], W=[h1[fc]])
                S.op('pool', lambda g: g.tensor_tensor(hd[:, fc, :], h1[fc][:], cb[:], ALU.mult), R=[h1[fc], cb], W=[hd])

        def down(n):
            e, tl = items[n]
            slot = e % 2
            tsl = slice(tl * NTL, (tl + 1) * NTL)
            hd = bufs.pop(n)
            for dc in range(8):
                pd = PD[dc % 2]
                for fc in range(2):
                    S.op('pe', lambda p: p.matmul(pd[:, :], Wde[slot][:, fc, dc * 128:(dc + 1) * 128], hd[:, fc, :], start=(fc == 0), stop=(fc == 1)), R=[Wde[slot], hd], W=[pd])
                at = acc_t[dc][tl]
                if e == 0:
                    S.op('act', lambda a: a.activation(acc[:, dc, tsl], pd[:, :], AF.Copy), R=[pd], W=[at])
                else:
                    S.op('dve', lambda v: v.tensor_tensor(acc[:, dc, tsl], pd[:, :], acc[:, dc, tsl], ALU.add), R=[pd, at], W=[at])

        gu(0)
        for n in range(len(items)):
            if n + 1 < len(items):
                gu(n + 1)
            e_, tl_ = items[n]
            if half > 0 and e_ == 0:
                epi_res(half - 1, 2 * tl_)
                epi_res(half - 1, 2 * tl_ + 1)
            down(n)
            if half > 0 and e_ == 0:
                epi_norm(half - 1, 2 * tl_)
                epi_norm(half - 1, 2 * tl_ + 1)
            if tl_ == 3 and (e_ + 2 < 16 or half + 1 < n_half):
                load_expert((e_ + 2) % 16, e_ % 2)
        if half == n_half - 1:
            for tlE in range(0, HT // NE, 2):
                epi_res(half, tlE)
                epi_res(half, tlE + 1)
                epi_norm(half, tlE)
                epi_norm(half, tlE + 1)
    S.end_phase()


def emit_A(S, io, c, n_tiles=8):
    S.begin_phase()
    NTL = 512
    ones_f = c['ones_f']
    modn = scol(S, "modnA", io['modn'], [128, 16])
    ang = scol(S, "angA", io['attn_norm_col'], [128, 8])
    Aa = S.sb([128, 8], F32, "AaA")
    S.op('dve', lambda v: v.scalar_tensor_tensor(Aa[:], modn[:, 8:16], 1.0, ang[:], ALU.add, ALU.mult), R=[modn, ang], W=[Aa])
    xt = [S.sb([128, 8, NTL], F32, "axt%d" % i) for i in range(2)]
    ub = [S.sb([128, 8, NTL], BF16, "aub%d" % i) for i in range(2)]
    sq = S.sb([128, 8, NTL], F32, "asq")
    lnt = S.sb([128, NTL], F32, "aln")
    rst = S.sb([128, NTL], F32, "ars")
    tmp = [S.sb([128, NTL], F32, "atmp%d" % i) for i in range(2)]
    PSS = bank(S, "PSSA")
    for t in range(n_tiles):
        sl = slice(t * NTL, (t + 1) * NTL)
        x = xt[t % 2]
        S.dma('sp', x[:], io['xT'][:, :, sl].rearrange("k p n -> p k n"), W=[x])
        u = ub[t % 2]
        norm_mod(S, x, Aa[:], modn[:, 0:8], [u], sq, ones_f, PSS, lnt, rst, tmp, [Aa, modn])
        S.dma('sp', io['uT_out'][:, :, sl].rearrange("k p n -> p k n"), u[:], R=[u])
    S.end_phase()


CONST_SPECS = {'c_ident_b': ([128, 128], BF16), 'c_ident_f': ([128, 128], F32), 'c_tri_b': ([128, 128], BF16),
               'c_E65': ([65, 64], F32)}


def _declare(nc, specs, kind):
    io = {}
    for name, (shape, dt) in specs.items():
        io[name] = nc.dram_tensor(name, list(shape), dt, kind=kind).ap()
    return io


def build_M():
    nc = bass.Bass("TRN2", target_bir_lowering=False)
    io = _declare(nc, {'c_col': ([128, 8], F32), 'w_ada': ([1024, 6144], F32), 'b_ada': ([1, 6144], F32)}, "ExternalInput")
    io.update(_declare(nc, {'mod_out': ([1, 6144], F32)}, "ExternalOutput"))
    with ExitStack() as st:
        S = Sched(nc, st)
        emit_mod(S, io)
        S.finish_all()
    return nc


A_IN = {'xT': ([8, 128, TOK], F32), 'modn': ([128, 16], F32), 'attn_norm_col': ([128, 8], F32)}


def build_A():
    nc = bass.Bass("TRN2", target_bir_lowering=False)
    io = _declare(nc, dict(A_IN, **CONST_SPECS), "ExternalInput")
    io.update(_declare(nc, {'uT_out': ([8, 128, TOK], BF16)}, "ExternalOutput"))
    with ExitStack() as st:
        S = Sched(nc, st)
        c = load_consts(S, io)
        emit_A(S, io, c)
        S.finish_all()
    return nc


B_IN = {'uT': ([8, 128, S_LEN], BF16), 'WB': ([1024, 1186], F32), 'wuq': ([256, 192], F32), 'wukv': ([128, 256], F32),
        'mla_ncol': ([128, 3], F32), 'mla_grow': ([1, 384], F32), 'c_rope': ([128, 4096], F32),
        'c_Bd': ([128, 128], F32), 'c_M4': ([128, 512], BF16), 'dil_gcol': ([128, 2], F32),
        'ml_conv': ([64, 10], F32), 'ml_gate': ([1, 2], F32), 'ml_hg': ([1, 128], F32)}


def build_B():
    nc = bass.Bass("TRN2", target_bir_lowering=False)
    io = _declare(nc, dict(B_IN, **CONST_SPECS), "ExternalInput")
    io.update(_declare(nc, {'yaT': ([128, S_LEN], BF16), 'ybT': ([128, S_LEN], BF16), 'ycT': ([128, S_LEN], BF16)}, "ExternalOutput"))
    with ExitStack() as st:
        S = Sched(nc, st)
        c = load_consts(S, io)
        emit_mlstm(S, io, c)
        emit_dil(S, io, c)
        emit_mla(S, io, c)
        S.finish_all()
    return nc


C_IN = {'xT': ([8, 128, TOK], F32), 'uT_loc': ([8, 128, TOK], BF16), 'yT': ([3, 4, 128, TOK], BF16),
        'w_gate': ([1024, 3072], F32), 'w_br': ([3, 512, 1024], F32), 'w_out': ([1024, 1024], F32),
        'w_router': ([1024, 20], F32), 'b_router': ([1, 20], F32),
        'w_eg': ([16, 1024, 256], F32), 'w_eu': ([16, 1024, 256], F32), 'w_ed': ([16, 256, 1024], F32),
        'modc': ([128, 48], F32), 'modn': ([128, 16], F32), 'ffn_norm_col': ([128, 8], F32), 'attn_norm_col': ([128, 8], F32),
        'c_Sel': ([16, 2048], F32)}


def build_C():
    nc = bass.Bass("TRN2", target_bir_lowering=False)
    io = _declare(nc, dict(C_IN, **CONST_SPECS), "ExternalInput")
    io.update(_declare(nc, {'xT_out': ([8, 128, TOK], F32), 'uT_out': ([8, 128, TOK], BF16)}, "ExternalOutput"))
    io.update(_declare(nc, {'mgT': ([8, 128, TOK], BF16), 'x1T': ([8, 128, TOK], F32), 'hfT': ([8, 128, TOK], BF16),
                            'cwT': ([16, TOK], F32)}, "Internal"))
    with ExitStack() as st:
        S = Sched(nc, st)
        c = load_consts(S, io)
        emit_C1a(S, io, c)
        emit_C1b(S, io, c)
        emit_C2(S, io, c, last_layer=False)
        S.finish_all()
    return nc


def col8(v):
    return np.ascontiguousarray(np.asarray(v, np.float32).reshape(8, 128).T)


def _run(nc, in_maps):
    res = run_bass_kernel_spmd(nc, in_maps, core_ids=list(range(8)))
    return res.results


def kernel(**inp):
    inp = {k: np.asarray(v) for k, v in inp.items()}
    x = inp['x'].astype(np.float32, copy=False)
    cst = host_consts()
    cst.update(host_consts_dil())
    sel = np.zeros((16, 16, 128), np.float32)
    for e in range(16):
        sel[e, e, :] = 1.0
    cst['c_Sel'] = np.ascontiguousarray(sel.reshape(16, 2048))
    base_c = {k: cst[k] for k in CONST_SPECS}

    ncM = build_M()
    maps = []
    for cidx in range(8):
        l, b = cidx // 2, cidx % 2
        maps.append({'c_col': col8(inp['c'][b]), 'w_ada': np.ascontiguousarray(inp['w_ada'][l]),
                     'b_ada': np.ascontiguousarray(inp['b_ada'][l][None, :])})
    r = _run(ncM, maps)
    mod = np.zeros((DEPTH, NB, 6, 1024), np.float32)
    for cidx in range(8):
        mod[cidx // 2, cidx % 2] = r[cidx]['mod_out'].reshape(6, 1024)

    def modcols(l, b, rows):
        return np.ascontiguousarray(np.concatenate([col8(mod[l, b, i]) for i in rows], axis=1))

    xT = []
    for cidx in range(8):
        b, j = cidx // 4, cidx % 4
        xT.append(np.ascontiguousarray(x[b, j * TOK:(j + 1) * TOK, :].T.reshape(8, 128, TOK)))
    ncA = build_A()
    maps = []
    for cidx in range(8):
        b = cidx // 4
        m = dict(base_c)
        m.update({'xT': xT[cidx], 'modn': modcols(0, b, (0, 1)), 'attn_norm_col': col8(inp['attn_norm'][0])})
        maps.append(m)
    r = _run(ncA, maps)
    uT_loc = [r[cidx]['uT_out'] for cidx in range(8)]

    ncB = build_B()
    ncC = build_C()
    for l in range(DEPTH):
        uT_full = [np.ascontiguousarray(np.concatenate([uT_loc[b * 4 + j] for j in range(4)], axis=2)) for b in range(NB)]
        maps = []
        for cidx in range(8):
            b, j = cidx // 4, cidx % 4
            m = dict(base_c)
            m.update(prep_B_weights(inp, l, j))
            m.update(prep_dil_vecs(inp, l))
            m.update(prep_mlstm_vecs(inp, l, j))
            m.update({'uT': uT_full[b], 'c_rope': cst['c_rope'], 'c_Bd': cst['c_Bd'], 'c_M4': cst['c_M4']})
            maps.append(m)
        rB = _run(ncB, maps)
        ln = min(l + 1, DEPTH - 1)
        wC = {'w_gate': np.ascontiguousarray(inp['w_in'][l][:, 3496:6568]),
              'w_br': np.ascontiguousarray(np.stack([inp['w_branch_a'][l], inp['w_branch_b'][l], inp['w_branch_c'][l]])),
              'w_out': np.ascontiguousarray(inp['w_out'][l]),
              'w_router': np.ascontiguousarray(np.concatenate([inp['w_router_group'][l], inp['w_router_expert'][l]], axis=1)),
              'b_router': np.ascontiguousarray(np.concatenate([inp['b_router_group'][l], inp['b_router_expert'][l]])[None, :]),
              'w_eg': np.ascontiguousarray(inp['w_exp_gate'][l]), 'w_eu': np.ascontiguousarray(inp['w_exp_up'][l]),
              'w_ed': np.ascontiguousarray(inp['w_exp_down'][l]),
              'ffn_norm_col': col8(inp['ffn_norm'][l]), 'attn_norm_col': col8(inp['attn_norm'][ln]), 'c_Sel': cst['c_Sel']}
        maps = []
        for cidx in range(8):
            b, j = cidx // 4, cidx % 4
            sl = slice(j * TOK, (j + 1) * TOK)
            yT = np.stack([np.stack([rB[b * 4 + jj][nm][:, sl] for jj in range(4)]) for nm in ('yaT', 'ybT', 'ycT')])
            m = dict(base_c)
            m.update(wC)
            m.update({'xT': xT[cidx], 'uT_loc': uT_loc[cidx], 'yT': np.ascontiguousarray(yT),
                      'modc': modcols(l, b, range(6)), 'modn': modcols(ln, b, (0, 1))})
            maps.append(m)
        rC = _run(ncC, maps)
        xT = [rC[cidx]['xT_out'] for cidx in range(8)]
        uT_loc = [rC[cidx]['uT_out'] for cidx in range(8)]

    out = np.empty((NB, S_LEN, D), np.float32)
    for cidx in range(8):
        b, j = cidx // 4, cidx % 4
        out[b, j * TOK:(j + 1) * TOK, :] = xT[cidx].reshape(D, TOK).T
    return out
```
